# Optimizing a Trainium2 kernel written in Bass

```python
import math
import jax
import jax.numpy as jnp
from jax import lax
import numpy as np

D_MODEL = 2048
BATCH = 2
SEQ = 16384
DEPTH = 2

D_MIX = D_MODEL
RET_W = 3 * D_MIX // 8
RET_HD = 128
RET_HEADS = RET_W // RET_HD
RET_CHUNK = 128
ROPE_BASE = 10000.0
RWKV_W = 3 * D_MIX // 8
RWKV_HD = 64
RWKV_HEADS = RWKV_W // RWKV_HD
DECAY_LORA = 64
AAA_LORA = 64
GATE_LORA = 128
S5_W = D_MIX - RET_W - RWKV_W
S5_GROUP = 16
S5_GROUPS = S5_W // S5_GROUP
S5_STATE = 64
RET_COLS = 4 * RET_W
RWKV_COLS = 3 * RWKV_W + 2 * DECAY_LORA + 2 * AAA_LORA + GATE_LORA
IN_COLS = RET_COLS + RWKV_COLS + S5_W
N_GROUPS = 4
EXPERTS_PER_GROUP = 4
N_EXPERTS = N_GROUPS * EXPERTS_PER_GROUP
TOP_EXPERTS = 2
EXPERT_FF = D_MODEL // 4
PLE_DIM = 256
LN_EPS = 1e-5
RWKV_LNX_EPS = 64e-5

kernel_name = 'hymba_style_hybrid_encoder'


def _layernorm(x, w, b, eps=LN_EPS):
    xf = x.astype(jnp.float32)
    mu = jnp.mean(xf, axis=-1, keepdims=True)
    var = jnp.mean(jnp.square(xf - mu), axis=-1, keepdims=True)
    y = (xf - mu) * lax.rsqrt(var + eps) * w.astype(jnp.float32) + b.astype(jnp.float32)
    return y.astype(x.dtype)


def _rope(t, positions):
    half = t.shape[-1] // 2
    inv = ROPE_BASE ** (-jnp.arange(half, dtype=jnp.float32) / half)
    ang = positions.astype(jnp.float32)[..., None] * inv
    cos = jnp.cos(ang)[:, :, None, :]
    sin = jnp.sin(ang)[:, :, None, :]
    t1, t2 = t[..., :half], t[..., half:]
    return jnp.concatenate([t1 * cos - t2 * sin, t1 * sin + t2 * cos], axis=-1)


def _retention_mixer(z, positions):
    B_, S_, _ = z.shape
    H, Dh, C = RET_HEADS, RET_HD, RET_CHUNK
    nC = S_ // C
    q, k, v, g = jnp.split(z.astype(jnp.float32), 4, axis=-1)
    q = _rope(q.reshape(B_, S_, H, Dh), positions) * (Dh ** -0.5)
    k = _rope(k.reshape(B_, S_, H, Dh), positions)
    v = v.reshape(B_, S_, H, Dh)
    qc, kc, vc = [t.reshape(B_, nC, C, H, Dh) for t in (q, k, v)]
    lg = jnp.log(1.0 - 2.0 ** (-5.0 - jnp.arange(H, dtype=jnp.float32)))
    pos = jnp.arange(C, dtype=jnp.float32)
    d_intra = jnp.exp(lg[:, None, None] * jnp.abs(pos[:, None] - pos[None, :]))
    scores = jnp.einsum('bnihd,bnjhd->bnhij', qc, kc) * d_intra
    y = jnp.einsum('bnhij,bnjhe->bnihe', scores, vc)
    kv_f = jnp.einsum('bnjhd,hj,bnjhe->nbhde', kc, jnp.exp(lg[:, None] * (C - 1 - pos)), vc)
    kv_b = jnp.einsum('bnjhd,hj,bnjhe->nbhde', kc, jnp.exp(lg[:, None] * pos), vc)
    decay_c = jnp.exp(lg * C)[None, :, None, None]

    def carry_step(state, kv):
        return decay_c * state + kv, state

    s0 = jnp.zeros((B_, H, Dh, Dh), jnp.float32)
    _, s_f = lax.scan(carry_step, s0, kv_f)
    _, s_b = lax.scan(carry_step, s0, kv_b, reverse=True)
    y = y + jnp.einsum('bnihd,hi,nbhde->bnihe', qc, jnp.exp(lg[:, None] * (pos + 1.0)), s_f)
    y = y + jnp.einsum('bnihd,hi,nbhde->bnihe', qc, jnp.exp(lg[:, None] * (C - pos)), s_b)
    y = y.reshape(B_, S_, H, Dh)
    y = y * lax.rsqrt(jnp.mean(jnp.square(y), axis=-1, keepdims=True) + 1e-6)
    return y.reshape(B_, S_, RET_W) * jax.nn.silu(g)


def _rwkv7_mixer(z, mu_prev, mu_next, w0, w_up, a0, a_up, g_up, k_k, k_a, r_k, lnx_w, lnx_b):
    B_, S_, _ = z.shape
    H, N = RWKV_HEADS, RWKV_HD
    zf = z.astype(jnp.float32)
    z_prev = jnp.pad(zf[:, :-1], ((0, 0), (1, 0), (0, 0)))
    z_next = jnp.pad(zf[:, 1:], ((0, 0), (0, 1), (0, 0)))
    zf = zf + mu_prev * (z_prev - zf) + mu_next * (z_next - zf)
    idx = [RWKV_W, 2 * RWKV_W, 3 * RWKV_W, 3 * RWKV_W + 2 * DECAY_LORA,
           3 * RWKV_W + 2 * DECAY_LORA + 2 * AAA_LORA]
    r, k, v, wd, ad, gd = jnp.split(zf, idx, axis=-1)
    w_raw = w0 + jnp.einsum('bsdr,drc->bsdc', jnp.tanh(wd.reshape(B_, S_, 2, DECAY_LORA)), w_up)
    decay = jnp.exp(-jnp.exp(-jax.nn.softplus(-w_raw) - 0.5))
    a = jax.nn.sigmoid(a0 + jnp.einsum('bsdr,drc->bsdc', ad.reshape(B_, S_, 2, AAA_LORA), a_up))
    g = jnp.einsum('bsr,rc->bsc', jax.nn.sigmoid(gd), g_up)
    kk = (k * k_k).reshape(B_, S_, H, N)
    kk = (kk / jnp.maximum(jnp.sqrt(jnp.sum(jnp.square(kk), axis=-1, keepdims=True)), 1e-12)).reshape(B_, S_, RWKV_W)
    k_dir = k[:, :, None, :] * (1.0 + (a - 1.0) * k_a)
    b_dir = kk[:, :, None, :] * a

    def both(t):
        return jnp.stack([t, t], axis=2)

    def time_major(t):
        t = jnp.stack([t[:, :, 0], jnp.flip(t[:, :, 1], axis=1)], axis=0)
        return t.reshape(2, B_, S_, H, N).transpose(2, 0, 1, 3, 4)

    xs = (time_major(both(r)), time_major(decay), time_major(k_dir),
          time_major(both(v)), time_major(both(kk)), time_major(b_dir))

    def step(state, inp):
        r_t, w_t, k_t, v_t, kk_t, b_t = inp
        sa = jnp.einsum('dbhvk,dbhk->dbhv', state, -kk_t)
        state = (state * w_t[..., None, :] + sa[..., :, None] * b_t[..., None, :]
                 + v_t[..., :, None] * k_t[..., None, :])
        return state, jnp.einsum('dbhvk,dbhk->dbhv', state, r_t)

    state0 = jnp.zeros((2, B_, H, N, N), jnp.float32)
    _, y = lax.scan(step, state0, xs)
    y = (y[:, 0] + jnp.flip(y[:, 1], axis=0)).transpose(1, 0, 2, 3)
    mu = jnp.mean(y, axis=-1, keepdims=True)
    var = jnp.mean(jnp.square(y - mu), axis=-1, keepdims=True)
    y = ((y - mu) * lax.rsqrt(var + RWKV_LNX_EPS)).reshape(B_, S_, RWKV_W) * lnx_w + lnx_b
    rh, kh, vh = [t.reshape(B_, S_, H, N) for t in (r, k, v)]
    bonus = jnp.sum(rh * kh * r_k, axis=-1, keepdims=True) * vh
    return (y + bonus.reshape(B_, S_, RWKV_W)) * g


def _cplx_affine_combine(e1, e2):
    a1r, a1i, b1r, b1i = e1
    a2r, a2i, b2r, b2i = e2
    return (a2r * a1r - a2i * a1i, a2r * a1i + a2i * a1r,
            a2r * b1r - a2i * b1i + b2r, a2r * b1i + a2i * b1r + b2i)


def _s5_mixer(u, lam_re, lam_im, log_dt, b_re, b_im, c_re, c_im, d_skip, glu_w, glu_b):
    B_, S_, _ = u.shape
    f32 = jnp.float32
    uf = u.astype(f32)
    ug = uf.reshape(B_, S_, S5_GROUPS, S5_GROUP)
    y = uf * d_skip.astype(f32)
    for d in range(2):
        lr, li = lam_re[d].astype(f32), lam_im[d].astype(f32)
        dt = jnp.exp(log_dt[d].astype(f32))[:, None]
        mag = jnp.exp(lr * dt)
        ab_re, ab_im = mag * jnp.cos(li * dt), mag * jnp.sin(li * dt)
        den = lr * lr + li * li
        nr, ni = ab_re - 1.0, ab_im
        cr = (nr * lr + ni * li) / den
        ci = (ni * lr - nr * li) / den
        br, bi = b_re[d].astype(f32), b_im[d].astype(f32)
        bb_re = cr[..., None] * br - ci[..., None] * bi
        bb_im = cr[..., None] * bi + ci[..., None] * br
        bu_re = jnp.einsum('bsgh,gph->bsgp', ug, bb_re)
        bu_im = jnp.einsum('bsgh,gph->bsgp', ug, bb_im)
        a_re = jnp.broadcast_to(ab_re, (1, S_) + ab_re.shape)
        a_im = jnp.broadcast_to(ab_im, (1, S_) + ab_im.shape)
        _, _, x_re, x_im = lax.associative_scan(
            _cplx_affine_combine, (a_re, a_im, bu_re, bu_im), reverse=(d == 1), axis=1)
        y_d = (jnp.einsum('ghp,bsgp->bsgh', c_re[d].astype(f32), x_re)
               - jnp.einsum('ghp,bsgp->bsgh', c_im[d].astype(f32), x_im))
        y = y + y_d.reshape(B_, S_, S5_W)
    zg = jax.nn.gelu(y)
    return zg * jax.nn.sigmoid(zg @ glu_w.astype(f32) + glu_b.astype(f32))


def _hier_moe(x, router_g, router_g_b, router_e, router_e_b, w1, w3, w2):
    B_, S_, D = x.shape
    xt = x.reshape(-1, D)
    g_logits = (xt @ router_g).astype(jnp.float32) + router_g_b.astype(jnp.float32)
    g_prob = jax.nn.softmax(g_logits, axis=-1)
    p_top, grp = lax.top_k(g_prob, 1)
    e_all = jnp.einsum('td,gde->tge', xt, router_e).astype(jnp.float32) + router_e_b.astype(jnp.float32)
    e_logits = jnp.take_along_axis(e_all, grp[:, :, None], axis=1)[:, 0]
    v_top, i_top = lax.top_k(e_logits, TOP_EXPERTS)
    w_top = jax.nn.softmax(v_top, axis=-1) * p_top
    eid = grp * EXPERTS_PER_GROUP + i_top
    gate = jnp.sum(jax.nn.one_hot(eid, N_EXPERTS, dtype=jnp.float32) * w_top[..., None], axis=1)
    y = jnp.zeros(xt.shape, jnp.float32)
    for e in range(N_EXPERTS):
        h = jax.nn.silu(xt @ w1[e]) * (xt @ w3[e])
        y = y + gate[:, e:e + 1] * (h @ w2[e])
    return y.reshape(B_, S_, D).astype(x.dtype)


def setup_inputs(seed: int = 0) -> dict:
    key = jax.random.key(seed)
    ks = jax.random.split(key, 40)
    L = DEPTH
    beta = (8.0 * DEPTH) ** -0.25

    def nrm(i, shape, scale):
        return scale * jax.random.normal(ks[i], shape, jnp.float32)

    def unif(i, shape, lo, hi):
        return jax.random.uniform(ks[i], shape, dtype=jnp.float32, minval=lo, maxval=hi)

    positions = (jnp.arange(SEQ, dtype=jnp.int32)[None, :]
                 + jax.random.randint(ks[2], (BATCH, 1), 0, 1024, dtype=jnp.int32))
    s5_lam_im = jnp.pi * jnp.arange(S5_STATE, dtype=jnp.float32) + nrm(18, (L, 2, S5_GROUPS, S5_STATE), 0.01)
    return {
        'x': nrm(0, (BATCH, SEQ, D_MODEL), 1.0),
        'p': nrm(1, (L, BATCH, SEQ, PLE_DIM), 1.0),
        'positions': positions,
        'w_in': nrm(3, (L, D_MODEL, IN_COLS), D_MODEL ** -0.5),
        'w_out': nrm(4, (L, D_MIX, D_MODEL), beta * D_MIX ** -0.5),
        'rwkv_mu_prev': unif(5, (L, RWKV_COLS), 0.0, 0.5),
        'rwkv_mu_next': unif(6, (L, RWKV_COLS), 0.0, 0.5),
        'rwkv_w0': unif(7, (L, 2, RWKV_W), -6.0, -1.0),
        'rwkv_w_up': nrm(8, (L, 2, DECAY_LORA, RWKV_W), 0.1 * DECAY_LORA ** -0.5),
        'rwkv_a0': nrm(9, (L, 2, RWKV_W), 0.1),
        'rwkv_a_up': nrm(10, (L, 2, AAA_LORA, RWKV_W), 0.5 * AAA_LORA ** -0.5),
        'rwkv_g_up': nrm(11, (L, GATE_LORA, RWKV_W), GATE_LORA ** -0.5),
        'rwkv_k_k': 0.85 + nrm(12, (L, RWKV_W), 0.05),
        'rwkv_k_a': 1.0 + nrm(13, (L, RWKV_W), 0.05),
        'rwkv_r_k': nrm(14, (L, RWKV_HEADS, RWKV_HD), 0.1),
        'rwkv_lnx_w': 1.0 + nrm(15, (L, RWKV_W), 0.05),
        'rwkv_lnx_b': nrm(16, (L, RWKV_W), 0.01),
        's5_lam_re': -0.5 + nrm(17, (L, 2, S5_GROUPS, S5_STATE), 0.01),
        's5_lam_im': s5_lam_im,
        's5_log_dt': unif(19, (L, 2, S5_GROUPS), math.log(1e-3), math.log(1e-1)),
        's5_b_re': nrm(20, (L, 2, S5_GROUPS, S5_STATE, S5_GROUP), (2.0 * S5_GROUP) ** -0.5),
        's5_b_im': nrm(21, (L, 2, S5_GROUPS, S5_STATE, S5_GROUP), (2.0 * S5_GROUP) ** -0.5),
        's5_c_re': nrm(22, (L, 2, S5_GROUPS, S5_GROUP, S5_STATE), (2.0 * S5_STATE) ** -0.5),
        's5_c_im': nrm(23, (L, 2, S5_GROUPS, S5_GROUP, S5_STATE), (2.0 * S5_STATE) ** -0.5),
        's5_d': nrm(24, (L, S5_W), 0.5),
        's5_glu_w': nrm(25, (L, S5_W, S5_W), S5_W ** -0.5),
        's5_glu_b': nrm(26, (L, S5_W), 0.01),
        'moe_router_g': nrm(27, (L, D_MODEL, N_GROUPS), D_MODEL ** -0.5),
        'moe_router_g_b': nrm(28, (L, N_GROUPS), 0.01),
        'moe_router_e': nrm(29, (L, N_GROUPS, D_MODEL, EXPERTS_PER_GROUP), D_MODEL ** -0.5),
        'moe_router_e_b': nrm(30, (L, N_GROUPS, EXPERTS_PER_GROUP), 0.01),
        'moe_w1': nrm(31, (L, N_EXPERTS, D_MODEL, EXPERT_FF), D_MODEL ** -0.5),
        'moe_w3': nrm(32, (L, N_EXPERTS, D_MODEL, EXPERT_FF), D_MODEL ** -0.5),
        'moe_w2': nrm(33, (L, N_EXPERTS, EXPERT_FF, D_MODEL), beta * EXPERT_FF ** -0.5),
        'ple_proj': nrm(34, (L, PLE_DIM, D_MODEL), beta * PLE_DIM ** -0.5),
        'ple_gate': nrm(35, (L, D_MODEL, D_MODEL), D_MODEL ** -0.5),
        'ln_w': 1.0 + nrm(36, (L, 3, D_MODEL), 0.05),
        'ln_b': nrm(37, (L, 3, D_MODEL), 0.01),
    }


def reference(x, p, positions, w_in, w_out, rwkv_mu_prev, rwkv_mu_next, rwkv_w0, rwkv_w_up,
              rwkv_a0, rwkv_a_up, rwkv_g_up, rwkv_k_k, rwkv_k_a, rwkv_r_k, rwkv_lnx_w, rwkv_lnx_b,
              s5_lam_re, s5_lam_im, s5_log_dt, s5_b_re, s5_b_im, s5_c_re, s5_c_im, s5_d,
              s5_glu_w, s5_glu_b, moe_router_g, moe_router_g_b, moe_router_e, moe_router_e_b,
              moe_w1, moe_w3, moe_w2, ple_proj, ple_gate, ln_w, ln_b):
    alpha = (2.0 * DEPTH) ** 0.25
    for i in range(DEPTH):
        z = x @ w_in[i]
        z_ret, z_rwkv, z_s5 = jnp.split(z, [RET_COLS, RET_COLS + RWKV_COLS], axis=-1)
        y_ret = _retention_mixer(z_ret, positions)
        y_rwkv = _rwkv7_mixer(z_rwkv, rwkv_mu_prev[i], rwkv_mu_next[i], rwkv_w0[i], rwkv_w_up[i],
                              rwkv_a0[i], rwkv_a_up[i], rwkv_g_up[i], rwkv_k_k[i], rwkv_k_a[i],
                              rwkv_r_k[i], rwkv_lnx_w[i], rwkv_lnx_b[i])
        y_s5 = _s5_mixer(z_s5, s5_lam_re[i], s5_lam_im[i], s5_log_dt[i], s5_b_re[i], s5_b_im[i],
                         s5_c_re[i], s5_c_im[i], s5_d[i], s5_glu_w[i], s5_glu_b[i])
        mix = jnp.concatenate([y_ret, y_rwkv, y_s5], axis=-1).astype(x.dtype) @ w_out[i]
        x = _layernorm(alpha * x + mix, ln_w[i, 0], ln_b[i, 0])
        moe = _hier_moe(x, moe_router_g[i], moe_router_g_b[i], moe_router_e[i], moe_router_e_b[i],
                        moe_w1[i], moe_w3[i], moe_w2[i])
        x = _layernorm(alpha * x + moe, ln_w[i, 1], ln_b[i, 1])
        ple = jax.nn.sigmoid(x @ ple_gate[i]) * (p[i] @ ple_proj[i])
        x = _layernorm(alpha * x + ple, ln_w[i, 2], ln_b[i, 2])
    return x
```

```python
import math
import numpy as np
import concourse.bass as bass
import concourse.mybir as mybir
from concourse.bass_utils import run_bass_kernel_spmd
from contextlib import ExitStack
F32 = mybir.dt.float32; BF16 = mybir.dt.bfloat16; I32 = mybir.dt.int32
AF = mybir.ActivationFunctionType; ALU = mybir.AluOpType; AX = mybir.AxisListType


class T:
    def __init__(self, ap, name):
        self.ap = ap; self.name = name
        self.w = None
        self.r = []
    def __getitem__(self, k):
        return self.ap[k]


class Prog:
    NDMA = 8
    SEM_MAX = 30000
    def __init__(self, nc):
        self.nc = nc
        self.eng = {'pe': nc.tensor, 'dve': nc.vector, 'act': nc.scalar, 'pool': nc.gpsimd, 'sp': nc.sync}
        self.gen = {e: 0 for e in ['pe', 'dve', 'act', 'pool']}
        self.sem = {e: nc.alloc_semaphore("sem_" + e) for e in ['pe', 'dve', 'act', 'pool']}
        self.cnt = {e: 0 for e in self.sem}
        self.seen = {e: {} for e in self.eng}
        self.dsem = {q: [nc.alloc_semaphore(f"dsem_{q}{i}") for i in range(self.NDMA)] for q in ['sp', 'pool']}
        self.dcnt = {q: [0] * self.NDMA for q in self.dsem}
        self.dgen = {q: [0] * self.NDMA for q in self.dsem}
        self.dnext = {q: 0 for q in self.dsem}
        self.semobj = {}
        for e, s in self.sem.items(): self.semobj[('c', e, 0)] = s
        for q in self.dsem:
            for i, s in enumerate(self.dsem[q]): self.semobj[('d', q, i, 0)] = s
        self.ninst = 0
        self.nsb = 0

    def sb(self, name, shape, dt=F32):
        return T(self.nc.alloc_sbuf_tensor(name, list(shape), dt).ap(), name)
    def ps(self, name, shape, dt=F32):
        return T(self.nc.alloc_psum_tensor(name, list(shape), dt).ap(), name)
    def dram(self, name, shape, dt=F32, kind="Internal"):
        return T(self.nc.dram_tensor(name, list(shape), dt, kind=kind).ap(), name)

    def _wait(self, e, tok):
        if tok is None: return
        key, val = tok
        if self.seen[e].get(key, 0) >= val: return
        self.eng[e].wait_ge(self.semobj[key], val)
        self.seen[e][key] = val

    def _deps(self, e, reads, writes):
        for b in reads:
            if b.w is not None and not (e == 'pe' and b.w[0][0:2] == ('c', 'pe')):
                self._wait(e, b.w)
        for b in writes:
            if b.w is not None and not (e == 'pe' and b.w[0][0:2] == ('c', 'pe')):
                self._wait(e, b.w)
            for tok in b.r:
                if e == 'pe' and tok[0][0:2] == ('c', 'pe'): continue
                self._wait(e, tok)

    def _mark(self, tok, reads, writes):
        for b in reads:
            b.r = [t for t in b.r if t[0] != tok[0]] + [tok]
        for b in writes:
            b.w = tok; b.r = []

    def op(self, e, fn, reads=(), writes=()):
        self._deps(e, reads, writes)
        if self.cnt[e] >= self.SEM_MAX:
            self.gen[e] += 1; self.cnt[e] = 0
            self.sem[e] = self.nc.alloc_semaphore(f"sem_{e}_{self.gen[e]}")
            self.semobj[('c', e, self.gen[e])] = self.sem[e]
        inst = fn()
        self.cnt[e] += 1
        inst.then_inc(self.sem[e], 1)
        tok = (('c', e, self.gen[e]), self.cnt[e])
        self._mark(tok, reads, writes)
        self.ninst += 1
        return inst

    def dma(self, q, out, in_, reads=(), writes=(), **kw):
        i = self.dnext[q]; self.dnext[q] = (i + 1) % self.NDMA
        key = ('d', q, i, self.dgen[q][i])
        if self.dcnt[q][i] > 0:
            self._wait(q, (key, self.dcnt[q][i]))
        if self.dcnt[q][i] >= self.SEM_MAX:
            self.dgen[q][i] += 1; self.dcnt[q][i] = 0
            self.dsem[q][i] = self.nc.alloc_semaphore(f"dsem_{q}{i}_{self.dgen[q][i]}")
            key = ('d', q, i, self.dgen[q][i])
            self.semobj[key] = self.dsem[q][i]
        self._deps(q, reads, writes)
        inst = self.eng[q].dma_start(out=out, in_=in_, **kw)
        self.dcnt[q][i] += 16
        inst.then_inc(self.dsem[q][i], 16)
        tok = (key, self.dcnt[q][i])
        self._mark(tok, reads, writes)
        self.ninst += 1
        return inst

    def finish(self, outs):
        for b in outs:
            self._wait('sp', b.w)
        for e in ['pe', 'dve', 'act', 'pool']:
            if self.cnt[e] > 0:
                self._wait('sp', (('c', e, self.gen[e]), self.cnt[e]))
        for q in self.dsem:
            for i in range(self.NDMA):
                if self.dcnt[q][i] > 0:
                    self._wait('sp', (('d', q, i, self.dgen[q][i]), self.dcnt[q][i]))

    def mm(self, out, o_ap, lhsT, l_ap, rhs, r_ap, start=True, stop=True):
        return self.op('pe', lambda: self.nc.tensor.matmul(o_ap, l_ap, r_ap, start=start, stop=stop),
                       reads=[lhsT, rhs], writes=[out])
    def tr(self, out, o_ap, in_, i_ap, ident, id_ap):
        return self.op('pe', lambda: self.nc.tensor.transpose(o_ap, i_ap, id_ap), reads=[in_, ident], writes=[out])
    def act(self, out, o_ap, in_, i_ap, func, bias=None, scale=1.0, extra_reads=(), e='act', accum=None):
        kw = {}
        if bias is not None: kw['bias'] = bias
        if accum is not None: kw['accum_out'] = accum[1]
        wr = [out] + ([accum[0]] if accum is not None else [])
        return self.op('act', lambda: self.nc.scalar.activation(out=o_ap, in_=i_ap, func=func, scale=scale, **kw),
                       reads=[in_] + list(extra_reads), writes=wr)
    def tt(self, e, out, o_ap, a, a_ap, b, b_ap, op):
        en = self.eng[e]
        return self.op(e, lambda: en.tensor_tensor(out=o_ap, in0=a_ap, in1=b_ap, op=op), reads=[a, b], writes=[out])
    def ts(self, e, out, o_ap, a, a_ap, s1, s2, op0, op1=None, extra_reads=(), accum=None):
        en = self.eng[e]
        kw = {}
        if op1 is not None: kw['op1'] = op1
        wr = [out]
        if accum is not None:
            kw['accum_out'] = accum[1]; wr.append(accum[0])
        return self.op(e, lambda: en.tensor_scalar(out=o_ap, in0=a_ap, scalar1=s1, scalar2=s2, op0=op0, **kw),
                       reads=[a] + list(extra_reads), writes=wr)
    def stt(self, out, o_ap, a, a_ap, s, b, b_ap, op0, op1, extra_reads=(), e='dve'):
        en = self.eng[e]
        return self.op(e, lambda: en.scalar_tensor_tensor(out=o_ap, in0=a_ap, scalar=s, in1=b_ap, op0=op0, op1=op1),
                       reads=[a, b] + list(extra_reads), writes=[out])
    def copy(self, e, out, o_ap, in_, i_ap):
        if e == 'act':
            return self.act(out, o_ap, in_, i_ap, AF.Copy)
        en = self.eng[e]
        return self.op(e, lambda: en.tensor_copy(out=o_ap, in_=i_ap), reads=[in_], writes=[out])
    def memset(self, e, out, o_ap, val):
        en = self.eng[e]
        return self.op(e, lambda: en.memset(o_ap, val), writes=[out])

class Stage:
    def __init__(self, p):
        self.p = p; self.es = ExitStack()
    def __enter__(self):
        self.es.__enter__(); return self
    def __exit__(self, *a):
        self.p.barrier()
        return self.es.__exit__(*a)
    def sb(self, name, shape, dt=F32):
        self.p.nsb += 1; name = f"s{self.p.nsb}_{name}"
        h = self.es.enter_context(self.p.nc.sbuf_tensor(name, list(shape), dt))
        return T(h.ap(), name)
    def ps(self, name, shape, dt=F32):
        self.p.nsb += 1; name = f"s{self.p.nsb}_{name}"
        h = self.es.enter_context(self.p.nc.psum_tensor(name, list(shape), dt))
        return T(h.ap(), name)

def _barrier(self):
    toks = []
    for e in ['pe', 'dve', 'act', 'pool']:
        if self.cnt[e] > 0: toks.append((('c', e, self.gen[e]), self.cnt[e]))
    for q in self.dsem:
        for i in range(self.NDMA):
            if self.dcnt[q][i] > 0: toks.append((('d', q, i, self.dgen[q][i]), self.dcnt[q][i]))
    for e in self.eng:
        for tok in toks:
            if tok[0][0:2] == ('c', e) and e == 'pe': continue
            self._wait(e, tok)
Prog.barrier = _barrier
Prog.stage = lambda self: Stage(self)

def _collective(self, kind, in_T, in_ap, out_T, out_ap, groups):
    if not hasattr(self, 'ccsem'):
        self.ccsem = self.nc.alloc_semaphore("ccsem"); self.ccnt = 0
        self.semobj[('cc',)] = self.ccsem
    self._deps('pool', [in_T], [out_T])
    inst = self.nc.gpsimd.collective_compute(kind, ALU.bypass, replica_groups=groups, ins=[in_ap], outs=[out_ap])
    self.ccnt += 1
    inst.then_inc(self.ccsem, 1)
    tok = (('cc',), self.ccnt)
    self._mark(tok, [in_T], [out_T])
    self.ninst += 1
Prog.collective = _collective

PI = math.pi
C1 = 6.28125
C2 = 2 * math.pi - C1
INV2PI = 1.0 / (2 * math.pi)

RET_W = 768; RWKV_W = 768; RET_COLS = 3072; RWKV_COLS = 2688
NZ = 2112
COLTILES = [(i * 128, 128) for i in range(8)] + [(1024, 128), (1152, 64), (1216, 128), (1344, 64), (1408, 128), (1536, 64),
                                                  (1600, 128), (1728, 128), (1856, 128), (1984, 128)]

def ret_heads(q):
    return [2 * q, 2 * q + 1] if q < 2 else [q + 2, q + 2]

def core_cols(q):
    cols = []
    for h in ret_heads(q):
        for part in range(4):
            cols += list(range(part * RET_W + h * 128, part * RET_W + (h + 1) * 128))
    base = RET_COLS
    for part in range(3):
        cols += list(range(base + part * RWKV_W + q * 192, base + part * RWKV_W + (q + 1) * 192))
    cols += list(range(base + 3 * RWKV_W, base + 3 * RWKV_W + 384))
    cols += list(range(RET_COLS + RWKV_COLS + q * 128, RET_COLS + RWKV_COLS + (q + 1) * 128))
    assert len(cols) == NZ
    return cols

def ret_consts(q):
    import numpy as np
    C = 128
    cols = []
    inv = (10000.0 ** (-(np.arange(128) % 64).astype(np.float32) / np.float32(64))).astype(np.float32)
    cols.append(inv[:, None]); cols.append(np.where(np.arange(128) < 64, -1.0, 1.0).astype(np.float32)[:, None])
    pos = np.arange(C, dtype=np.float64)
    for h in ret_heads(q):
        lg = np.log(1.0 - 2.0 ** (-5.0 - h))
        cols.append(np.exp(lg * (C - 1 - pos))[:, None])
        cols.append(np.exp(lg * pos)[:, None])
        cols.append(np.full((128, 1), np.exp(lg * C)))
        cols.append(np.exp(lg * np.abs(pos[:, None] - pos[None, :])))
        cols.append(np.tile(np.exp(lg * (pos + 1.0))[None, :], (128, 4)))
        cols.append(np.tile(np.exp(lg * (C - pos))[None, :], (128, 4)))
    return np.concatenate(cols, axis=1).astype(np.float32)
RC_SLOT = 3 + 128 + 512 + 512
RC_N = 2 + 2 * RC_SLOT

def stage_inproj(p, S, io, x_f32, xsrc=None):
    nc = p.nc
    Q4 = S // 4
    with p.stage() as st:
        wb = st.sb("winb", [128, 16, NZ], BF16)
        wst = [st.sb(f"wst{i}", [128, NZ]) for i in range(2)]
        for k in range(16):
            s = wst[k % 2]
            p.dma('sp', s[:, :], io['w_in'][k * 128:(k + 1) * 128, :], reads=[io['w_in']], writes=[s])
            p.copy(['dve', 'pool'][k % 2], wb, wb[:, k, :], s, s[:, :])
        xb = [st.sb(f"xb{i}", [128, 16, 512], BF16) for i in range(2)]
        if x_f32:
            xf = [st.sb(f"xf{i}", [128, 8, 512]) for i in range(2)]
        zo = [st.sb(f"zo{i}", [128, 512]) for i in range(4)]
        ps = [st.ps(f"ps{i}", [128, 512]) for i in range(4)]
        n = 0
        for tt in range(S // 512):
            r = (tt * 512) // Q4; off = tt * 512 - r * Q4
            src = xsrc(r) if xsrc is not None else io['xT'][r].rearrange("(k p) t -> p k t", p=128)
            x = xb[tt % 2]
            if x_f32:
                for hf in range(2):
                    p.dma('sp', xf[hf][:, :, :], src[:, hf * 8:(hf + 1) * 8, off:off + 512], reads=[io['xT']], writes=[xf[hf]])
                    p.copy(['dve', 'pool'][hf], x, x[:, hf * 8:(hf + 1) * 8, :], xf[hf], xf[hf][:, :, :])
            else:
                p.dma('sp', x[:, :, :], src[:, :, off:off + 512], reads=[io['xT']], writes=[x])
            for ci, (c0, w) in enumerate(COLTILES):
                P_ = ps[n % 4]; z = zo[n % 4]
                for k in range(16):
                    p.mm(P_, P_[0:w, :], wb, wb[:, k, c0:c0 + w], x, x[:, k, :], start=(k == 0), stop=(k == 15))
                if n % 2 == 0:
                    p.act(z, z[0:w, :], P_, P_[0:w, :], AF.Copy)
                else:
                    p.copy('dve', z, z[0:w, :], P_, P_[0:w, :])
                p.dma('pool', io['zT'][c0:c0 + w, tt * 512:(tt + 1) * 512], z[0:w, :], reads=[z], writes=[io['zT']])
                n += 1

def stage_retention(p, S, io):
    nc = p.nc
    NCK = S // 128; NTT = S // 512; Q4 = S // 4
    zT = io['zT']
    with p.stage() as st:
        rc = st.sb("rc", [128, RC_N])
        p.dma('sp', rc[:, :], io['rconst'][:, :], reads=[io['rconst']], writes=[rc])
        ident_f = st.sb("ident_f", [128, 128]); ident = st.sb("ident", [128, 128], BF16)
        p.dma('sp', ident_f[:, :], io['ident'][:, :], reads=[io['ident']], writes=[ident_f])
        p.copy('dve', ident, ident[:, :], ident_f, ident_f[:, :])
        posi = st.sb("posi", [128, 512], I32); ang = st.sb("ang", [128, 512]); a2 = st.sb("a2", [128, 512])
        ki = st.sb("ki", [128, 512], I32); kf = st.sb("kf", [128, 512])
        cos = st.sb("cos", [128, 512]); sin = st.sb("sin", [128, 512])
        zq = [st.sb(f"zq{i}", [128, 512]) for i in range(2)]; zs = [st.sb(f"zs{i}", [128, 512]) for i in range(2)]
        t1 = st.sb("t1", [128, 512]); t2 = st.sb("t2", [128, 512])
        rot = [st.sb(f"rot{i}", [128, 512], BF16) for i in range(2)]
        vf = st.sb("vf", [128, 512]); vb = st.sb("vb", [128, 512], BF16)
        tok = [st.sb(f"tok{i}", [128, 4, 128], BF16) for i in range(2)]
        ptr = [st.ps(f"ptr{i}", [128, 512], BF16) for i in range(2)]
        n = 0
        for tt in range(NTT):
            ts_ = slice(tt * 512, (tt + 1) * 512)
            p.dma('sp', posi[:, :], io['pos'].ap[ts_].rearrange("(o t) -> o t", o=1).partition_broadcast(128), reads=[io['pos']], writes=[posi])
            p.ts('dve', ang, ang[:, :], posi, posi[:, :], rc[:, 0:1], None, ALU.mult, extra_reads=[rc])
            for (shift, dst, scale) in [(0.0, sin, rc[:, 1:2]), (PI / 2, cos, 1.0)]:
                if shift != 0.0:
                    p.ts('pool', a2, a2[:, :], ang, ang[:, :], shift, None, ALU.add)
                    src = a2
                else:
                    src = ang
                p.ts('dve', ki, ki[:, :], src, src[:, :], INV2PI, None, ALU.mult)
                p.copy('pool', kf, kf[:, :], ki, ki[:, :])
                p.stt(a2, a2[:, :], kf, kf[:, :], -C1, src, src[:, :], ALU.mult, ALU.add)
                p.stt(a2, a2[:, :], kf, kf[:, :], -C2, a2, a2[:, :], ALU.mult, ALU.add)
                p.ts('pool', a2, a2[:, :], a2, a2[:, :], -PI, PI, ALU.max, ALU.min)
                p.act(dst, dst[:, :], a2, a2[:, :], AF.Sin, scale=scale, extra_reads=[rc])
            for s in range(2):
                base = s * 512
                for which, (r0, scl, dstd) in enumerate([(base, 128.0 ** -0.5, io['qrT']), (base + 128, 1.0, io['krT'])]):
                    z = zq[which]; zw = zs[which]
                    p.dma('sp', z[:, :], zT[r0:r0 + 128, ts_], reads=[zT], writes=[z])
                    p.dma('sp', zw[0:64, :], zT[r0 + 64:r0 + 128, ts_], reads=[zT], writes=[zw])
                    p.dma('sp', zw[64:128, :], zT[r0:r0 + 64, ts_], reads=[zT], writes=[zw])
                    p.stt(t1, t1[:, :], z, z[:, :], scl, cos, cos[:, :], ALU.mult, ALU.mult)
                    p.stt(t2, t2[:, :], zw, zw[:, :], scl, sin, sin[:, :], ALU.mult, ALU.mult, e='dve')
                    ro = rot[which]
                    p.tt('pool', ro, ro[:, :], t1, t1[:, :], t2, t2[:, :], ALU.add)
                    p.dma('pool', dstd[s, :, ts_], ro[:, :], reads=[ro], writes=[dstd])
                p.dma('sp', vf[:, :], zT[base + 256:base + 384, ts_], reads=[zT], writes=[vf])
                p.copy('pool', vb, vb[:, :], vf, vf[:, :])
                for (srcb, dstd) in [(rot[1], io['ktok']), (vb, io['vtok'])]:
                    P_ = ptr[n % 2]; tk = tok[n % 2]; n += 1
                    for c4 in range(4):
                        p.tr(P_, P_[:, c4 * 128:(c4 + 1) * 128], srcb, srcb[:, c4 * 128:(c4 + 1) * 128], ident, ident[:, :])
                    p.act(tk, tk[:, :, :], P_, P_[:, :].rearrange("p (c d) -> p c d", c=4), AF.Copy)
                    p.dma('pool', dstd[s, tt * 4:(tt + 1) * 4].rearrange("c j d -> j c d"), tk[:, :, :], reads=[tk], writes=[dstd])
    with p.stage() as st:
        rc = st.sb("rc", [128, RC_N])
        p.dma('sp', rc[:, :], io['rconst'][:, :], reads=[io['rconst']], writes=[rc])
        Sf = [st.sb(f"Sb{s}", [128, 128]) for s in range(2)]
        Sb = [[st.sb(f"Sbb{s}_{i}", [128, 128], BF16) for i in range(2)] for s in range(2)]
        kt = [[st.sb(f"kt{s}_{i}", [128, 4, 128], BF16) for i in range(2)] for s in range(2)]
        vt = [[st.sb(f"vt{s}_{i}", [128, 4, 128], BF16) for i in range(2)] for s in range(2)]
        kd = [[st.sb(f"kd{s}_{i}", [128, 4, 128], BF16) for i in range(2)] for s in range(2)]
        pkv = [st.ps(f"pkv{i}", [128, 128]) for i in range(2)]
        for s in range(2):
            p.memset('dve', Sf[s], Sf[s][:, :], 0.0)
            p.memset('pool', Sb[s][0], Sb[s][0][:, :], 0.0)
            p.memset('pool', Sb[s][1], Sb[s][1][:, :], 0.0)
        for tt in range(NTT - 1, -1, -1):
            for s in range(2):
                o = 2 + s * RC_SLOT
                k_ = kt[s][tt % 2]; v_ = vt[s][tt % 2]; d_ = kd[s][tt % 2]
                p.dma('sp', k_[:, :, :], io['ktok'][s, tt * 4:(tt + 1) * 4].rearrange("c j d -> j c d"), reads=[io['ktok']], writes=[k_])
                p.dma('sp', v_[:, :, :], io['vtok'][s, tt * 4:(tt + 1) * 4].rearrange("c j d -> j c d"), reads=[io['vtok']], writes=[v_])
                p.ts('pool', d_, d_[:, :, :], k_, k_[:, :, :], rc[:, o + 1:o + 2], None, ALU.mult, extra_reads=[rc])
                for c4 in range(3, -1, -1):
                    c = tt * 4 + c4
                    cur = Sb[s][c % 2]; nxt = Sb[s][(c + 1) % 2]
                    p.dma('pool', io['sbd'][s, c], cur[:, :], reads=[cur], writes=[io['sbd']])
                    if c == 0: continue
                    P_ = pkv[s]
                    p.mm(P_, P_[:, :], d_, d_[:, c4, :], v_, v_[:, c4, :])
                    p.stt(Sf[s], Sf[s][:, :], Sf[s], Sf[s][:, :], rc[:, o + 2:o + 3], P_, P_[:, :], ALU.mult, ALU.add, extra_reads=[rc])
                    p.act(nxt, nxt[:, :], Sf[s], Sf[s][:, :], AF.Copy)
    with p.stage() as st:
        rc = st.sb("rc", [128, RC_N])
        p.dma('sp', rc[:, :], io['rconst'][:, :], reads=[io['rconst']], writes=[rc])
        ones = st.sb("ones", [128, 128]); p.memset('dve', ones, ones[:, :], 1.0)
        Sf = [st.sb(f"Sf{s}", [128, 128]) for s in range(2)]
        Sfb = [[st.sb(f"Sfb{s}_{i}", [128, 128], BF16) for i in range(2)] for s in range(2)]
        bufs = {}
        for s in range(2):
            for nm, shp, dt in [('q', [128, 512], BF16), ('k', [128, 512], BF16), ('kt', [128, 4, 128], BF16), ('vt', [128, 4, 128], BF16),
                                ('sb', [128, 4, 128], BF16), ('g', [128, 512], F32), ('qf', [128, 512], BF16), ('qb', [128, 512], BF16),
                                ('kd', [128, 4, 128], BF16), ('scm', [128, 512], BF16), ('sq', [128, 512], F32), ('rs', [128, 512], F32),
                                ('sg', [128, 512], F32), ('t', [128, 512], F32), ('o', [128, 512], BF16)]:
                bufs[(s, nm)] = st.sb(f"r3{nm}{s}", shp, dt)
        psc = [st.ps(f"psc{i}", [128, 128]) for i in range(2)]
        pyT = [st.ps(f"pyT{i}", [128, 512]) for i in range(2)]
        pkv = [st.ps(f"pkv3{i}", [128, 128]) for i in range(2)]
        pss = st.ps("pss", [128, 512])
        for s in range(2):
            p.memset('dve', Sf[s], Sf[s][:, :], 0.0)
            p.memset('pool', Sfb[s][0], Sfb[s][0][:, :], 0.0)
        for tt in range(NTT):
            ts_ = slice(tt * 512, (tt + 1) * 512)
            r = (tt * 512) // Q4; off = tt * 512 - r * Q4
            for s in range(2):
                o = 2 + s * RC_SLOT
                B = lambda nm: bufs[(s, nm)]
                p.dma('sp', B('q')[:, :], io['qrT'][s, :, ts_], reads=[io['qrT']], writes=[B('q')])
                p.dma('sp', B('k')[:, :], io['krT'][s, :, ts_], reads=[io['krT']], writes=[B('k')])
                p.dma('sp', B('kt')[:, :, :], io['ktok'][s, tt * 4:(tt + 1) * 4].rearrange("c j d -> j c d"), reads=[io['ktok']], writes=[B('kt')])
                p.dma('sp', B('vt')[:, :, :], io['vtok'][s, tt * 4:(tt + 1) * 4].rearrange("c j d -> j c d"), reads=[io['vtok']], writes=[B('vt')])
                p.dma('sp', B('sb')[:, :, :], io['sbd'][s, tt * 4:(tt + 1) * 4].rearrange("c d e -> d c e"), reads=[io['sbd']], writes=[B('sb')])
                p.dma('sp', B('g')[:, :], zT[s * 512 + 384:s * 512 + 512, ts_], reads=[zT], writes=[B('g')])
                p.tt('pool', B('qf'), B('qf')[:, :], B('q'), B('q')[:, :], rc, rc[:, o + 3 + 128:o + 3 + 128 + 512], ALU.mult)
                p.tt('pool', B('qb'), B('qb')[:, :], B('q'), B('q')[:, :], rc, rc[:, o + 3 + 640:o + 3 + 640 + 512], ALU.mult)
                p.ts('pool', B('kd'), B('kd')[:, :, :], B('kt'), B('kt')[:, :, :], rc[:, o:o + 1], None, ALU.mult, extra_reads=[rc])
                p.act(B('sg'), B('sg')[:, :], B('g'), B('g')[:, :], AF.Silu)
                Y = pyT[s]
                for c4 in range(4):
                    c = tt * 4 + c4
                    cs = slice(c4 * 128, (c4 + 1) * 128)
                    cur = Sfb[s][c % 2]; nxt = Sfb[s][(c + 1) % 2]
                    SC = psc[c % 2]
                    p.mm(SC, SC[:, :], B('k'), B('k')[:, cs], B('q'), B('q')[:, cs])
                    p.tt('dve', B('scm'), B('scm')[:, cs], SC, SC[:, :], rc, rc[:, o + 3:o + 3 + 128], ALU.mult)
                    p.mm(Y, Y[:, cs], B('vt'), B('vt')[:, c4, :], B('scm'), B('scm')[:, cs], start=True, stop=False)
                    p.mm(Y, Y[:, cs], cur, cur[:, :], B('qf'), B('qf')[:, cs], start=False, stop=False)
                    p.mm(Y, Y[:, cs], B('sb'), B('sb')[:, c4, :], B('qb'), B('qb')[:, cs], start=False, stop=True)
                    if c < NCK - 1:
                        P_ = pkv[s]
                        p.mm(P_, P_[:, :], B('kd'), B('kd')[:, c4, :], B('vt'), B('vt')[:, c4, :])
                        p.stt(Sf[s], Sf[s][:, :], Sf[s], Sf[s][:, :], rc[:, o + 2:o + 3], P_, P_[:, :], ALU.mult, ALU.add, extra_reads=[rc])
                        p.act(nxt, nxt[:, :], Sf[s], Sf[s][:, :], AF.Copy)
                p.act(B('sq'), B('sq')[:, :], Y, Y[:, :], AF.Square)
                p.mm(pss, pss[:, :], ones, ones[:, :], B('sq'), B('sq')[:, :])
                p.act(B('rs'), B('rs')[:, :], pss, pss[:, :], AF.Sqrt, bias=1e-6, scale=1.0 / 128)
                p.op('dve', lambda: nc.vector.reciprocal(out=B('rs')[:, :], in_=B('rs')[:, :]), reads=[B('rs')], writes=[B('rs')])
                p.tt('dve', B('t'), B('t')[:, :], Y, Y[:, :], B('rs'), B('rs')[:, :], ALU.mult)
                p.tt('pool', B('o'), B('o')[:, :], B('t'), B('t')[:, :], B('sg'), B('sg')[:, :], ALU.mult)
                p.dma('pool', io['yT'][r, s * 128:(s + 1) * 128, off:off + 512], B('o')[:, :], reads=[B('o')], writes=[io['yT']])

ZS5 = 1984
def sin_reduce(p, out, src, shift, ki, kf, tmp, scale=1.0, extra_reads=()):
    sl = tuple(slice(None) for _ in src.ap.shape)
    if shift != 0.0:
        p.ts('pool', tmp, tmp[sl], src, src[sl], shift, None, ALU.add)
        s = tmp
    else:
        s = src
    p.ts('dve', ki, ki[sl], s, s[sl], INV2PI, None, ALU.mult)
    p.copy('pool', kf, kf[sl], ki, ki[sl])
    p.stt(tmp, tmp[sl], kf, kf[sl], -C1, s, s[sl], ALU.mult, ALU.add)
    p.stt(tmp, tmp[sl], kf, kf[sl], -C2, tmp, tmp[sl], ALU.mult, ALU.add)
    p.ts('pool', tmp, tmp[sl], tmp, tmp[sl], -PI, PI, ALU.max, ALU.min)
    p.act(out, out[sl], tmp, tmp[sl], AF.Sin, scale=scale, extra_reads=extra_reads)

def s5_host_layout(q, lam_re, lam_im, log_dt, b_re, b_im, c_re, c_im, d_skip):
    import numpy as np
    gs = slice(8 * q, 8 * q + 8)
    lr = lam_re[:, gs, :]; li = lam_im[:, gs, :]; ld = log_dt[:, gs]
    rows = np.stack([lr.reshape(-1), li.reshape(-1), np.repeat(ld.reshape(-1), 64)], 0).astype(np.float32)
    def col(a):
        return np.ascontiguousarray(a.reshape(2, 4, 2, 64).transpose(2, 3, 0, 1).reshape(128, 8))
    cols = np.concatenate([col(lr), col(li), col(np.repeat(ld[:, :, None], 64, axis=2))], 1).astype(np.float32)
    bl = np.zeros((2, 128, 2, 4, 128), np.float32)
    cl = np.zeros((2, 128, 2, 4, 128), np.float32)
    for d in range(2):
        for g in range(8):
            j, gp = g // 2, g % 2
            for ri, (bsrc, csrc) in enumerate([(b_re, c_re), (b_im, c_im)]):
                bl[ri, g * 16:(g + 1) * 16, d, j, gp * 64:(gp + 1) * 64] = bsrc[d, 8 * q + g].T
                cl[ri, gp * 64:(gp + 1) * 64, d, j, g * 16:(g + 1) * 16] = csrc[d, 8 * q + g].T
    dcol = d_skip[128 * q:128 * (q + 1)].reshape(128, 1).astype(np.float32)
    return dict(s5rows=rows, s5cols=cols, s5bl=bl.reshape(2, 128, 1024), s5cl=cl.reshape(2, 128, 1024), s5d=dcol,
                s5iota=np.tile(np.arange(512, dtype=np.float32)[None, :], (128, 1)))

def stage_s5(p, S, io):
    nc = p.nc
    NTT = S // 512; Q4 = S // 4; TC = 512
    zT = io['zT']
    with p.stage() as st:
        bb = [st.sb(f"bb{i}", [128, 1024], BF16) for i in range(2)]
        cc = [st.sb(f"cc{i}", [128, 1024], BF16) for i in range(2)]
        rho = st.sb("rho", [128, 8]); cT = st.sb("cT", [128, 8]); sT = st.sb("sT", [128, 8]); thc = st.sb("thc", [128, 8])
        dcol = st.sb("dcol", [128, 1])
        p.dma('sp', dcol[:, :], io['s5d'][:, :], reads=[io['s5d']], writes=[dcol])
        with p.stage() as s2:
            f = lambda nm: s2.sb(nm, [128, 1024])
            lr, li, ld, dt, mag, th, sn, cs, t1, t2, t3, kf, tmp, cr, ci = [f(n) for n in
                ['lr', 'li', 'ld', 'dt', 'mag', 'th', 'sn', 'cs', 't1', 't2', 't3', 'kf', 'tmp', 'cr', 'ci']]
            ki = s2.sb("ki", [128, 1024], I32)
            for k, t in enumerate([lr, li, ld]):
                p.dma('sp', t[:, :], io['s5rows'][k:k + 1, :].partition_broadcast(128), reads=[io['s5rows']], writes=[t])
            p.act(dt, dt[:, :], ld, ld[:, :], AF.Exp)
            p.tt('dve', t1, t1[:, :], lr, lr[:, :], dt, dt[:, :], ALU.mult)
            p.act(mag, mag[:, :], t1, t1[:, :], AF.Exp)
            p.tt('dve', th, th[:, :], li, li[:, :], dt, dt[:, :], ALU.mult)
            sin_reduce(p, sn, th, 0.0, ki, kf, tmp)
            sin_reduce(p, cs, th, PI / 2, ki, kf, tmp)
            p.tt('dve', cs, cs[:, :], cs, cs[:, :], mag, mag[:, :], ALU.mult)
            p.tt('dve', sn, sn[:, :], sn, sn[:, :], mag, mag[:, :], ALU.mult)
            p.ts('dve', cs, cs[:, :], cs, cs[:, :], -1.0, None, ALU.add)
            p.tt('dve', t1, t1[:, :], lr, lr[:, :], lr, lr[:, :], ALU.mult)
            p.tt('dve', t2, t2[:, :], li, li[:, :], li, li[:, :], ALU.mult)
            p.tt('dve', t1, t1[:, :], t1, t1[:, :], t2, t2[:, :], ALU.add)
            p.op('dve', lambda: nc.vector.reciprocal(out=t1[:, :], in_=t1[:, :]), reads=[t1], writes=[t1])
            p.tt('dve', t2, t2[:, :], cs, cs[:, :], lr, lr[:, :], ALU.mult)
            p.tt('dve', t3, t3[:, :], sn, sn[:, :], li, li[:, :], ALU.mult)
            p.tt('dve', t2, t2[:, :], t2, t2[:, :], t3, t3[:, :], ALU.add)
            p.tt('dve', cr, cr[:, :], t2, t2[:, :], t1, t1[:, :], ALU.mult)
            p.tt('dve', t2, t2[:, :], sn, sn[:, :], lr, lr[:, :], ALU.mult)
            p.tt('dve', t3, t3[:, :], cs, cs[:, :], li, li[:, :], ALU.mult)
            p.tt('dve', t2, t2[:, :], t2, t2[:, :], t3, t3[:, :], ALU.subtract)
            p.tt('dve', ci, ci[:, :], t2, t2[:, :], t1, t1[:, :], ALU.mult)
            br = lr; bi = li
            p.dma('sp', br[:, :], io['s5bl'][0], reads=[io['s5bl']], writes=[br])
            p.dma('sp', bi[:, :], io['s5bl'][1], reads=[io['s5bl']], writes=[bi])
            p.tt('dve', t1, t1[:, :], cr, cr[:, :], br, br[:, :], ALU.mult)
            p.tt('dve', t2, t2[:, :], ci, ci[:, :], bi, bi[:, :], ALU.mult)
            p.tt('dve', bb[0], bb[0][:, :], t1, t1[:, :], t2, t2[:, :], ALU.subtract)
            p.tt('dve', t1, t1[:, :], cr, cr[:, :], bi, bi[:, :], ALU.mult)
            p.tt('dve', t2, t2[:, :], ci, ci[:, :], br, br[:, :], ALU.mult)
            p.tt('dve', bb[1], bb[1][:, :], t1, t1[:, :], t2, t2[:, :], ALU.add)
            p.dma('sp', t1[:, :], io['s5cl'][0], reads=[io['s5cl']], writes=[t1])
            p.dma('sp', t2[:, :], io['s5cl'][1], reads=[io['s5cl']], writes=[t2])
            p.copy('dve', cc[0], cc[0][:, :], t1, t1[:, :])
            p.ts('dve', cc[1], cc[1][:, :], t2, t2[:, :], -1.0, None, ALU.mult)
            c24 = s2.sb("c24", [128, 24]); dtc = s2.sb("dtc", [128, 8]); tq = s2.sb("tq", [128, 8]); tq2 = s2.sb("tq2", [128, 8])
            ki8 = s2.sb("ki8", [128, 8], I32); kf8 = s2.sb("kf8", [128, 8]); tmp8 = s2.sb("tmp8", [128, 8])
            p.dma('sp', c24[:, :], io['s5cols'][:, :], reads=[io['s5cols']], writes=[c24])
            p.act(dtc, dtc[:, :], c24, c24[:, 16:24], AF.Exp)
            p.tt('dve', tq, tq[:, :], c24, c24[:, 0:8], dtc, dtc[:, :], ALU.mult)
            p.act(rho, rho[:, :], tq, tq[:, :], AF.Exp)
            p.tt('dve', thc, thc[:, :], c24, c24[:, 8:16], dtc, dtc[:, :], ALU.mult)
            p.ts('dve', tq2, tq2[:, :], thc, thc[:, :], float(TC), None, ALU.mult)
            sin_reduce(p, sT, tq2, 0.0, ki8, kf8, tmp8)
            sin_reduce(p, cT, tq2, PI / 2, ki8, kf8, tmp8)
        iota = st.sb("iota", [128, TC]); p.dma('sp', iota[:, :], io['s5iota'][:, :], reads=[io['s5iota']], writes=[iota])
        cosT = [st.sb(f"cost{k}", [128, TC]) for k in range(8)]; sinT = [st.sb(f"sint{k}", [128, TC]) for k in range(8)]
        rhoT = [st.sb(f"rhot{k}", [128, TC]) for k in range(8)]
        ang = st.sb("ang", [128, TC]); kiT = st.sb("kiT", [128, TC], I32); kfT = st.sb("kfT", [128, TC]); tmpT = st.sb("tmpT", [128, TC])
        tb = st.sb("tb", [128, TC])
        for k in range(8):
            p.ts('dve', ang, ang[:, :], iota, iota[:, :], thc[:, k:k + 1], None, ALU.mult, extra_reads=[thc])
            for (shift, dst) in [(0.0, sinT[k]), (PI / 2, cosT[k])]:
                if k < 4:
                    sin_reduce(p, dst, ang, shift, kiT, kfT, tmpT)
                else:
                    sin_reduce(p, tb, ang, shift, kiT, kfT, tmpT)
                    p.copy('dve', dst, dst[:, :], tb, tb[:, ::-1])
            p.ts('dve', rhoT[k], rhoT[k][:, :], iota, iota[:, :], 0.0, rho[:, k:k + 1], ALU.mult, ALU.add, extra_reads=[rho])
        uf = [st.sb(f"uf{i}", [128, TC]) for i in range(2)]; ub = [st.sb(f"ub{i}", [128, TC], BF16) for i in range(2)]
        brs = [st.sb(f"brs{i}", [128, TC]) for i in range(2)]; bis = [st.sb(f"bis{i}", [128, TC]) for i in range(2)]
        t1 = st.sb("t1", [128, TC]); t2 = st.sb("t2", [128, TC]); t3 = st.sb("t3", [128, TC]); t4 = st.sb("t4", [128, TC])
        mre = st.sb("mre", [128, TC]); mim = st.sb("mim", [128, TC])
        xr = [st.sb(f"xr{i}", [128, TC]) for i in range(2)]; xi = [st.sb(f"xi{i}", [128, TC]) for i in range(2)]
        xre = [st.sb(f"xre{i}", [128, TC], BF16) for i in range(2)]; xim = [st.sb(f"xim{i}", [128, TC], BF16) for i in range(2)]
        init = st.sb("init", [128, 16]); p.memset('dve', init, init[:, :], 0.0)
        tn = st.sb("tn", [128, 4])
        yo = [st.sb(f"yo{i}", [128, TC]) for i in range(2)]
        yfl = st.sb("yfl", [128, TC]); yb16 = [st.sb(f"yb16{i}", [128, TC], BF16) for i in range(2)]
        pb_re = [st.ps(f"pbre{i}", [128, TC]) for i in range(2)]; pb_im = [st.ps(f"pbim{i}", [128, TC]) for i in range(2)]
        py = [st.ps(f"py{i}", [128, TC]) for i in range(2)]
        n = 0
        for d in range(2):
            order = range(NTT) if d == 0 else range(NTT - 1, -1, -1)
            for it, tt in enumerate(order):
                ts_ = slice(tt * TC, (tt + 1) * TC)
                r = (tt * TC) // Q4; off = tt * TC - r * Q4
                u_f = uf[it % 2]; u_b = ub[it % 2]
                p.dma('sp', u_f[:, :], zT[ZS5:ZS5 + 128, ts_], reads=[zT], writes=[u_f])
                p.copy('pool', u_b, u_b[:, :], u_f, u_f[:, :])
                Y = py[it % 2]
                for j in range(4):
                    k = d * 4 + j
                    blk = slice(k * 128, (k + 1) * 128)
                    PR = pb_re[n % 2]; PI_ = pb_im[n % 2]; b_r = brs[n % 2]; b_i = bis[n % 2]
                    x_r = xr[n % 2]; x_i = xi[n % 2]; xo_r = xre[n % 2]; xo_i = xim[n % 2]; n += 1
                    p.mm(PR, PR[:, :], bb[0], bb[0][:, blk], u_b, u_b[:, :])
                    p.mm(PI_, PI_[:, :], bb[1], bb[1][:, blk], u_b, u_b[:, :])
                    p.act(b_r, b_r[:, :], PR, PR[:, :], AF.Copy)
                    p.act(b_i, b_i[:, :], PI_, PI_[:, :], AF.Copy)
                    p.tt('dve', t1, t1[:, :], b_r, b_r[:, :], cosT[k], cosT[k][:, :], ALU.mult)
                    p.tt('pool', t2, t2[:, :], b_i, b_i[:, :], sinT[k], sinT[k][:, :], ALU.mult)
                    p.tt('dve', mre, mre[:, :], t1, t1[:, :], t2, t2[:, :], ALU.add)
                    p.tt('pool', t3, t3[:, :], b_i, b_i[:, :], cosT[k], cosT[k][:, :], ALU.mult)
                    p.tt('dve', t4, t4[:, :], b_r, b_r[:, :], sinT[k], sinT[k][:, :], ALU.mult)
                    p.tt('pool', mim, mim[:, :], t3, t3[:, :], t4, t4[:, :], ALU.subtract)
                    if d == 0:
                        vw = lambda a: a[:, :]
                        last = slice(TC - 1, TC)
                    else:
                        vw = lambda a: a[:, ::-1]
                        last = slice(0, 1)
                    p.op('dve', lambda: nc.vector.tensor_tensor_scan(out=vw(x_r), data0=rhoT[k][:, :], data1=vw(mre), initial=init[:, k:k + 1],
                                                                      op0=ALU.mult, op1=ALU.add), reads=[rhoT[k], mre, init], writes=[x_r])
                    p.op('dve', lambda: nc.vector.tensor_tensor_scan(out=vw(x_i), data0=rhoT[k][:, :], data1=vw(mim), initial=init[:, 8 + k:9 + k],
                                                                      op0=ALU.mult, op1=ALU.add), reads=[rhoT[k], mim, init], writes=[x_i])
                    p.ts('pool', tn, tn[:, 0:1], x_r, x_r[:, last], cT[:, k:k + 1], None, ALU.mult, extra_reads=[cT])
                    p.ts('pool', tn, tn[:, 1:2], x_r, x_r[:, last], sT[:, k:k + 1], None, ALU.mult, extra_reads=[sT])
                    p.stt(tn, tn[:, 2:3], x_i, x_i[:, last], sT[:, k:k + 1], tn, tn[:, 0:1], ALU.mult, ALU.subtract, extra_reads=[sT])
                    p.ts('pool', init, init[:, k:k + 1], tn, tn[:, 2:3], -1.0, None, ALU.mult)
                    p.stt(init, init[:, 8 + k:9 + k], x_i, x_i[:, last], cT[:, k:k + 1], tn, tn[:, 1:2], ALU.mult, ALU.add, extra_reads=[cT])
                    p.tt('dve', t1, t1[:, :], x_r, x_r[:, :], cosT[k], cosT[k][:, :], ALU.mult)
                    p.tt('pool', t2, t2[:, :], x_i, x_i[:, :], sinT[k], sinT[k][:, :], ALU.mult)
                    p.tt('dve', xo_r, xo_r[:, :], t1, t1[:, :], t2, t2[:, :], ALU.subtract)
                    p.tt('pool', t3, t3[:, :], x_r, x_r[:, :], sinT[k], sinT[k][:, :], ALU.mult)
                    p.tt('dve', t4, t4[:, :], x_i, x_i[:, :], cosT[k], cosT[k][:, :], ALU.mult)
                    p.tt('pool', xo_i, xo_i[:, :], t3, t3[:, :], t4, t4[:, :], ALU.add)
                    p.mm(Y, Y[:, :], cc[0], cc[0][:, blk], xo_r, xo_r[:, :], start=(j == 0), stop=False)
                    p.mm(Y, Y[:, :], cc[1], cc[1][:, blk], xo_i, xo_i[:, :], start=False, stop=(j == 3))
                if d == 0:
                    y_o = yo[it % 2]
                    p.act(y_o, y_o[:, :], Y, Y[:, :], AF.Copy)
                    p.dma('pool', io['yf'][:, ts_], y_o[:, :], reads=[y_o], writes=[io['yf']])
                else:
                    y_o = yo[it % 2]; yb = yb16[it % 2]
                    p.dma('sp', yfl[:, :], io['yf'][:, ts_], reads=[io['yf']], writes=[yfl])
                    p.tt('dve', y_o, y_o[:, :], Y, Y[:, :], yfl, yfl[:, :], ALU.add)
                    p.stt(y_o, y_o[:, :], u_f, u_f[:, :], dcol[:, 0:1], y_o, y_o[:, :], ALU.mult, ALU.add, extra_reads=[dcol])
                    p.act(yb, yb[:, :], y_o, y_o[:, :], AF.Gelu_apprx_tanh)
                    p.dma('pool', io['yT'][r, 448:576, off:off + TC], yb[:, :], reads=[yb], writes=[io['yT']])

ZR, ZK, ZV, ZWD, ZAD, ZGD = 1024, 1216, 1408, 1600, 1728, 1856
RW_SU, RW_SL, RW_U, RW_L, RW_I, RW_BLK, RW_MF, RW_MB, RW_N = 0, 384, 768, 1152, 1536, 1920, 2048, 2560, 3072
NEG_EXP_HALF = -math.exp(-0.5)

def rwkv_consts():
    import numpy as np
    i = np.arange(128)
    su = (i[:, None] < i[None, :]).astype(np.float32); sl = su.T.copy()
    u = (i[:, None] <= i[None, :]).astype(np.float32); l = u.T.copy()
    I = np.eye(128, dtype=np.float32)
    blk = np.zeros((128, 128), np.float32); blk[:64, :64] = 1; blk[64:, 64:] = 1
    t = np.arange(512)
    mf = (t % 128 != 0).astype(np.float32); mb = (t % 128 != 127).astype(np.float32)
    return np.concatenate([np.tile(su, (1, 3)), np.tile(sl, (1, 3)), np.tile(u, (1, 3)), np.tile(l, (1, 3)), np.tile(I, (1, 3)), blk,
                           np.tile(mf[None], (128, 1)), np.tile(mb[None], (128, 1))], axis=1).astype(np.float32)

def rwkv_host_layout(q, mu_prev, mu_next, w0, w_up, a0, a_up, g_up, k_k, k_a, r_k, lnx_w, lnx_b):
    import numpy as np
    rwp = np.zeros((128, 55), np.float32)
    rkf = r_k.reshape(-1)
    for hh in range(3):
        ch = q * 192 + hh * 64 + np.arange(64)
        b = hh * 15
        for j, part in enumerate([0, 768, 1536]):
            rwp[:64, b + 2 * j] = mu_prev[part + ch]; rwp[:64, b + 2 * j + 1] = mu_next[part + ch]
        rwp[:64, b + 6] = k_k[ch]; rwp[:64, b + 7] = k_a[ch]; rwp[:64, b + 8] = rkf[ch]; rwp[:64, b + 9] = lnx_w[ch]; rwp[:64, b + 10] = lnx_b[ch]
        rwp[:64, b + 11] = w0[0, ch]; rwp[:64, b + 12] = w0[1, ch]; rwp[:64, b + 13] = a0[0, ch]; rwp[:64, b + 14] = a0[1, ch]
    for j, part in enumerate([2304, 2432]):
        for d in range(2):
            rwp[:64, 45 + 4 * j + 2 * d] = mu_prev[part + d * 64:part + (d + 1) * 64]
            rwp[:64, 46 + 4 * j + 2 * d] = mu_next[part + d * 64:part + (d + 1) * 64]
    rwp[:, 53] = mu_prev[2560:2688]; rwp[:, 54] = mu_next[2560:2688]
    cs = slice(q * 192, (q + 1) * 192)
    return dict(rwp=rwp, rw_wup=np.ascontiguousarray(np.stack([w_up[0][:, cs], w_up[1][:, cs]], 1)),
                rw_aup=np.ascontiguousarray(np.stack([a_up[0][:, cs], a_up[1][:, cs]], 1)),
                rw_gup=np.ascontiguousarray(g_up[:, cs]))

RW_DEBUG = [None]
def stage_rwkv(p, S, io):
    nc = p.nc
    NTT = S // 512; Q4 = S // 4
    zT = io['zT']
    H3 = range(3)
    with p.stage() as st:
        rwp = st.sb("rwp", [128, 55]); p.dma('sp', rwp[:, :], io['rwp'][:, :], reads=[io['rwp']], writes=[rwp])
        cst = st.sb("rwc", [128, RW_N]); p.dma('sp', cst[:, :], io['rwconst'][:, :], reads=[io['rwconst']], writes=[cst])
        ident_f = st.sb("ident_f", [128, 128]); ident = st.sb("ident", [128, 128], BF16)
        p.dma('sp', ident_f[:, :], io['ident'][:, :], reads=[io['ident']], writes=[ident_f])
        p.copy('dve', ident, ident[:, :], ident_f, ident_f[:, :])
        wtmp = st.sb("wtmp", [128, 384])
        wup = st.sb("wupb", [64, 2, 192], BF16); aup = st.sb("aupb", [64, 2, 192], BF16); gup = st.sb("gupb", [128, 192], BF16)
        p.dma('sp', wtmp[0:64, :], io['rw_wup'].ap.rearrange("r d c -> r (d c)"), reads=[io['rw_wup']], writes=[wtmp])
        p.copy('dve', wup, wup[:, :, :], wtmp, wtmp[0:64, :].rearrange("r (d c) -> r d c", d=2))
        p.dma('sp', wtmp[0:64, :], io['rw_aup'].ap.rearrange("r d c -> r (d c)"), reads=[io['rw_aup']], writes=[wtmp])
        p.copy('dve', aup, aup[:, :, :], wtmp, wtmp[0:64, :].rearrange("r (d c) -> r d c", d=2))
        p.dma('sp', wtmp[:, 0:192], io['rw_gup'][:, :], reads=[io['rw_gup']], writes=[wtmp])
        p.copy('dve', gup, gup[:, :], wtmp, wtmp[:, 0:192])
        c0 = st.sb("c0", [128, 14])
        pairs = [0, 2, 4, 15, 17, 19, 30, 32, 34, 45, 47, 49, 51, 53]
        for i, col in enumerate(pairs):
            p.tt('dve', c0, c0[:, i:i + 1], rwp, rwp[:, col:col + 1], rwp, rwp[:, col + 1:col + 2], ALU.add)
        p.ts('dve', c0, c0[:, :], c0, c0[:, :], -1.0, 1.0, ALU.mult, ALU.add)
        def fb(nm, P_=64, n=512, dt=F32): return st.sb(nm, [P_, n], dt)
        zh = [fb(f"zh{i}", 128, 514) for i in range(3)]
        sh = {}
        for hh in H3:
            for nm in ['r', 'k', 'v']:
                sh[(hh, nm)] = fb(f"sh{nm}{hh}")
        shwd = fb("shwd"); shad = fb("shad"); shgd = fb("shgd", 128)
        twd = fb("twd", dt=BF16); adb = fb("adb", dt=BF16); sgd = fb("sgd", 128, dt=BF16)
        lw = [fb(f"lw{h}") for h in H3]; cin = [fb(f"cin{h}") for h in H3]; cex = [fb(f"cex{h}") for h in H3]
        Ein = [fb(f"Ein{h}") for h in H3]; Eex = [fb(f"Eex{h}") for h in H3]; Eni = [fb(f"Eni{h}") for h in H3]
        aT = [fb(f"aT{h}") for h in H3]; kk = [fb(f"kk{h}") for h in H3]
        tA = [fb(f"tA{h}") for h in H3]; tB = [fb(f"tB{h}") for h in H3]
        at_ = [fb(f"at{h}", dt=BF16) for h in H3]; bt_ = [fb(f"bt{h}", dt=BF16) for h in H3]
        kt_ = [fb(f"kt{h}", dt=BF16) for h in H3]; rt_ = [fb(f"rt{h}", dt=BF16) for h in H3]
        vb_ = [fb(f"vb{h}", dt=BF16) for h in H3]
        tok = [st.sb(f"tok{i}", [128, 4, 192], BF16) for i in range(2)]
        W3 = lambda nm, dt=BF16: st.sb(nm, [128, 384], dt)
        M = [W3(f"M{i}") for i in range(2)]; N = [W3(f"N{i}") for i in range(2)]
        Pm = [W3(f"P{i}") for i in range(2)]; Qm = [W3(f"Q{i}") for i in range(2)]
        AKT = W3("AKT"); RBT = W3("RBT"); RKT = W3("RKT")
        AKV = st.sb("AKV", [128, 192], BF16); UV = st.sb("UV", [128, 192]); U = st.sb("U", [128, 192], BF16)
        TAT = st.sb("TAT", [64, 384], BF16)
        KVW = st.sb("KVW", [64, 192])
        S0 = st.sb("S0", [64, 192])
        S0b = [st.sb(f"S0b{i}", [64, 192], BF16) for i in range(2)]
        tS = st.sb("tS", [64, 192])
        ydall = st.sb("ydall", [64, 3, 512]); y0l = [fb(f"y0l{h}") for h in H3]
        pp1 = [fb(f"pp1{h}") for h in H3]; pp2 = [fb(f"pp2{h}") for h in H3]; pp3 = [fb(f"pp3{h}") for h in H3]
        yob = [fb(f"yob{h}", dt=BF16) for h in H3]
        pg = [st.ps(f"pg{i}", [128, 512]) for i in range(4)]
        pch = st.ps("pch", [128, 512])
        pY = [st.ps(f"pY{i}", [128, 512]) for i in range(2)]
        ptk = st.ps("ptk", [128, 1024], BF16)
        gcount = [0]
        def PG():
            gcount[0] += 1
            return pg[gcount[0] % 4]
        ones64 = cst[0:64, RW_BLK:RW_BLK + 64]
        def blocksum(src):
            G = PG()
            p.mm(G, G[0:64, :], cst, ones64, src, src[0:64, :])
            return G

        for d in range(2):
            p.memset('dve', S0, S0[:, :], 0.0)
            p.memset('pool', S0b[0], S0b[0][:, :], 0.0)
            p.memset('pool', S0b[1], S0b[1][:, :], 0.0)
            order = list(range(NTT)) if d == 0 else list(range(NTT - 1, -1, -1))
            mSU = RW_SU if d == 0 else RW_SL; mSL = RW_SL if d == 0 else RW_SU; mU = RW_U if d == 0 else RW_L
            gchunk = 0
            for tt in order:
                t0 = tt * 512
                r_ = t0 // Q4; off = t0 - r_ * Q4
                hbc = [0]
                def shift(row0, P_, dst, c0col, mpcol):
                    z = zh[hbc[0] % 3]; hbc[0] += 1
                    lo = max(t0 - 1, 0); hi = min(t0 + 513, S)
                    if t0 == 0: p.memset('pool', z, z[0:P_, 0:1], 0.0)
                    if t0 + 513 > S: p.memset('pool', z, z[0:P_, 513:514], 0.0)
                    p.dma('sp', z[0:P_, lo - (t0 - 1):hi - (t0 - 1)], zT[row0:row0 + P_, lo:hi], reads=[zT], writes=[z])
                    p.ts('pool', dst, dst[0:P_, :], z, z[0:P_, 1:513], c0[0:P_, c0col:c0col + 1], None, ALU.mult, extra_reads=[c0])
                    p.stt(dst, dst[0:P_, :], z, z[0:P_, 0:512], rwp[0:P_, mpcol:mpcol + 1], dst, dst[0:P_, :], ALU.mult, ALU.add, extra_reads=[rwp])
                    p.stt(dst, dst[0:P_, :], z, z[0:P_, 2:514], rwp[0:P_, mpcol + 1:mpcol + 2], dst, dst[0:P_, :], ALU.mult, ALU.add, extra_reads=[rwp])
                for hh in H3:
                    for j, (nm, zrow) in enumerate([('r', ZR), ('k', ZK), ('v', ZV)]):
                        shift(zrow + hh * 64, 64, sh[(hh, nm)], hh * 3 + j, hh * 15 + 2 * j)
                shift(ZWD + d * 64, 64, shwd, 9 + d, 45 + 2 * d)
                shift(ZAD + d * 64, 64, shad, 11 + d, 49 + 2 * d)
                p.act(twd, twd[:, :], shwd, shwd[:, :], AF.Tanh)
                p.copy('pool', adb, adb[:, :], shad, shad[:, :])
                if d == 1:
                    shift(ZGD, 128, shgd, 13, 53)
                    p.act(sgd, sgd[:, :], shgd, shgd[:, :], AF.Sigmoid)
                for hh in H3:
                    b = hh * 15
                    cs_ = slice(hh * 64, (hh + 1) * 64)
                    G = PG()
                    p.mm(G, G[0:64, :], wup, wup[:, d, cs_], twd, twd[:, :])
                    p.act(lw[hh], lw[hh][:, :], G, G[0:64, :], AF.Sigmoid, bias=rwp[0:64, b + 11 + d:b + 12 + d], extra_reads=[rwp])
                    p.ts('pool', lw[hh], lw[hh][:, :], lw[hh], lw[hh][:, :], NEG_EXP_HALF, None, ALU.mult)
                    if d == 0:
                        p.op('dve', lambda: nc.vector.tensor_tensor_scan(out=cin[hh][:, :], data0=cst[0:64, RW_MF:RW_MF + 512], data1=lw[hh][:, :],
                                                                          initial=0.0, op0=ALU.mult, op1=ALU.add), reads=[cst, lw[hh]], writes=[cin[hh]])
                    else:
                        mbv = cst[0:64, RW_MB:RW_MB + 512]
                        p.op('dve', lambda: nc.vector.tensor_tensor_scan(out=cin[hh][:, ::-1], data0=mbv[:, ::-1], data1=lw[hh][:, ::-1],
                                                                          initial=0.0, op0=ALU.mult, op1=ALU.add), reads=[cst, lw[hh]], writes=[cin[hh]])
                    p.tt('pool', cex[hh], cex[hh][:, :], cin[hh], cin[hh][:, :], lw[hh], lw[hh][:, :], ALU.subtract)
                    p.act(Ein[hh], Ein[hh][:, :], cin[hh], cin[hh][:, :], AF.Exp)
                    p.act(Eex[hh], Eex[hh][:, :], cex[hh], cex[hh][:, :], AF.Exp)
                    p.act(Eni[hh], Eni[hh][:, :], cin[hh], cin[hh][:, :], AF.Exp, scale=-1.0)
                    G = PG()
                    p.mm(G, G[0:64, :], aup, aup[:, d, cs_], adb, adb[:, :])
                    p.act(aT[hh], aT[hh][:, :], G, G[0:64, :], AF.Sigmoid, bias=rwp[0:64, b + 13 + d:b + 14 + d], extra_reads=[rwp])
                    ks = sh[(hh, 'k')]
                    p.ts('pool', kk[hh], kk[hh][:, :], ks, ks[:, :], rwp[0:64, b + 6:b + 7], None, ALU.mult, extra_reads=[rwp])
                    p.tt('pool', tA[hh], tA[hh][:, :], kk[hh], kk[hh][:, :], kk[hh], kk[hh][:, :], ALU.mult)
                    G = blocksum(tA[hh])
                    p.act(tB[hh], tB[hh][:, :], G, G[0:64, :], AF.Sqrt)
                    p.ts('dve', tB[hh], tB[hh][:, :], tB[hh], tB[hh][:, :], 1e-12, None, ALU.max)
                    p.op('dve', lambda: nc.vector.reciprocal(out=tB[hh][:, :], in_=tB[hh][:, :]), reads=[tB[hh]], writes=[tB[hh]])
                    p.tt('dve', kk[hh], kk[hh][:, :], kk[hh], kk[hh][:, :], tB[hh], tB[hh][:, :], ALU.mult)
                    p.stt(at_[hh], at_[hh][:, :], kk[hh], kk[hh][:, :], -1.0, Eex[hh], Eex[hh][:, :], ALU.mult, ALU.mult)
                    p.tt('pool', tA[hh], tA[hh][:, :], kk[hh], kk[hh][:, :], aT[hh], aT[hh][:, :], ALU.mult)
                    p.tt('pool', bt_[hh], bt_[hh][:, :], tA[hh], tA[hh][:, :], Eni[hh], Eni[hh][:, :], ALU.mult)
                    p.ts('dve', tB[hh], tB[hh][:, :], aT[hh], aT[hh][:, :], -1.0, rwp[0:64, b + 7:b + 8], ALU.add, ALU.mult, extra_reads=[rwp])
                    p.stt(tB[hh], tB[hh][:, :], tB[hh], tB[hh][:, :], 1.0, ks, ks[:, :], ALU.add, ALU.mult)
                    p.tt('dve', kt_[hh], kt_[hh][:, :], tB[hh], tB[hh][:, :], Eni[hh], Eni[hh][:, :], ALU.mult)
                    rs_ = sh[(hh, 'r')]
                    p.tt('pool', rt_[hh], rt_[hh][:, :], rs_, rs_[:, :], Ein[hh], Ein[hh][:, :], ALU.mult)
                    p.copy('pool', vb_[hh], vb_[hh][:, :], sh[(hh, 'v')], sh[(hh, 'v')][:, :])
                if RW_DEBUG[0] == 'prep': return
                corder = range(4) if d == 0 else range(3, -1, -1)
                for c4 in corder:
                    cs = slice(c4 * 128, (c4 + 1) * 128)
                    widx = c4 * 128 + 127 if d == 0 else c4 * 128
                    tk = tok[gchunk % 2]
                    cur = S0b[gchunk % 2]; nxt = S0b[(gchunk + 1) % 2]
                    Yp = pY[gchunk % 2]
                    gchunk += 1
                    for j, srcs in enumerate([at_, bt_, kt_, vb_]):
                        for hh in H3:
                            p.tr(ptk, ptk[:, j * 192 + hh * 64:j * 192 + (hh + 1) * 64], srcs[hh], srcs[hh][:, cs], ident, ident[0:64, 0:64])
                    p.act(tk, tk[:, :, :], ptk, ptk[:, 0:768].rearrange("p (j c) -> p j c", j=4), AF.Copy)
                    if RW_DEBUG[0] == 'tok': return
                    HS = [slice(hh * 128, (hh + 1) * 128) for hh in H3]
                    VS = [slice(hh * 64, (hh + 1) * 64) for hh in H3]
                    GM = PG(); GN = PG()
                    for hh in H3:
                        p.mm(GM, GM[:, HS[hh]], bt_[hh], bt_[hh][:, cs], at_[hh], at_[hh][:, cs])
                        p.mm(GN, GN[:, HS[hh]], at_[hh], at_[hh][:, cs], bt_[hh], bt_[hh][:, cs])
                    p.tt('dve', M[0], M[0][:, :], GM, GM[:, 0:384], cst, cst[:, mSU:mSU + 384], ALU.mult)
                    p.tt('dve', N[0], N[0][:, :], GN, GN[:, 0:384], cst, cst[:, mSL:mSL + 384], ALU.mult)
                    p.tt('pool', Pm[0], Pm[0][:, :], M[0], M[0][:, :], cst, cst[:, RW_I:RW_I + 384], ALU.add)
                    p.tt('pool', Qm[0], Qm[0][:, :], N[0], N[0][:, :], cst, cst[:, RW_I:RW_I + 384], ALU.add)
                    G1 = PG(); G2 = PG()
                    for hh in H3:
                        p.mm(G1, G1[:, HS[hh]], kt_[hh], kt_[hh][:, cs], at_[hh], at_[hh][:, cs])
                        p.mm(G2, G2[:, HS[hh]], bt_[hh], bt_[hh][:, cs], rt_[hh], rt_[hh][:, cs])
                    p.tt('dve', AKT, AKT[:, :], G1, G1[:, 0:384], cst, cst[:, mSU:mSU + 384], ALU.mult)
                    p.tt('dve', RBT, RBT[:, :], G2, G2[:, 0:384], cst, cst[:, mU:mU + 384], ALU.mult)
                    G3 = PG()
                    for hh in H3:
                        p.mm(G3, G3[:, HS[hh]], kt_[hh], kt_[hh][:, cs], rt_[hh], rt_[hh][:, cs])
                    p.tt('dve', RKT, RKT[:, :], G3, G3[:, 0:384], cst, cst[:, mU:mU + 384], ALU.mult)
                    if RW_DEBUG[0] == 'step1': return
                    for lev in range(1, 7):
                        Mo, No = M[(lev - 1) % 2], N[(lev - 1) % 2]; Mn, Nn = M[lev % 2], N[lev % 2]
                        Po, Qo = Pm[(lev - 1) % 2], Qm[(lev - 1) % 2]; Pn, Qn = Pm[lev % 2], Qm[lev % 2]
                        GM = PG()
                        for hh in H3:
                            p.mm(GM, GM[:, HS[hh]], No, No[:, HS[hh]], Mo, Mo[:, HS[hh]])
                        p.act(Mn, Mn[:, :], GM, GM[:, 0:384], AF.Copy)
                        if lev < 6:
                            GN = PG()
                            for hh in H3:
                                p.mm(GN, GN[:, HS[hh]], Mo, Mo[:, HS[hh]], No, No[:, HS[hh]])
                            p.act(Nn, Nn[:, :], GN, GN[:, 0:384], AF.Copy)
                        GP = PG()
                        for hh in H3:
                            p.mm(GP, GP[:, HS[hh]], Qo, Qo[:, HS[hh]], Mn, Mn[:, HS[hh]])
                        p.tt('dve', Pn, Pn[:, :], GP, GP[:, 0:384], Po, Po[:, :], ALU.add)
                        if lev < 6:
                            GQ = PG()
                            for hh in H3:
                                p.mm(GQ, GQ[:, HS[hh]], Po, Po[:, HS[hh]], Nn, Nn[:, HS[hh]])
                            p.tt('dve', Qn, Qn[:, :], GQ, GQ[:, 0:384], Qo, Qo[:, :], ALU.add)
                    PT = Pm[0]
                    if RW_DEBUG[0] == 'step2': return
                    G = PG()
                    for hh in H3:
                        p.mm(G, G[:, VS[hh]], AKT, AKT[:, HS[hh]], tk, tk[:, 3, VS[hh]])
                    p.act(AKV, AKV[:, :], G, G[:, 0:192], AF.Copy)
                    G = PG()
                    for hh in H3:
                        p.mm(G, G[:, VS[hh]], PT, PT[:, HS[hh]], AKV, AKV[:, VS[hh]])
                    p.act(UV, UV[:, :], G, G[:, 0:192], AF.Copy)
                    G = PG()
                    for hh in H3:
                        p.mm(G, G[0:64, HS[hh]], tk, tk[:, 0, VS[hh]], PT, PT[:, HS[hh]])
                    p.act(TAT, TAT[:, :], G, G[0:64, 0:384], AF.Copy)
                    G = PG()
                    for hh in H3:
                        p.mm(G, G[0:64, VS[hh]], tk, tk[:, 2, VS[hh]], tk, tk[:, 3, VS[hh]])
                    for hh in H3:
                        p.ts('dve', KVW, KVW[:, VS[hh]], G, G[0:64, VS[hh]], Ein[hh][:, widx:widx + 1], None, ALU.mult, extra_reads=[Ein[hh]])
                    if RW_DEBUG[0] == 'step3': return
                    for hh in H3:
                        p.mm(pch, pch[:, VS[hh]], TAT, TAT[:, HS[hh]], cur, cur[:, VS[hh]])
                    p.tt('dve', U, U[:, :], pch, pch[:, 0:192], UV, UV[:, :], ALU.add)
                    for hh in H3:
                        p.mm(Yp, Yp[0:64, HS[hh]], cur, cur[:, VS[hh]], rt_[hh], rt_[hh][:, cs], start=True, stop=False)
                        p.mm(Yp, Yp[0:64, HS[hh]], U, U[:, VS[hh]], RBT, RBT[:, HS[hh]], start=False, stop=False)
                        p.mm(Yp, Yp[0:64, HS[hh]], tk, tk[:, 3, VS[hh]], RKT, RKT[:, HS[hh]], start=False, stop=True)
                    p.act(ydall, ydall[:, :, cs], Yp, Yp[0:64, 0:384].rearrange("p (h t) -> p h t", h=3), AF.Copy)
                    for hh in H3:
                        p.mm(pch, pch[0:64, 256 + hh * 64:256 + (hh + 1) * 64], tk, tk[:, 1, VS[hh]], U, U[:, VS[hh]])
                    p.tt('dve', tS, tS[:, :], pch, pch[0:64, 256:448], S0, S0[:, :], ALU.add)
                    for hh in H3:
                        p.stt(S0, S0[:, VS[hh]], tS, tS[:, VS[hh]], Ein[hh][:, widx:widx + 1], KVW, KVW[:, VS[hh]], ALU.mult, ALU.add, extra_reads=[Ein[hh]])
                    p.act(nxt, nxt[:, :], S0, S0[:, :], AF.Copy)
                    if RW_DEBUG[0] == 'chunk': return
                for hh in H3:
                    b = hh * 15
                    rows = slice(hh * 64, (hh + 1) * 64)
                    if d == 0:
                        p.dma('pool', io['y0'][rows, t0:t0 + 512], ydall[:, hh, :], reads=[ydall], writes=[io['y0']])
                    else:
                        p.dma('sp', y0l[hh][:, :], io['y0'][rows, t0:t0 + 512], reads=[io['y0']], writes=[y0l[hh]])
                        y = pp3[hh]
                        p.tt('dve', y, y[:, :], ydall, ydall[:, hh, :], y0l[hh], y0l[hh][:, :], ALU.add)
                        G1 = blocksum(y)
                        p.tt('pool', pp1[hh], pp1[hh][:, :], y, y[:, :], y, y[:, :], ALU.mult)
                        G2 = blocksum(pp1[hh])
                        mean = pp2[hh]; var = tA[hh]
                        p.act(mean, mean[:, :], G1, G1[0:64, :], AF.Copy, scale=1.0 / 64)
                        p.tt('pool', pp1[hh], pp1[hh][:, :], mean, mean[:, :], mean, mean[:, :], ALU.mult)
                        p.stt(var, var[:, :], G2, G2[0:64, :], 1.0 / 64, pp1[hh], pp1[hh][:, :], ALU.mult, ALU.subtract)
                        p.act(var, var[:, :], var, var[:, :], AF.Sqrt, bias=64e-5)
                        p.op('dve', lambda: nc.vector.reciprocal(out=var[:, :], in_=var[:, :]), reads=[var], writes=[var])
                        p.tt('pool', y, y[:, :], y, y[:, :], mean, mean[:, :], ALU.subtract)
                        p.tt('pool', y, y[:, :], y, y[:, :], var, var[:, :], ALU.mult)
                        p.ts('dve', y, y[:, :], y, y[:, :], rwp[0:64, b + 9:b + 10], rwp[0:64, b + 10:b + 11], ALU.mult, ALU.add, extra_reads=[rwp])
                        rs_ = sh[(hh, 'r')]; ks = sh[(hh, 'k')]; vs_ = sh[(hh, 'v')]
                        p.stt(pp1[hh], pp1[hh][:, :], rs_, rs_[:, :], rwp[0:64, b + 8:b + 9], ks, ks[:, :], ALU.mult, ALU.mult, extra_reads=[rwp])
                        G3 = blocksum(pp1[hh])
                        p.tt('dve', pp2[hh], pp2[hh][:, :], G3, G3[0:64, :], vs_, vs_[:, :], ALU.mult)
                        p.tt('pool', y, y[:, :], y, y[:, :], pp2[hh], pp2[hh][:, :], ALU.add)
                        G4 = PG()
                        p.mm(G4, G4[0:64, :], gup, gup[:, rows], sgd, sgd[:, :])
                        p.tt('dve', yob[hh], yob[hh][:, :], y, y[:, :], G4, G4[0:64, :], ALU.mult)
                        p.dma('pool', io['yT'][r_, 256 + hh * 64:256 + (hh + 1) * 64, off:off + 512], yob[hh][:, :], reads=[yob[hh]], writes=[io['yT']])

ALPHA = float(2.0 ** 0.5)
LN_EPS = 1e-5

def out_chunks():
    ch = []
    for q in range(4):
        heads = [2 * q, 2 * q + 1] if q < 2 else [q + 2]
        for s, h in enumerate(heads):
            ch.append((q, s * 128, 128, h * 128))
        ch.append((q, 256, 128, 768 + q * 192))
        ch.append((q, 384, 64, 768 + q * 192 + 128))
        ch.append((q, 448, 128, 1536 + q * 128))
    return ch

def ln_tile(p, u, lnw, lnb, scr, outf, outb, pfx, eps=LN_EPS):
    nc = p.nc
    st6 = scr['st6']; mv = scr['mv']; rs = scr['rs']; nm = scr['nm']; xn = u
    for c in range(4):
        p.op('dve', lambda c=c: nc.vector.bn_stats(out=st6[:, c * 6:(c + 1) * 6], in_=u[:, c * 512:(c + 1) * 512]), reads=[u], writes=[st6])
    p.op('dve', lambda: nc.vector.bn_aggr(out=mv[:, 0:2], in_=st6[:, 0:24]), reads=[st6], writes=[mv])
    p.ts('dve', rs, rs[:, 0:1], mv, mv[:, 1:2], eps, None, ALU.add)
    p.act(rs, rs[:, 0:1], rs, rs[:, 0:1], AF.Sqrt)
    p.op('dve', lambda: nc.vector.reciprocal(out=rs[:, 0:1], in_=rs[:, 0:1]), reads=[rs], writes=[rs])
    p.ts('dve', nm, nm[:, 0:1], mv, mv[:, 0:1], rs[:, 0:1], -1.0, ALU.mult, ALU.mult, extra_reads=[rs])
    p.act(xn, xn[:, :], u, u[:, :], AF.Identity, bias=nm[:, 0:1], scale=rs[:, 0:1], extra_reads=[nm, rs])
    p.tt('pool', xn, xn[:, :], xn, xn[:, :], lnw, lnw[:, :], ALU.mult)
    p.tt('dve', outf, outf[:, :], xn, xn[:, :], lnb, lnb[:, :], ALU.add)
    if outb is not None:
        p.tt('pool', outb, outb[:, :], xn, xn[:, :], lnb, lnb[:, :], ALU.add)

def ln_scratch(st, pfx):
    return dict(st6=st.sb(pfx + "st6", [128, 24]), mv=st.sb(pfx + "mv", [128, 2]), rs=st.sb(pfx + "rs", [128, 1]),
                nm=st.sb(pfx + "nm", [128, 1]))

def load_bc(p, st, name, src_ap, n, q='sp'):
    t = st.sb(name, [128, n])
    p.dma(q, t[:, :], src_ap.partition_broadcast(128), writes=[t])
    return t

def transpose_tile(p, xb, ident, pst, xT, tcol, evac_e='act'):
    for k in range(16):
        p.tr(pst, pst[:, k * 128:(k + 1) * 128], xb, xb[:, k * 128:(k + 1) * 128], ident, ident[:, :])
    src = pst[:, :].rearrange("p (k t) -> p k t", k=16)
    dst = xT[:, :, tcol:tcol + 128]
    if evac_e == 'act':
        p.act(xT, dst, pst, src, AF.Copy)
    else:
        p.copy(evac_e, xT, dst, pst, src)

def cast_weights(p, srcs, dsts, n_per):
    with p.stage() as st:
        CH = 4096
        stg = [st.sb(f"cw_s{i}", [128, CH], F32) for i in range(3)]
        ob = [st.sb(f"cw_o{i}", [128, CH], BF16) for i in range(3)]
        i = 0
        engs = ['dve', 'pool', 'act']
        for (sT, sap), (dT, dap) in zip(srcs, dsts):
            n = sap.shape[1]
            for c0 in range(0, n, CH):
                w = min(CH, n - c0)
                s = stg[i % 3]; o = ob[i % 3]
                p.dma('sp', s[:, 0:w], sap[:, c0:c0 + w], reads=[sT], writes=[s])
                p.copy(engs[i % 3], o, o[:, 0:w], s, s[:, 0:w])
                p.dma('pool', dap[:, c0:c0 + w], o[:, 0:w], reads=[o], writes=[dT])
                i += 1

def phase2(p, T_, io, layer_last=False, ST=512):
    nc = p.nc
    NT = T_ // 128
    chunks = out_chunks()
    NCH = len(chunks)
    srcs = []; dsts = []
    for nm in ['w1', 'w3', 'w2']:
        s = io[nm]; d = io[nm + 'b']
        srcs.append((s, s.ap.rearrange("e a b -> (e a b)").rearrange("(p n) -> p n", p=128)))
        dsts.append((d, d.ap.rearrange("e a b -> (e a b)").rearrange("(p n) -> p n", p=128)))
    cast_weights(p, srcs, dsts, None)

    with p.stage() as st:
        ident_f = st.sb("ident_f", [128, 128]); ident = st.sb("ident", [128, 128], BF16)
        p.dma('sp', ident_f[:, :], io['ident'][:, :], reads=[io['ident']], writes=[ident_f])
        p.copy('dve', ident, ident[:, :], ident_f, ident_f[:, :])
        wout = st.sb("wout", [128, NCH, 2048], BF16)
        wst = [st.sb(f"wst{i}", [128, 2048]) for i in range(2)]
        for j, (q, off, sz, r0) in enumerate(chunks):
            s = wst[j % 2]
            p.dma('sp', s[0:sz, :], io['w_out'][r0:r0 + sz, :], reads=[io['w_out']], writes=[s])
            p.copy(['dve', 'pool'][j % 2], wout, wout[0:sz, j, :], s, s[0:sz, :])
        lnw = load_bc(p, st, "lnw", io['ln_w'][0:1, :], 2048); lnb = load_bc(p, st, "lnb", io['ln_b'][0:1, :], 2048)
        scr = ln_scratch(st, "a")
        gluw_f = st.sb("gluw_f", [128, 4, 512]); gluw = st.sb("gluw", [128, 4, 512], BF16); glub = st.sb("glub", [128, 4])
        p.dma('sp', gluw_f[:, :, :], io['glu_w'].ap.rearrange("(k p) c -> p k c", p=128), reads=[io['glu_w']], writes=[gluw_f])
        p.copy('dve', gluw, gluw[:, :, :], gluw_f, gluw_f[:, :, :])
        p.dma('sp', glub[:, :], io['glu_bc'][:, :], reads=[io['glu_bc']], writes=[glub])
        ys5 = [st.sb(f"ys5{i}", [128, 4, 512], BF16) for i in range(2)]
        sgl = st.sb("sgl", [128, 512])
        s5j = [j for j, (q, off, sz, r0) in enumerate(chunks) if off == 448]
        ybuf = [st.sb(f"ybuf{i}", [128, NCH, 512], BF16) for i in range(2)]
        xr = wst
        u = st.sb("u", [128, 2048])
        x1f = [st.sb(f"x1f{i}", [128, 2048]) for i in range(1)]
        x1b = st.sb("x1b", [128, 2048], BF16)
        x1T = [st.sb(f"x1T{i}", [128, 16, 512], BF16) for i in range(1)]
        pm = [st.ps(f"pm{i}", [128, 512]) for i in range(4)]
        pst = st.ps("pst", [128, 2048], BF16)
        x1T_d = io['x1T'].ap.rearrange("(k p) t -> p k t", p=128)
        GT = min(4, NT)
        for t in range(NT):
            g = t // GT; tt_ = t % GT
            yb = ybuf[g % 2]
            if tt_ == 0:
                for j, (q, off, sz, r0) in enumerate(chunks):
                    p.dma('sp', yb[0:sz, j, 0:GT * 128], io['yT'][q, off:off + sz, g * GT * 128:(g + 1) * GT * 128], reads=[io['yT']], writes=[yb])
                W_ = GT * 128
                y5 = ys5[g % 2]
                for qo in range(4):
                    G = pm[qo]
                    for qi in range(4):
                        p.mm(G, G[:, 0:W_], gluw, gluw[:, qi, qo * 128:(qo + 1) * 128], yb, yb[:, s5j[qi], 0:W_], start=(qi == 0), stop=(qi == 3))
                    p.act(sgl, sgl[:, 0:W_], G, G[:, 0:W_], AF.Sigmoid, bias=glub[:, qo:qo + 1], extra_reads=[glub])
                    p.tt('dve', y5, y5[:, qo, 0:W_], sgl, sgl[:, 0:W_], yb, yb[:, s5j[qo], 0:W_], ALU.mult)
            xrt = xr[t % 2]
            p.dma('sp', xrt[:, :], io['xres'][t * 128:(t + 1) * 128, :], reads=[io['xres']], writes=[xrt])
            for c in range(4):
                for j, (q, off, sz, r0) in enumerate(chunks):
                    if j in s5j:
                        y5 = ys5[g % 2]
                        p.mm(pm[c], pm[c][:, :], y5, y5[:, s5j.index(j), tt_ * 128:(tt_ + 1) * 128], wout, wout[0:sz, j, c * 512:(c + 1) * 512],
                             start=(j == 0), stop=(j == NCH - 1))
                    else:
                        p.mm(pm[c], pm[c][:, :], yb, yb[0:sz, j, tt_ * 128:(tt_ + 1) * 128], wout, wout[0:sz, j, c * 512:(c + 1) * 512],
                             start=(j == 0), stop=(j == NCH - 1))
                p.stt(u, u[:, c * 512:(c + 1) * 512], xrt, xrt[:, c * 512:(c + 1) * 512], ALPHA, pm[c], pm[c][:, :], ALU.mult, ALU.add)
            xf = x1f[0]
            ln_tile(p, u, lnw, lnb, scr, xf, x1b, "a")
            p.dma('pool', io['x1'][t * 128:(t + 1) * 128, :], xf[:, :], reads=[xf], writes=[io['x1']])
            xT = x1T[0]
            transpose_tile(p, x1b, ident, pst, xT, tt_ * 128)
            if tt_ == GT - 1:
                p.dma('pool', x1T_d[:, :, g * GT * 128:(g + 1) * GT * 128], xT[:, :, 0:GT * 128], reads=[xT], writes=[io['x1T']])

    with p.stage() as st:
        ident_f = st.sb("ident_f", [128, 128]); ident = st.sb("ident", [128, 128], BF16)
        p.dma('sp', ident_f[:, :], io['ident'][:, :], reads=[io['ident']], writes=[ident_f])
        p.copy('dve', ident, ident[:, :], ident_f, ident_f[:, :])
        wr_f = st.sb("wr_f", [128, 16, 20]); wr = st.sb("wr", [128, 16, 20], BF16)
        p.dma('sp', wr_f[:, :, 0:4], io['rg'].ap.rearrange("(k p) g -> p k g", p=128), reads=[io['rg']], writes=[wr_f])
        for g in range(4):
            p.dma('sp', wr_f[:, :, 4 + 4 * g:8 + 4 * g], io['re'][g].rearrange("(k p) e -> p k e", p=128), reads=[io['re']], writes=[wr_f])
        p.copy('dve', wr, wr[:, :, :], wr_f, wr_f[:, :, :])
        rb = st.sb("rb", [128, 20])
        p.dma('sp', rb[:, 0:4], io['rgb'].ap.rearrange("(o g) -> o g", o=1).partition_broadcast(128), reads=[io['rgb']], writes=[rb])
        p.dma('sp', rb[:, 4:20], io['reb'].ap.rearrange("(o g) e -> o (g e)", o=1).partition_broadcast(128), reads=[io['reb']], writes=[rb])
        NS = ST // 128
        x1T = [st.sb(f"mx1T{i}", [128, 16, ST], BF16) for i in range(2)]
        yacc = st.sb("yacc", [128, NS, 2048])
        gate = st.sb("gate", [128, NS, 16])
        w1b = [st.sb(f"w1b{i}", [128, 16, 512], BF16) for i in range(1)]
        w3b = [st.sb(f"w3b{i}", [128, 16, 512], BF16) for i in range(1)]
        w2b = [st.sb(f"w2b{i}", [128, 4, 2048], BF16) for i in range(1)]
        hT = [st.sb(f"hT{i}", [128, 4, ST], BF16) for i in range(2)]
        sl = [st.sb(f"sl{i}", [128, ST]) for i in range(2)]
        lg = st.sb("lg", [128, 20]); r1 = st.sb("r1", [128, 8]); mg = st.sb("mg", [128, 4]); es = st.sb("es", [128, 4])
        tmp16 = st.sb("tmp16", [128, 16]); m1 = st.sb("m1", [128, 4]); m2 = st.sb("m2", [128, 4]); e2 = st.sb("e2", [128, 4])
        gi = st.sb("gi", [128, 4])
        pa = [st.ps(f"pa{i}", [128, 512]) for i in range(2)]
        pb = [st.ps(f"pb{i}", [128, 512]) for i in range(2)]
        py = [st.ps(f"py{i}", [128, 512]) for i in range(2)]
        x1T_d = io['x1T'].ap.rearrange("(k p) t -> p k t", p=128)
        NSUP = T_ // ST
        wcount = 0
        for s in range(NSUP):
            xT = x1T[s % 2]
            p.dma('sp', xT[:, :, :], x1T_d[:, :, s * ST:(s + 1) * ST], reads=[io['x1T']], writes=[xT])
            for m in range(NS):
                pl = pa[m % 2]
                for k in range(16):
                    p.mm(pl, pl[:, 0:20], xT, xT[:, k, m * 128:(m + 1) * 128], wr, wr[:, k, :], start=(k == 0), stop=(k == 15))
                p.tt('dve', lg, lg[:, :], pl, pl[:, 0:20], rb, rb[:, :], ALU.add)
                p.op('dve', lambda: nc.vector.tensor_reduce(out=r1[:, 0:1], in_=lg[:, 0:4], axis=AX.X, op=ALU.max), reads=[lg], writes=[r1])
                p.ts('dve', mg, mg[:, :], lg, lg[:, 0:4], r1[:, 0:1], None, ALU.is_equal, extra_reads=[r1])
                p.ts('dve', r1, r1[:, 1:2], r1, r1[:, 0:1], -1.0, None, ALU.mult)
                p.act(tmp16, tmp16[:, 0:4], lg, lg[:, 0:4], AF.Exp, bias=r1[:, 1:2], extra_reads=[r1], accum=(r1, r1[:, 2:3]))
                p.op('dve', lambda: nc.vector.reciprocal(out=r1[:, 3:4], in_=r1[:, 2:3]), reads=[r1], writes=[r1])
                p.ts('dve', es, es[:, :], lg, lg[:, 4:8], mg[:, 0:1], None, ALU.mult, extra_reads=[mg])
                for g in range(1, 4):
                    p.stt(es, es[:, :], lg, lg[:, 4 + 4 * g:8 + 4 * g], mg[:, g:g + 1], es, es[:, :], ALU.mult, ALU.add, extra_reads=[mg])
                p.op('dve', lambda: nc.vector.tensor_reduce(out=r1[:, 4:5], in_=es[:, :], axis=AX.X, op=ALU.max), reads=[es], writes=[r1])
                p.ts('dve', m1, m1[:, :], es, es[:, :], r1[:, 4:5], None, ALU.is_equal, extra_reads=[r1])
                p.stt(e2, e2[:, :], m1, m1[:, :], -1e30, es, es[:, :], ALU.mult, ALU.add)
                p.op('dve', lambda: nc.vector.tensor_reduce(out=r1[:, 5:6], in_=e2[:, :], axis=AX.X, op=ALU.max), reads=[e2], writes=[r1])
                p.ts('dve', m2, m2[:, :], e2, e2[:, :], r1[:, 5:6], None, ALU.is_equal, extra_reads=[r1])
                p.tt('dve', r1, r1[:, 6:7], r1, r1[:, 4:5], r1, r1[:, 5:6], ALU.subtract)
                p.act(r1, r1[:, 6:7], r1, r1[:, 6:7], AF.Sigmoid)
                p.ts('dve', r1, r1[:, 7:8], r1, r1[:, 6:7], -1.0, 1.0, ALU.mult, ALU.add)
                p.ts('dve', gi, gi[:, :], m1, m1[:, :], r1[:, 6:7], None, ALU.mult, extra_reads=[r1])
                p.stt(gi, gi[:, :], m2, m2[:, :], r1[:, 7:8], gi, gi[:, :], ALU.mult, ALU.add, extra_reads=[r1])
                p.ts('dve', gi, gi[:, :], gi, gi[:, :], r1[:, 3:4], None, ALU.mult, extra_reads=[r1])
                for g in range(4):
                    p.ts('dve', gate, gate[:, m, 4 * g:4 * g + 4], gi, gi[:, :], mg[:, g:g + 1], None, ALU.mult, extra_reads=[mg])
            for e in range(16):
                wb = 0
                p.dma('sp', w1b[wb][:, :, :], io['w1b'][e].rearrange("(k p) f -> p k f", p=128), reads=[io['w1b']], writes=[w1b[wb]])
                p.dma('sp', w3b[wb][:, :, :], io['w3b'][e].rearrange("(k p) f -> p k f", p=128), reads=[io['w3b']], writes=[w3b[wb]])
                p.dma('sp', w2b[wb][:, :, :], io['w2b'][e].rearrange("(k p) f -> p k f", p=128), reads=[io['w2b']], writes=[w2b[wb]])
                h = hT[e % 2]
                for f in range(4):
                    a = pa[f % 2]; b = pb[f % 2]
                    for k in range(16):
                        p.mm(a, a[:, 0:ST], w1b[wb], w1b[wb][:, k, f * 128:(f + 1) * 128], xT, xT[:, k, :], start=(k == 0), stop=(k == 15))
                    for k in range(16):
                        p.mm(b, b[:, 0:ST], w3b[wb], w3b[wb][:, k, f * 128:(f + 1) * 128], xT, xT[:, k, :], start=(k == 0), stop=(k == 15))
                    s_ = sl[f % 2]
                    p.act(s_, s_[:, :], a, a[:, 0:ST], AF.Silu)
                    p.tt('dve', h, h[:, f, :], s_, s_[:, :], b, b[:, 0:ST], ALU.mult)
                i = 0
                for m in range(NS):
                    for c in range(4):
                        y = py[i % 2]; i += 1
                        for f in range(4):
                            p.mm(y, y[:, :], h, h[:, f, m * 128:(m + 1) * 128], w2b[wb], w2b[wb][:, f, c * 512:(c + 1) * 512], start=(f == 0), stop=(f == 3))
                        if e == 0:
                            p.ts('dve', yacc, yacc[:, m, c * 512:(c + 1) * 512], y, y[:, :], gate[:, m, e:e + 1], None, ALU.mult, extra_reads=[gate])
                        else:
                            p.stt(yacc, yacc[:, m, c * 512:(c + 1) * 512], y, y[:, :], gate[:, m, e:e + 1], yacc, yacc[:, m, c * 512:(c + 1) * 512],
                                  ALU.mult, ALU.add, extra_reads=[gate])
            for m in range(NS):
                t = s * NS + m
                p.dma('pool', io['moe'][t * 128:(t + 1) * 128, :], yacc[:, m, :], reads=[yacc], writes=[io['moe']])

    with p.stage() as st:
        ident_f = st.sb("ident_f", [128, 128]); ident = st.sb("ident", [128, 128], BF16)
        p.dma('sp', ident_f[:, :], io['ident'][:, :], reads=[io['ident']], writes=[ident_f])
        p.copy('dve', ident, ident[:, :], ident_f, ident_f[:, :])
        lnw2 = load_bc(p, st, "lnw2", io['ln_w'][1:2, :], 2048); lnb2 = load_bc(p, st, "lnb2", io['ln_b'][1:2, :], 2048)
        lnw3 = load_bc(p, st, "lnw3", io['ln_w'][2:3, :], 2048); lnb3 = load_bc(p, st, "lnb3", io['ln_b'][2:3, :], 2048)
        scr = ln_scratch(st, "c")
        pg = st.sb("pg", [128, 16, 2048], BF16); pp = st.sb("pp", [128, 2, 2048], BF16)
        wst = [st.sb(f"wst{i}", [128, 2048]) for i in range(2)]
        for k in range(16):
            s = wst[k % 2]
            p.dma('sp', s[:, :], io['ple_gate'][k * 128:(k + 1) * 128, :], reads=[io['ple_gate']], writes=[s])
            p.copy(['dve', 'pool'][k % 2], pg, pg[:, k, :], s, s[:, :])
        for k in range(2):
            s = wst[k % 2]
            p.dma('sp', s[:, :], io['ple_proj'][k * 128:(k + 1) * 128, :], reads=[io['ple_proj']], writes=[s])
            p.copy(['dve', 'pool'][k % 2], pp, pp[:, k, :], s, s[:, :])
        xr = wst[0]; mo = wst[1]
        x2T = st.sb("cx2T", [128, 16, 128], BF16)
        pTf = st.sb("pTf", [128, 2, 128]); pTb = st.sb("pTb", [128, 2, 128], BF16)
        sg = st.sb("sg", [128, 512])
        u = st.sb("u", [128, 2048])
        x2f = st.sb("x2f", [128, 2048]); x2b = st.sb("x2b", [128, 2048], BF16)
        x3f = st.sb("x3f", [128, 2048]); x3b = st.sb("x3b", [128, 2048], BF16)
        x3T = [st.sb(f"x3T{i}", [128, 16, 512], BF16) for i in range(2)]
        pgp = [st.ps(f"pgp{i}", [128, 512]) for i in range(2)]
        ppp = [st.ps(f"ppp{i}", [128, 512]) for i in range(2)]
        pst = st.ps("pst", [128, 2048], BF16)
        xoT_d = io['xoT'].ap.rearrange("(k p) t -> p k t", p=128)
        pT_d = io['pT'].ap.rearrange("(k p) t -> p k t", p=128)
        GT = min(4, NT)
        for t in range(NT):
            g = t // GT; tt_ = t % GT
            rows = slice(t * 128, (t + 1) * 128)
            p.dma('sp', xr[:, :], io['x1'][rows, :], reads=[io['x1']], writes=[xr])
            p.dma('sp', mo[:, :], io['moe'][rows, :], reads=[io['moe']], writes=[mo])
            p.dma('sp', pTf[:, :, :], pT_d[:, :, rows], reads=[io['pT']], writes=[pTf])
            p.copy('pool', pTb, pTb[:, :, :], pTf, pTf[:, :, :])
            p.stt(u, u[:, :], xr, xr[:, :], ALPHA, mo, mo[:, :], ALU.mult, ALU.add)
            ln_tile(p, u, lnw2, lnb2, scr, x2f, x2b, "c")
            transpose_tile(p, x2b, ident, pst, x2T, 0)
            for c in range(4):
                cs = slice(c * 512, (c + 1) * 512)
                G = pgp[c % 2]; PP = ppp[c % 2]
                for k in range(16):
                    p.mm(G, G[:, :], x2T, x2T[:, k, :], pg, pg[:, k, cs], start=(k == 0), stop=(k == 15))
                for k in range(2):
                    p.mm(PP, PP[:, :], pTb, pTb[:, k, :], pp, pp[:, k, cs], start=(k == 0), stop=(k == 1))
                p.act(sg, sg[:, :], G, G[:, :], AF.Sigmoid)
                p.tt('dve', sg, sg[:, :], sg, sg[:, :], PP, PP[:, :], ALU.mult)
                p.stt(u, u[:, cs], x2f, x2f[:, cs], ALPHA, sg, sg[:, :], ALU.mult, ALU.add)
            ln_tile(p, u, lnw3, lnb3, scr, x3f, x3b, "c")
            p.dma('pool', io['xo'][rows, :], x3f[:, :], reads=[x3f], writes=[io['xo']])
            x3 = x3T[g % 2]
            transpose_tile(p, x3b, ident, pst, x3, tt_ * 128)
            if tt_ == GT - 1:
                p.dma('pool', xoT_d[:, :, g * GT * 128:(g + 1) * GT * 128], x3[:, :, 0:GT * 128], reads=[x3], writes=[io['xoT']])


_S = 16384
_D = 2048
_T = 4096
_PROGS = {}
_GROUPS = [[0, 1, 2, 3], [4, 5, 6, 7]]
_P1_KEYS = [('w_in', [_D, NZ]), ('rwp', [128, 55]), ('rw_wup', [64, 2, 192]), ('rw_aup', [64, 2, 192]), ('rw_gup', [128, 192]),
            ('s5rows', [3, 1024]), ('s5cols', [128, 24]), ('s5bl', [2, 128, 1024]), ('s5cl', [2, 128, 1024]), ('s5d', [128, 1])]
_P2_KEYS = [('w_out', [_D, _D]), ('ln_w', [3, _D]), ('ln_b', [3, _D]), ('rg', [_D, 4]), ('rgb', [4]), ('re', [4, _D, 4]), ('reb', [4, 4]),
            ('w1', [16, _D, 512]), ('w3', [16, _D, 512]), ('w2', [16, 512, _D]), ('glu_w', [512, 512]), ('glu_bc', [128, 4]),
            ('ple_proj', [256, _D]), ('ple_gate', [_D, _D]), ('pT', [256, None])]

def stage_select(p, gath, rmask, yTr):
    with p.stage() as st:
        mk = st.sb("mk", [128, 4]); p.dma('sp', mk[:, :], rmask[:, :], reads=[rmask], writes=[mk])
        cand = [[st.sb(f"cand{i}_{r}", [128, _T], BF16) for r in range(4)] for i in range(2)]
        acc = [st.sb(f"acc{i}", [128, _T], BF16) for i in range(2)]
        n = 0
        for q in range(4):
            for i in range(6):
                r0 = i * 96; sz = 96
                cd = cand[n % 2]; a = acc[n % 2]
                e = 'dve'; n += 1
                for r in range(4):
                    p.dma('sp', cd[r][0:sz, :], gath[r * 6 + i, q * 96:(q + 1) * 96, :], reads=[gath], writes=[cd[r]])
                p.ts(e, a, a[0:sz, :], cd[0], cd[0][0:sz, :], mk[0:sz, 0:1], None, ALU.mult, extra_reads=[mk])
                for r in range(1, 4):
                    p.stt(a, a[0:sz, :], cd[r], cd[r][0:sz, :], mk[0:sz, r:r + 1], a, a[0:sz, :], ALU.mult, ALU.add, extra_reads=[mk], e=e)
                p.dma('pool', yTr[q, r0:r0 + sz, :], a[0:sz, :], reads=[a], writes=[yTr])

def _build_fused():
    S = _S; D = _D; T_ = _T
    nc = bass.Bass("TRN2", target_bir_lowering=False)
    p = Prog(nc)
    E = {}
    def ext(name, shape, dt=F32):
        E[name] = p.dram(name, shape, dt, kind="ExternalInput"); return E[name]
    ext('xT0', [4, D, T_]); ext('xres', [T_, D]); ext('pos', [S], I32); ext('rconst', [128, RC_N]); ext('ident', [128, 128])
    ext('rwconst', [128, RW_N]); ext('s5iota', [128, 512]); ext('rmask', [128, 4])
    for L in range(2):
        for k, sh in _P1_KEYS + _P2_KEYS:
            ext(f"{k}_{L}", [T_ if s is None else s for s in sh])
    xo_final = p.dram('xo_final', [T_, D], F32, kind="ExternalOutput")
    sc = {}
    sc['zT'] = p.dram('zT', [NZ, S], F32)
    for nm in ['qrT', 'krT']: sc[nm] = p.dram(nm, [2, 128, S], BF16)
    for nm in ['ktok', 'vtok', 'sbd']: sc[nm] = p.dram(nm, [2, S // 128, 128, 128], BF16)
    sc['yf'] = p.dram('yf', [128, S]); sc['y0'] = p.dram('y0', [192, S])
    yTloc = p.dram('yTloc', [4, 576, T_], BF16)
    gath = p.dram('gath', [24, 4 * 96, T_], BF16)
    yTr = p.dram('yTr', [4, 576, T_], BF16)
    sc2 = dict(x1=p.dram('x1', [T_, D]), x1T=p.dram('x1T', [D, T_], BF16), moe=p.dram('moe', [T_, D]),
               w1b=p.dram('w1b', [16, D, 512], BF16), w3b=p.dram('w3b', [16, D, 512], BF16), w2b=p.dram('w2b', [16, 512, D], BF16))
    xo0 = p.dram('xo0', [T_, D]); xoT = p.dram('xoT', [D, T_], BF16); xoT_dummy = p.dram('xoT_dummy', [D, T_], BF16)
    xTg = p.dram('xTg', [16, 4 * 128, T_], BF16)
    for L in range(2):
        io1 = dict(sc)
        for k, _ in _P1_KEYS: io1[k] = E[f"{k}_{L}"]
        io1.update(pos=E['pos'], rconst=E['rconst'], ident=E['ident'], rwconst=E['rwconst'], s5iota=E['s5iota'], yT=yTloc)
        io1['xT'] = E['xT0'] if L == 0 else xTg
        xsrc = None
        if L == 1:
            xsrc = lambda r: xTg.ap[:, r * 128:(r + 1) * 128, :].rearrange("k p t -> p k t")
        stage_inproj(p, S, io1, L == 0, xsrc=xsrc)
        stage_retention(p, S, io1)
        stage_s5(p, S, io1)
        stage_rwkv(p, S, io1)
        for r in range(4):
            for i in range(6):
                p.collective("AllGather", yTloc, yTloc.ap[r, i * 96:(i + 1) * 96, :], gath, gath.ap[r * 6 + i], _GROUPS)
        stage_select(p, gath, E['rmask'], yTr)
        io2 = dict(sc2)
        for k, _ in _P2_KEYS: io2[k] = E[f"{k}_{L}"]
        io2.update(yT=yTr, ident=E['ident'], xres=(E['xres'] if L == 0 else xo0), xo=(xo0 if L == 0 else xo_final),
                   xoT=(xoT if L == 0 else xoT_dummy))
        phase2(p, T_, io2, ST=512)
        if L == 0:
            for k in range(16):
                p.collective("AllGather", xoT, xoT.ap[k * 128:(k + 1) * 128, :], xTg, xTg.ap[k], _GROUPS)
    p.finish([xo_final])
    return nc

def kernel(**inp):
    x = np.ascontiguousarray(np.asarray(inp['x'], dtype=np.float32))
    S = _S; D = _D
    if 'f' not in _PROGS:
        _PROGS['f'] = _build_fused()
    ident = np.eye(128, dtype=np.float32)
    rwc = rwkv_consts()
    positions = np.asarray(inp['positions']).astype(np.int32)
    g = lambda k: np.asarray(inp[k])
    maps = []
    for c in range(8):
        b, q = c // 4, c % 4
        r = q
        rmask = np.zeros((128, 4), np.float32); rmask[:, r] = 1.0
        m = dict(xT0=np.ascontiguousarray(x[b].T.reshape(D, 4, S // 4).transpose(1, 0, 2)),
                 xres=np.ascontiguousarray(x[b, r * _T:(r + 1) * _T]), pos=np.ascontiguousarray(positions[b]),
                 rconst=ret_consts(q), ident=ident, rwconst=rwc, rmask=rmask)
        for L in range(2):
            d1 = dict(w_in=np.ascontiguousarray(g('w_in')[L][:, core_cols(q)]))
            d1.update(rwkv_host_layout(q, g('rwkv_mu_prev')[L], g('rwkv_mu_next')[L], g('rwkv_w0')[L], g('rwkv_w_up')[L], g('rwkv_a0')[L],
                                       g('rwkv_a_up')[L], g('rwkv_g_up')[L], g('rwkv_k_k')[L], g('rwkv_k_a')[L], g('rwkv_r_k')[L],
                                       g('rwkv_lnx_w')[L], g('rwkv_lnx_b')[L]))
            s5 = s5_host_layout(q, g('s5_lam_re')[L], g('s5_lam_im')[L], g('s5_log_dt')[L], g('s5_b_re')[L], g('s5_b_im')[L],
                                g('s5_c_re')[L], g('s5_c_im')[L], g('s5_d')[L])
            m['s5iota'] = s5.pop('s5iota')
            d1.update(s5)
            d2 = dict(w_out=g('w_out')[L], ln_w=g('ln_w')[L], ln_b=g('ln_b')[L],
                      rg=g('moe_router_g')[L], rgb=g('moe_router_g_b')[L], re=g('moe_router_e')[L], reb=g('moe_router_e_b')[L],
                      w1=g('moe_w1')[L], w3=g('moe_w3')[L], w2=g('moe_w2')[L],
                      glu_w=g('s5_glu_w')[L], glu_bc=np.ascontiguousarray(g('s5_glu_b')[L].reshape(4, 128).T),
                      ple_proj=g('ple_proj')[L], ple_gate=g('ple_gate')[L],
                      pT=np.ascontiguousarray(g('p')[L, b, r * _T:(r + 1) * _T].T))
            for k, v in list(d1.items()) + list(d2.items()):
                m[f"{k}_{L}"] = np.ascontiguousarray(v)
        maps.append(m)
    res = run_bass_kernel_spmd(_PROGS['f'], maps, core_ids=list(range(8)))
    out = np.empty_like(x)
    for c in range(8):
        b, r = c // 4, c % 4
        out[b, r * _T:(r + 1) * _T] = np.asarray(res.results[c]['xo_final'])
    return out
```

```python
import math
import numpy as np
import concourse.bass as bass
import concourse.mybir as mybir
from concourse.bass_utils import run_bass_kernel_spmd
from contextlib import ExitStack
F32 = mybir.dt.float32; BF16 = mybir.dt.bfloat16; I32 = mybir.dt.int32
AF = mybir.ActivationFunctionType; ALU = mybir.AluOpType; AX = mybir.AxisListType


class T:
    def __init__(self, ap, name):
        self.ap = ap; self.name = name
        self.w = None
        self.r = []
    def __getitem__(self, k):
        return self.ap[k]


class Prog:
    NDMA = 8
    SEM_MAX = 30000
    def __init__(self, nc):
        self.nc = nc
        self.eng = {'pe': nc.tensor, 'dve': nc.vector, 'act': nc.scalar, 'pool': nc.gpsimd, 'sp': nc.sync}
        self.gen = {e: 0 for e in ['pe', 'dve', 'act', 'pool']}
        self.sem = {e: nc.alloc_semaphore("sem_" + e) for e in ['pe', 'dve', 'act', 'pool']}
        self.cnt = {e: 0 for e in self.sem}
        self.seen = {e: {} for e in self.eng}
        self.dsem = {q: [nc.alloc_semaphore(f"dsem_{q}{i}") for i in range(self.NDMA)] for q in ['sp', 'pool']}
        self.dcnt = {q: [0] * self.NDMA for q in self.dsem}
        self.dgen = {q: [0] * self.NDMA for q in self.dsem}
        self.dnext = {q: 0 for q in self.dsem}
        self.semobj = {}
        for e, s in self.sem.items(): self.semobj[('c', e, 0)] = s
        for q in self.dsem:
            for i, s in enumerate(self.dsem[q]): self.semobj[('d', q, i, 0)] = s
        self.ninst = 0
        self.nsb = 0

    def sb(self, name, shape, dt=F32):
        return T(self.nc.alloc_sbuf_tensor(name, list(shape), dt).ap(), name)
    def ps(self, name, shape, dt=F32):
        return T(self.nc.alloc_psum_tensor(name, list(shape), dt).ap(), name)
    def dram(self, name, shape, dt=F32, kind="Internal"):
        return T(self.nc.dram_tensor(name, list(shape), dt, kind=kind).ap(), name)

    def _wait(self, e, tok):
        if tok is None: return
        key, val = tok
        if self.seen[e].get(key, 0) >= val: return
        self.eng[e].wait_ge(self.semobj[key], val)
        self.seen[e][key] = val

    def _deps(self, e, reads, writes):
        for b in reads:
            if b.w is not None and not (e == 'pe' and b.w[0][0:2] == ('c', 'pe')):
                self._wait(e, b.w)
        for b in writes:
            if b.w is not None and not (e == 'pe' and b.w[0][0:2] == ('c', 'pe')):
                self._wait(e, b.w)
            for tok in b.r:
                if e == 'pe' and tok[0][0:2] == ('c', 'pe'): continue
                self._wait(e, tok)

    def _mark(self, tok, reads, writes):
        for b in reads:
            b.r = [t for t in b.r if t[0] != tok[0]] + [tok]
        for b in writes:
            b.w = tok; b.r = []

    def op(self, e, fn, reads=(), writes=()):
        self._deps(e, reads, writes)
        if self.cnt[e] >= self.SEM_MAX:
            self.gen[e] += 1; self.cnt[e] = 0
            self.sem[e] = self.nc.alloc_semaphore(f"sem_{e}_{self.gen[e]}")
            self.semobj[('c', e, self.gen[e])] = self.sem[e]
        inst = fn()
        self.cnt[e] += 1
        inst.then_inc(self.sem[e], 1)
        tok = (('c', e, self.gen[e]), self.cnt[e])
        self._mark(tok, reads, writes)
        self.ninst += 1
        return inst

    def dma(self, q, out, in_, reads=(), writes=(), **kw):
        i = self.dnext[q]; self.dnext[q] = (i + 1) % self.NDMA
        key = ('d', q, i, self.dgen[q][i])
        if self.dcnt[q][i] > 0:
            self._wait(q, (key, self.dcnt[q][i]))
        if self.dcnt[q][i] >= self.SEM_MAX:
            self.dgen[q][i] += 1; self.dcnt[q][i] = 0
            self.dsem[q][i] = self.nc.alloc_semaphore(f"dsem_{q}{i}_{self.dgen[q][i]}")
            key = ('d', q, i, self.dgen[q][i])
            self.semobj[key] = self.dsem[q][i]
        self._deps(q, reads, writes)
        inst = self.eng[q].dma_start(out=out, in_=in_, **kw)
        self.dcnt[q][i] += 16
        inst.then_inc(self.dsem[q][i], 16)
        tok = (key, self.dcnt[q][i])
        self._mark(tok, reads, writes)
        self.ninst += 1
        return inst

    def finish(self, outs):
        for b in outs:
            self._wait('sp', b.w)
        for e in ['pe', 'dve', 'act', 'pool']:
            if self.cnt[e] > 0:
                self._wait('sp', (('c', e, self.gen[e]), self.cnt[e]))
        for q in self.dsem:
            for i in range(self.NDMA):
                if self.dcnt[q][i] > 0:
                    self._wait('sp', (('d', q, i, self.dgen[q][i]), self.dcnt[q][i]))

    def mm(self, out, o_ap, lhsT, l_ap, rhs, r_ap, start=True, stop=True):
        return self.op('pe', lambda: self.nc.tensor.matmul(o_ap, l_ap, r_ap, start=start, stop=stop),
                       reads=[lhsT, rhs], writes=[out])
    def tr(self, out, o_ap, in_, i_ap, ident, id_ap):
        return self.op('pe', lambda: self.nc.tensor.transpose(o_ap, i_ap, id_ap), reads=[in_, ident], writes=[out])
    def act(self, out, o_ap, in_, i_ap, func, bias=None, scale=1.0, extra_reads=(), e='act', accum=None):
        kw = {}
        if bias is not None: kw['bias'] = bias
        if accum is not None: kw['accum_out'] = accum[1]
        wr = [out] + ([accum[0]] if accum is not None else [])
        return self.op('act', lambda: self.nc.scalar.activation(out=o_ap, in_=i_ap, func=func, scale=scale, **kw),
                       reads=[in_] + list(extra_reads), writes=wr)
    def tt(self, e, out, o_ap, a, a_ap, b, b_ap, op):
        en = self.eng[e]
        return self.op(e, lambda: en.tensor_tensor(out=o_ap, in0=a_ap, in1=b_ap, op=op), reads=[a, b], writes=[out])
    def ts(self, e, out, o_ap, a, a_ap, s1, s2, op0, op1=None, extra_reads=(), accum=None):
        en = self.eng[e]
        kw = {}
        if op1 is not None: kw['op1'] = op1
        wr = [out]
        if accum is not None:
            kw['accum_out'] = accum[1]; wr.append(accum[0])
        return self.op(e, lambda: en.tensor_scalar(out=o_ap, in0=a_ap, scalar1=s1, scalar2=s2, op0=op0, **kw),
                       reads=[a] + list(extra_reads), writes=wr)
    def stt(self, out, o_ap, a, a_ap, s, b, b_ap, op0, op1, extra_reads=(), e='dve'):
        en = self.eng[e]
        return self.op(e, lambda: en.scalar_tensor_tensor(out=o_ap, in0=a_ap, scalar=s, in1=b_ap, op0=op0, op1=op1),
                       reads=[a, b] + list(extra_reads), writes=[out])
    def copy(self, e, out, o_ap, in_, i_ap):
        if e == 'act':
            return self.act(out, o_ap, in_, i_ap, AF.Copy)
        en = self.eng[e]
        return self.op(e, lambda: en.tensor_copy(out=o_ap, in_=i_ap), reads=[in_], writes=[out])
    def memset(self, e, out, o_ap, val):
        en = self.eng[e]
        return self.op(e, lambda: en.memset(o_ap, val), writes=[out])

class Stage:
    def __init__(self, p):
        self.p = p; self.es = ExitStack()
    def __enter__(self):
        self.es.__enter__(); return self
    def __exit__(self, *a):
        self.p.barrier()
        return self.es.__exit__(*a)
    def sb(self, name, shape, dt=F32):
        self.p.nsb += 1; name = f"s{self.p.nsb}_{name}"
        h = self.es.enter_context(self.p.nc.sbuf_tensor(name, list(shape), dt))
        return T(h.ap(), name)
    def ps(self, name, shape, dt=F32):
        self.p.nsb += 1; name = f"s{self.p.nsb}_{name}"
        h = self.es.enter_context(self.p.nc.psum_tensor(name, list(shape), dt))
        return T(h.ap(), name)

def _barrier(self):
    toks = []
    for e in ['pe', 'dve', 'act', 'pool']:
        if self.cnt[e] > 0: toks.append((('c', e, self.gen[e]), self.cnt[e]))
    for q in self.dsem:
        for i in range(self.NDMA):
            if self.dcnt[q][i] > 0: toks.append((('d', q, i, self.dgen[q][i]), self.dcnt[q][i]))
    for e in self.eng:
        for tok in toks:
            if tok[0][0:2] == ('c', e) and e == 'pe': continue
            self._wait(e, tok)
Prog.barrier = _barrier
Prog.stage = lambda self: Stage(self)

def _collective(self, kind, in_T, in_ap, out_T, out_ap, groups):
    if not hasattr(self, 'ccsem'):
        self.ccsem = self.nc.alloc_semaphore("ccsem"); self.ccnt = 0
        self.semobj[('cc',)] = self.ccsem
    self._deps('pool', [in_T], [out_T])
    inst = self.nc.gpsimd.collective_compute(kind, ALU.bypass, replica_groups=groups, ins=[in_ap], outs=[out_ap])
    self.ccnt += 1
    inst.then_inc(self.ccsem, 1)
    tok = (('cc',), self.ccnt)
    self._mark(tok, [in_T], [out_T])
    self.ninst += 1
Prog.collective = _collective

PI = math.pi
C1 = 6.28125
C2 = 2 * math.pi - C1
INV2PI = 1.0 / (2 * math.pi)

RET_W = 768; RWKV_W = 768; RET_COLS = 3072; RWKV_COLS = 2688
NZ = 2112
COLTILES = [(i * 128, 128) for i in range(8)] + [(1024, 128), (1152, 64), (1216, 128), (1344, 64), (1408, 128), (1536, 64),
                                                  (1600, 128), (1728, 128), (1856, 128), (1984, 128)]

def ret_heads(q):
    return [2 * q, 2 * q + 1] if q < 2 else [q + 2, q + 2]

def core_cols(q):
    cols = []
    for h in ret_heads(q):
        for part in range(4):
            cols += list(range(part * RET_W + h * 128, part * RET_W + (h + 1) * 128))
    base = RET_COLS
    for part in range(3):
        cols += list(range(base + part * RWKV_W + q * 192, base + part * RWKV_W + (q + 1) * 192))
    cols += list(range(base + 3 * RWKV_W, base + 3 * RWKV_W + 384))
    cols += list(range(RET_COLS + RWKV_COLS + q * 128, RET_COLS + RWKV_COLS + (q + 1) * 128))
    assert len(cols) == NZ
    return cols

def ret_consts(q):
    import numpy as np
    C = 128
    cols = []
    inv = (10000.0 ** (-(np.arange(128) % 64).astype(np.float32) / np.float32(64))).astype(np.float32)
    cols.append(inv[:, None]); cols.append(np.where(np.arange(128) < 64, -1.0, 1.0).astype(np.float32)[:, None])
    pos = np.arange(C, dtype=np.float64)
    for h in ret_heads(q):
        lg = np.log(1.0 - 2.0 ** (-5.0 - h))
        cols.append(np.exp(lg * (C - 1 - pos))[:, None])
        cols.append(np.exp(lg * pos)[:, None])
        cols.append(np.full((128, 1), np.exp(lg * C)))
        cols.append(np.exp(lg * np.abs(pos[:, None] - pos[None, :])))
        cols.append(np.tile(np.exp(lg * (pos + 1.0))[None, :], (128, 4)))
        cols.append(np.tile(np.exp(lg * (C - pos))[None, :], (128, 4)))
    return np.concatenate(cols, axis=1).astype(np.float32)
RC_SLOT = 3 + 128 + 512 + 512
RC_N = 2 + 2 * RC_SLOT

def stage_inproj(p, S, io, x_f32, xsrc=None):
    nc = p.nc
    Q4 = S // 4
    with p.stage() as st:
        wb = st.sb("winb", [128, 16, NZ], BF16)
        wst = [st.sb(f"wst{i}", [128, NZ]) for i in range(2)]
        for k in range(16):
            s = wst[k % 2]
            p.dma('sp', s[:, :], io['w_in'][k * 128:(k + 1) * 128, :], reads=[io['w_in']], writes=[s])
            p.copy(['dve', 'pool'][k % 2], wb, wb[:, k, :], s, s[:, :])
        xb = [st.sb(f"xb{i}", [128, 16, 512], BF16) for i in range(2)]
        if x_f32:
            xf = [st.sb(f"xf{i}", [128, 8, 512]) for i in range(2)]
        zo = [st.sb(f"zo{i}", [128, 512]) for i in range(4)]
        ps = [st.ps(f"ps{i}", [128, 512]) for i in range(4)]
        n = 0
        for tt in range(S // 512):
            r = (tt * 512) // Q4; off = tt * 512 - r * Q4
            src = xsrc(r) if xsrc is not None else io['xT'][r].rearrange("(k p) t -> p k t", p=128)
            x = xb[tt % 2]
            if x_f32:
                for hf in range(2):
                    p.dma('sp', xf[hf][:, :, :], src[:, hf * 8:(hf + 1) * 8, off:off + 512], reads=[io['xT']], writes=[xf[hf]])
                    p.copy(['dve', 'pool'][hf], x, x[:, hf * 8:(hf + 1) * 8, :], xf[hf], xf[hf][:, :, :])
            else:
                p.dma('sp', x[:, :, :], src[:, :, off:off + 512], reads=[io['xT']], writes=[x])
            for ci, (c0, w) in enumerate(COLTILES):
                P_ = ps[n % 4]; z = zo[n % 4]
                for k in range(16):
                    p.mm(P_, P_[0:w, :], wb, wb[:, k, c0:c0 + w], x, x[:, k, :], start=(k == 0), stop=(k == 15))
                if n % 2 == 0:
                    p.act(z, z[0:w, :], P_, P_[0:w, :], AF.Copy)
                else:
                    p.copy('dve', z, z[0:w, :], P_, P_[0:w, :])
                p.dma('pool', io['zT'][c0:c0 + w, tt * 512:(tt + 1) * 512], z[0:w, :], reads=[z], writes=[io['zT']])
                n += 1

def stage_retention(p, S, io):
    nc = p.nc
    NCK = S // 128; NTT = S // 512; Q4 = S // 4
    zT = io['zT']
    with p.stage() as st:
        rc = st.sb("rc", [128, RC_N])
        p.dma('sp', rc[:, :], io['rconst'][:, :], reads=[io['rconst']], writes=[rc])
        ident_f = st.sb("ident_f", [128, 128]); ident = st.sb("ident", [128, 128], BF16)
        p.dma('sp', ident_f[:, :], io['ident'][:, :], reads=[io['ident']], writes=[ident_f])
        p.copy('dve', ident, ident[:, :], ident_f, ident_f[:, :])
        posi = st.sb("posi", [128, 512], I32); ang = st.sb("ang", [128, 512]); a2 = st.sb("a2", [128, 512])
        ki = st.sb("ki", [128, 512], I32); kf = st.sb("kf", [128, 512])
        cos = st.sb("cos", [128, 512]); sin = st.sb("sin", [128, 512])
        zq = [st.sb(f"zq{i}", [128, 512]) for i in range(2)]; zs = [st.sb(f"zs{i}", [128, 512]) for i in range(2)]
        t1 = st.sb("t1", [128, 512]); t2 = st.sb("t2", [128, 512])
        rot = [st.sb(f"rot{i}", [128, 512], BF16) for i in range(2)]
        vf = st.sb("vf", [128, 512]); vb = st.sb("vb", [128, 512], BF16)
        tok = [st.sb(f"tok{i}", [128, 4, 128], BF16) for i in range(2)]
        ptr = [st.ps(f"ptr{i}", [128, 512], BF16) for i in range(2)]
        n = 0
        for tt in range(NTT):
            ts_ = slice(tt * 512, (tt + 1) * 512)
            p.dma('sp', posi[:, :], io['pos'].ap[ts_].rearrange("(o t) -> o t", o=1).partition_broadcast(128), reads=[io['pos']], writes=[posi])
            p.ts('dve', ang, ang[:, :], posi, posi[:, :], rc[:, 0:1], None, ALU.mult, extra_reads=[rc])
            for (shift, dst, scale) in [(0.0, sin, rc[:, 1:2]), (PI / 2, cos, 1.0)]:
                if shift != 0.0:
                    p.ts('pool', a2, a2[:, :], ang, ang[:, :], shift, None, ALU.add)
                    src = a2
                else:
                    src = ang
                p.ts('dve', ki, ki[:, :], src, src[:, :], INV2PI, None, ALU.mult)
                p.copy('act', kf, kf[:, :], ki, ki[:, :])
                p.stt(a2, a2[:, :], kf, kf[:, :], -C1, src, src[:, :], ALU.mult, ALU.add)
                p.stt(a2, a2[:, :], kf, kf[:, :], -C2, a2, a2[:, :], ALU.mult, ALU.add)
                p.ts('pool', a2, a2[:, :], a2, a2[:, :], -PI, PI, ALU.max, ALU.min)
                p.act(dst, dst[:, :], a2, a2[:, :], AF.Sin, scale=scale, extra_reads=[rc])
            for s in range(2):
                base = s * 512
                for which, (r0, scl, dstd) in enumerate([(base, 128.0 ** -0.5, io['qrT']), (base + 128, 1.0, io['krT'])]):
                    z = zq[which]; zw = zs[which]
                    p.dma('sp', z[:, :], zT[r0:r0 + 128, ts_], reads=[zT], writes=[z])
                    p.dma('sp', zw[0:64, :], zT[r0 + 64:r0 + 128, ts_], reads=[zT], writes=[zw])
                    p.dma('sp', zw[64:128, :], zT[r0:r0 + 64, ts_], reads=[zT], writes=[zw])
                    p.stt(t1, t1[:, :], z, z[:, :], scl, cos, cos[:, :], ALU.mult, ALU.mult)
                    p.stt(t2, t2[:, :], zw, zw[:, :], scl, sin, sin[:, :], ALU.mult, ALU.mult, e='dve')
                    ro = rot[which]
                    p.tt(['pool', 'dve'][which], ro, ro[:, :], t1, t1[:, :], t2, t2[:, :], ALU.add)
                    p.dma('pool', dstd[s, :, ts_], ro[:, :], reads=[ro], writes=[dstd])
                p.dma('sp', vf[:, :], zT[base + 256:base + 384, ts_], reads=[zT], writes=[vf])
                p.copy('act', vb, vb[:, :], vf, vf[:, :])
                for (srcb, dstd) in [(rot[1], io['ktok']), (vb, io['vtok'])]:
                    P_ = ptr[n % 2]; tk = tok[n % 2]; n += 1
                    for c4 in range(4):
                        p.tr(P_, P_[:, c4 * 128:(c4 + 1) * 128], srcb, srcb[:, c4 * 128:(c4 + 1) * 128], ident, ident[:, :])
                    p.act(tk, tk[:, :, :], P_, P_[:, :].rearrange("p (c d) -> p c d", c=4), AF.Copy)
                    p.dma('pool', dstd[s, tt * 4:(tt + 1) * 4].rearrange("c j d -> j c d"), tk[:, :, :], reads=[tk], writes=[dstd])
    with p.stage() as st:
        rc = st.sb("rc", [128, RC_N])
        p.dma('sp', rc[:, :], io['rconst'][:, :], reads=[io['rconst']], writes=[rc])
        Sf = [st.sb(f"Sb{s}", [128, 128]) for s in range(2)]
        Sb = [[st.sb(f"Sbb{s}_{i}", [128, 128], BF16) for i in range(2)] for s in range(2)]
        kt = [[st.sb(f"kt{s}_{i}", [128, 4, 128], BF16) for i in range(2)] for s in range(2)]
        vt = [[st.sb(f"vt{s}_{i}", [128, 4, 128], BF16) for i in range(2)] for s in range(2)]
        kd = [[st.sb(f"kd{s}_{i}", [128, 4, 128], BF16) for i in range(2)] for s in range(2)]
        pkv = [st.ps(f"pkv{i}", [128, 128]) for i in range(2)]
        for s in range(2):
            p.memset('dve', Sf[s], Sf[s][:, :], 0.0)
            p.memset('pool', Sb[s][0], Sb[s][0][:, :], 0.0)
            p.memset('pool', Sb[s][1], Sb[s][1][:, :], 0.0)
        for tt in range(NTT - 1, -1, -1):
            for s in range(2):
                o = 2 + s * RC_SLOT
                k_ = kt[s][tt % 2]; v_ = vt[s][tt % 2]; d_ = kd[s][tt % 2]
                p.dma('sp', k_[:, :, :], io['ktok'][s, tt * 4:(tt + 1) * 4].rearrange("c j d -> j c d"), reads=[io['ktok']], writes=[k_])
                p.dma('sp', v_[:, :, :], io['vtok'][s, tt * 4:(tt + 1) * 4].rearrange("c j d -> j c d"), reads=[io['vtok']], writes=[v_])
                p.ts('pool', d_, d_[:, :, :], k_, k_[:, :, :], rc[:, o + 1:o + 2], None, ALU.mult, extra_reads=[rc])
                for c4 in range(3, -1, -1):
                    c = tt * 4 + c4
                    cur = Sb[s][c % 2]; nxt = Sb[s][(c + 1) % 2]
                    p.dma('pool', io['sbd'][s, c], cur[:, :], reads=[cur], writes=[io['sbd']])
                    if c == 0: continue
                    P_ = pkv[s]
                    p.mm(P_, P_[:, :], d_, d_[:, c4, :], v_, v_[:, c4, :])
                    p.stt(Sf[s], Sf[s][:, :], Sf[s], Sf[s][:, :], rc[:, o + 2:o + 3], P_, P_[:, :], ALU.mult, ALU.add, extra_reads=[rc])
                    p.act(nxt, nxt[:, :], Sf[s], Sf[s][:, :], AF.Copy)
    with p.stage() as st:
        rc = st.sb("rc", [128, RC_N])
        p.dma('sp', rc[:, :], io['rconst'][:, :], reads=[io['rconst']], writes=[rc])
        ones = st.sb("ones", [128, 128]); p.memset('dve', ones, ones[:, :], 1.0)
        Sf = [st.sb(f"Sf{s}", [128, 128]) for s in range(2)]
        Sfb = [[st.sb(f"Sfb{s}_{i}", [128, 128], BF16) for i in range(2)] for s in range(2)]
        bufs = {}
        for s in range(2):
            for nm, shp, dt in [('q', [128, 512], BF16), ('k', [128, 512], BF16), ('kt', [128, 4, 128], BF16), ('vt', [128, 4, 128], BF16),
                                ('sb', [128, 4, 128], BF16), ('g', [128, 512], F32), ('qf', [128, 512], BF16), ('qb', [128, 512], BF16),
                                ('kd', [128, 4, 128], BF16), ('scm', [128, 512], BF16), ('sq', [128, 512], F32), ('rs', [128, 512], F32),
                                ('sg', [128, 512], F32), ('t', [128, 512], F32), ('o', [128, 512], BF16)]:
                bufs[(s, nm)] = st.sb(f"r3{nm}{s}", shp, dt)
        psc = [st.ps(f"psc{i}", [128, 128]) for i in range(2)]
        pyT = [st.ps(f"pyT{i}", [128, 512]) for i in range(2)]
        pkv = [st.ps(f"pkv3{i}", [128, 128]) for i in range(2)]
        pss = st.ps("pss", [128, 512])
        for s in range(2):
            p.memset('dve', Sf[s], Sf[s][:, :], 0.0)
            p.memset('pool', Sfb[s][0], Sfb[s][0][:, :], 0.0)
        for tt in range(NTT):
            ts_ = slice(tt * 512, (tt + 1) * 512)
            r = (tt * 512) // Q4; off = tt * 512 - r * Q4
            for s in range(2):
                o = 2 + s * RC_SLOT
                B = lambda nm: bufs[(s, nm)]
                p.dma('sp', B('q')[:, :], io['qrT'][s, :, ts_], reads=[io['qrT']], writes=[B('q')])
                p.dma('sp', B('k')[:, :], io['krT'][s, :, ts_], reads=[io['krT']], writes=[B('k')])
                p.dma('sp', B('kt')[:, :, :], io['ktok'][s, tt * 4:(tt + 1) * 4].rearrange("c j d -> j c d"), reads=[io['ktok']], writes=[B('kt')])
                p.dma('sp', B('vt')[:, :, :], io['vtok'][s, tt * 4:(tt + 1) * 4].rearrange("c j d -> j c d"), reads=[io['vtok']], writes=[B('vt')])
                p.dma('sp', B('sb')[:, :, :], io['sbd'][s, tt * 4:(tt + 1) * 4].rearrange("c d e -> d c e"), reads=[io['sbd']], writes=[B('sb')])
                p.dma('sp', B('g')[:, :], zT[s * 512 + 384:s * 512 + 512, ts_], reads=[zT], writes=[B('g')])
                p.tt('pool', B('qf'), B('qf')[:, :], B('q'), B('q')[:, :], rc, rc[:, o + 3 + 128:o + 3 + 128 + 512], ALU.mult)
                p.tt('dve', B('qb'), B('qb')[:, :], B('q'), B('q')[:, :], rc, rc[:, o + 3 + 640:o + 3 + 640 + 512], ALU.mult)
                p.ts('pool', B('kd'), B('kd')[:, :, :], B('kt'), B('kt')[:, :, :], rc[:, o:o + 1], None, ALU.mult, extra_reads=[rc])
                p.act(B('sg'), B('sg')[:, :], B('g'), B('g')[:, :], AF.Silu)
                Y = pyT[s]
                for c4 in range(4):
                    c = tt * 4 + c4
                    cs = slice(c4 * 128, (c4 + 1) * 128)
                    cur = Sfb[s][c % 2]; nxt = Sfb[s][(c + 1) % 2]
                    SC = psc[c % 2]
                    p.mm(SC, SC[:, :], B('k'), B('k')[:, cs], B('q'), B('q')[:, cs])
                    p.tt('dve', B('scm'), B('scm')[:, cs], SC, SC[:, :], rc, rc[:, o + 3:o + 3 + 128], ALU.mult)
                    p.mm(Y, Y[:, cs], B('vt'), B('vt')[:, c4, :], B('scm'), B('scm')[:, cs], start=True, stop=False)
                    p.mm(Y, Y[:, cs], cur, cur[:, :], B('qf'), B('qf')[:, cs], start=False, stop=False)
                    p.mm(Y, Y[:, cs], B('sb'), B('sb')[:, c4, :], B('qb'), B('qb')[:, cs], start=False, stop=True)
                    if c < NCK - 1:
                        P_ = pkv[s]
                        p.mm(P_, P_[:, :], B('kd'), B('kd')[:, c4, :], B('vt'), B('vt')[:, c4, :])
                        p.stt(Sf[s], Sf[s][:, :], Sf[s], Sf[s][:, :], rc[:, o + 2:o + 3], P_, P_[:, :], ALU.mult, ALU.add, extra_reads=[rc])
                        p.act(nxt, nxt[:, :], Sf[s], Sf[s][:, :], AF.Copy)
                p.act(B('sq'), B('sq')[:, :], Y, Y[:, :], AF.Square)
                p.mm(pss, pss[:, :], ones, ones[:, :], B('sq'), B('sq')[:, :])
                p.act(B('rs'), B('rs')[:, :], pss, pss[:, :], AF.Sqrt, bias=1e-6, scale=1.0 / 128)
                p.op('dve', lambda: nc.vector.reciprocal(out=B('rs')[:, :], in_=B('rs')[:, :]), reads=[B('rs')], writes=[B('rs')])
                p.tt('dve', B('t'), B('t')[:, :], Y, Y[:, :], B('rs'), B('rs')[:, :], ALU.mult)
                p.tt('pool', B('o'), B('o')[:, :], B('t'), B('t')[:, :], B('sg'), B('sg')[:, :], ALU.mult)
                p.dma('pool', io['yT'][r, s * 128:(s + 1) * 128, off:off + 512], B('o')[:, :], reads=[B('o')], writes=[io['yT']])

ZS5 = 1984
def sin_reduce(p, out, src, shift, ki, kf, tmp, scale=1.0, extra_reads=()):
    sl = tuple(slice(None) for _ in src.ap.shape)
    if shift != 0.0:
        p.ts('pool', tmp, tmp[sl], src, src[sl], shift, None, ALU.add)
        s = tmp
    else:
        s = src
    p.ts('dve', ki, ki[sl], s, s[sl], INV2PI, None, ALU.mult)
    p.copy('pool', kf, kf[sl], ki, ki[sl])
    p.stt(tmp, tmp[sl], kf, kf[sl], -C1, s, s[sl], ALU.mult, ALU.add)
    p.stt(tmp, tmp[sl], kf, kf[sl], -C2, tmp, tmp[sl], ALU.mult, ALU.add)
    p.ts('pool', tmp, tmp[sl], tmp, tmp[sl], -PI, PI, ALU.max, ALU.min)
    p.act(out, out[sl], tmp, tmp[sl], AF.Sin, scale=scale, extra_reads=extra_reads)

def s5_host_layout(q, lam_re, lam_im, log_dt, b_re, b_im, c_re, c_im, d_skip):
    import numpy as np
    gs = slice(8 * q, 8 * q + 8)
    lr = lam_re[:, gs, :]; li = lam_im[:, gs, :]; ld = log_dt[:, gs]
    rows = np.stack([lr.reshape(-1), li.reshape(-1), np.repeat(ld.reshape(-1), 64)], 0).astype(np.float32)
    def col(a):
        return np.ascontiguousarray(a.reshape(2, 4, 2, 64).transpose(2, 3, 0, 1).reshape(128, 8))
    cols = np.concatenate([col(lr), col(li), col(np.repeat(ld[:, :, None], 64, axis=2))], 1).astype(np.float32)
    bl = np.zeros((2, 128, 2, 4, 128), np.float32)
    cl = np.zeros((2, 128, 2, 4, 128), np.float32)
    for d in range(2):
        for g in range(8):
            j, gp = g // 2, g % 2
            for ri, (bsrc, csrc) in enumerate([(b_re, c_re), (b_im, c_im)]):
                bl[ri, g * 16:(g + 1) * 16, d, j, gp * 64:(gp + 1) * 64] = bsrc[d, 8 * q + g].T
                cl[ri, gp * 64:(gp + 1) * 64, d, j, g * 16:(g + 1) * 16] = csrc[d, 8 * q + g].T
    dcol = d_skip[128 * q:128 * (q + 1)].reshape(128, 1).astype(np.float32)
    return dict(s5rows=rows, s5cols=cols, s5bl=bl.reshape(2, 128, 1024), s5cl=cl.reshape(2, 128, 1024), s5d=dcol,
                s5iota=np.tile(np.arange(512, dtype=np.float32)[None, :], (128, 1)))

def stage_s5(p, S, io):
    nc = p.nc
    NTT = S // 512; Q4 = S // 4; TC = 512
    zT = io['zT']
    with p.stage() as st:
        bb = [st.sb(f"bb{i}", [128, 1024], BF16) for i in range(2)]
        cc = [st.sb(f"cc{i}", [128, 1024], BF16) for i in range(2)]
        rho = st.sb("rho", [128, 8]); cT = st.sb("cT", [128, 8]); sT = st.sb("sT", [128, 8]); thc = st.sb("thc", [128, 8])
        dcol = st.sb("dcol", [128, 1])
        p.dma('sp', dcol[:, :], io['s5d'][:, :], reads=[io['s5d']], writes=[dcol])
        with p.stage() as s2:
            f = lambda nm: s2.sb(nm, [128, 1024])
            lr, li, ld, dt, mag, th, sn, cs, t1, t2, t3, kf, tmp, cr, ci = [f(n) for n in
                ['lr', 'li', 'ld', 'dt', 'mag', 'th', 'sn', 'cs', 't1', 't2', 't3', 'kf', 'tmp', 'cr', 'ci']]
            ki = s2.sb("ki", [128, 1024], I32)
            for k, t in enumerate([lr, li, ld]):
                p.dma('sp', t[:, :], io['s5rows'][k:k + 1, :].partition_broadcast(128), reads=[io['s5rows']], writes=[t])
            p.act(dt, dt[:, :], ld, ld[:, :], AF.Exp)
            p.tt('dve', t1, t1[:, :], lr, lr[:, :], dt, dt[:, :], ALU.mult)
            p.act(mag, mag[:, :], t1, t1[:, :], AF.Exp)
            p.tt('dve', th, th[:, :], li, li[:, :], dt, dt[:, :], ALU.mult)
            sin_reduce(p, sn, th, 0.0, ki, kf, tmp)
            sin_reduce(p, cs, th, PI / 2, ki, kf, tmp)
            p.tt('dve', cs, cs[:, :], cs, cs[:, :], mag, mag[:, :], ALU.mult)
            p.tt('dve', sn, sn[:, :], sn, sn[:, :], mag, mag[:, :], ALU.mult)
            p.ts('dve', cs, cs[:, :], cs, cs[:, :], -1.0, None, ALU.add)
            p.tt('dve', t1, t1[:, :], lr, lr[:, :], lr, lr[:, :], ALU.mult)
            p.tt('dve', t2, t2[:, :], li, li[:, :], li, li[:, :], ALU.mult)
            p.tt('dve', t1, t1[:, :], t1, t1[:, :], t2, t2[:, :], ALU.add)
            p.op('dve', lambda: nc.vector.reciprocal(out=t1[:, :], in_=t1[:, :]), reads=[t1], writes=[t1])
            p.tt('dve', t2, t2[:, :], cs, cs[:, :], lr, lr[:, :], ALU.mult)
            p.tt('dve', t3, t3[:, :], sn, sn[:, :], li, li[:, :], ALU.mult)
            p.tt('dve', t2, t2[:, :], t2, t2[:, :], t3, t3[:, :], ALU.add)
            p.tt('dve', cr, cr[:, :], t2, t2[:, :], t1, t1[:, :], ALU.mult)
            p.tt('dve', t2, t2[:, :], sn, sn[:, :], lr, lr[:, :], ALU.mult)
            p.tt('dve', t3, t3[:, :], cs, cs[:, :], li, li[:, :], ALU.mult)
            p.tt('dve', t2, t2[:, :], t2, t2[:, :], t3, t3[:, :], ALU.subtract)
            p.tt('dve', ci, ci[:, :], t2, t2[:, :], t1, t1[:, :], ALU.mult)
            br = lr; bi = li
            p.dma('sp', br[:, :], io['s5bl'][0], reads=[io['s5bl']], writes=[br])
            p.dma('sp', bi[:, :], io['s5bl'][1], reads=[io['s5bl']], writes=[bi])
            p.tt('dve', t1, t1[:, :], cr, cr[:, :], br, br[:, :], ALU.mult)
            p.tt('dve', t2, t2[:, :], ci, ci[:, :], bi, bi[:, :], ALU.mult)
            p.tt('dve', bb[0], bb[0][:, :], t1, t1[:, :], t2, t2[:, :], ALU.subtract)
            p.tt('dve', t1, t1[:, :], cr, cr[:, :], bi, bi[:, :], ALU.mult)
            p.tt('dve', t2, t2[:, :], ci, ci[:, :], br, br[:, :], ALU.mult)
            p.tt('dve', bb[1], bb[1][:, :], t1, t1[:, :], t2, t2[:, :], ALU.add)
            p.dma('sp', t1[:, :], io['s5cl'][0], reads=[io['s5cl']], writes=[t1])
            p.dma('sp', t2[:, :], io['s5cl'][1], reads=[io['s5cl']], writes=[t2])
            p.copy('dve', cc[0], cc[0][:, :], t1, t1[:, :])
            p.ts('dve', cc[1], cc[1][:, :], t2, t2[:, :], -1.0, None, ALU.mult)
            c24 = s2.sb("c24", [128, 24]); dtc = s2.sb("dtc", [128, 8]); tq = s2.sb("tq", [128, 8]); tq2 = s2.sb("tq2", [128, 8])
            ki8 = s2.sb("ki8", [128, 8], I32); kf8 = s2.sb("kf8", [128, 8]); tmp8 = s2.sb("tmp8", [128, 8])
            p.dma('sp', c24[:, :], io['s5cols'][:, :], reads=[io['s5cols']], writes=[c24])
            p.act(dtc, dtc[:, :], c24, c24[:, 16:24], AF.Exp)
            p.tt('dve', tq, tq[:, :], c24, c24[:, 0:8], dtc, dtc[:, :], ALU.mult)
            p.act(rho, rho[:, :], tq, tq[:, :], AF.Exp)
            p.tt('dve', thc, thc[:, :], c24, c24[:, 8:16], dtc, dtc[:, :], ALU.mult)
            p.ts('dve', tq2, tq2[:, :], thc, thc[:, :], float(TC), None, ALU.mult)
            sin_reduce(p, sT, tq2, 0.0, ki8, kf8, tmp8)
            sin_reduce(p, cT, tq2, PI / 2, ki8, kf8, tmp8)
        iota = st.sb("iota", [128, TC]); p.dma('sp', iota[:, :], io['s5iota'][:, :], reads=[io['s5iota']], writes=[iota])
        cosT = [st.sb(f"cost{k}", [128, TC]) for k in range(8)]; sinT = [st.sb(f"sint{k}", [128, TC]) for k in range(8)]
        rhoT = [st.sb(f"rhot{k}", [128, TC]) for k in range(8)]
        ang = st.sb("ang", [128, TC]); kiT = st.sb("kiT", [128, TC], I32); kfT = st.sb("kfT", [128, TC]); tmpT = st.sb("tmpT", [128, TC])
        tb = st.sb("tb", [128, TC])
        for k in range(8):
            p.ts('dve', ang, ang[:, :], iota, iota[:, :], thc[:, k:k + 1], None, ALU.mult, extra_reads=[thc])
            for (shift, dst) in [(0.0, sinT[k]), (PI / 2, cosT[k])]:
                if k < 4:
                    sin_reduce(p, dst, ang, shift, kiT, kfT, tmpT)
                else:
                    sin_reduce(p, tb, ang, shift, kiT, kfT, tmpT)
                    p.copy('dve', dst, dst[:, :], tb, tb[:, ::-1])
            p.ts('dve', rhoT[k], rhoT[k][:, :], iota, iota[:, :], 0.0, rho[:, k:k + 1], ALU.mult, ALU.add, extra_reads=[rho])
        uf = [st.sb(f"uf{i}", [128, TC]) for i in range(2)]; ub = [st.sb(f"ub{i}", [128, TC], BF16) for i in range(2)]
        brs = [st.sb(f"brs{i}", [128, TC]) for i in range(2)]; bis = [st.sb(f"bis{i}", [128, TC]) for i in range(2)]
        t1 = st.sb("t1", [128, TC]); t2 = st.sb("t2", [128, TC]); t3 = st.sb("t3", [128, TC]); t4 = st.sb("t4", [128, TC])
        mre = st.sb("mre", [128, TC]); mim = st.sb("mim", [128, TC])
        xr = [st.sb(f"xr{i}", [128, TC]) for i in range(2)]; xi = [st.sb(f"xi{i}", [128, TC]) for i in range(2)]
        xre = [st.sb(f"xre{i}", [128, TC], BF16) for i in range(2)]; xim = [st.sb(f"xim{i}", [128, TC], BF16) for i in range(2)]
        init = st.sb("init", [128, 16]); p.memset('dve', init, init[:, :], 0.0)
        tn = st.sb("tn", [128, 4])
        yo = [st.sb(f"yo{i}", [128, TC]) for i in range(2)]
        yfl = st.sb("yfl", [128, TC]); yb16 = [st.sb(f"yb16{i}", [128, TC], BF16) for i in range(2)]
        pb_re = [st.ps(f"pbre{i}", [128, TC]) for i in range(2)]; pb_im = [st.ps(f"pbim{i}", [128, TC]) for i in range(2)]
        py = [st.ps(f"py{i}", [128, TC]) for i in range(2)]
        n = 0
        for d in range(2):
            order = range(NTT) if d == 0 else range(NTT - 1, -1, -1)
            for it, tt in enumerate(order):
                ts_ = slice(tt * TC, (tt + 1) * TC)
                r = (tt * TC) // Q4; off = tt * TC - r * Q4
                u_f = uf[it % 2]; u_b = ub[it % 2]
                p.dma('sp', u_f[:, :], zT[ZS5:ZS5 + 128, ts_], reads=[zT], writes=[u_f])
                p.copy('pool', u_b, u_b[:, :], u_f, u_f[:, :])
                Y = py[it % 2]
                for j in range(4):
                    k = d * 4 + j
                    blk = slice(k * 128, (k + 1) * 128)
                    PR = pb_re[n % 2]; PI_ = pb_im[n % 2]; b_r = brs[n % 2]; b_i = bis[n % 2]
                    x_r = xr[n % 2]; x_i = xi[n % 2]; xo_r = xre[n % 2]; xo_i = xim[n % 2]; n += 1
                    p.mm(PR, PR[:, :], bb[0], bb[0][:, blk], u_b, u_b[:, :])
                    p.mm(PI_, PI_[:, :], bb[1], bb[1][:, blk], u_b, u_b[:, :])
                    p.act(b_r, b_r[:, :], PR, PR[:, :], AF.Copy)
                    p.act(b_i, b_i[:, :], PI_, PI_[:, :], AF.Copy)
                    p.tt('dve', t1, t1[:, :], b_r, b_r[:, :], cosT[k], cosT[k][:, :], ALU.mult)
                    p.tt('pool', t2, t2[:, :], b_i, b_i[:, :], sinT[k], sinT[k][:, :], ALU.mult)
                    p.tt('dve', mre, mre[:, :], t1, t1[:, :], t2, t2[:, :], ALU.add)
                    p.tt('dve', t3, t3[:, :], b_i, b_i[:, :], cosT[k], cosT[k][:, :], ALU.mult)
                    p.tt('dve', t4, t4[:, :], b_r, b_r[:, :], sinT[k], sinT[k][:, :], ALU.mult)
                    p.tt('pool', mim, mim[:, :], t3, t3[:, :], t4, t4[:, :], ALU.subtract)
                    if d == 0:
                        vw = lambda a: a[:, :]
                        last = slice(TC - 1, TC)
                    else:
                        vw = lambda a: a[:, ::-1]
                        last = slice(0, 1)
                    p.op('dve', lambda: nc.vector.tensor_tensor_scan(out=vw(x_r), data0=rhoT[k][:, :], data1=vw(mre), initial=init[:, k:k + 1],
                                                                      op0=ALU.mult, op1=ALU.add), reads=[rhoT[k], mre, init], writes=[x_r])
                    p.op('dve', lambda: nc.vector.tensor_tensor_scan(out=vw(x_i), data0=rhoT[k][:, :], data1=vw(mim), initial=init[:, 8 + k:9 + k],
                                                                      op0=ALU.mult, op1=ALU.add), reads=[rhoT[k], mim, init], writes=[x_i])
                    p.ts('pool', tn, tn[:, 0:1], x_r, x_r[:, last], cT[:, k:k + 1], None, ALU.mult, extra_reads=[cT])
                    p.ts('pool', tn, tn[:, 1:2], x_r, x_r[:, last], sT[:, k:k + 1], None, ALU.mult, extra_reads=[sT])
                    p.stt(tn, tn[:, 2:3], x_i, x_i[:, last], sT[:, k:k + 1], tn, tn[:, 0:1], ALU.mult, ALU.subtract, extra_reads=[sT])
                    p.ts('pool', init, init[:, k:k + 1], tn, tn[:, 2:3], -1.0, None, ALU.mult)
                    p.stt(init, init[:, 8 + k:9 + k], x_i, x_i[:, last], cT[:, k:k + 1], tn, tn[:, 1:2], ALU.mult, ALU.add, extra_reads=[cT])
                    p.tt('dve', t1, t1[:, :], x_r, x_r[:, :], cosT[k], cosT[k][:, :], ALU.mult)
                    p.tt('pool', t2, t2[:, :], x_i, x_i[:, :], sinT[k], sinT[k][:, :], ALU.mult)
                    p.tt('dve', xo_r, xo_r[:, :], t1, t1[:, :], t2, t2[:, :], ALU.subtract)
                    p.tt('dve', t3, t3[:, :], x_r, x_r[:, :], sinT[k], sinT[k][:, :], ALU.mult)
                    p.tt('dve', t4, t4[:, :], x_i, x_i[:, :], cosT[k], cosT[k][:, :], ALU.mult)
                    p.tt('pool', xo_i, xo_i[:, :], t3, t3[:, :], t4, t4[:, :], ALU.add)
                    p.mm(Y, Y[:, :], cc[0], cc[0][:, blk], xo_r, xo_r[:, :], start=(j == 0), stop=False)
                    p.mm(Y, Y[:, :], cc[1], cc[1][:, blk], xo_i, xo_i[:, :], start=False, stop=(j == 3))
                if d == 0:
                    y_o = yo[it % 2]
                    p.act(y_o, y_o[:, :], Y, Y[:, :], AF.Copy)
                    p.dma('pool', io['yf'][:, ts_], y_o[:, :], reads=[y_o], writes=[io['yf']])
                else:
                    y_o = yo[it % 2]; yb = yb16[it % 2]
                    p.dma('sp', yfl[:, :], io['yf'][:, ts_], reads=[io['yf']], writes=[yfl])
                    p.tt('dve', y_o, y_o[:, :], Y, Y[:, :], yfl, yfl[:, :], ALU.add)
                    p.stt(y_o, y_o[:, :], u_f, u_f[:, :], dcol[:, 0:1], y_o, y_o[:, :], ALU.mult, ALU.add, extra_reads=[dcol])
                    p.act(yb, yb[:, :], y_o, y_o[:, :], AF.Gelu_apprx_tanh)
                    p.dma('pool', io['yT'][r, 448:576, off:off + TC], yb[:, :], reads=[yb], writes=[io['yT']])

ZR, ZK, ZV, ZWD, ZAD, ZGD = 1024, 1216, 1408, 1600, 1728, 1856
RW_SU, RW_SL, RW_U, RW_L, RW_I, RW_BLK, RW_MF, RW_MB, RW_N = 0, 768, 1536, 2304, 3072, 3840, 3968, 4480, 4992
NEG_EXP_HALF = -math.exp(-0.5)

def rwkv_consts():
    import numpy as np
    i = np.arange(128)
    su = (i[:, None] < i[None, :]).astype(np.float32); sl = su.T.copy()
    u = (i[:, None] <= i[None, :]).astype(np.float32); l = u.T.copy()
    I = np.eye(128, dtype=np.float32)
    blk = np.zeros((128, 128), np.float32); blk[:64, :64] = 1; blk[64:, 64:] = 1
    t = np.arange(512)
    mf = (t % 128 != 0).astype(np.float32); mb = (t % 128 != 127).astype(np.float32)
    return np.concatenate([np.tile(su, (1, 6)), np.tile(sl, (1, 6)), np.tile(u, (1, 6)), np.tile(l, (1, 6)), np.tile(I, (1, 6)), blk,
                           np.tile(mf[None], (128, 1)), np.tile(mb[None], (128, 1))], axis=1).astype(np.float32)

def rwkv_host_layout(q, mu_prev, mu_next, w0, w_up, a0, a_up, g_up, k_k, k_a, r_k, lnx_w, lnx_b):
    import numpy as np
    rwp = np.zeros((128, 55), np.float32)
    rkf = r_k.reshape(-1)
    for hh in range(3):
        ch = q * 192 + hh * 64 + np.arange(64)
        b = hh * 15
        for j, part in enumerate([0, 768, 1536]):
            rwp[:64, b + 2 * j] = mu_prev[part + ch]; rwp[:64, b + 2 * j + 1] = mu_next[part + ch]
        rwp[:64, b + 6] = k_k[ch]; rwp[:64, b + 7] = k_a[ch]; rwp[:64, b + 8] = rkf[ch]; rwp[:64, b + 9] = lnx_w[ch]; rwp[:64, b + 10] = lnx_b[ch]
        rwp[:64, b + 11] = w0[0, ch]; rwp[:64, b + 12] = w0[1, ch]; rwp[:64, b + 13] = a0[0, ch]; rwp[:64, b + 14] = a0[1, ch]
    for j, part in enumerate([2304, 2432]):
        for d in range(2):
            rwp[:64, 45 + 4 * j + 2 * d] = mu_prev[part + d * 64:part + (d + 1) * 64]
            rwp[:64, 46 + 4 * j + 2 * d] = mu_next[part + d * 64:part + (d + 1) * 64]
    rwp[:, 53] = mu_prev[2560:2688]; rwp[:, 54] = mu_next[2560:2688]
    cs = slice(q * 192, (q + 1) * 192)
    return dict(rwp=rwp, rw_wup=np.ascontiguousarray(np.stack([w_up[0][:, cs], w_up[1][:, cs]], 1)),
                rw_aup=np.ascontiguousarray(np.stack([a_up[0][:, cs], a_up[1][:, cs]], 1)),
                rw_gup=np.ascontiguousarray(g_up[:, cs]))

RW_DEBUG = [None]
def stage_rwkv(p, S, io):
    nc = p.nc
    NTT = S // 512; Q4 = S // 4
    zT = io['zT']
    H3 = range(3)
    with p.stage() as st:
        rwp = st.sb("rwp", [128, 55]); p.dma('sp', rwp[:, :], io['rwp'][:, :], reads=[io['rwp']], writes=[rwp])
        cst = st.sb("rwc", [128, RW_N]); p.dma('sp', cst[:, :], io['rwconst'][:, :], reads=[io['rwconst']], writes=[cst])
        ident_f = st.sb("ident_f", [128, 128]); ident = st.sb("ident", [128, 128], BF16)
        p.dma('sp', ident_f[:, :], io['ident'][:, :], reads=[io['ident']], writes=[ident_f])
        p.copy('dve', ident, ident[:, :], ident_f, ident_f[:, :])
        wtmp = st.sb("wtmp", [128, 384])
        wup = st.sb("wupb", [64, 2, 192], BF16); aup = st.sb("aupb", [64, 2, 192], BF16); gup = st.sb("gupb", [128, 192], BF16)
        p.dma('sp', wtmp[0:64, :], io['rw_wup'].ap.rearrange("r d c -> r (d c)"), reads=[io['rw_wup']], writes=[wtmp])
        p.copy('dve', wup, wup[:, :, :], wtmp, wtmp[0:64, :].rearrange("r (d c) -> r d c", d=2))
        p.dma('sp', wtmp[0:64, :], io['rw_aup'].ap.rearrange("r d c -> r (d c)"), reads=[io['rw_aup']], writes=[wtmp])
        p.copy('dve', aup, aup[:, :, :], wtmp, wtmp[0:64, :].rearrange("r (d c) -> r d c", d=2))
        p.dma('sp', wtmp[:, 0:192], io['rw_gup'][:, :], reads=[io['rw_gup']], writes=[wtmp])
        p.copy('dve', gup, gup[:, :], wtmp, wtmp[:, 0:192])
        c0 = st.sb("c0", [128, 14])
        pairs = [0, 2, 4, 15, 17, 19, 30, 32, 34, 45, 47, 49, 51, 53]
        for i, col in enumerate(pairs):
            p.tt('dve', c0, c0[:, i:i + 1], rwp, rwp[:, col:col + 1], rwp, rwp[:, col + 1:col + 2], ALU.add)
        p.ts('dve', c0, c0[:, :], c0, c0[:, :], -1.0, 1.0, ALU.mult, ALU.add)
        def fb(nm, P_=64, n=512, dt=F32): return st.sb(nm, [P_, n], dt)
        zh = [fb(f"zh{i}", 128, 514) for i in range(3)]
        sh = {}
        for hh in H3:
            for nm in ['r', 'k', 'v']:
                sh[(hh, nm)] = fb(f"sh{nm}{hh}")
        shwd = fb("shwd"); shad = fb("shad"); shgd = fb("shgd", 128)
        twd = fb("twd", dt=BF16); adb = fb("adb", dt=BF16); sgd = fb("sgd", 128, dt=BF16)
        lw = [fb(f"lw{h}") for h in H3]; cin = [fb(f"cin{h}") for h in H3]; cex = [fb(f"cex{h}") for h in H3]
        Ein = [fb(f"Ein{h}") for h in H3]; Eex = [fb(f"Eex{h}") for h in H3]; Eni = [fb(f"Eni{h}") for h in H3]
        aT = [fb(f"aT{h}") for h in H3]; kk = [fb(f"kk{h}") for h in H3]
        tA = [fb(f"tA{h}") for h in H3]; tB = [fb(f"tB{h}") for h in H3]
        at_ = [fb(f"at{h}", dt=BF16) for h in H3]; bt_ = [fb(f"bt{h}", dt=BF16) for h in H3]
        kt_ = [fb(f"kt{h}", dt=BF16) for h in H3]; rt_ = [fb(f"rt{h}", dt=BF16) for h in H3]
        vb_ = [fb(f"vb{h}", dt=BF16) for h in H3]
        tok = [st.sb(f"tok{i}", [128, 4, 192], BF16) for i in range(4)]
        W3 = lambda nm, dt=BF16: st.sb(nm, [128, 768], dt)
        M = [W3(f"M{i}") for i in range(2)]; N = [W3(f"N{i}") for i in range(2)]
        Pm = [W3(f"P{i}") for i in range(2)]; Qm = [W3(f"Q{i}") for i in range(2)]
        AKT = W3("AKT"); RBT = W3("RBT"); RKT = W3("RKT")
        AKV = st.sb("AKV", [128, 384], BF16); UV = st.sb("UV", [128, 384]); U = st.sb("U", [128, 192], BF16)
        TAT = st.sb("TAT", [64, 768], BF16)
        KVW = st.sb("KVW", [64, 384])
        S0 = st.sb("S0", [64, 192])
        S0b = [st.sb(f"S0b{i}", [64, 192], BF16) for i in range(2)]
        tS = st.sb("tS", [64, 192])
        ydall = st.sb("ydall", [64, 3, 512]); y0l = [fb(f"y0l{h}") for h in H3]
        pp1 = [fb(f"pp1{h}") for h in H3]; pp2 = [fb(f"pp2{h}") for h in H3]; pp3 = [fb(f"pp3{h}") for h in H3]
        yob = [fb(f"yob{h}", dt=BF16) for h in H3]
        pg = [st.ps(f"pg{i}", [128, 1024]) for i in range(2)]
        pch = st.ps("pch", [128, 512])
        pY = [st.ps(f"pY{i}", [128, 512]) for i in range(2)]
        ptk = st.ps("ptk", [128, 1024], BF16)
        gcount = [0]
        def PG():
            gcount[0] += 1
            return pg[gcount[0] % 2]
        ones64 = cst[0:64, RW_BLK:RW_BLK + 64]
        def blocksum(src):
            G = PG()
            p.mm(G, G[0:64, 0:512], cst, ones64, src, src[0:64, :])
            return G

        for d in range(2):
            p.memset('dve', S0, S0[:, :], 0.0)
            p.memset('pool', S0b[0], S0b[0][:, :], 0.0)
            p.memset('pool', S0b[1], S0b[1][:, :], 0.0)
            order = list(range(NTT)) if d == 0 else list(range(NTT - 1, -1, -1))
            mSU = RW_SU if d == 0 else RW_SL; mSL = RW_SL if d == 0 else RW_SU; mU = RW_U if d == 0 else RW_L
            gchunk = 0
            for tt in order:
                t0 = tt * 512
                r_ = t0 // Q4; off = t0 - r_ * Q4
                hbc = [0]
                def shift(row0, P_, dst, c0col, mpcol):
                    z = zh[hbc[0] % 3]; hbc[0] += 1
                    lo = max(t0 - 1, 0); hi = min(t0 + 513, S)
                    if t0 == 0: p.memset('pool', z, z[0:P_, 0:1], 0.0)
                    if t0 + 513 > S: p.memset('pool', z, z[0:P_, 513:514], 0.0)
                    p.dma('sp', z[0:P_, lo - (t0 - 1):hi - (t0 - 1)], zT[row0:row0 + P_, lo:hi], reads=[zT], writes=[z])
                    p.act(dst, dst[0:P_, :], z, z[0:P_, 1:513], AF.Identity, scale=c0[0:P_, c0col:c0col + 1], extra_reads=[c0])
                    p.stt(dst, dst[0:P_, :], z, z[0:P_, 0:512], rwp[0:P_, mpcol:mpcol + 1], dst, dst[0:P_, :], ALU.mult, ALU.add, extra_reads=[rwp])
                    p.stt(dst, dst[0:P_, :], z, z[0:P_, 2:514], rwp[0:P_, mpcol + 1:mpcol + 2], dst, dst[0:P_, :], ALU.mult, ALU.add, extra_reads=[rwp])
                for hh in H3:
                    for j, (nm, zrow) in enumerate([('r', ZR), ('k', ZK), ('v', ZV)]):
                        shift(zrow + hh * 64, 64, sh[(hh, nm)], hh * 3 + j, hh * 15 + 2 * j)
                shift(ZWD + d * 64, 64, shwd, 9 + d, 45 + 2 * d)
                shift(ZAD + d * 64, 64, shad, 11 + d, 49 + 2 * d)
                p.act(twd, twd[:, :], shwd, shwd[:, :], AF.Tanh)
                p.copy('act', adb, adb[:, :], shad, shad[:, :])
                if d == 1:
                    shift(ZGD, 128, shgd, 13, 53)
                    p.act(sgd, sgd[:, :], shgd, shgd[:, :], AF.Sigmoid)
                for hh in H3:
                    b = hh * 15
                    cs_ = slice(hh * 64, (hh + 1) * 64)
                    G = PG()
                    p.mm(G, G[0:64, 0:512], wup, wup[:, d, cs_], twd, twd[:, :])
                    p.act(lw[hh], lw[hh][:, :], G, G[0:64, 0:512], AF.Sigmoid, bias=rwp[0:64, b + 11 + d:b + 12 + d], extra_reads=[rwp])
                    if d == 0:
                        p.op('dve', lambda: nc.vector.tensor_tensor_scan(out=cin[hh][:, :], data0=cst[0:64, RW_MF:RW_MF + 512], data1=lw[hh][:, :],
                                                                          initial=0.0, op0=ALU.mult, op1=ALU.add), reads=[cst, lw[hh]], writes=[cin[hh]])
                    else:
                        mbv = cst[0:64, RW_MB:RW_MB + 512]
                        p.op('dve', lambda: nc.vector.tensor_tensor_scan(out=cin[hh][:, ::-1], data0=mbv[:, ::-1], data1=lw[hh][:, ::-1],
                                                                          initial=0.0, op0=ALU.mult, op1=ALU.add), reads=[cst, lw[hh]], writes=[cin[hh]])
                    p.tt('dve', cex[hh], cex[hh][:, :], cin[hh], cin[hh][:, :], lw[hh], lw[hh][:, :], ALU.subtract)
                    p.act(Ein[hh], Ein[hh][:, :], cin[hh], cin[hh][:, :], AF.Exp, scale=NEG_EXP_HALF)
                    p.act(Eex[hh], Eex[hh][:, :], cex[hh], cex[hh][:, :], AF.Exp, scale=NEG_EXP_HALF)
                    p.act(Eni[hh], Eni[hh][:, :], cin[hh], cin[hh][:, :], AF.Exp, scale=-NEG_EXP_HALF)
                    G = PG()
                    p.mm(G, G[0:64, 0:512], aup, aup[:, d, cs_], adb, adb[:, :])
                    p.act(aT[hh], aT[hh][:, :], G, G[0:64, 0:512], AF.Sigmoid, bias=rwp[0:64, b + 13 + d:b + 14 + d], extra_reads=[rwp])
                    ks = sh[(hh, 'k')]
                    p.act(kk[hh], kk[hh][:, :], ks, ks[:, :], AF.Identity, scale=rwp[0:64, b + 6:b + 7], extra_reads=[rwp])
                    p.act(tA[hh], tA[hh][:, :], ks, ks[:, :], AF.Square, scale=rwp[0:64, b + 6:b + 7], extra_reads=[rwp])
                    G = blocksum(tA[hh])
                    p.act(tB[hh], tB[hh][:, :], G, G[0:64, 0:512], AF.Sqrt)
                    p.ts('dve', tB[hh], tB[hh][:, :], tB[hh], tB[hh][:, :], 1e-12, None, ALU.max)
                    p.op('dve', lambda: nc.vector.reciprocal(out=tB[hh][:, :], in_=tB[hh][:, :]), reads=[tB[hh]], writes=[tB[hh]])
                    p.tt('dve', kk[hh], kk[hh][:, :], kk[hh], kk[hh][:, :], tB[hh], tB[hh][:, :], ALU.mult)
                    p.stt(at_[hh], at_[hh][:, :], kk[hh], kk[hh][:, :], -1.0, Eex[hh], Eex[hh][:, :], ALU.mult, ALU.mult)
                    p.tt('dve', tA[hh], tA[hh][:, :], kk[hh], kk[hh][:, :], aT[hh], aT[hh][:, :], ALU.mult)
                    p.tt('pool', bt_[hh], bt_[hh][:, :], tA[hh], tA[hh][:, :], Eni[hh], Eni[hh][:, :], ALU.mult)
                    p.ts('dve', tB[hh], tB[hh][:, :], aT[hh], aT[hh][:, :], -1.0, rwp[0:64, b + 7:b + 8], ALU.add, ALU.mult, extra_reads=[rwp])
                    p.stt(tB[hh], tB[hh][:, :], tB[hh], tB[hh][:, :], 1.0, ks, ks[:, :], ALU.add, ALU.mult)
                    p.tt('dve', kt_[hh], kt_[hh][:, :], tB[hh], tB[hh][:, :], Eni[hh], Eni[hh][:, :], ALU.mult)
                    rs_ = sh[(hh, 'r')]
                    p.tt('pool', rt_[hh], rt_[hh][:, :], rs_, rs_[:, :], Ein[hh], Ein[hh][:, :], ALU.mult)
                    p.copy('act', vb_[hh], vb_[hh][:, :], sh[(hh, 'v')], sh[(hh, 'v')][:, :])
                if RW_DEBUG[0] == 'prep': return
                pairs = [(0, 1), (2, 3)] if d == 0 else [(3, 2), (1, 0)]
                HS = [slice(i * 128, (i + 1) * 128) for i in range(6)]
                VS = [slice(i * 64, (i + 1) * 64) for i in range(6)]
                for pr in pairs:
                    CS = [slice(c4 * 128, (c4 + 1) * 128) for c4 in pr]
                    tks = []
                    for ci, c4 in enumerate(pr):
                        tk = tok[(gchunk + ci) % 4]; tks.append(tk)
                        for j, srcs in enumerate([at_, bt_, kt_, vb_]):
                            for hh in H3:
                                p.tr(ptk, ptk[:, j * 192 + hh * 64:j * 192 + (hh + 1) * 64], srcs[hh], srcs[hh][:, CS[ci]], ident, ident[0:64, 0:64])
                        p.act(tk, tk[:, :, :], ptk, ptk[:, 0:768].rearrange("p (j c) -> p j c", j=4), AF.Copy)
                    BL = [(ci, hh) for ci in range(2) for hh in H3]
                    def gram(dst, A_, B_, mask):
                        G = PG()
                        for bi, (ci, hh) in enumerate(BL):
                            p.mm(G, G[:, HS[bi]], A_[hh], A_[hh][:, CS[ci]], B_[hh], B_[hh][:, CS[ci]])
                        p.tt('dve', dst, dst[:, :], G, G[:, 0:768], cst, cst[:, mask:mask + 768], ALU.mult)
                    gram(M[0], bt_, at_, mSU)
                    gram(N[0], at_, bt_, mSL)
                    p.tt('dve', Pm[0], Pm[0][:, :], M[0], M[0][:, :], cst, cst[:, RW_I:RW_I + 768], ALU.add)
                    p.tt('pool', Qm[0], Qm[0][:, :], N[0], N[0][:, :], cst, cst[:, RW_I:RW_I + 768], ALU.add)
                    gram(AKT, kt_, at_, mSU)
                    gram(RBT, bt_, rt_, mU)
                    gram(RKT, kt_, rt_, mU)
                    for lev in range(1, 7):
                        Mo, No = M[(lev - 1) % 2], N[(lev - 1) % 2]; Mn, Nn = M[lev % 2], N[lev % 2]
                        Po, Qo = Pm[(lev - 1) % 2], Qm[(lev - 1) % 2]; Pn, Qn = Pm[lev % 2], Qm[lev % 2]
                        GM = PG()
                        for bi in range(6):
                            p.mm(GM, GM[:, HS[bi]], No, No[:, HS[bi]], Mo, Mo[:, HS[bi]])
                        p.act(Mn, Mn[:, :], GM, GM[:, 0:768], AF.Copy)
                        if lev < 6:
                            GN = PG()
                            for bi in range(6):
                                p.mm(GN, GN[:, HS[bi]], Mo, Mo[:, HS[bi]], No, No[:, HS[bi]])
                            p.act(Nn, Nn[:, :], GN, GN[:, 0:768], AF.Copy)
                        GP = PG()
                        for bi in range(6):
                            p.mm(GP, GP[:, HS[bi]], Qo, Qo[:, HS[bi]], Mn, Mn[:, HS[bi]])
                        p.tt('dve', Pn, Pn[:, :], GP, GP[:, 0:768], Po, Po[:, :], ALU.add)
                        if lev < 6:
                            GQ = PG()
                            for bi in range(6):
                                p.mm(GQ, GQ[:, HS[bi]], Po, Po[:, HS[bi]], Nn, Nn[:, HS[bi]])
                            p.tt('dve', Qn, Qn[:, :], GQ, GQ[:, 0:768], Qo, Qo[:, :], ALU.add)
                    PT = Pm[0]
                    G = PG()
                    for bi, (ci, hh) in enumerate(BL):
                        p.mm(G, G[:, VS[bi]], AKT, AKT[:, HS[bi]], tks[ci], tks[ci][:, 3, VS[hh]])
                    p.act(AKV, AKV[:, :], G, G[:, 0:384], AF.Copy)
                    G = PG()
                    for bi, (ci, hh) in enumerate(BL):
                        p.mm(G, G[:, VS[bi]], PT, PT[:, HS[bi]], AKV, AKV[:, VS[bi]])
                    p.act(UV, UV[:, :], G, G[:, 0:384], AF.Copy)
                    G = PG()
                    for bi, (ci, hh) in enumerate(BL):
                        p.mm(G, G[0:64, HS[bi]], tks[ci], tks[ci][:, 0, VS[hh]], PT, PT[:, HS[bi]])
                    p.act(TAT, TAT[:, :], G, G[0:64, 0:768], AF.Copy)
                    G = PG()
                    for bi, (ci, hh) in enumerate(BL):
                        p.mm(G, G[0:64, VS[bi]], tks[ci], tks[ci][:, 2, VS[hh]], tks[ci], tks[ci][:, 3, VS[hh]])
                    for bi, (ci, hh) in enumerate(BL):
                        c4 = pr[ci]
                        widx = c4 * 128 + 127 if d == 0 else c4 * 128
                        p.ts('dve', KVW, KVW[:, VS[bi]], G, G[0:64, VS[bi]], Ein[hh][:, widx:widx + 1], None, ALU.mult, extra_reads=[Ein[hh]])
                    for ci, c4 in enumerate(pr):
                        cs = CS[ci]; tk = tks[ci]
                        widx = c4 * 128 + 127 if d == 0 else c4 * 128
                        cur = S0b[gchunk % 2]; nxt = S0b[(gchunk + 1) % 2]
                        Yp = pY[gchunk % 2]
                        gchunk += 1
                        for hh in H3:
                            p.mm(pch, pch[:, VS[hh]], TAT, TAT[:, HS[ci * 3 + hh]], cur, cur[:, VS[hh]])
                        p.tt('dve', U, U[:, :], pch, pch[:, 0:192], UV, UV[:, ci * 192:(ci + 1) * 192], ALU.add)
                        for hh in H3:
                            bi = ci * 3 + hh
                            p.mm(Yp, Yp[0:64, HS[hh]], cur, cur[:, VS[hh]], rt_[hh], rt_[hh][:, cs], start=True, stop=False)
                            p.mm(Yp, Yp[0:64, HS[hh]], U, U[:, VS[hh]], RBT, RBT[:, HS[bi]], start=False, stop=False)
                            p.mm(Yp, Yp[0:64, HS[hh]], tk, tk[:, 3, VS[hh]], RKT, RKT[:, HS[bi]], start=False, stop=True)
                        p.act(ydall, ydall[:, :, cs], Yp, Yp[0:64, 0:384].rearrange("p (h t) -> p h t", h=3), AF.Copy)
                        for hh in H3:
                            p.mm(pch, pch[0:64, 256 + hh * 64:256 + (hh + 1) * 64], tk, tk[:, 1, VS[hh]], U, U[:, VS[hh]])
                        p.tt('dve', tS, tS[:, :], pch, pch[0:64, 256:448], S0, S0[:, :], ALU.add)
                        for hh in H3:
                            p.stt(S0, S0[:, VS[hh]], tS, tS[:, VS[hh]], Ein[hh][:, widx:widx + 1], KVW, KVW[:, VS[ci * 3 + hh]], ALU.mult, ALU.add,
                                  extra_reads=[Ein[hh]])
                        p.act(nxt, nxt[:, :], S0, S0[:, :], AF.Copy)
                for hh in H3:
                    b = hh * 15
                    rows = slice(hh * 64, (hh + 1) * 64)
                    if d == 0:
                        p.dma('pool', io['y0'][rows, t0:t0 + 512], ydall[:, hh, :], reads=[ydall], writes=[io['y0']])
                    else:
                        p.dma('sp', y0l[hh][:, :], io['y0'][rows, t0:t0 + 512], reads=[io['y0']], writes=[y0l[hh]])
                        y = pp3[hh]
                        p.tt('dve', y, y[:, :], ydall, ydall[:, hh, :], y0l[hh], y0l[hh][:, :], ALU.add)
                        G1 = blocksum(y)
                        p.tt('pool', pp1[hh], pp1[hh][:, :], y, y[:, :], y, y[:, :], ALU.mult)
                        G2 = blocksum(pp1[hh])
                        mean = pp2[hh]; var = tA[hh]
                        p.act(mean, mean[:, :], G1, G1[0:64, 0:512], AF.Copy, scale=1.0 / 64)
                        p.tt('pool', pp1[hh], pp1[hh][:, :], mean, mean[:, :], mean, mean[:, :], ALU.mult)
                        p.stt(var, var[:, :], G2, G2[0:64, 0:512], 1.0 / 64, pp1[hh], pp1[hh][:, :], ALU.mult, ALU.subtract)
                        p.act(var, var[:, :], var, var[:, :], AF.Sqrt, bias=64e-5)
                        p.op('dve', lambda: nc.vector.reciprocal(out=var[:, :], in_=var[:, :]), reads=[var], writes=[var])
                        p.tt('pool', y, y[:, :], y, y[:, :], mean, mean[:, :], ALU.subtract)
                        p.tt('pool', y, y[:, :], y, y[:, :], var, var[:, :], ALU.mult)
                        p.ts('dve', y, y[:, :], y, y[:, :], rwp[0:64, b + 9:b + 10], rwp[0:64, b + 10:b + 11], ALU.mult, ALU.add, extra_reads=[rwp])
                        rs_ = sh[(hh, 'r')]; ks = sh[(hh, 'k')]; vs_ = sh[(hh, 'v')]
                        p.stt(pp1[hh], pp1[hh][:, :], rs_, rs_[:, :], rwp[0:64, b + 8:b + 9], ks, ks[:, :], ALU.mult, ALU.mult, extra_reads=[rwp])
                        G3 = blocksum(pp1[hh])
                        p.tt('dve', pp2[hh], pp2[hh][:, :], G3, G3[0:64, 0:512], vs_, vs_[:, :], ALU.mult)
                        p.tt('pool', y, y[:, :], y, y[:, :], pp2[hh], pp2[hh][:, :], ALU.add)
                        G4 = PG()
                        p.mm(G4, G4[0:64, 0:512], gup, gup[:, rows], sgd, sgd[:, :])
                        p.tt('dve', yob[hh], yob[hh][:, :], y, y[:, :], G4, G4[0:64, 0:512], ALU.mult)
                        p.dma('pool', io['yT'][r_, 256 + hh * 64:256 + (hh + 1) * 64, off:off + 512], yob[hh][:, :], reads=[yob[hh]], writes=[io['yT']])

ALPHA = float(2.0 ** 0.5)
LN_EPS = 1e-5

def out_chunks():
    ch = []
    for q in range(4):
        heads = [2 * q, 2 * q + 1] if q < 2 else [q + 2]
        for s, h in enumerate(heads):
            ch.append((q, s * 128, 128, h * 128))
        ch.append((q, 256, 128, 768 + q * 192))
        ch.append((q, 384, 64, 768 + q * 192 + 128))
        ch.append((q, 448, 128, 1536 + q * 128))
    return ch

def ln_tile(p, u, lnw, lnb, scr, outf, outb, pfx, eps=LN_EPS):
    nc = p.nc
    st6 = scr['st6']; mv = scr['mv']; rs = scr['rs']; nm = scr['nm']; xn = u
    for c in range(4):
        p.op('dve', lambda c=c: nc.vector.bn_stats(out=st6[:, c * 6:(c + 1) * 6], in_=u[:, c * 512:(c + 1) * 512]), reads=[u], writes=[st6])
    p.op('dve', lambda: nc.vector.bn_aggr(out=mv[:, 0:2], in_=st6[:, 0:24]), reads=[st6], writes=[mv])
    p.ts('dve', rs, rs[:, 0:1], mv, mv[:, 1:2], eps, None, ALU.add)
    p.act(rs, rs[:, 0:1], rs, rs[:, 0:1], AF.Sqrt)
    p.op('dve', lambda: nc.vector.reciprocal(out=rs[:, 0:1], in_=rs[:, 0:1]), reads=[rs], writes=[rs])
    p.ts('dve', nm, nm[:, 0:1], mv, mv[:, 0:1], rs[:, 0:1], -1.0, ALU.mult, ALU.mult, extra_reads=[rs])
    p.act(xn, xn[:, :], u, u[:, :], AF.Identity, bias=nm[:, 0:1], scale=rs[:, 0:1], extra_reads=[nm, rs])
    p.tt('pool', xn, xn[:, :], xn, xn[:, :], lnw, lnw[:, :], ALU.mult)
    p.tt('dve', outf, outf[:, :], xn, xn[:, :], lnb, lnb[:, :], ALU.add)
    if outb is not None:
        p.tt('pool', outb, outb[:, :], xn, xn[:, :], lnb, lnb[:, :], ALU.add)

def ln_scratch(st, pfx):
    return dict(st6=st.sb(pfx + "st6", [128, 24]), mv=st.sb(pfx + "mv", [128, 2]), rs=st.sb(pfx + "rs", [128, 1]),
                nm=st.sb(pfx + "nm", [128, 1]))

def load_bc(p, st, name, src_ap, n, q='sp'):
    t = st.sb(name, [128, n])
    p.dma(q, t[:, :], src_ap.partition_broadcast(128), writes=[t])
    return t

def transpose_tile(p, xb, ident, pst, xT, tcol, evac_e='act'):
    for k in range(16):
        p.tr(pst, pst[:, k * 128:(k + 1) * 128], xb, xb[:, k * 128:(k + 1) * 128], ident, ident[:, :])
    src = pst[:, :].rearrange("p (k t) -> p k t", k=16)
    dst = xT[:, :, tcol:tcol + 128]
    if evac_e == 'act':
        p.act(xT, dst, pst, src, AF.Copy)
    else:
        p.copy(evac_e, xT, dst, pst, src)

def cast_weights(p, srcs, dsts, n_per):
    with p.stage() as st:
        CH = 4096
        stg = [st.sb(f"cw_s{i}", [128, CH], F32) for i in range(3)]
        ob = [st.sb(f"cw_o{i}", [128, CH], BF16) for i in range(3)]
        i = 0
        engs = ['dve', 'pool', 'act']
        for (sT, sap), (dT, dap) in zip(srcs, dsts):
            n = sap.shape[1]
            for c0 in range(0, n, CH):
                w = min(CH, n - c0)
                s = stg[i % 3]; o = ob[i % 3]
                p.dma('sp', s[:, 0:w], sap[:, c0:c0 + w], reads=[sT], writes=[s])
                p.copy(engs[i % 3], o, o[:, 0:w], s, s[:, 0:w])
                p.dma('pool', dap[:, c0:c0 + w], o[:, 0:w], reads=[o], writes=[dT])
                i += 1

def phase2(p, T_, io, layer_last=False, ST=512):
    nc = p.nc
    NT = T_ // 128
    chunks = out_chunks()
    NCH = len(chunks)
    srcs = []; dsts = []
    for nm in ['w1', 'w3', 'w2']:
        s = io[nm]; d = io[nm + 'b']
        srcs.append((s, s.ap.rearrange("e a b -> (e a b)").rearrange("(p n) -> p n", p=128)))
        dsts.append((d, d.ap.rearrange("e a b -> (e a b)").rearrange("(p n) -> p n", p=128)))
    cast_weights(p, srcs, dsts, None)

    with p.stage() as st:
        ident_f = st.sb("ident_f", [128, 128]); ident = st.sb("ident", [128, 128], BF16)
        p.dma('sp', ident_f[:, :], io['ident'][:, :], reads=[io['ident']], writes=[ident_f])
        p.copy('dve', ident, ident[:, :], ident_f, ident_f[:, :])
        wout = st.sb("wout", [128, NCH, 2048], BF16)
        wst = [st.sb(f"wst{i}", [128, 2048]) for i in range(2)]
        for j, (q, off, sz, r0) in enumerate(chunks):
            s = wst[j % 2]
            p.dma('sp', s[0:sz, :], io['w_out'][r0:r0 + sz, :], reads=[io['w_out']], writes=[s])
            p.copy(['dve', 'pool'][j % 2], wout, wout[0:sz, j, :], s, s[0:sz, :])
        lnw = load_bc(p, st, "lnw", io['ln_w'][0:1, :], 2048); lnb = load_bc(p, st, "lnb", io['ln_b'][0:1, :], 2048)
        scr = ln_scratch(st, "a")
        gluw_f = st.sb("gluw_f", [128, 4, 512]); gluw = st.sb("gluw", [128, 4, 512], BF16); glub = st.sb("glub", [128, 4])
        p.dma('sp', gluw_f[:, :, :], io['glu_w'].ap.rearrange("(k p) c -> p k c", p=128), reads=[io['glu_w']], writes=[gluw_f])
        p.copy('dve', gluw, gluw[:, :, :], gluw_f, gluw_f[:, :, :])
        p.dma('sp', glub[:, :], io['glu_bc'][:, :], reads=[io['glu_bc']], writes=[glub])
        ys5 = [st.sb(f"ys5{i}", [128, 4, 512], BF16) for i in range(2)]
        sgl = st.sb("sgl", [128, 512])
        s5j = [j for j, (q, off, sz, r0) in enumerate(chunks) if off == 448]
        ybuf = [st.sb(f"ybuf{i}", [128, NCH, 512], BF16) for i in range(2)]
        xr = wst
        u = st.sb("u", [128, 2048])
        x1f = [st.sb(f"x1f{i}", [128, 2048]) for i in range(1)]
        x1b = st.sb("x1b", [128, 2048], BF16)
        x1T = [st.sb(f"x1T{i}", [128, 16, 512], BF16) for i in range(1)]
        pm = [st.ps(f"pm{i}", [128, 512]) for i in range(4)]
        pst = st.ps("pst", [128, 2048], BF16)
        x1T_d = io['x1T'].ap.rearrange("(k p) t -> p k t", p=128)
        GT = min(4, NT)
        for t in range(NT):
            g = t // GT; tt_ = t % GT
            yb = ybuf[g % 2]
            if tt_ == 0:
                for j, (q, off, sz, r0) in enumerate(chunks):
                    p.dma('sp', yb[0:sz, j, 0:GT * 128], io['yT'][q, off:off + sz, g * GT * 128:(g + 1) * GT * 128], reads=[io['yT']], writes=[yb])
                W_ = GT * 128
                y5 = ys5[g % 2]
                for qo in range(4):
                    G = pm[qo]
                    for qi in range(4):
                        p.mm(G, G[:, 0:W_], gluw, gluw[:, qi, qo * 128:(qo + 1) * 128], yb, yb[:, s5j[qi], 0:W_], start=(qi == 0), stop=(qi == 3))
                    p.act(sgl, sgl[:, 0:W_], G, G[:, 0:W_], AF.Sigmoid, bias=glub[:, qo:qo + 1], extra_reads=[glub])
                    p.tt('dve', y5, y5[:, qo, 0:W_], sgl, sgl[:, 0:W_], yb, yb[:, s5j[qo], 0:W_], ALU.mult)
            xrt = xr[t % 2]
            p.dma('sp', xrt[:, :], io['xres'][t * 128:(t + 1) * 128, :], reads=[io['xres']], writes=[xrt])
            for c in range(4):
                for j, (q, off, sz, r0) in enumerate(chunks):
                    if j in s5j:
                        y5 = ys5[g % 2]
                        p.mm(pm[c], pm[c][:, :], y5, y5[:, s5j.index(j), tt_ * 128:(tt_ + 1) * 128], wout, wout[0:sz, j, c * 512:(c + 1) * 512],
                             start=(j == 0), stop=(j == NCH - 1))
                    else:
                        p.mm(pm[c], pm[c][:, :], yb, yb[0:sz, j, tt_ * 128:(tt_ + 1) * 128], wout, wout[0:sz, j, c * 512:(c + 1) * 512],
                             start=(j == 0), stop=(j == NCH - 1))
                p.stt(u, u[:, c * 512:(c + 1) * 512], xrt, xrt[:, c * 512:(c + 1) * 512], ALPHA, pm[c], pm[c][:, :], ALU.mult, ALU.add)
            xf = x1f[0]
            ln_tile(p, u, lnw, lnb, scr, xf, x1b, "a")
            p.dma('pool', io['x1'][t * 128:(t + 1) * 128, :], xf[:, :], reads=[xf], writes=[io['x1']])
            xT = x1T[0]
            transpose_tile(p, x1b, ident, pst, xT, tt_ * 128)
            if tt_ == GT - 1:
                p.dma('pool', x1T_d[:, :, g * GT * 128:(g + 1) * GT * 128], xT[:, :, 0:GT * 128], reads=[xT], writes=[io['x1T']])

    with p.stage() as st:
        ident_f = st.sb("ident_f", [128, 128]); ident = st.sb("ident", [128, 128], BF16)
        p.dma('sp', ident_f[:, :], io['ident'][:, :], reads=[io['ident']], writes=[ident_f])
        p.copy('dve', ident, ident[:, :], ident_f, ident_f[:, :])
        wr_f = st.sb("wr_f", [128, 16, 20]); wr = st.sb("wr", [128, 16, 20], BF16)
        p.dma('sp', wr_f[:, :, 0:4], io['rg'].ap.rearrange("(k p) g -> p k g", p=128), reads=[io['rg']], writes=[wr_f])
        for g in range(4):
            p.dma('sp', wr_f[:, :, 4 + 4 * g:8 + 4 * g], io['re'][g].rearrange("(k p) e -> p k e", p=128), reads=[io['re']], writes=[wr_f])
        p.copy('dve', wr, wr[:, :, :], wr_f, wr_f[:, :, :])
        rb = st.sb("rb", [128, 20])
        p.dma('sp', rb[:, 0:4], io['rgb'].ap.rearrange("(o g) -> o g", o=1).partition_broadcast(128), reads=[io['rgb']], writes=[rb])
        p.dma('sp', rb[:, 4:20], io['reb'].ap.rearrange("(o g) e -> o (g e)", o=1).partition_broadcast(128), reads=[io['reb']], writes=[rb])
        NS = ST // 128
        x1T = [st.sb(f"mx1T{i}", [128, 16, ST], BF16) for i in range(2)]
        yacc = st.sb("yacc", [128, NS, 2048])
        gate = st.sb("gate", [128, NS, 16])
        w1b = [st.sb(f"w1b{i}", [128, 16, 512], BF16) for i in range(1)]
        w3b = [st.sb(f"w3b{i}", [128, 16, 512], BF16) for i in range(1)]
        w2b = [st.sb(f"w2b{i}", [128, 4, 2048], BF16) for i in range(1)]
        hT = [st.sb(f"hT{i}", [128, 4, ST], BF16) for i in range(2)]
        sl = [st.sb(f"sl{i}", [128, ST]) for i in range(2)]
        lg = st.sb("lg", [128, 20]); r1 = st.sb("r1", [128, 8]); mg = st.sb("mg", [128, 4]); es = st.sb("es", [128, 4])
        tmp16 = st.sb("tmp16", [128, 16]); m1 = st.sb("m1", [128, 4]); m2 = st.sb("m2", [128, 4]); e2 = st.sb("e2", [128, 4])
        gi = st.sb("gi", [128, 4])
        pa = [st.ps(f"pa{i}", [128, 512]) for i in range(2)]
        pb = [st.ps(f"pb{i}", [128, 512]) for i in range(2)]
        py = [st.ps(f"py{i}", [128, 512]) for i in range(2)]
        x1T_d = io['x1T'].ap.rearrange("(k p) t -> p k t", p=128)
        NSUP = T_ // ST
        wcount = 0
        for s in range(NSUP):
            xT = x1T[s % 2]
            p.dma('sp', xT[:, :, :], x1T_d[:, :, s * ST:(s + 1) * ST], reads=[io['x1T']], writes=[xT])
            for m in range(NS):
                pl = pa[m % 2]
                for k in range(16):
                    p.mm(pl, pl[:, 0:20], xT, xT[:, k, m * 128:(m + 1) * 128], wr, wr[:, k, :], start=(k == 0), stop=(k == 15))
                p.tt('dve', lg, lg[:, :], pl, pl[:, 0:20], rb, rb[:, :], ALU.add)
                p.op('dve', lambda: nc.vector.tensor_reduce(out=r1[:, 0:1], in_=lg[:, 0:4], axis=AX.X, op=ALU.max), reads=[lg], writes=[r1])
                p.ts('dve', mg, mg[:, :], lg, lg[:, 0:4], r1[:, 0:1], None, ALU.is_equal, extra_reads=[r1])
                p.ts('dve', r1, r1[:, 1:2], r1, r1[:, 0:1], -1.0, None, ALU.mult)
                p.act(tmp16, tmp16[:, 0:4], lg, lg[:, 0:4], AF.Exp, bias=r1[:, 1:2], extra_reads=[r1], accum=(r1, r1[:, 2:3]))
                p.op('dve', lambda: nc.vector.reciprocal(out=r1[:, 3:4], in_=r1[:, 2:3]), reads=[r1], writes=[r1])
                p.ts('dve', es, es[:, :], lg, lg[:, 4:8], mg[:, 0:1], None, ALU.mult, extra_reads=[mg])
                for g in range(1, 4):
                    p.stt(es, es[:, :], lg, lg[:, 4 + 4 * g:8 + 4 * g], mg[:, g:g + 1], es, es[:, :], ALU.mult, ALU.add, extra_reads=[mg])
                p.op('dve', lambda: nc.vector.tensor_reduce(out=r1[:, 4:5], in_=es[:, :], axis=AX.X, op=ALU.max), reads=[es], writes=[r1])
                p.ts('dve', m1, m1[:, :], es, es[:, :], r1[:, 4:5], None, ALU.is_equal, extra_reads=[r1])
                p.stt(e2, e2[:, :], m1, m1[:, :], -1e30, es, es[:, :], ALU.mult, ALU.add)
                p.op('dve', lambda: nc.vector.tensor_reduce(out=r1[:, 5:6], in_=e2[:, :], axis=AX.X, op=ALU.max), reads=[e2], writes=[r1])
                p.ts('dve', m2, m2[:, :], e2, e2[:, :], r1[:, 5:6], None, ALU.is_equal, extra_reads=[r1])
                p.tt('dve', r1, r1[:, 6:7], r1, r1[:, 4:5], r1, r1[:, 5:6], ALU.subtract)
                p.act(r1, r1[:, 6:7], r1, r1[:, 6:7], AF.Sigmoid)
                p.ts('dve', r1, r1[:, 7:8], r1, r1[:, 6:7], -1.0, 1.0, ALU.mult, ALU.add)
                p.ts('dve', gi, gi[:, :], m1, m1[:, :], r1[:, 6:7], None, ALU.mult, extra_reads=[r1])
                p.stt(gi, gi[:, :], m2, m2[:, :], r1[:, 7:8], gi, gi[:, :], ALU.mult, ALU.add, extra_reads=[r1])
                p.ts('dve', gi, gi[:, :], gi, gi[:, :], r1[:, 3:4], None, ALU.mult, extra_reads=[r1])
                for g in range(4):
                    p.ts('dve', gate, gate[:, m, 4 * g:4 * g + 4], gi, gi[:, :], mg[:, g:g + 1], None, ALU.mult, extra_reads=[mg])
            for e in range(16):
                wb = 0
                p.dma('sp', w1b[wb][:, :, :], io['w1b'][e].rearrange("(k p) f -> p k f", p=128), reads=[io['w1b']], writes=[w1b[wb]])
                p.dma('sp', w3b[wb][:, :, :], io['w3b'][e].rearrange("(k p) f -> p k f", p=128), reads=[io['w3b']], writes=[w3b[wb]])
                p.dma('sp', w2b[wb][:, :, :], io['w2b'][e].rearrange("(k p) f -> p k f", p=128), reads=[io['w2b']], writes=[w2b[wb]])
                h = hT[e % 2]
                for f in range(4):
                    a = pa[f % 2]; b = pb[f % 2]
                    for k in range(16):
                        p.mm(a, a[:, 0:ST], w1b[wb], w1b[wb][:, k, f * 128:(f + 1) * 128], xT, xT[:, k, :], start=(k == 0), stop=(k == 15))
                    for k in range(16):
                        p.mm(b, b[:, 0:ST], w3b[wb], w3b[wb][:, k, f * 128:(f + 1) * 128], xT, xT[:, k, :], start=(k == 0), stop=(k == 15))
                    s_ = sl[f % 2]
                    p.act(s_, s_[:, :], a, a[:, 0:ST], AF.Silu)
                    p.tt('dve', h, h[:, f, :], s_, s_[:, :], b, b[:, 0:ST], ALU.mult)
                i = 0
                for m in range(NS):
                    for c in range(4):
                        y = py[i % 2]; i += 1
                        for f in range(4):
                            p.mm(y, y[:, :], h, h[:, f, m * 128:(m + 1) * 128], w2b[wb], w2b[wb][:, f, c * 512:(c + 1) * 512], start=(f == 0), stop=(f == 3))
                        if e == 0:
                            p.ts('dve', yacc, yacc[:, m, c * 512:(c + 1) * 512], y, y[:, :], gate[:, m, e:e + 1], None, ALU.mult, extra_reads=[gate])
                        else:
                            p.stt(yacc, yacc[:, m, c * 512:(c + 1) * 512], y, y[:, :], gate[:, m, e:e + 1], yacc, yacc[:, m, c * 512:(c + 1) * 512],
                                  ALU.mult, ALU.add, extra_reads=[gate])
            for m in range(NS):
                t = s * NS + m
                p.dma('pool', io['moe'][t * 128:(t + 1) * 128, :], yacc[:, m, :], reads=[yacc], writes=[io['moe']])

    with p.stage() as st:
        ident_f = st.sb("ident_f", [128, 128]); ident = st.sb("ident", [128, 128], BF16)
        p.dma('sp', ident_f[:, :], io['ident'][:, :], reads=[io['ident']], writes=[ident_f])
        p.copy('dve', ident, ident[:, :], ident_f, ident_f[:, :])
        lnw2 = load_bc(p, st, "lnw2", io['ln_w'][1:2, :], 2048); lnb2 = load_bc(p, st, "lnb2", io['ln_b'][1:2, :], 2048)
        lnw3 = load_bc(p, st, "lnw3", io['ln_w'][2:3, :], 2048); lnb3 = load_bc(p, st, "lnb3", io['ln_b'][2:3, :], 2048)
        scr = ln_scratch(st, "c")
        pg = st.sb("pg", [128, 16, 2048], BF16); pp = st.sb("pp", [128, 2, 2048], BF16)
        wst = [st.sb(f"wst{i}", [128, 2048]) for i in range(2)]
        for k in range(16):
            s = wst[k % 2]
            p.dma('sp', s[:, :], io['ple_gate'][k * 128:(k + 1) * 128, :], reads=[io['ple_gate']], writes=[s])
            p.copy(['dve', 'pool'][k % 2], pg, pg[:, k, :], s, s[:, :])
        for k in range(2):
            s = wst[k % 2]
            p.dma('sp', s[:, :], io['ple_proj'][k * 128:(k + 1) * 128, :], reads=[io['ple_proj']], writes=[s])
            p.copy(['dve', 'pool'][k % 2], pp, pp[:, k, :], s, s[:, :])
        xr = wst[0]; mo = wst[1]
        x2T = st.sb("cx2T", [128, 16, 128], BF16)
        pTf = st.sb("pTf", [128, 2, 128]); pTb = st.sb("pTb", [128, 2, 128], BF16)
        sg = st.sb("sg", [128, 512])
        u = st.sb("u", [128, 2048])
        x2f = st.sb("x2f", [128, 2048]); x2b = st.sb("x2b", [128, 2048], BF16)
        x3f = st.sb("x3f", [128, 2048]); x3b = st.sb("x3b", [128, 2048], BF16)
        x3T = [st.sb(f"x3T{i}", [128, 16, 512], BF16) for i in range(2)]
        pgp = [st.ps(f"pgp{i}", [128, 512]) for i in range(2)]
        ppp = [st.ps(f"ppp{i}", [128, 512]) for i in range(2)]
        pst = st.ps("pst", [128, 2048], BF16)
        xoT_d = io['xoT'].ap.rearrange("(k p) t -> p k t", p=128)
        pT_d = io['pT'].ap.rearrange("(k p) t -> p k t", p=128)
        GT = min(4, NT)
        for t in range(NT):
            g = t // GT; tt_ = t % GT
            rows = slice(t * 128, (t + 1) * 128)
            p.dma('sp', xr[:, :], io['x1'][rows, :], reads=[io['x1']], writes=[xr])
            p.dma('sp', mo[:, :], io['moe'][rows, :], reads=[io['moe']], writes=[mo])
            p.dma('sp', pTf[:, :, :], pT_d[:, :, rows], reads=[io['pT']], writes=[pTf])
            p.copy('pool', pTb, pTb[:, :, :], pTf, pTf[:, :, :])
            p.stt(u, u[:, :], xr, xr[:, :], ALPHA, mo, mo[:, :], ALU.mult, ALU.add)
            ln_tile(p, u, lnw2, lnb2, scr, x2f, x2b, "c")
            transpose_tile(p, x2b, ident, pst, x2T, 0)
            for c in range(4):
                cs = slice(c * 512, (c + 1) * 512)
                G = pgp[c % 2]; PP = ppp[c % 2]
                for k in range(16):
                    p.mm(G, G[:, :], x2T, x2T[:, k, :], pg, pg[:, k, cs], start=(k == 0), stop=(k == 15))
                for k in range(2):
                    p.mm(PP, PP[:, :], pTb, pTb[:, k, :], pp, pp[:, k, cs], start=(k == 0), stop=(k == 1))
                p.act(sg, sg[:, :], G, G[:, :], AF.Sigmoid)
                p.tt('dve', sg, sg[:, :], sg, sg[:, :], PP, PP[:, :], ALU.mult)
                p.stt(u, u[:, cs], x2f, x2f[:, cs], ALPHA, sg, sg[:, :], ALU.mult, ALU.add)
            ln_tile(p, u, lnw3, lnb3, scr, x3f, x3b, "c")
            p.dma('pool', io['xo'][rows, :], x3f[:, :], reads=[x3f], writes=[io['xo']])
            x3 = x3T[g % 2]
            transpose_tile(p, x3b, ident, pst, x3, tt_ * 128)
            if tt_ == GT - 1:
                p.dma('pool', xoT_d[:, :, g * GT * 128:(g + 1) * GT * 128], x3[:, :, 0:GT * 128], reads=[x3], writes=[io['xoT']])


_S = 16384
_D = 2048
_T = 4096
_PROGS = {}
_GROUPS = [[0, 1, 2, 3], [4, 5, 6, 7]]
_P1_KEYS = [('w_in', [_D, NZ]), ('rwp', [128, 55]), ('rw_wup', [64, 2, 192]), ('rw_aup', [64, 2, 192]), ('rw_gup', [128, 192]),
            ('s5rows', [3, 1024]), ('s5cols', [128, 24]), ('s5bl', [2, 128, 1024]), ('s5cl', [2, 128, 1024]), ('s5d', [128, 1])]
_P2_KEYS = [('w_out', [_D, _D]), ('ln_w', [3, _D]), ('ln_b', [3, _D]), ('rg', [_D, 4]), ('rgb', [4]), ('re', [4, _D, 4]), ('reb', [4, 4]),
            ('w1', [16, _D, 512]), ('w3', [16, _D, 512]), ('w2', [16, 512, _D]), ('glu_w', [512, 512]), ('glu_bc', [128, 4]),
            ('ple_proj', [256, _D]), ('ple_gate', [_D, _D]), ('pT', [256, None])]

def stage_select(p, gath, rmask, yTr):
    with p.stage() as st:
        mk = st.sb("mk", [128, 4]); p.dma('sp', mk[:, :], rmask[:, :], reads=[rmask], writes=[mk])
        cand = [[st.sb(f"cand{i}_{r}", [128, _T], BF16) for r in range(4)] for i in range(2)]
        acc = [st.sb(f"acc{i}", [128, _T], BF16) for i in range(2)]
        n = 0
        for q in range(4):
            for i in range(6):
                r0 = i * 96; sz = 96
                cd = cand[n % 2]; a = acc[n % 2]
                e = 'dve'; n += 1
                for r in range(4):
                    p.dma('sp', cd[r][0:sz, :], gath[r * 6 + i, q * 96:(q + 1) * 96, :], reads=[gath], writes=[cd[r]])
                p.ts(e, a, a[0:sz, :], cd[0], cd[0][0:sz, :], mk[0:sz, 0:1], None, ALU.mult, extra_reads=[mk])
                for r in range(1, 4):
                    p.stt(a, a[0:sz, :], cd[r], cd[r][0:sz, :], mk[0:sz, r:r + 1], a, a[0:sz, :], ALU.mult, ALU.add, extra_reads=[mk], e=e)
                p.dma('pool', yTr[q, r0:r0 + sz, :], a[0:sz, :], reads=[a], writes=[yTr])

def _build_fused():
    S = _S; D = _D; T_ = _T
    nc = bass.Bass("TRN2", target_bir_lowering=False)
    p = Prog(nc)
    E = {}
    def ext(name, shape, dt=F32):
        E[name] = p.dram(name, shape, dt, kind="ExternalInput"); return E[name]
    ext('xT0', [4, D, T_]); ext('xres', [T_, D]); ext('pos', [S], I32); ext('rconst', [128, RC_N]); ext('ident', [128, 128])
    ext('rwconst', [128, RW_N]); ext('s5iota', [128, 512]); ext('rmask', [128, 4])
    for L in range(2):
        for k, sh in _P1_KEYS + _P2_KEYS:
            ext(f"{k}_{L}", [T_ if s is None else s for s in sh])
    xo_final = p.dram('xo_final', [T_, D], F32, kind="ExternalOutput")
    sc = {}
    sc['zT'] = p.dram('zT', [NZ, S], F32)
    for nm in ['qrT', 'krT']: sc[nm] = p.dram(nm, [2, 128, S], BF16)
    for nm in ['ktok', 'vtok', 'sbd']: sc[nm] = p.dram(nm, [2, S // 128, 128, 128], BF16)
    sc['yf'] = p.dram('yf', [128, S]); sc['y0'] = p.dram('y0', [192, S])
    yTloc = p.dram('yTloc', [4, 576, T_], BF16)
    gath = p.dram('gath', [24, 4 * 96, T_], BF16)
    yTr = p.dram('yTr', [4, 576, T_], BF16)
    sc2 = dict(x1=p.dram('x1', [T_, D]), x1T=p.dram('x1T', [D, T_], BF16), moe=p.dram('moe', [T_, D]),
               w1b=p.dram('w1b', [16, D, 512], BF16), w3b=p.dram('w3b', [16, D, 512], BF16), w2b=p.dram('w2b', [16, 512, D], BF16))
    xo0 = p.dram('xo0', [T_, D]); xoT = p.dram('xoT', [D, T_], BF16); xoT_dummy = p.dram('xoT_dummy', [D, T_], BF16)
    xTg = p.dram('xTg', [16, 4 * 128, T_], BF16)
    for L in range(2):
        io1 = dict(sc)
        for k, _ in _P1_KEYS: io1[k] = E[f"{k}_{L}"]
        io1.update(pos=E['pos'], rconst=E['rconst'], ident=E['ident'], rwconst=E['rwconst'], s5iota=E['s5iota'], yT=yTloc)
        io1['xT'] = E['xT0'] if L == 0 else xTg
        xsrc = None
        if L == 1:
            xsrc = lambda r: xTg.ap[:, r * 128:(r + 1) * 128, :].rearrange("k p t -> p k t")
        stage_inproj(p, S, io1, L == 0, xsrc=xsrc)
        stage_retention(p, S, io1)
        stage_s5(p, S, io1)
        stage_rwkv(p, S, io1)
        for r in range(4):
            for i in range(6):
                p.collective("AllGather", yTloc, yTloc.ap[r, i * 96:(i + 1) * 96, :], gath, gath.ap[r * 6 + i], _GROUPS)
        stage_select(p, gath, E['rmask'], yTr)
        io2 = dict(sc2)
        for k, _ in _P2_KEYS: io2[k] = E[f"{k}_{L}"]
        io2.update(yT=yTr, ident=E['ident'], xres=(E['xres'] if L == 0 else xo0), xo=(xo0 if L == 0 else xo_final),
                   xoT=(xoT if L == 0 else xoT_dummy))
        phase2(p, T_, io2, ST=512)
        if L == 0:
            for k in range(16):
                p.collective("AllGather", xoT, xoT.ap[k * 128:(k + 1) * 128, :], xTg, xTg.ap[k], _GROUPS)
    p.finish([xo_final])
    return nc

def kernel(**inp):
    x = np.ascontiguousarray(np.asarray(inp['x'], dtype=np.float32))
    S = _S; D = _D
    if 'f' not in _PROGS:
        _PROGS['f'] = _build_fused()
    ident = np.eye(128, dtype=np.float32)
    rwc = rwkv_consts()
    positions = np.asarray(inp['positions']).astype(np.int32)
    g = lambda k: np.asarray(inp[k])
    maps = []
    for c in range(8):
        b, q = c // 4, c % 4
        r = q
        rmask = np.zeros((128, 4), np.float32); rmask[:, r] = 1.0
        m = dict(xT0=np.ascontiguousarray(x[b].T.reshape(D, 4, S // 4).transpose(1, 0, 2)),
                 xres=np.ascontiguousarray(x[b, r * _T:(r + 1) * _T]), pos=np.ascontiguousarray(positions[b]),
                 rconst=ret_consts(q), ident=ident, rwconst=rwc, rmask=rmask)
        for L in range(2):
            d1 = dict(w_in=np.ascontiguousarray(g('w_in')[L][:, core_cols(q)]))
            d1.update(rwkv_host_layout(q, g('rwkv_mu_prev')[L], g('rwkv_mu_next')[L], g('rwkv_w0')[L], g('rwkv_w_up')[L], g('rwkv_a0')[L],
                                       g('rwkv_a_up')[L], g('rwkv_g_up')[L], g('rwkv_k_k')[L], g('rwkv_k_a')[L], g('rwkv_r_k')[L],
                                       g('rwkv_lnx_w')[L], g('rwkv_lnx_b')[L]))
            s5 = s5_host_layout(q, g('s5_lam_re')[L], g('s5_lam_im')[L], g('s5_log_dt')[L], g('s5_b_re')[L], g('s5_b_im')[L],
                                g('s5_c_re')[L], g('s5_c_im')[L], g('s5_d')[L])
            m['s5iota'] = s5.pop('s5iota')
            d1.update(s5)
            d2 = dict(w_out=g('w_out')[L], ln_w=g('ln_w')[L], ln_b=g('ln_b')[L],
                      rg=g('moe_router_g')[L], rgb=g('moe_router_g_b')[L], re=g('moe_router_e')[L], reb=g('moe_router_e_b')[L],
                      w1=g('moe_w1')[L], w3=g('moe_w3')[L], w2=g('moe_w2')[L],
                      glu_w=g('s5_glu_w')[L], glu_bc=np.ascontiguousarray(g('s5_glu_b')[L].reshape(4, 128).T),
                      ple_proj=g('ple_proj')[L], ple_gate=g('ple_gate')[L],
                      pT=np.ascontiguousarray(g('p')[L, b, r * _T:(r + 1) * _T].T))
            for k, v in list(d1.items()) + list(d2.items()):
                m[f"{k}_{L}"] = np.ascontiguousarray(v)
        maps.append(m)
    res = run_bass_kernel_spmd(_PROGS['f'], maps, core_ids=list(range(8)))
    out = np.empty_like(x)
    for c in range(8):
        b, r = c // 4, c % 4
        out[b, r * _T:(r + 1) * _T] = np.asarray(res.results[c]['xo_final'])
    return out
```

```python
import math
import numpy as np
import concourse.bass as bass
import concourse.mybir as mybir
from concourse.bass_utils import run_bass_kernel_spmd
from contextlib import ExitStack
F32 = mybir.dt.float32; BF16 = mybir.dt.bfloat16; I32 = mybir.dt.int32
AF = mybir.ActivationFunctionType; ALU = mybir.AluOpType; AX = mybir.AxisListType


class T:
    def __init__(self, ap, name):
        self.ap = ap; self.name = name
        self.w = None
        self.r = []
    def __getitem__(self, k):
        return self.ap[k]


class Prog:
    NDMA = 8
    SEM_MAX = 30000
    def __init__(self, nc):
        self.nc = nc
        self.eng = {'pe': nc.tensor, 'dve': nc.vector, 'act': nc.scalar, 'pool': nc.gpsimd, 'sp': nc.sync}
        self.gen = {e: 0 for e in ['pe', 'dve', 'act', 'pool']}
        self.sem = {e: nc.alloc_semaphore("sem_" + e) for e in ['pe', 'dve', 'act', 'pool']}
        self.cnt = {e: 0 for e in self.sem}
        self.seen = {e: {} for e in self.eng}
        self.dsem = {q: [nc.alloc_semaphore(f"dsem_{q}{i}") for i in range(self.NDMA)] for q in ['sp', 'pool']}
        self.dcnt = {q: [0] * self.NDMA for q in self.dsem}
        self.dgen = {q: [0] * self.NDMA for q in self.dsem}
        self.dnext = {q: 0 for q in self.dsem}
        self.semobj = {}
        for e, s in self.sem.items(): self.semobj[('c', e, 0)] = s
        for q in self.dsem:
            for i, s in enumerate(self.dsem[q]): self.semobj[('d', q, i, 0)] = s
        self.ninst = 0
        self.nsb = 0

    def sb(self, name, shape, dt=F32):
        return T(self.nc.alloc_sbuf_tensor(name, list(shape), dt).ap(), name)
    def ps(self, name, shape, dt=F32):
        return T(self.nc.alloc_psum_tensor(name, list(shape), dt).ap(), name)
    def dram(self, name, shape, dt=F32, kind="Internal"):
        return T(self.nc.dram_tensor(name, list(shape), dt, kind=kind).ap(), name)

    def _wait(self, e, tok):
        if tok is None: return
        key, val = tok
        if self.seen[e].get(key, 0) >= val: return
        self.eng[e].wait_ge(self.semobj[key], val)
        self.seen[e][key] = val

    def _deps(self, e, reads, writes):
        for b in reads:
            if b.w is not None and not (e == 'pe' and b.w[0][0:2] == ('c', 'pe')):
                self._wait(e, b.w)
        for b in writes:
            if b.w is not None and not (e == 'pe' and b.w[0][0:2] == ('c', 'pe')):
                self._wait(e, b.w)
            for tok in b.r:
                if e == 'pe' and tok[0][0:2] == ('c', 'pe'): continue
                self._wait(e, tok)

    def _mark(self, tok, reads, writes):
        for b in reads:
            b.r = [t for t in b.r if t[0] != tok[0]] + [tok]
        for b in writes:
            b.w = tok; b.r = []

    def op(self, e, fn, reads=(), writes=()):
        self._deps(e, reads, writes)
        if self.cnt[e] >= self.SEM_MAX:
            self.gen[e] += 1; self.cnt[e] = 0
            self.sem[e] = self.nc.alloc_semaphore(f"sem_{e}_{self.gen[e]}")
            self.semobj[('c', e, self.gen[e])] = self.sem[e]
        inst = fn()
        self.cnt[e] += 1
        inst.then_inc(self.sem[e], 1)
        tok = (('c', e, self.gen[e]), self.cnt[e])
        self._mark(tok, reads, writes)
        self.ninst += 1
        return inst

    def dma(self, q, out, in_, reads=(), writes=(), **kw):
        i = self.dnext[q]; self.dnext[q] = (i + 1) % self.NDMA
        key = ('d', q, i, self.dgen[q][i])
        if self.dcnt[q][i] > 0:
            self._wait(q, (key, self.dcnt[q][i]))
        if self.dcnt[q][i] >= self.SEM_MAX:
            self.dgen[q][i] += 1; self.dcnt[q][i] = 0
            self.dsem[q][i] = self.nc.alloc_semaphore(f"dsem_{q}{i}_{self.dgen[q][i]}")
            key = ('d', q, i, self.dgen[q][i])
            self.semobj[key] = self.dsem[q][i]
        self._deps(q, reads, writes)
        inst = self.eng[q].dma_start(out=out, in_=in_, **kw)
        self.dcnt[q][i] += 16
        inst.then_inc(self.dsem[q][i], 16)
        tok = (key, self.dcnt[q][i])
        self._mark(tok, reads, writes)
        self.ninst += 1
        return inst

    def finish(self, outs):
        for b in outs:
            self._wait('sp', b.w)
        for e in ['pe', 'dve', 'act', 'pool']:
            if self.cnt[e] > 0:
                self._wait('sp', (('c', e, self.gen[e]), self.cnt[e]))
        for q in self.dsem:
            for i in range(self.NDMA):
                if self.dcnt[q][i] > 0:
                    self._wait('sp', (('d', q, i, self.dgen[q][i]), self.dcnt[q][i]))

    def mm(self, out, o_ap, lhsT, l_ap, rhs, r_ap, start=True, stop=True):
        return self.op('pe', lambda: self.nc.tensor.matmul(o_ap, l_ap, r_ap, start=start, stop=stop),
                       reads=[lhsT, rhs], writes=[out])
    def tr(self, out, o_ap, in_, i_ap, ident, id_ap):
        return self.op('pe', lambda: self.nc.tensor.transpose(o_ap, i_ap, id_ap), reads=[in_, ident], writes=[out])
    def act(self, out, o_ap, in_, i_ap, func, bias=None, scale=1.0, extra_reads=(), e='act', accum=None):
        kw = {}
        if bias is not None: kw['bias'] = bias
        if accum is not None: kw['accum_out'] = accum[1]
        wr = [out] + ([accum[0]] if accum is not None else [])
        return self.op('act', lambda: self.nc.scalar.activation(out=o_ap, in_=i_ap, func=func, scale=scale, **kw),
                       reads=[in_] + list(extra_reads), writes=wr)
    def tt(self, e, out, o_ap, a, a_ap, b, b_ap, op):
        en = self.eng[e]
        return self.op(e, lambda: en.tensor_tensor(out=o_ap, in0=a_ap, in1=b_ap, op=op), reads=[a, b], writes=[out])
    def ts(self, e, out, o_ap, a, a_ap, s1, s2, op0, op1=None, extra_reads=(), accum=None):
        en = self.eng[e]
        kw = {}
        if op1 is not None: kw['op1'] = op1
        wr = [out]
        if accum is not None:
            kw['accum_out'] = accum[1]; wr.append(accum[0])
        return self.op(e, lambda: en.tensor_scalar(out=o_ap, in0=a_ap, scalar1=s1, scalar2=s2, op0=op0, **kw),
                       reads=[a] + list(extra_reads), writes=wr)
    def stt(self, out, o_ap, a, a_ap, s, b, b_ap, op0, op1, extra_reads=(), e='dve'):
        en = self.eng[e]
        return self.op(e, lambda: en.scalar_tensor_tensor(out=o_ap, in0=a_ap, scalar=s, in1=b_ap, op0=op0, op1=op1),
                       reads=[a, b] + list(extra_reads), writes=[out])
    def copy(self, e, out, o_ap, in_, i_ap):
        if e == 'act':
            return self.act(out, o_ap, in_, i_ap, AF.Copy)
        en = self.eng[e]
        return self.op(e, lambda: en.tensor_copy(out=o_ap, in_=i_ap), reads=[in_], writes=[out])
    def memset(self, e, out, o_ap, val):
        en = self.eng[e]
        return self.op(e, lambda: en.memset(o_ap, val), writes=[out])

class Stage:
    def __init__(self, p):
        self.p = p; self.es = ExitStack()
    def __enter__(self):
        self.es.__enter__(); return self
    def __exit__(self, *a):
        self.p.barrier()
        return self.es.__exit__(*a)
    def sb(self, name, shape, dt=F32):
        self.p.nsb += 1; name = f"s{self.p.nsb}_{name}"
        h = self.es.enter_context(self.p.nc.sbuf_tensor(name, list(shape), dt))
        return T(h.ap(), name)
    def ps(self, name, shape, dt=F32):
        self.p.nsb += 1; name = f"s{self.p.nsb}_{name}"
        h = self.es.enter_context(self.p.nc.psum_tensor(name, list(shape), dt))
        return T(h.ap(), name)

def _barrier(self):
    toks = []
    for e in ['pe', 'dve', 'act', 'pool']:
        if self.cnt[e] > 0: toks.append((('c', e, self.gen[e]), self.cnt[e]))
    for q in self.dsem:
        for i in range(self.NDMA):
            if self.dcnt[q][i] > 0: toks.append((('d', q, i, self.dgen[q][i]), self.dcnt[q][i]))
    for e in self.eng:
        for tok in toks:
            if tok[0][0:2] == ('c', e) and e == 'pe': continue
            self._wait(e, tok)
Prog.barrier = _barrier
Prog.stage = lambda self: Stage(self)

def _collective(self, kind, in_T, in_ap, out_T, out_ap, groups):
    if not hasattr(self, 'ccsem'):
        self.ccsem = self.nc.alloc_semaphore("ccsem"); self.ccnt = 0
        self.semobj[('cc',)] = self.ccsem
    self._deps('pool', [in_T], [out_T])
    inst = self.nc.gpsimd.collective_compute(kind, ALU.bypass, replica_groups=groups, ins=[in_ap], outs=[out_ap])
    self.ccnt += 1
    inst.then_inc(self.ccsem, 1)
    tok = (('cc',), self.ccnt)
    self._mark(tok, [in_T], [out_T])
    self.ninst += 1
Prog.collective = _collective

PI = math.pi
C1 = 6.28125
C2 = 2 * math.pi - C1
INV2PI = 1.0 / (2 * math.pi)

RET_W = 768; RWKV_W = 768; RET_COLS = 3072; RWKV_COLS = 2688
NZ = 2112
COLTILES = [(i * 128, 128) for i in range(8)] + [(1024, 128), (1152, 64), (1216, 128), (1344, 64), (1408, 128), (1536, 64),
                                                  (1600, 128), (1728, 128), (1856, 128), (1984, 128)]

def ret_heads(q):
    return [2 * q, 2 * q + 1] if q < 2 else [q + 2, q + 2]

def core_cols(q):
    cols = []
    for h in ret_heads(q):
        for part in range(4):
            cols += list(range(part * RET_W + h * 128, part * RET_W + (h + 1) * 128))
    base = RET_COLS
    for part in range(3):
        cols += list(range(base + part * RWKV_W + q * 192, base + part * RWKV_W + (q + 1) * 192))
    cols += list(range(base + 3 * RWKV_W, base + 3 * RWKV_W + 384))
    cols += list(range(RET_COLS + RWKV_COLS + q * 128, RET_COLS + RWKV_COLS + (q + 1) * 128))
    assert len(cols) == NZ
    return cols

def ret_consts(q):
    import numpy as np
    C = 128
    cols = []
    inv = (10000.0 ** (-(np.arange(128) % 64).astype(np.float32) / np.float32(64))).astype(np.float32)
    cols.append(inv[:, None]); cols.append(np.where(np.arange(128) < 64, -1.0, 1.0).astype(np.float32)[:, None])
    pos = np.arange(C, dtype=np.float64)
    for h in ret_heads(q):
        lg = np.log(1.0 - 2.0 ** (-5.0 - h))
        cols.append(np.exp(lg * (C - 1 - pos))[:, None])
        cols.append(np.exp(lg * pos)[:, None])
        cols.append(np.full((128, 1), np.exp(lg * C)))
        cols.append(np.exp(lg * np.abs(pos[:, None] - pos[None, :])))
        cols.append(np.tile(np.exp(lg * (pos + 1.0))[None, :], (128, 4)))
        cols.append(np.tile(np.exp(lg * (C - pos))[None, :], (128, 4)))
    return np.concatenate(cols, axis=1).astype(np.float32)
RC_SLOT = 3 + 128 + 512 + 512
RC_N = 2 + 2 * RC_SLOT

def stage_inproj(p, S, io, x_f32, xsrc=None):
    nc = p.nc
    Q4 = S // 4
    with p.stage() as st:
        wb = st.sb("winb", [128, 16, NZ], BF16)
        wst = [st.sb(f"wst{i}", [128, NZ]) for i in range(2)]
        for k in range(16):
            s = wst[k % 2]
            p.dma('sp', s[:, :], io['w_in'][k * 128:(k + 1) * 128, :], reads=[io['w_in']], writes=[s])
            p.copy(['dve', 'pool'][k % 2], wb, wb[:, k, :], s, s[:, :])
        xb = [st.sb(f"xb{i}", [128, 16, 512], BF16) for i in range(2)]
        if x_f32:
            xf = [st.sb(f"xf{i}", [128, 8, 512]) for i in range(2)]
        zo = [st.sb(f"zo{i}", [128, 512]) for i in range(4)]
        ps = [st.ps(f"ps{i}", [128, 512]) for i in range(4)]
        n = 0
        for tt in range(S // 512):
            r = (tt * 512) // Q4; off = tt * 512 - r * Q4
            src = xsrc(r) if xsrc is not None else io['xT'][r].rearrange("(k p) t -> p k t", p=128)
            x = xb[tt % 2]
            if x_f32:
                for hf in range(2):
                    p.dma('sp', xf[hf][:, :, :], src[:, hf * 8:(hf + 1) * 8, off:off + 512], reads=[io['xT']], writes=[xf[hf]])
                    p.copy(['dve', 'act'][hf], x, x[:, hf * 8:(hf + 1) * 8, :], xf[hf], xf[hf][:, :, :])
            else:
                p.dma('sp', x[:, :, :], src[:, :, off:off + 512], reads=[io['xT']], writes=[x])
            for ci, (c0, w) in enumerate(COLTILES):
                P_ = ps[n % 4]; z = zo[n % 4]
                for k in range(16):
                    p.mm(P_, P_[0:w, :], wb, wb[:, k, c0:c0 + w], x, x[:, k, :], start=(k == 0), stop=(k == 15))
                if n % 2 == 0:
                    p.act(z, z[0:w, :], P_, P_[0:w, :], AF.Copy)
                else:
                    p.copy('dve', z, z[0:w, :], P_, P_[0:w, :])
                p.dma('pool', io['zT'][c0:c0 + w, tt * 512:(tt + 1) * 512], z[0:w, :], reads=[z], writes=[io['zT']])
                n += 1

def stage_retention(p, S, io):
    nc = p.nc
    NCK = S // 128; NTT = S // 512; Q4 = S // 4
    zT = io['zT']
    with p.stage() as st:
        rc = st.sb("rc", [128, RC_N])
        p.dma('sp', rc[:, :], io['rconst'][:, :], reads=[io['rconst']], writes=[rc])
        ident_f = st.sb("ident_f", [128, 128]); ident = st.sb("ident", [128, 128], BF16)
        p.dma('sp', ident_f[:, :], io['ident'][:, :], reads=[io['ident']], writes=[ident_f])
        p.copy('dve', ident, ident[:, :], ident_f, ident_f[:, :])
        posi = st.sb("posi", [128, 512], I32); ang = st.sb("ang", [128, 512]); a2 = st.sb("a2", [128, 512])
        ki = st.sb("ki", [128, 512], I32); kf = st.sb("kf", [128, 512])
        cos = st.sb("cos", [128, 512]); sin = st.sb("sin", [128, 512])
        zq = [st.sb(f"zq{i}", [128, 512]) for i in range(2)]; zs = [st.sb(f"zs{i}", [128, 512]) for i in range(2)]
        t1 = st.sb("t1", [128, 512]); t2 = st.sb("t2", [128, 512])
        rot = [st.sb(f"rot{i}", [128, 512], BF16) for i in range(2)]
        vf = st.sb("vf", [128, 512]); vb = st.sb("vb", [128, 512], BF16)
        tok = [st.sb(f"tok{i}", [128, 4, 128], BF16) for i in range(2)]
        ptr = [st.ps(f"ptr{i}", [128, 512], BF16) for i in range(2)]
        n = 0
        for tt in range(NTT):
            ts_ = slice(tt * 512, (tt + 1) * 512)
            p.dma('sp', posi[:, :], io['pos'].ap[ts_].rearrange("(o t) -> o t", o=1).partition_broadcast(128), reads=[io['pos']], writes=[posi])
            p.ts('dve', ang, ang[:, :], posi, posi[:, :], rc[:, 0:1], None, ALU.mult, extra_reads=[rc])
            for (shift, dst, scale) in [(0.0, sin, rc[:, 1:2]), (PI / 2, cos, 1.0)]:
                if shift != 0.0:
                    p.ts('dve', a2, a2[:, :], ang, ang[:, :], shift, None, ALU.add)
                    src = a2
                else:
                    src = ang
                p.ts('dve', ki, ki[:, :], src, src[:, :], INV2PI, None, ALU.mult)
                p.copy('act', kf, kf[:, :], ki, ki[:, :])
                p.stt(a2, a2[:, :], kf, kf[:, :], -C1, src, src[:, :], ALU.mult, ALU.add)
                p.stt(a2, a2[:, :], kf, kf[:, :], -C2, a2, a2[:, :], ALU.mult, ALU.add)
                p.ts('pool', a2, a2[:, :], a2, a2[:, :], -PI, PI, ALU.max, ALU.min)
                p.act(dst, dst[:, :], a2, a2[:, :], AF.Sin, scale=scale, extra_reads=[rc])
            for s in range(2):
                base = s * 512
                for which, (r0, scl, dstd) in enumerate([(base, 128.0 ** -0.5, io['qrT']), (base + 128, 1.0, io['krT'])]):
                    z = zq[which]; zw = zs[which]
                    p.dma('sp', z[:, :], zT[r0:r0 + 128, ts_], reads=[zT], writes=[z])
                    p.dma('sp', zw[0:64, :], zT[r0 + 64:r0 + 128, ts_], reads=[zT], writes=[zw])
                    p.dma('sp', zw[64:128, :], zT[r0:r0 + 64, ts_], reads=[zT], writes=[zw])
                    p.stt(t1, t1[:, :], z, z[:, :], scl, cos, cos[:, :], ALU.mult, ALU.mult)
                    p.stt(t2, t2[:, :], zw, zw[:, :], scl, sin, sin[:, :], ALU.mult, ALU.mult, e='dve')
                    ro = rot[which]
                    p.tt(['pool', 'dve'][which], ro, ro[:, :], t1, t1[:, :], t2, t2[:, :], ALU.add)
                    p.dma('pool', dstd[s, :, ts_], ro[:, :], reads=[ro], writes=[dstd])
                p.dma('sp', vf[:, :], zT[base + 256:base + 384, ts_], reads=[zT], writes=[vf])
                p.copy('act', vb, vb[:, :], vf, vf[:, :])
                for (srcb, dstd) in [(rot[1], io['ktok']), (vb, io['vtok'])]:
                    P_ = ptr[n % 2]; tk = tok[n % 2]; n += 1
                    for c4 in range(4):
                        p.tr(P_, P_[:, c4 * 128:(c4 + 1) * 128], srcb, srcb[:, c4 * 128:(c4 + 1) * 128], ident, ident[:, :])
                    p.act(tk, tk[:, :, :], P_, P_[:, :].rearrange("p (c d) -> p c d", c=4), AF.Copy)
                    p.dma('pool', dstd[s, tt * 4:(tt + 1) * 4].rearrange("c j d -> j c d"), tk[:, :, :], reads=[tk], writes=[dstd])
    with p.stage() as st:
        rc = st.sb("rc", [128, RC_N])
        p.dma('sp', rc[:, :], io['rconst'][:, :], reads=[io['rconst']], writes=[rc])
        Sf = [st.sb(f"Sb{s}", [128, 128]) for s in range(2)]
        Sb = [[st.sb(f"Sbb{s}_{i}", [128, 128], BF16) for i in range(2)] for s in range(2)]
        kt = [[st.sb(f"kt{s}_{i}", [128, 4, 128], BF16) for i in range(2)] for s in range(2)]
        vt = [[st.sb(f"vt{s}_{i}", [128, 4, 128], BF16) for i in range(2)] for s in range(2)]
        kd = [[st.sb(f"kd{s}_{i}", [128, 4, 128], BF16) for i in range(2)] for s in range(2)]
        pkv = [st.ps(f"pkv{i}", [128, 128]) for i in range(2)]
        for s in range(2):
            p.memset('dve', Sf[s], Sf[s][:, :], 0.0)
            p.memset('pool', Sb[s][0], Sb[s][0][:, :], 0.0)
            p.memset('pool', Sb[s][1], Sb[s][1][:, :], 0.0)
        for tt in range(NTT - 1, -1, -1):
            for s in range(2):
                o = 2 + s * RC_SLOT
                k_ = kt[s][tt % 2]; v_ = vt[s][tt % 2]; d_ = kd[s][tt % 2]
                p.dma('sp', k_[:, :, :], io['ktok'][s, tt * 4:(tt + 1) * 4].rearrange("c j d -> j c d"), reads=[io['ktok']], writes=[k_])
                p.dma('sp', v_[:, :, :], io['vtok'][s, tt * 4:(tt + 1) * 4].rearrange("c j d -> j c d"), reads=[io['vtok']], writes=[v_])
                p.act(d_, d_[:, :, :], k_, k_[:, :, :], AF.Identity, scale=rc[:, o + 1:o + 2], extra_reads=[rc])
                for c4 in range(3, -1, -1):
                    c = tt * 4 + c4
                    cur = Sb[s][c % 2]; nxt = Sb[s][(c + 1) % 2]
                    p.dma('pool', io['sbd'][s, c], cur[:, :], reads=[cur], writes=[io['sbd']])
                    if c == 0: continue
                    P_ = pkv[s]
                    p.mm(P_, P_[:, :], d_, d_[:, c4, :], v_, v_[:, c4, :])
                    p.stt(Sf[s], Sf[s][:, :], Sf[s], Sf[s][:, :], rc[:, o + 2:o + 3], P_, P_[:, :], ALU.mult, ALU.add, extra_reads=[rc])
                    p.act(nxt, nxt[:, :], Sf[s], Sf[s][:, :], AF.Copy)
    with p.stage() as st:
        rc = st.sb("rc", [128, RC_N])
        p.dma('sp', rc[:, :], io['rconst'][:, :], reads=[io['rconst']], writes=[rc])
        ones = st.sb("ones", [128, 128]); p.memset('dve', ones, ones[:, :], 1.0)
        Sf = [st.sb(f"Sf{s}", [128, 128]) for s in range(2)]
        Sfb = [[st.sb(f"Sfb{s}_{i}", [128, 128], BF16) for i in range(2)] for s in range(2)]
        bufs = {}
        for s in range(2):
            for nm, shp, dt in [('q', [128, 512], BF16), ('k', [128, 512], BF16), ('kt', [128, 4, 128], BF16), ('vt', [128, 4, 128], BF16),
                                ('sb', [128, 4, 128], BF16), ('g', [128, 512], F32), ('qf', [128, 512], BF16), ('qb', [128, 512], BF16),
                                ('kd', [128, 4, 128], BF16), ('scm', [128, 512], BF16), ('sq', [128, 512], F32), ('rs', [128, 512], F32),
                                ('sg', [128, 512], F32), ('t', [128, 512], F32), ('o', [128, 512], BF16)]:
                bufs[(s, nm)] = st.sb(f"r3{nm}{s}", shp, dt)
        psc = [st.ps(f"psc{i}", [128, 128]) for i in range(2)]
        pyT = [st.ps(f"pyT{i}", [128, 512]) for i in range(2)]
        pkv = [st.ps(f"pkv3{i}", [128, 128]) for i in range(2)]
        pss = st.ps("pss", [128, 512])
        for s in range(2):
            p.memset('dve', Sf[s], Sf[s][:, :], 0.0)
            p.memset('pool', Sfb[s][0], Sfb[s][0][:, :], 0.0)
        for tt in range(NTT):
            ts_ = slice(tt * 512, (tt + 1) * 512)
            r = (tt * 512) // Q4; off = tt * 512 - r * Q4
            for s in range(2):
                o = 2 + s * RC_SLOT
                B = lambda nm: bufs[(s, nm)]
                p.dma('sp', B('q')[:, :], io['qrT'][s, :, ts_], reads=[io['qrT']], writes=[B('q')])
                p.dma('sp', B('k')[:, :], io['krT'][s, :, ts_], reads=[io['krT']], writes=[B('k')])
                p.dma('sp', B('kt')[:, :, :], io['ktok'][s, tt * 4:(tt + 1) * 4].rearrange("c j d -> j c d"), reads=[io['ktok']], writes=[B('kt')])
                p.dma('sp', B('vt')[:, :, :], io['vtok'][s, tt * 4:(tt + 1) * 4].rearrange("c j d -> j c d"), reads=[io['vtok']], writes=[B('vt')])
                p.dma('sp', B('sb')[:, :, :], io['sbd'][s, tt * 4:(tt + 1) * 4].rearrange("c d e -> d c e"), reads=[io['sbd']], writes=[B('sb')])
                p.dma('sp', B('g')[:, :], zT[s * 512 + 384:s * 512 + 512, ts_], reads=[zT], writes=[B('g')])
                p.tt('pool', B('qf'), B('qf')[:, :], B('q'), B('q')[:, :], rc, rc[:, o + 3 + 128:o + 3 + 128 + 512], ALU.mult)
                p.tt('dve', B('qb'), B('qb')[:, :], B('q'), B('q')[:, :], rc, rc[:, o + 3 + 640:o + 3 + 640 + 512], ALU.mult)
                p.act(B('kd'), B('kd')[:, :, :], B('kt'), B('kt')[:, :, :], AF.Identity, scale=rc[:, o:o + 1], extra_reads=[rc])
                p.act(B('sg'), B('sg')[:, :], B('g'), B('g')[:, :], AF.Silu)
                Y = pyT[s]
                for c4 in range(4):
                    c = tt * 4 + c4
                    cs = slice(c4 * 128, (c4 + 1) * 128)
                    cur = Sfb[s][c % 2]; nxt = Sfb[s][(c + 1) % 2]
                    SC = psc[c % 2]
                    p.mm(SC, SC[:, :], B('k'), B('k')[:, cs], B('q'), B('q')[:, cs])
                    p.tt('dve', B('scm'), B('scm')[:, cs], SC, SC[:, :], rc, rc[:, o + 3:o + 3 + 128], ALU.mult)
                    p.mm(Y, Y[:, cs], B('vt'), B('vt')[:, c4, :], B('scm'), B('scm')[:, cs], start=True, stop=False)
                    p.mm(Y, Y[:, cs], cur, cur[:, :], B('qf'), B('qf')[:, cs], start=False, stop=False)
                    p.mm(Y, Y[:, cs], B('sb'), B('sb')[:, c4, :], B('qb'), B('qb')[:, cs], start=False, stop=True)
                    if c < NCK - 1:
                        P_ = pkv[s]
                        p.mm(P_, P_[:, :], B('kd'), B('kd')[:, c4, :], B('vt'), B('vt')[:, c4, :])
                        p.stt(Sf[s], Sf[s][:, :], Sf[s], Sf[s][:, :], rc[:, o + 2:o + 3], P_, P_[:, :], ALU.mult, ALU.add, extra_reads=[rc])
                        p.act(nxt, nxt[:, :], Sf[s], Sf[s][:, :], AF.Copy)
                p.act(B('sq'), B('sq')[:, :], Y, Y[:, :], AF.Square)
                p.mm(pss, pss[:, :], ones, ones[:, :], B('sq'), B('sq')[:, :])
                p.act(B('rs'), B('rs')[:, :], pss, pss[:, :], AF.Sqrt, bias=1e-6, scale=1.0 / 128)
                p.op('dve', lambda: nc.vector.reciprocal(out=B('rs')[:, :], in_=B('rs')[:, :]), reads=[B('rs')], writes=[B('rs')])
                p.tt('dve', B('t'), B('t')[:, :], Y, Y[:, :], B('rs'), B('rs')[:, :], ALU.mult)
                p.tt('pool', B('o'), B('o')[:, :], B('t'), B('t')[:, :], B('sg'), B('sg')[:, :], ALU.mult)
                p.dma('pool', io['yT'][r, s * 128:(s + 1) * 128, off:off + 512], B('o')[:, :], reads=[B('o')], writes=[io['yT']])

ZS5 = 1984
def sin_reduce(p, out, src, shift, ki, kf, tmp, scale=1.0, extra_reads=()):
    sl = tuple(slice(None) for _ in src.ap.shape)
    if shift != 0.0:
        p.ts('pool', tmp, tmp[sl], src, src[sl], shift, None, ALU.add)
        s = tmp
    else:
        s = src
    p.ts('dve', ki, ki[sl], s, s[sl], INV2PI, None, ALU.mult)
    p.copy('pool', kf, kf[sl], ki, ki[sl])
    p.stt(tmp, tmp[sl], kf, kf[sl], -C1, s, s[sl], ALU.mult, ALU.add)
    p.stt(tmp, tmp[sl], kf, kf[sl], -C2, tmp, tmp[sl], ALU.mult, ALU.add)
    p.ts('pool', tmp, tmp[sl], tmp, tmp[sl], -PI, PI, ALU.max, ALU.min)
    p.act(out, out[sl], tmp, tmp[sl], AF.Sin, scale=scale, extra_reads=extra_reads)

def s5_host_layout(q, lam_re, lam_im, log_dt, b_re, b_im, c_re, c_im, d_skip):
    import numpy as np
    gs = slice(8 * q, 8 * q + 8)
    lr = lam_re[:, gs, :]; li = lam_im[:, gs, :]; ld = log_dt[:, gs]
    rows = np.stack([lr.reshape(-1), li.reshape(-1), np.repeat(ld.reshape(-1), 64)], 0).astype(np.float32)
    def col(a):
        return np.ascontiguousarray(a.reshape(2, 4, 2, 64).transpose(2, 3, 0, 1).reshape(128, 8))
    cols = np.concatenate([col(lr), col(li), col(np.repeat(ld[:, :, None], 64, axis=2))], 1).astype(np.float32)
    bl = np.zeros((2, 128, 2, 4, 128), np.float32)
    cl = np.zeros((2, 128, 2, 4, 128), np.float32)
    for d in range(2):
        for g in range(8):
            j, gp = g // 2, g % 2
            for ri, (bsrc, csrc) in enumerate([(b_re, c_re), (b_im, c_im)]):
                bl[ri, g * 16:(g + 1) * 16, d, j, gp * 64:(gp + 1) * 64] = bsrc[d, 8 * q + g].T
                cl[ri, gp * 64:(gp + 1) * 64, d, j, g * 16:(g + 1) * 16] = csrc[d, 8 * q + g].T
    dcol = d_skip[128 * q:128 * (q + 1)].reshape(128, 1).astype(np.float32)
    return dict(s5rows=rows, s5cols=cols, s5bl=bl.reshape(2, 128, 1024), s5cl=cl.reshape(2, 128, 1024), s5d=dcol,
                s5iota=np.tile(np.arange(512, dtype=np.float32)[None, :], (128, 1)))

def stage_s5(p, S, io):
    nc = p.nc
    NTT = S // 512; Q4 = S // 4; TC = 512
    zT = io['zT']
    with p.stage() as st:
        bb = [st.sb(f"bb{i}", [128, 1024], BF16) for i in range(2)]
        cc = [st.sb(f"cc{i}", [128, 1024], BF16) for i in range(2)]
        rho = st.sb("rho", [128, 8]); cT = st.sb("cT", [128, 8]); sT = st.sb("sT", [128, 8]); thc = st.sb("thc", [128, 8])
        dcol = st.sb("dcol", [128, 1])
        p.dma('sp', dcol[:, :], io['s5d'][:, :], reads=[io['s5d']], writes=[dcol])
        with p.stage() as s2:
            f = lambda nm: s2.sb(nm, [128, 1024])
            lr, li, ld, dt, mag, th, sn, cs, t1, t2, t3, kf, tmp, cr, ci = [f(n) for n in
                ['lr', 'li', 'ld', 'dt', 'mag', 'th', 'sn', 'cs', 't1', 't2', 't3', 'kf', 'tmp', 'cr', 'ci']]
            ki = s2.sb("ki", [128, 1024], I32)
            for k, t in enumerate([lr, li, ld]):
                p.dma('sp', t[:, :], io['s5rows'][k:k + 1, :].partition_broadcast(128), reads=[io['s5rows']], writes=[t])
            p.act(dt, dt[:, :], ld, ld[:, :], AF.Exp)
            p.tt('dve', t1, t1[:, :], lr, lr[:, :], dt, dt[:, :], ALU.mult)
            p.act(mag, mag[:, :], t1, t1[:, :], AF.Exp)
            p.tt('dve', th, th[:, :], li, li[:, :], dt, dt[:, :], ALU.mult)
            sin_reduce(p, sn, th, 0.0, ki, kf, tmp)
            sin_reduce(p, cs, th, PI / 2, ki, kf, tmp)
            p.tt('dve', cs, cs[:, :], cs, cs[:, :], mag, mag[:, :], ALU.mult)
            p.tt('dve', sn, sn[:, :], sn, sn[:, :], mag, mag[:, :], ALU.mult)
            p.ts('dve', cs, cs[:, :], cs, cs[:, :], -1.0, None, ALU.add)
            p.tt('dve', t1, t1[:, :], lr, lr[:, :], lr, lr[:, :], ALU.mult)
            p.tt('dve', t2, t2[:, :], li, li[:, :], li, li[:, :], ALU.mult)
            p.tt('dve', t1, t1[:, :], t1, t1[:, :], t2, t2[:, :], ALU.add)
            p.op('dve', lambda: nc.vector.reciprocal(out=t1[:, :], in_=t1[:, :]), reads=[t1], writes=[t1])
            p.tt('dve', t2, t2[:, :], cs, cs[:, :], lr, lr[:, :], ALU.mult)
            p.tt('dve', t3, t3[:, :], sn, sn[:, :], li, li[:, :], ALU.mult)
            p.tt('dve', t2, t2[:, :], t2, t2[:, :], t3, t3[:, :], ALU.add)
            p.tt('dve', cr, cr[:, :], t2, t2[:, :], t1, t1[:, :], ALU.mult)
            p.tt('dve', t2, t2[:, :], sn, sn[:, :], lr, lr[:, :], ALU.mult)
            p.tt('dve', t3, t3[:, :], cs, cs[:, :], li, li[:, :], ALU.mult)
            p.tt('dve', t2, t2[:, :], t2, t2[:, :], t3, t3[:, :], ALU.subtract)
            p.tt('dve', ci, ci[:, :], t2, t2[:, :], t1, t1[:, :], ALU.mult)
            br = lr; bi = li
            p.dma('sp', br[:, :], io['s5bl'][0], reads=[io['s5bl']], writes=[br])
            p.dma('sp', bi[:, :], io['s5bl'][1], reads=[io['s5bl']], writes=[bi])
            p.tt('dve', t1, t1[:, :], cr, cr[:, :], br, br[:, :], ALU.mult)
            p.tt('dve', t2, t2[:, :], ci, ci[:, :], bi, bi[:, :], ALU.mult)
            p.tt('dve', bb[0], bb[0][:, :], t1, t1[:, :], t2, t2[:, :], ALU.subtract)
            p.tt('dve', t1, t1[:, :], cr, cr[:, :], bi, bi[:, :], ALU.mult)
            p.tt('dve', t2, t2[:, :], ci, ci[:, :], br, br[:, :], ALU.mult)
            p.tt('dve', bb[1], bb[1][:, :], t1, t1[:, :], t2, t2[:, :], ALU.add)
            p.dma('sp', t1[:, :], io['s5cl'][0], reads=[io['s5cl']], writes=[t1])
            p.dma('sp', t2[:, :], io['s5cl'][1], reads=[io['s5cl']], writes=[t2])
            p.copy('dve', cc[0], cc[0][:, :], t1, t1[:, :])
            p.ts('dve', cc[1], cc[1][:, :], t2, t2[:, :], -1.0, None, ALU.mult)
            c24 = s2.sb("c24", [128, 24]); dtc = s2.sb("dtc", [128, 8]); tq = s2.sb("tq", [128, 8]); tq2 = s2.sb("tq2", [128, 8])
            ki8 = s2.sb("ki8", [128, 8], I32); kf8 = s2.sb("kf8", [128, 8]); tmp8 = s2.sb("tmp8", [128, 8])
            p.dma('sp', c24[:, :], io['s5cols'][:, :], reads=[io['s5cols']], writes=[c24])
            p.act(dtc, dtc[:, :], c24, c24[:, 16:24], AF.Exp)
            p.tt('dve', tq, tq[:, :], c24, c24[:, 0:8], dtc, dtc[:, :], ALU.mult)
            p.act(rho, rho[:, :], tq, tq[:, :], AF.Exp)
            p.tt('dve', thc, thc[:, :], c24, c24[:, 8:16], dtc, dtc[:, :], ALU.mult)
            p.ts('dve', tq2, tq2[:, :], thc, thc[:, :], float(TC), None, ALU.mult)
            sin_reduce(p, sT, tq2, 0.0, ki8, kf8, tmp8)
            sin_reduce(p, cT, tq2, PI / 2, ki8, kf8, tmp8)
        iota = st.sb("iota", [128, TC]); p.dma('sp', iota[:, :], io['s5iota'][:, :], reads=[io['s5iota']], writes=[iota])
        cosT = [st.sb(f"cost{k}", [128, TC]) for k in range(8)]; sinT = [st.sb(f"sint{k}", [128, TC]) for k in range(8)]
        rhoT = [st.sb(f"rhot{k}", [128, TC]) for k in range(8)]
        ang = st.sb("ang", [128, TC]); kiT = st.sb("kiT", [128, TC], I32); kfT = st.sb("kfT", [128, TC]); tmpT = st.sb("tmpT", [128, TC])
        tb = st.sb("tb", [128, TC])
        for k in range(8):
            p.ts('dve', ang, ang[:, :], iota, iota[:, :], thc[:, k:k + 1], None, ALU.mult, extra_reads=[thc])
            for (shift, dst) in [(0.0, sinT[k]), (PI / 2, cosT[k])]:
                if k < 4:
                    sin_reduce(p, dst, ang, shift, kiT, kfT, tmpT)
                else:
                    sin_reduce(p, tb, ang, shift, kiT, kfT, tmpT)
                    p.copy('dve', dst, dst[:, :], tb, tb[:, ::-1])
            p.ts('dve', rhoT[k], rhoT[k][:, :], iota, iota[:, :], 0.0, rho[:, k:k + 1], ALU.mult, ALU.add, extra_reads=[rho])
        uf = [st.sb(f"uf{i}", [128, TC]) for i in range(2)]; ub = [st.sb(f"ub{i}", [128, TC], BF16) for i in range(2)]
        brs = [st.sb(f"brs{i}", [128, TC]) for i in range(2)]; bis = [st.sb(f"bis{i}", [128, TC]) for i in range(2)]
        t1 = st.sb("t1", [128, TC]); t2 = st.sb("t2", [128, TC]); t3 = st.sb("t3", [128, TC]); t4 = st.sb("t4", [128, TC])
        mre = st.sb("mre", [128, TC]); mim = st.sb("mim", [128, TC])
        xr = [st.sb(f"xr{i}", [128, TC]) for i in range(2)]; xi = [st.sb(f"xi{i}", [128, TC]) for i in range(2)]
        xre = [st.sb(f"xre{i}", [128, TC], BF16) for i in range(2)]; xim = [st.sb(f"xim{i}", [128, TC], BF16) for i in range(2)]
        init = st.sb("init", [128, 16]); p.memset('dve', init, init[:, :], 0.0)
        tn = st.sb("tn", [128, 4])
        yo = [st.sb(f"yo{i}", [128, TC]) for i in range(2)]
        yfl = st.sb("yfl", [128, TC]); yb16 = [st.sb(f"yb16{i}", [128, TC], BF16) for i in range(2)]
        pb_re = [st.ps(f"pbre{i}", [128, TC]) for i in range(2)]; pb_im = [st.ps(f"pbim{i}", [128, TC]) for i in range(2)]
        py = [st.ps(f"py{i}", [128, TC]) for i in range(2)]
        n = 0
        for d in range(2):
            order = range(NTT) if d == 0 else range(NTT - 1, -1, -1)
            for it, tt in enumerate(order):
                ts_ = slice(tt * TC, (tt + 1) * TC)
                r = (tt * TC) // Q4; off = tt * TC - r * Q4
                u_f = uf[it % 2]; u_b = ub[it % 2]
                p.dma('sp', u_f[:, :], zT[ZS5:ZS5 + 128, ts_], reads=[zT], writes=[u_f])
                p.copy('pool', u_b, u_b[:, :], u_f, u_f[:, :])
                Y = py[it % 2]
                for j in range(4):
                    k = d * 4 + j
                    blk = slice(k * 128, (k + 1) * 128)
                    PR = pb_re[n % 2]; PI_ = pb_im[n % 2]; b_r = brs[n % 2]; b_i = bis[n % 2]
                    x_r = xr[n % 2]; x_i = xi[n % 2]; xo_r = xre[n % 2]; xo_i = xim[n % 2]; n += 1
                    p.mm(PR, PR[:, :], bb[0], bb[0][:, blk], u_b, u_b[:, :])
                    p.mm(PI_, PI_[:, :], bb[1], bb[1][:, blk], u_b, u_b[:, :])
                    p.act(b_r, b_r[:, :], PR, PR[:, :], AF.Copy)
                    p.act(b_i, b_i[:, :], PI_, PI_[:, :], AF.Copy)
                    p.tt('dve', t1, t1[:, :], b_r, b_r[:, :], cosT[k], cosT[k][:, :], ALU.mult)
                    p.tt('pool', t2, t2[:, :], b_i, b_i[:, :], sinT[k], sinT[k][:, :], ALU.mult)
                    p.tt('dve', mre, mre[:, :], t1, t1[:, :], t2, t2[:, :], ALU.add)
                    p.tt('dve', t3, t3[:, :], b_i, b_i[:, :], cosT[k], cosT[k][:, :], ALU.mult)
                    p.tt('dve', t4, t4[:, :], b_r, b_r[:, :], sinT[k], sinT[k][:, :], ALU.mult)
                    p.tt('pool', mim, mim[:, :], t3, t3[:, :], t4, t4[:, :], ALU.subtract)
                    if d == 0:
                        vw = lambda a: a[:, :]
                        last = slice(TC - 1, TC)
                    else:
                        vw = lambda a: a[:, ::-1]
                        last = slice(0, 1)
                    p.op('dve', lambda: nc.vector.tensor_tensor_scan(out=vw(x_r), data0=rhoT[k][:, :], data1=vw(mre), initial=init[:, k:k + 1],
                                                                      op0=ALU.mult, op1=ALU.add), reads=[rhoT[k], mre, init], writes=[x_r])
                    p.op('dve', lambda: nc.vector.tensor_tensor_scan(out=vw(x_i), data0=rhoT[k][:, :], data1=vw(mim), initial=init[:, 8 + k:9 + k],
                                                                      op0=ALU.mult, op1=ALU.add), reads=[rhoT[k], mim, init], writes=[x_i])
                    p.ts('pool', tn, tn[:, 0:1], x_r, x_r[:, last], cT[:, k:k + 1], None, ALU.mult, extra_reads=[cT])
                    p.ts('pool', tn, tn[:, 1:2], x_r, x_r[:, last], sT[:, k:k + 1], None, ALU.mult, extra_reads=[sT])
                    p.stt(tn, tn[:, 2:3], x_i, x_i[:, last], sT[:, k:k + 1], tn, tn[:, 0:1], ALU.mult, ALU.subtract, extra_reads=[sT])
                    p.ts('pool', init, init[:, k:k + 1], tn, tn[:, 2:3], -1.0, None, ALU.mult)
                    p.stt(init, init[:, 8 + k:9 + k], x_i, x_i[:, last], cT[:, k:k + 1], tn, tn[:, 1:2], ALU.mult, ALU.add, extra_reads=[cT])
                    p.tt('dve', t1, t1[:, :], x_r, x_r[:, :], cosT[k], cosT[k][:, :], ALU.mult)
                    p.tt('pool', t2, t2[:, :], x_i, x_i[:, :], sinT[k], sinT[k][:, :], ALU.mult)
                    p.tt('dve', xo_r, xo_r[:, :], t1, t1[:, :], t2, t2[:, :], ALU.subtract)
                    p.tt('dve', t3, t3[:, :], x_r, x_r[:, :], sinT[k], sinT[k][:, :], ALU.mult)
                    p.tt('dve', t4, t4[:, :], x_i, x_i[:, :], cosT[k], cosT[k][:, :], ALU.mult)
                    p.tt('pool', xo_i, xo_i[:, :], t3, t3[:, :], t4, t4[:, :], ALU.add)
                    p.mm(Y, Y[:, :], cc[0], cc[0][:, blk], xo_r, xo_r[:, :], start=(j == 0), stop=False)
                    p.mm(Y, Y[:, :], cc[1], cc[1][:, blk], xo_i, xo_i[:, :], start=False, stop=(j == 3))
                if d == 0:
                    y_o = yo[it % 2]
                    p.act(y_o, y_o[:, :], Y, Y[:, :], AF.Copy)
                    p.dma('pool', io['yf'][:, ts_], y_o[:, :], reads=[y_o], writes=[io['yf']])
                else:
                    y_o = yo[it % 2]; yb = yb16[it % 2]
                    p.dma('sp', yfl[:, :], io['yf'][:, ts_], reads=[io['yf']], writes=[yfl])
                    p.tt('dve', y_o, y_o[:, :], Y, Y[:, :], yfl, yfl[:, :], ALU.add)
                    p.stt(y_o, y_o[:, :], u_f, u_f[:, :], dcol[:, 0:1], y_o, y_o[:, :], ALU.mult, ALU.add, extra_reads=[dcol])
                    p.act(yb, yb[:, :], y_o, y_o[:, :], AF.Gelu_apprx_tanh)
                    p.dma('pool', io['yT'][r, 448:576, off:off + TC], yb[:, :], reads=[yb], writes=[io['yT']])

ZR, ZK, ZV, ZWD, ZAD, ZGD = 1024, 1216, 1408, 1600, 1728, 1856
RW_SU, RW_SL, RW_U, RW_L, RW_I, RW_BLK, RW_MF, RW_MB, RW_N = 0, 768, 1536, 2304, 3072, 3840, 3968, 4480, 4992
NEG_EXP_HALF = -math.exp(-0.5)

def rwkv_consts():
    import numpy as np
    i = np.arange(128)
    su = (i[:, None] < i[None, :]).astype(np.float32); sl = su.T.copy()
    u = (i[:, None] <= i[None, :]).astype(np.float32); l = u.T.copy()
    I = np.eye(128, dtype=np.float32)
    blk = np.zeros((128, 128), np.float32); blk[:64, :64] = 1; blk[64:, 64:] = 1
    t = np.arange(512)
    mf = (t % 128 != 0).astype(np.float32); mb = (t % 128 != 127).astype(np.float32)
    return np.concatenate([np.tile(su, (1, 6)), np.tile(sl, (1, 6)), np.tile(u, (1, 6)), np.tile(l, (1, 6)), np.tile(I, (1, 6)), blk,
                           np.tile(mf[None], (128, 1)), np.tile(mb[None], (128, 1))], axis=1).astype(np.float32)

def rwkv_host_layout(q, mu_prev, mu_next, w0, w_up, a0, a_up, g_up, k_k, k_a, r_k, lnx_w, lnx_b):
    import numpy as np
    rwp = np.zeros((128, 55), np.float32)
    rkf = r_k.reshape(-1)
    for hh in range(3):
        ch = q * 192 + hh * 64 + np.arange(64)
        b = hh * 15
        for j, part in enumerate([0, 768, 1536]):
            rwp[:64, b + 2 * j] = mu_prev[part + ch]; rwp[:64, b + 2 * j + 1] = mu_next[part + ch]
        rwp[:64, b + 6] = k_k[ch]; rwp[:64, b + 7] = k_a[ch]; rwp[:64, b + 8] = rkf[ch]; rwp[:64, b + 9] = lnx_w[ch]; rwp[:64, b + 10] = lnx_b[ch]
        rwp[:64, b + 11] = w0[0, ch]; rwp[:64, b + 12] = w0[1, ch]; rwp[:64, b + 13] = a0[0, ch]; rwp[:64, b + 14] = a0[1, ch]
    for j, part in enumerate([2304, 2432]):
        for d in range(2):
            rwp[:64, 45 + 4 * j + 2 * d] = mu_prev[part + d * 64:part + (d + 1) * 64]
            rwp[:64, 46 + 4 * j + 2 * d] = mu_next[part + d * 64:part + (d + 1) * 64]
    rwp[:, 53] = mu_prev[2560:2688]; rwp[:, 54] = mu_next[2560:2688]
    cs = slice(q * 192, (q + 1) * 192)
    return dict(rwp=rwp, rw_wup=np.ascontiguousarray(np.stack([w_up[0][:, cs], w_up[1][:, cs]], 1)),
                rw_aup=np.ascontiguousarray(np.stack([a_up[0][:, cs], a_up[1][:, cs]], 1)),
                rw_gup=np.ascontiguousarray(g_up[:, cs]))

RW_DEBUG = [None]
def stage_rwkv(p, S, io):
    nc = p.nc
    NTT = S // 512; Q4 = S // 4
    zT = io['zT']
    H3 = range(3)
    with p.stage() as st:
        rwp = st.sb("rwp", [128, 55]); p.dma('sp', rwp[:, :], io['rwp'][:, :], reads=[io['rwp']], writes=[rwp])
        cst = st.sb("rwc", [128, RW_N]); p.dma('sp', cst[:, :], io['rwconst'][:, :], reads=[io['rwconst']], writes=[cst])
        ident_f = st.sb("ident_f", [128, 128]); ident = st.sb("ident", [128, 128], BF16)
        p.dma('sp', ident_f[:, :], io['ident'][:, :], reads=[io['ident']], writes=[ident_f])
        p.copy('dve', ident, ident[:, :], ident_f, ident_f[:, :])
        wtmp = st.sb("wtmp", [128, 384])
        wup = st.sb("wupb", [64, 2, 192], BF16); aup = st.sb("aupb", [64, 2, 192], BF16); gup = st.sb("gupb", [128, 192], BF16)
        p.dma('sp', wtmp[0:64, :], io['rw_wup'].ap.rearrange("r d c -> r (d c)"), reads=[io['rw_wup']], writes=[wtmp])
        p.copy('dve', wup, wup[:, :, :], wtmp, wtmp[0:64, :].rearrange("r (d c) -> r d c", d=2))
        p.dma('sp', wtmp[0:64, :], io['rw_aup'].ap.rearrange("r d c -> r (d c)"), reads=[io['rw_aup']], writes=[wtmp])
        p.copy('dve', aup, aup[:, :, :], wtmp, wtmp[0:64, :].rearrange("r (d c) -> r d c", d=2))
        p.dma('sp', wtmp[:, 0:192], io['rw_gup'][:, :], reads=[io['rw_gup']], writes=[wtmp])
        p.copy('dve', gup, gup[:, :], wtmp, wtmp[:, 0:192])
        c0 = st.sb("c0", [128, 14])
        pairs = [0, 2, 4, 15, 17, 19, 30, 32, 34, 45, 47, 49, 51, 53]
        for i, col in enumerate(pairs):
            p.tt('dve', c0, c0[:, i:i + 1], rwp, rwp[:, col:col + 1], rwp, rwp[:, col + 1:col + 2], ALU.add)
        p.ts('dve', c0, c0[:, :], c0, c0[:, :], -1.0, 1.0, ALU.mult, ALU.add)
        def fb(nm, P_=64, n=512, dt=F32): return st.sb(nm, [P_, n], dt)
        zh = [fb(f"zh{i}", 128, 514) for i in range(3)]
        sh = {}
        for hh in H3:
            for nm in ['r', 'k', 'v']:
                sh[(hh, nm)] = fb(f"sh{nm}{hh}")
        shwd = fb("shwd"); shad = fb("shad"); shgd = fb("shgd", 128)
        twd = fb("twd", dt=BF16); adb = fb("adb", dt=BF16); sgd = fb("sgd", 128, dt=BF16)
        lw = [fb(f"lw{h}") for h in H3]; cin = [fb(f"cin{h}") for h in H3]; cex = [fb(f"cex{h}") for h in H3]
        Ein = [fb(f"Ein{h}") for h in H3]; Eex = [fb(f"Eex{h}") for h in H3]; Eni = [fb(f"Eni{h}") for h in H3]
        aT = [fb(f"aT{h}") for h in H3]; kk = [fb(f"kk{h}") for h in H3]
        tA = [fb(f"tA{h}") for h in H3]; tB = [fb(f"tB{h}") for h in H3]
        at_ = [fb(f"at{h}", dt=BF16) for h in H3]; bt_ = [fb(f"bt{h}", dt=BF16) for h in H3]
        kt_ = [fb(f"kt{h}", dt=BF16) for h in H3]; rt_ = [fb(f"rt{h}", dt=BF16) for h in H3]
        vb_ = [fb(f"vb{h}", dt=BF16) for h in H3]
        tok = [st.sb(f"tok{i}", [128, 4, 192], BF16) for i in range(4)]
        W3 = lambda nm, dt=BF16: st.sb(nm, [128, 768], dt)
        M = [W3(f"M{i}") for i in range(2)]; N = [W3(f"N{i}") for i in range(2)]
        Pm = [W3(f"P{i}") for i in range(2)]; Qm = [W3(f"Q{i}") for i in range(2)]
        AKT = W3("AKT"); RBT = W3("RBT"); RKT = W3("RKT")
        AKV = st.sb("AKV", [128, 384], BF16); UV = st.sb("UV", [128, 384]); U = st.sb("U", [128, 192], BF16)
        TAT = st.sb("TAT", [64, 768], BF16)
        KVW = st.sb("KVW", [64, 384])
        S0 = st.sb("S0", [64, 192])
        S0b = [st.sb(f"S0b{i}", [64, 192], BF16) for i in range(2)]
        tS = st.sb("tS", [64, 192])
        ydall = st.sb("ydall", [64, 3, 512]); y0l = [fb(f"y0l{h}") for h in H3]
        pp1 = [fb(f"pp1{h}") for h in H3]; pp2 = [fb(f"pp2{h}") for h in H3]; pp3 = [fb(f"pp3{h}") for h in H3]
        yob = [fb(f"yob{h}", dt=BF16) for h in H3]
        pg = [st.ps(f"pg{i}", [128, 1024]) for i in range(2)]
        pch = st.ps("pch", [128, 512])
        pY = [st.ps(f"pY{i}", [128, 512]) for i in range(2)]
        ptk = st.ps("ptk", [128, 1024], BF16)
        gcount = [0]
        def PG():
            gcount[0] += 1
            return pg[gcount[0] % 2]
        ones64 = cst[0:64, RW_BLK:RW_BLK + 64]
        def blocksum(src):
            G = PG()
            p.mm(G, G[0:64, 0:512], cst, ones64, src, src[0:64, :])
            return G

        for d in range(2):
            p.memset('dve', S0, S0[:, :], 0.0)
            p.memset('pool', S0b[0], S0b[0][:, :], 0.0)
            p.memset('pool', S0b[1], S0b[1][:, :], 0.0)
            order = list(range(NTT)) if d == 0 else list(range(NTT - 1, -1, -1))
            mSU = RW_SU if d == 0 else RW_SL; mSL = RW_SL if d == 0 else RW_SU; mU = RW_U if d == 0 else RW_L
            gchunk = 0
            for tt in order:
                t0 = tt * 512
                r_ = t0 // Q4; off = t0 - r_ * Q4
                hbc = [0]
                def shift(row0, P_, dst, c0col, mpcol):
                    z = zh[hbc[0] % 3]; hbc[0] += 1
                    lo = max(t0 - 1, 0); hi = min(t0 + 513, S)
                    if t0 == 0: p.memset('pool', z, z[0:P_, 0:1], 0.0)
                    if t0 + 513 > S: p.memset('pool', z, z[0:P_, 513:514], 0.0)
                    p.dma('sp', z[0:P_, lo - (t0 - 1):hi - (t0 - 1)], zT[row0:row0 + P_, lo:hi], reads=[zT], writes=[z])
                    p.act(dst, dst[0:P_, :], z, z[0:P_, 1:513], AF.Identity, scale=c0[0:P_, c0col:c0col + 1], extra_reads=[c0])
                    p.stt(dst, dst[0:P_, :], z, z[0:P_, 0:512], rwp[0:P_, mpcol:mpcol + 1], dst, dst[0:P_, :], ALU.mult, ALU.add, extra_reads=[rwp])
                    p.stt(dst, dst[0:P_, :], z, z[0:P_, 2:514], rwp[0:P_, mpcol + 1:mpcol + 2], dst, dst[0:P_, :], ALU.mult, ALU.add, extra_reads=[rwp])
                for hh in H3:
                    for j, (nm, zrow) in enumerate([('r', ZR), ('k', ZK), ('v', ZV)]):
                        shift(zrow + hh * 64, 64, sh[(hh, nm)], hh * 3 + j, hh * 15 + 2 * j)
                shift(ZWD + d * 64, 64, shwd, 9 + d, 45 + 2 * d)
                shift(ZAD + d * 64, 64, shad, 11 + d, 49 + 2 * d)
                p.act(twd, twd[:, :], shwd, shwd[:, :], AF.Tanh)
                p.copy('act', adb, adb[:, :], shad, shad[:, :])
                if d == 1:
                    shift(ZGD, 128, shgd, 13, 53)
                    p.act(sgd, sgd[:, :], shgd, shgd[:, :], AF.Sigmoid)
                for hh in H3:
                    b = hh * 15
                    cs_ = slice(hh * 64, (hh + 1) * 64)
                    G = PG()
                    p.mm(G, G[0:64, 0:512], wup, wup[:, d, cs_], twd, twd[:, :])
                    p.act(lw[hh], lw[hh][:, :], G, G[0:64, 0:512], AF.Sigmoid, bias=rwp[0:64, b + 11 + d:b + 12 + d], extra_reads=[rwp])
                    if d == 0:
                        p.op('dve', lambda: nc.vector.tensor_tensor_scan(out=cin[hh][:, :], data0=cst[0:64, RW_MF:RW_MF + 512], data1=lw[hh][:, :],
                                                                          initial=0.0, op0=ALU.mult, op1=ALU.add), reads=[cst, lw[hh]], writes=[cin[hh]])
                    else:
                        mbv = cst[0:64, RW_MB:RW_MB + 512]
                        p.op('dve', lambda: nc.vector.tensor_tensor_scan(out=cin[hh][:, ::-1], data0=mbv[:, ::-1], data1=lw[hh][:, ::-1],
                                                                          initial=0.0, op0=ALU.mult, op1=ALU.add), reads=[cst, lw[hh]], writes=[cin[hh]])
                    p.tt('dve', cex[hh], cex[hh][:, :], cin[hh], cin[hh][:, :], lw[hh], lw[hh][:, :], ALU.subtract)
                    p.act(Ein[hh], Ein[hh][:, :], cin[hh], cin[hh][:, :], AF.Exp, scale=NEG_EXP_HALF)
                    p.act(Eex[hh], Eex[hh][:, :], cex[hh], cex[hh][:, :], AF.Exp, scale=NEG_EXP_HALF)
                    p.act(Eni[hh], Eni[hh][:, :], cin[hh], cin[hh][:, :], AF.Exp, scale=-NEG_EXP_HALF)
                    G = PG()
                    p.mm(G, G[0:64, 0:512], aup, aup[:, d, cs_], adb, adb[:, :])
                    p.act(aT[hh], aT[hh][:, :], G, G[0:64, 0:512], AF.Sigmoid, bias=rwp[0:64, b + 13 + d:b + 14 + d], extra_reads=[rwp])
                    ks = sh[(hh, 'k')]
                    p.act(kk[hh], kk[hh][:, :], ks, ks[:, :], AF.Identity, scale=rwp[0:64, b + 6:b + 7], extra_reads=[rwp])
                    p.act(tA[hh], tA[hh][:, :], ks, ks[:, :], AF.Square, scale=rwp[0:64, b + 6:b + 7], extra_reads=[rwp])
                    G = blocksum(tA[hh])
                    p.act(tB[hh], tB[hh][:, :], G, G[0:64, 0:512], AF.Sqrt)
                    p.ts('dve', tB[hh], tB[hh][:, :], tB[hh], tB[hh][:, :], 1e-12, None, ALU.max)
                    p.op('dve', lambda: nc.vector.reciprocal(out=tB[hh][:, :], in_=tB[hh][:, :]), reads=[tB[hh]], writes=[tB[hh]])
                    p.tt('dve', kk[hh], kk[hh][:, :], kk[hh], kk[hh][:, :], tB[hh], tB[hh][:, :], ALU.mult)
                    p.stt(at_[hh], at_[hh][:, :], kk[hh], kk[hh][:, :], -1.0, Eex[hh], Eex[hh][:, :], ALU.mult, ALU.mult)
                    p.tt('dve', tA[hh], tA[hh][:, :], kk[hh], kk[hh][:, :], aT[hh], aT[hh][:, :], ALU.mult)
                    p.tt('pool', bt_[hh], bt_[hh][:, :], tA[hh], tA[hh][:, :], Eni[hh], Eni[hh][:, :], ALU.mult)
                    p.ts('dve', tB[hh], tB[hh][:, :], aT[hh], aT[hh][:, :], -1.0, rwp[0:64, b + 7:b + 8], ALU.add, ALU.mult, extra_reads=[rwp])
                    p.stt(tB[hh], tB[hh][:, :], tB[hh], tB[hh][:, :], 1.0, ks, ks[:, :], ALU.add, ALU.mult)
                    p.tt('dve', kt_[hh], kt_[hh][:, :], tB[hh], tB[hh][:, :], Eni[hh], Eni[hh][:, :], ALU.mult)
                    rs_ = sh[(hh, 'r')]
                    p.tt('pool', rt_[hh], rt_[hh][:, :], rs_, rs_[:, :], Ein[hh], Ein[hh][:, :], ALU.mult)
                    p.copy('act', vb_[hh], vb_[hh][:, :], sh[(hh, 'v')], sh[(hh, 'v')][:, :])
                if RW_DEBUG[0] == 'prep': return
                pairs = [(0, 1), (2, 3)] if d == 0 else [(3, 2), (1, 0)]
                HS = [slice(i * 128, (i + 1) * 128) for i in range(6)]
                VS = [slice(i * 64, (i + 1) * 64) for i in range(6)]
                for pr in pairs:
                    CS = [slice(c4 * 128, (c4 + 1) * 128) for c4 in pr]
                    tks = []
                    for ci, c4 in enumerate(pr):
                        tk = tok[(gchunk + ci) % 4]; tks.append(tk)
                        for j, srcs in enumerate([at_, bt_, kt_, vb_]):
                            for hh in H3:
                                p.tr(ptk, ptk[:, j * 192 + hh * 64:j * 192 + (hh + 1) * 64], srcs[hh], srcs[hh][:, CS[ci]], ident, ident[0:64, 0:64])
                        p.act(tk, tk[:, :, :], ptk, ptk[:, 0:768].rearrange("p (j c) -> p j c", j=4), AF.Copy)
                    BL = [(ci, hh) for ci in range(2) for hh in H3]
                    def gram(dst, A_, B_, mask):
                        G = PG()
                        for bi, (ci, hh) in enumerate(BL):
                            p.mm(G, G[:, HS[bi]], A_[hh], A_[hh][:, CS[ci]], B_[hh], B_[hh][:, CS[ci]])
                        p.tt('dve', dst, dst[:, :], G, G[:, 0:768], cst, cst[:, mask:mask + 768], ALU.mult)
                    gram(M[0], bt_, at_, mSU)
                    gram(N[0], at_, bt_, mSL)
                    p.tt('dve', Pm[0], Pm[0][:, :], M[0], M[0][:, :], cst, cst[:, RW_I:RW_I + 768], ALU.add)
                    p.tt('pool', Qm[0], Qm[0][:, :], N[0], N[0][:, :], cst, cst[:, RW_I:RW_I + 768], ALU.add)
                    gram(AKT, kt_, at_, mSU)
                    gram(RBT, bt_, rt_, mU)
                    gram(RKT, kt_, rt_, mU)
                    for lev in range(1, 7):
                        Mo, No = M[(lev - 1) % 2], N[(lev - 1) % 2]; Mn, Nn = M[lev % 2], N[lev % 2]
                        Po, Qo = Pm[(lev - 1) % 2], Qm[(lev - 1) % 2]; Pn, Qn = Pm[lev % 2], Qm[lev % 2]
                        GM = PG()
                        for bi in range(6):
                            p.mm(GM, GM[:, HS[bi]], No, No[:, HS[bi]], Mo, Mo[:, HS[bi]])
                        p.act(Mn, Mn[:, :], GM, GM[:, 0:768], AF.Copy)
                        if lev < 6:
                            GN = PG()
                            for bi in range(6):
                                p.mm(GN, GN[:, HS[bi]], Mo, Mo[:, HS[bi]], No, No[:, HS[bi]])
                            p.act(Nn, Nn[:, :], GN, GN[:, 0:768], AF.Copy)
                        GP = PG()
                        for bi in range(6):
                            p.mm(GP, GP[:, HS[bi]], Qo, Qo[:, HS[bi]], Mn, Mn[:, HS[bi]])
                        p.tt('dve', Pn, Pn[:, :], GP, GP[:, 0:768], Po, Po[:, :], ALU.add)
                        if lev < 6:
                            GQ = PG()
                            for bi in range(6):
                                p.mm(GQ, GQ[:, HS[bi]], Po, Po[:, HS[bi]], Nn, Nn[:, HS[bi]])
                            p.tt('dve', Qn, Qn[:, :], GQ, GQ[:, 0:768], Qo, Qo[:, :], ALU.add)
                    PT = Pm[0]
                    G = PG()
                    for bi, (ci, hh) in enumerate(BL):
                        p.mm(G, G[:, VS[bi]], AKT, AKT[:, HS[bi]], tks[ci], tks[ci][:, 3, VS[hh]])
                    p.act(AKV, AKV[:, :], G, G[:, 0:384], AF.Copy)
                    G = PG()
                    for bi, (ci, hh) in enumerate(BL):
                        p.mm(G, G[:, VS[bi]], PT, PT[:, HS[bi]], AKV, AKV[:, VS[bi]])
                    p.act(UV, UV[:, :], G, G[:, 0:384], AF.Copy)
                    G = PG()
                    for bi, (ci, hh) in enumerate(BL):
                        p.mm(G, G[0:64, HS[bi]], tks[ci], tks[ci][:, 0, VS[hh]], PT, PT[:, HS[bi]])
                    p.act(TAT, TAT[:, :], G, G[0:64, 0:768], AF.Copy)
                    G = PG()
                    for bi, (ci, hh) in enumerate(BL):
                        p.mm(G, G[0:64, VS[bi]], tks[ci], tks[ci][:, 2, VS[hh]], tks[ci], tks[ci][:, 3, VS[hh]])
                    for bi, (ci, hh) in enumerate(BL):
                        c4 = pr[ci]
                        widx = c4 * 128 + 127 if d == 0 else c4 * 128
                        p.ts('dve', KVW, KVW[:, VS[bi]], G, G[0:64, VS[bi]], Ein[hh][:, widx:widx + 1], None, ALU.mult, extra_reads=[Ein[hh]])
                    for ci, c4 in enumerate(pr):
                        cs = CS[ci]; tk = tks[ci]
                        widx = c4 * 128 + 127 if d == 0 else c4 * 128
                        cur = S0b[gchunk % 2]; nxt = S0b[(gchunk + 1) % 2]
                        Yp = pY[gchunk % 2]
                        gchunk += 1
                        for hh in H3:
                            p.mm(pch, pch[:, VS[hh]], TAT, TAT[:, HS[ci * 3 + hh]], cur, cur[:, VS[hh]])
                        p.tt('dve', U, U[:, :], pch, pch[:, 0:192], UV, UV[:, ci * 192:(ci + 1) * 192], ALU.add)
                        for hh in H3:
                            bi = ci * 3 + hh
                            p.mm(Yp, Yp[0:64, HS[hh]], cur, cur[:, VS[hh]], rt_[hh], rt_[hh][:, cs], start=True, stop=False)
                            p.mm(Yp, Yp[0:64, HS[hh]], U, U[:, VS[hh]], RBT, RBT[:, HS[bi]], start=False, stop=False)
                            p.mm(Yp, Yp[0:64, HS[hh]], tk, tk[:, 3, VS[hh]], RKT, RKT[:, HS[bi]], start=False, stop=True)
                        p.act(ydall, ydall[:, :, cs], Yp, Yp[0:64, 0:384].rearrange("p (h t) -> p h t", h=3), AF.Copy)
                        for hh in H3:
                            p.mm(pch, pch[0:64, 256 + hh * 64:256 + (hh + 1) * 64], tk, tk[:, 1, VS[hh]], U, U[:, VS[hh]])
                        p.tt('dve', tS, tS[:, :], pch, pch[0:64, 256:448], S0, S0[:, :], ALU.add)
                        for hh in H3:
                            p.stt(S0, S0[:, VS[hh]], tS, tS[:, VS[hh]], Ein[hh][:, widx:widx + 1], KVW, KVW[:, VS[ci * 3 + hh]], ALU.mult, ALU.add,
                                  extra_reads=[Ein[hh]])
                        p.act(nxt, nxt[:, :], S0, S0[:, :], AF.Copy)
                for hh in H3:
                    b = hh * 15
                    rows = slice(hh * 64, (hh + 1) * 64)
                    if d == 0:
                        p.dma('pool', io['y0'][rows, t0:t0 + 512], ydall[:, hh, :], reads=[ydall], writes=[io['y0']])
                    else:
                        p.dma('sp', y0l[hh][:, :], io['y0'][rows, t0:t0 + 512], reads=[io['y0']], writes=[y0l[hh]])
                        y = pp3[hh]
                        p.tt('dve', y, y[:, :], ydall, ydall[:, hh, :], y0l[hh], y0l[hh][:, :], ALU.add)
                        G1 = blocksum(y)
                        p.tt('pool', pp1[hh], pp1[hh][:, :], y, y[:, :], y, y[:, :], ALU.mult)
                        G2 = blocksum(pp1[hh])
                        mean = pp2[hh]; var = tA[hh]
                        p.act(mean, mean[:, :], G1, G1[0:64, 0:512], AF.Copy, scale=1.0 / 64)
                        p.tt('pool', pp1[hh], pp1[hh][:, :], mean, mean[:, :], mean, mean[:, :], ALU.mult)
                        p.stt(var, var[:, :], G2, G2[0:64, 0:512], 1.0 / 64, pp1[hh], pp1[hh][:, :], ALU.mult, ALU.subtract)
                        p.act(var, var[:, :], var, var[:, :], AF.Sqrt, bias=64e-5)
                        p.op('dve', lambda: nc.vector.reciprocal(out=var[:, :], in_=var[:, :]), reads=[var], writes=[var])
                        p.tt('pool', y, y[:, :], y, y[:, :], mean, mean[:, :], ALU.subtract)
                        p.tt('pool', y, y[:, :], y, y[:, :], var, var[:, :], ALU.mult)
                        p.ts('dve', y, y[:, :], y, y[:, :], rwp[0:64, b + 9:b + 10], rwp[0:64, b + 10:b + 11], ALU.mult, ALU.add, extra_reads=[rwp])
                        rs_ = sh[(hh, 'r')]; ks = sh[(hh, 'k')]; vs_ = sh[(hh, 'v')]
                        p.stt(pp1[hh], pp1[hh][:, :], rs_, rs_[:, :], rwp[0:64, b + 8:b + 9], ks, ks[:, :], ALU.mult, ALU.mult, extra_reads=[rwp])
                        G3 = blocksum(pp1[hh])
                        p.tt('dve', pp2[hh], pp2[hh][:, :], G3, G3[0:64, 0:512], vs_, vs_[:, :], ALU.mult)
                        p.tt('pool', y, y[:, :], y, y[:, :], pp2[hh], pp2[hh][:, :], ALU.add)
                        G4 = PG()
                        p.mm(G4, G4[0:64, 0:512], gup, gup[:, rows], sgd, sgd[:, :])
                        p.tt('dve', yob[hh], yob[hh][:, :], y, y[:, :], G4, G4[0:64, 0:512], ALU.mult)
                        p.dma('pool', io['yT'][r_, 256 + hh * 64:256 + (hh + 1) * 64, off:off + 512], yob[hh][:, :], reads=[yob[hh]], writes=[io['yT']])

ALPHA = float(2.0 ** 0.5)
LN_EPS = 1e-5

def out_chunks():
    ch = []
    for q in range(4):
        heads = [2 * q, 2 * q + 1] if q < 2 else [q + 2]
        for s, h in enumerate(heads):
            ch.append((q, s * 128, 128, h * 128))
        ch.append((q, 256, 128, 768 + q * 192))
        ch.append((q, 384, 64, 768 + q * 192 + 128))
        ch.append((q, 448, 128, 1536 + q * 128))
    return ch

def ln_tile(p, u, lnw, lnb, scr, outf, outb, pfx, eps=LN_EPS):
    nc = p.nc
    st6 = scr['st6']; mv = scr['mv']; rs = scr['rs']; nm = scr['nm']; xn = u
    for c in range(4):
        p.op('dve', lambda c=c: nc.vector.bn_stats(out=st6[:, c * 6:(c + 1) * 6], in_=u[:, c * 512:(c + 1) * 512]), reads=[u], writes=[st6])
    p.op('dve', lambda: nc.vector.bn_aggr(out=mv[:, 0:2], in_=st6[:, 0:24]), reads=[st6], writes=[mv])
    p.ts('dve', rs, rs[:, 0:1], mv, mv[:, 1:2], eps, None, ALU.add)
    p.act(rs, rs[:, 0:1], rs, rs[:, 0:1], AF.Sqrt)
    p.op('dve', lambda: nc.vector.reciprocal(out=rs[:, 0:1], in_=rs[:, 0:1]), reads=[rs], writes=[rs])
    p.ts('dve', nm, nm[:, 0:1], mv, mv[:, 0:1], rs[:, 0:1], -1.0, ALU.mult, ALU.mult, extra_reads=[rs])
    p.act(xn, xn[:, :], u, u[:, :], AF.Identity, bias=nm[:, 0:1], scale=rs[:, 0:1], extra_reads=[nm, rs])
    p.tt('dve', xn, xn[:, :], xn, xn[:, :], lnw, lnw[:, :], ALU.mult)
    p.tt('dve', outf, outf[:, :], xn, xn[:, :], lnb, lnb[:, :], ALU.add)
    if outb is not None:
        p.act(outb, outb[:, :], outf, outf[:, :], AF.Copy)

def ln_scratch(st, pfx):
    return dict(st6=st.sb(pfx + "st6", [128, 24]), mv=st.sb(pfx + "mv", [128, 2]), rs=st.sb(pfx + "rs", [128, 1]),
                nm=st.sb(pfx + "nm", [128, 1]))

def load_bc(p, st, name, src_ap, n, q='sp'):
    t = st.sb(name, [128, n])
    p.dma(q, t[:, :], src_ap.partition_broadcast(128), writes=[t])
    return t

def transpose_tile(p, xb, ident, pst, xT, tcol, evac_e='act'):
    for k in range(16):
        p.tr(pst, pst[:, k * 128:(k + 1) * 128], xb, xb[:, k * 128:(k + 1) * 128], ident, ident[:, :])
    src = pst[:, :].rearrange("p (k t) -> p k t", k=16)
    dst = xT[:, :, tcol:tcol + 128]
    if evac_e == 'act':
        p.act(xT, dst, pst, src, AF.Copy)
    else:
        p.copy(evac_e, xT, dst, pst, src)

def cast_weights(p, srcs, dsts, n_per):
    with p.stage() as st:
        CH = 4096
        stg = [st.sb(f"cw_s{i}", [128, CH], F32) for i in range(3)]
        ob = [st.sb(f"cw_o{i}", [128, CH], BF16) for i in range(3)]
        i = 0
        engs = ['dve', 'pool', 'act']
        for (sT, sap), (dT, dap) in zip(srcs, dsts):
            n = sap.shape[1]
            for c0 in range(0, n, CH):
                w = min(CH, n - c0)
                s = stg[i % 3]; o = ob[i % 3]
                p.dma('sp', s[:, 0:w], sap[:, c0:c0 + w], reads=[sT], writes=[s])
                p.copy(engs[i % 3], o, o[:, 0:w], s, s[:, 0:w])
                p.dma('pool', dap[:, c0:c0 + w], o[:, 0:w], reads=[o], writes=[dT])
                i += 1

def precast_dma(p, io):
    for nm in ['w1', 'w3', 'w2']:
        sT = io[nm]; dT = io[nm + 'b']
        sv = sT.ap.rearrange("e a b -> (e a b)").rearrange("(r c) -> r c", c=2048)
        dv = dT.ap.rearrange("e a b -> (e a b)").rearrange("(r c) -> r c", c=2048)
        for r0 in range(0, 8192, 1024):
            p.dma('pool', dv[r0:r0 + 1024, :], sv[r0:r0 + 1024, :], reads=[sT], writes=[dT])

def phase2(p, T_, io, layer_last=False, ST=512, precast=True, want_xoT=True):
    nc = p.nc
    NT = T_ // 128
    chunks = out_chunks()
    NCH = len(chunks)
    srcs = []; dsts = []
    for nm in ['w1', 'w3', 'w2']:
        s = io[nm]; d = io[nm + 'b']
        srcs.append((s, s.ap.rearrange("e a b -> (e a b)").rearrange("(p n) -> p n", p=128)))
        dsts.append((d, d.ap.rearrange("e a b -> (e a b)").rearrange("(p n) -> p n", p=128)))
    if precast:
        cast_weights(p, srcs, dsts, None)

    with p.stage() as st:
        ident_f = st.sb("ident_f", [128, 128]); ident = st.sb("ident", [128, 128], BF16)
        p.dma('sp', ident_f[:, :], io['ident'][:, :], reads=[io['ident']], writes=[ident_f])
        p.copy('dve', ident, ident[:, :], ident_f, ident_f[:, :])
        wout = st.sb("wout", [128, NCH, 2048], BF16)
        wst = [st.sb(f"wst{i}", [128, 2048]) for i in range(2)]
        for j, (q, off, sz, r0) in enumerate(chunks):
            s = wst[j % 2]
            p.dma('sp', s[0:sz, :], io['w_out'][r0:r0 + sz, :], reads=[io['w_out']], writes=[s])
            p.copy(['dve', 'pool'][j % 2], wout, wout[0:sz, j, :], s, s[0:sz, :])
        lnw = load_bc(p, st, "lnw", io['ln_w'][0:1, :], 2048); lnb = load_bc(p, st, "lnb", io['ln_b'][0:1, :], 2048)
        scr = ln_scratch(st, "a")
        gluw_f = st.sb("gluw_f", [128, 4, 512]); gluw = st.sb("gluw", [128, 4, 512], BF16); glub = st.sb("glub", [128, 4])
        p.dma('sp', gluw_f[:, :, :], io['glu_w'].ap.rearrange("(k p) c -> p k c", p=128), reads=[io['glu_w']], writes=[gluw_f])
        p.copy('dve', gluw, gluw[:, :, :], gluw_f, gluw_f[:, :, :])
        p.dma('sp', glub[:, :], io['glu_bc'][:, :], reads=[io['glu_bc']], writes=[glub])
        ys5 = [st.sb(f"ys5{i}", [128, 4, 512], BF16) for i in range(2)]
        sgl = st.sb("sgl", [128, 512])
        s5j = [j for j, (q, off, sz, r0) in enumerate(chunks) if off == 448]
        ybuf = [st.sb(f"ybuf{i}", [128, NCH, 512], BF16) for i in range(2)]
        xr = wst
        u = st.sb("u", [128, 2048])
        x1f = [st.sb(f"x1f{i}", [128, 2048]) for i in range(1)]
        x1b = st.sb("x1b", [128, 2048], BF16)
        x1T = [st.sb(f"x1T{i}", [128, 16, 512], BF16) for i in range(1)]
        pm = [st.ps(f"pm{i}", [128, 512]) for i in range(4)]
        pst = st.ps("pst", [128, 2048], BF16)
        x1T_d = io['x1T'].ap.rearrange("(k p) t -> p k t", p=128)
        GT = min(4, NT)
        for t in range(NT):
            g = t // GT; tt_ = t % GT
            yb = ybuf[g % 2]
            if tt_ == 0:
                for j, (q, off, sz, r0) in enumerate(chunks):
                    p.dma('sp', yb[0:sz, j, 0:GT * 128], io['yT'][q, off:off + sz, g * GT * 128:(g + 1) * GT * 128], reads=[io['yT']], writes=[yb])
                W_ = GT * 128
                y5 = ys5[g % 2]
                for qo in range(4):
                    G = pm[qo]
                    for qi in range(4):
                        p.mm(G, G[:, 0:W_], gluw, gluw[:, qi, qo * 128:(qo + 1) * 128], yb, yb[:, s5j[qi], 0:W_], start=(qi == 0), stop=(qi == 3))
                    p.act(sgl, sgl[:, 0:W_], G, G[:, 0:W_], AF.Sigmoid, bias=glub[:, qo:qo + 1], extra_reads=[glub])
                    p.tt('dve', y5, y5[:, qo, 0:W_], sgl, sgl[:, 0:W_], yb, yb[:, s5j[qo], 0:W_], ALU.mult)
            xrt = xr[t % 2]
            p.dma('sp', xrt[:, :], io['xres'][t * 128:(t + 1) * 128, :], reads=[io['xres']], writes=[xrt])
            for c in range(4):
                for j, (q, off, sz, r0) in enumerate(chunks):
                    if j in s5j:
                        y5 = ys5[g % 2]
                        p.mm(pm[c], pm[c][:, :], y5, y5[:, s5j.index(j), tt_ * 128:(tt_ + 1) * 128], wout, wout[0:sz, j, c * 512:(c + 1) * 512],
                             start=(j == 0), stop=(j == NCH - 1))
                    else:
                        p.mm(pm[c], pm[c][:, :], yb, yb[0:sz, j, tt_ * 128:(tt_ + 1) * 128], wout, wout[0:sz, j, c * 512:(c + 1) * 512],
                             start=(j == 0), stop=(j == NCH - 1))
                p.stt(u, u[:, c * 512:(c + 1) * 512], xrt, xrt[:, c * 512:(c + 1) * 512], ALPHA, pm[c], pm[c][:, :], ALU.mult, ALU.add)
            xf = x1f[0]
            ln_tile(p, u, lnw, lnb, scr, xf, x1b, "a")
            p.dma('pool', io['x1'][t * 128:(t + 1) * 128, :], xf[:, :], reads=[xf], writes=[io['x1']])
            xT = x1T[0]
            transpose_tile(p, x1b, ident, pst, xT, tt_ * 128)
            if tt_ == GT - 1:
                p.dma('pool', x1T_d[:, :, g * GT * 128:(g + 1) * GT * 128], xT[:, :, 0:GT * 128], reads=[xT], writes=[io['x1T']])

    with p.stage() as st:
        ident_f = st.sb("ident_f", [128, 128]); ident = st.sb("ident", [128, 128], BF16)
        p.dma('sp', ident_f[:, :], io['ident'][:, :], reads=[io['ident']], writes=[ident_f])
        p.copy('dve', ident, ident[:, :], ident_f, ident_f[:, :])
        wr_f = st.sb("wr_f", [128, 16, 20]); wr = st.sb("wr", [128, 16, 20], BF16)
        p.dma('sp', wr_f[:, :, 0:4], io['rg'].ap.rearrange("(k p) g -> p k g", p=128), reads=[io['rg']], writes=[wr_f])
        for g in range(4):
            p.dma('sp', wr_f[:, :, 4 + 4 * g:8 + 4 * g], io['re'][g].rearrange("(k p) e -> p k e", p=128), reads=[io['re']], writes=[wr_f])
        p.copy('dve', wr, wr[:, :, :], wr_f, wr_f[:, :, :])
        rb = st.sb("rb", [128, 20])
        p.dma('sp', rb[:, 0:4], io['rgb'].ap.rearrange("(o g) -> o g", o=1).partition_broadcast(128), reads=[io['rgb']], writes=[rb])
        p.dma('sp', rb[:, 4:20], io['reb'].ap.rearrange("(o g) e -> o (g e)", o=1).partition_broadcast(128), reads=[io['reb']], writes=[rb])
        NS = ST // 128
        x1T = [st.sb(f"mx1T{i}", [128, 16, ST], BF16) for i in range(2)]
        yacc = st.sb("yacc", [128, NS, 2048])
        gate = st.sb("gate", [128, NS, 16])
        w1b = [st.sb(f"w1b{i}", [128, 16, 512], BF16) for i in range(1)]
        w3b = [st.sb(f"w3b{i}", [128, 16, 512], BF16) for i in range(1)]
        w2b = [st.sb(f"w2b{i}", [128, 4, 2048], BF16) for i in range(1)]
        hT = [st.sb(f"hT{i}", [128, 4, ST], BF16) for i in range(2)]
        sl = [st.sb(f"sl{i}", [128, ST]) for i in range(2)]
        lg = st.sb("lg", [128, 20]); r1 = st.sb("r1", [128, 8]); mg = st.sb("mg", [128, 4]); es = st.sb("es", [128, 4])
        tmp16 = st.sb("tmp16", [128, 16]); m1 = st.sb("m1", [128, 4]); m2 = st.sb("m2", [128, 4]); e2 = st.sb("e2", [128, 4])
        gi = st.sb("gi", [128, 4])
        pa = [st.ps(f"pa{i}", [128, 512]) for i in range(2)]
        pb = [st.ps(f"pb{i}", [128, 512]) for i in range(2)]
        py = [st.ps(f"py{i}", [128, 512]) for i in range(2)]
        x1T_d = io['x1T'].ap.rearrange("(k p) t -> p k t", p=128)
        NSUP = T_ // ST
        wcount = 0
        for s in range(NSUP):
            xT = x1T[s % 2]
            p.dma('sp', xT[:, :, :], x1T_d[:, :, s * ST:(s + 1) * ST], reads=[io['x1T']], writes=[xT])
            for m in range(NS):
                pl = pa[m % 2]
                for k in range(16):
                    p.mm(pl, pl[:, 0:20], xT, xT[:, k, m * 128:(m + 1) * 128], wr, wr[:, k, :], start=(k == 0), stop=(k == 15))
                p.tt('dve', lg, lg[:, :], pl, pl[:, 0:20], rb, rb[:, :], ALU.add)
                p.op('dve', lambda: nc.vector.tensor_reduce(out=r1[:, 0:1], in_=lg[:, 0:4], axis=AX.X, op=ALU.max), reads=[lg], writes=[r1])
                p.ts('dve', mg, mg[:, :], lg, lg[:, 0:4], r1[:, 0:1], None, ALU.is_equal, extra_reads=[r1])
                p.ts('dve', r1, r1[:, 1:2], r1, r1[:, 0:1], -1.0, None, ALU.mult)
                p.act(tmp16, tmp16[:, 0:4], lg, lg[:, 0:4], AF.Exp, bias=r1[:, 1:2], extra_reads=[r1], accum=(r1, r1[:, 2:3]))
                p.op('dve', lambda: nc.vector.reciprocal(out=r1[:, 3:4], in_=r1[:, 2:3]), reads=[r1], writes=[r1])
                p.ts('dve', es, es[:, :], lg, lg[:, 4:8], mg[:, 0:1], None, ALU.mult, extra_reads=[mg])
                for g in range(1, 4):
                    p.stt(es, es[:, :], lg, lg[:, 4 + 4 * g:8 + 4 * g], mg[:, g:g + 1], es, es[:, :], ALU.mult, ALU.add, extra_reads=[mg])
                p.op('dve', lambda: nc.vector.tensor_reduce(out=r1[:, 4:5], in_=es[:, :], axis=AX.X, op=ALU.max), reads=[es], writes=[r1])
                p.ts('dve', m1, m1[:, :], es, es[:, :], r1[:, 4:5], None, ALU.is_equal, extra_reads=[r1])
                p.stt(e2, e2[:, :], m1, m1[:, :], -1e30, es, es[:, :], ALU.mult, ALU.add)
                p.op('dve', lambda: nc.vector.tensor_reduce(out=r1[:, 5:6], in_=e2[:, :], axis=AX.X, op=ALU.max), reads=[e2], writes=[r1])
                p.ts('dve', m2, m2[:, :], e2, e2[:, :], r1[:, 5:6], None, ALU.is_equal, extra_reads=[r1])
                p.tt('dve', r1, r1[:, 6:7], r1, r1[:, 4:5], r1, r1[:, 5:6], ALU.subtract)
                p.act(r1, r1[:, 6:7], r1, r1[:, 6:7], AF.Sigmoid)
                p.ts('dve', r1, r1[:, 7:8], r1, r1[:, 6:7], -1.0, 1.0, ALU.mult, ALU.add)
                p.ts('dve', gi, gi[:, :], m1, m1[:, :], r1[:, 6:7], None, ALU.mult, extra_reads=[r1])
                p.stt(gi, gi[:, :], m2, m2[:, :], r1[:, 7:8], gi, gi[:, :], ALU.mult, ALU.add, extra_reads=[r1])
                p.ts('dve', gi, gi[:, :], gi, gi[:, :], r1[:, 3:4], None, ALU.mult, extra_reads=[r1])
                for g in range(4):
                    p.ts('dve', gate, gate[:, m, 4 * g:4 * g + 4], gi, gi[:, :], mg[:, g:g + 1], None, ALU.mult, extra_reads=[mg])
            for e in range(16):
                wb = 0
                p.dma('sp', w1b[wb][:, :, :], io['w1b'][e].rearrange("(k p) f -> p k f", p=128), reads=[io['w1b']], writes=[w1b[wb]])
                p.dma('sp', w3b[wb][:, :, :], io['w3b'][e].rearrange("(k p) f -> p k f", p=128), reads=[io['w3b']], writes=[w3b[wb]])
                p.dma('sp', w2b[wb][:, :, :], io['w2b'][e].rearrange("(k p) f -> p k f", p=128), reads=[io['w2b']], writes=[w2b[wb]])
                h = hT[e % 2]
                for f in range(4):
                    a = pa[f % 2]; b = pb[f % 2]
                    for k in range(16):
                        p.mm(a, a[:, 0:ST], w1b[wb], w1b[wb][:, k, f * 128:(f + 1) * 128], xT, xT[:, k, :], start=(k == 0), stop=(k == 15))
                    for k in range(16):
                        p.mm(b, b[:, 0:ST], w3b[wb], w3b[wb][:, k, f * 128:(f + 1) * 128], xT, xT[:, k, :], start=(k == 0), stop=(k == 15))
                    s_ = sl[f % 2]
                    p.act(s_, s_[:, :], a, a[:, 0:ST], AF.Silu)
                    p.tt('dve', h, h[:, f, :], s_, s_[:, :], b, b[:, 0:ST], ALU.mult)
                i = 0
                for m in range(NS):
                    for c in range(4):
                        y = py[i % 2]; i += 1
                        for f in range(4):
                            p.mm(y, y[:, :], h, h[:, f, m * 128:(m + 1) * 128], w2b[wb], w2b[wb][:, f, c * 512:(c + 1) * 512], start=(f == 0), stop=(f == 3))
                        if e == 0:
                            p.ts('dve', yacc, yacc[:, m, c * 512:(c + 1) * 512], y, y[:, :], gate[:, m, e:e + 1], None, ALU.mult, extra_reads=[gate])
                        else:
                            p.stt(yacc, yacc[:, m, c * 512:(c + 1) * 512], y, y[:, :], gate[:, m, e:e + 1], yacc, yacc[:, m, c * 512:(c + 1) * 512],
                                  ALU.mult, ALU.add, extra_reads=[gate])
            for m in range(NS):
                t = s * NS + m
                p.dma('pool', io['moe'][t * 128:(t + 1) * 128, :], yacc[:, m, :], reads=[yacc], writes=[io['moe']])

    with p.stage() as st:
        ident_f = st.sb("ident_f", [128, 128]); ident = st.sb("ident", [128, 128], BF16)
        p.dma('sp', ident_f[:, :], io['ident'][:, :], reads=[io['ident']], writes=[ident_f])
        p.copy('dve', ident, ident[:, :], ident_f, ident_f[:, :])
        lnw2 = load_bc(p, st, "lnw2", io['ln_w'][1:2, :], 2048); lnb2 = load_bc(p, st, "lnb2", io['ln_b'][1:2, :], 2048)
        lnw3 = load_bc(p, st, "lnw3", io['ln_w'][2:3, :], 2048); lnb3 = load_bc(p, st, "lnb3", io['ln_b'][2:3, :], 2048)
        scr = ln_scratch(st, "c")
        pg = st.sb("pg", [128, 16, 2048], BF16); pp = st.sb("pp", [128, 2, 2048], BF16)
        wst = [st.sb(f"wst{i}", [128, 2048]) for i in range(2)]
        for k in range(16):
            s = wst[k % 2]
            p.dma('sp', s[:, :], io['ple_gate'][k * 128:(k + 1) * 128, :], reads=[io['ple_gate']], writes=[s])
            p.copy(['dve', 'pool'][k % 2], pg, pg[:, k, :], s, s[:, :])
        for k in range(2):
            s = wst[k % 2]
            p.dma('sp', s[:, :], io['ple_proj'][k * 128:(k + 1) * 128, :], reads=[io['ple_proj']], writes=[s])
            p.copy(['dve', 'pool'][k % 2], pp, pp[:, k, :], s, s[:, :])
        xr = wst[0]; mo = wst[1]
        x2T = st.sb("cx2T", [128, 16, 128], BF16)
        pTf = st.sb("pTf", [128, 2, 128]); pTb = st.sb("pTb", [128, 2, 128], BF16)
        sg = st.sb("sg", [128, 512])
        u = st.sb("u", [128, 2048])
        x2f = st.sb("x2f", [128, 2048]); x2b = st.sb("x2b", [128, 2048], BF16)
        x3f = st.sb("x3f", [128, 2048]); x3b = st.sb("x3b", [128, 2048], BF16)
        x3T = [st.sb(f"x3T{i}", [128, 16, 512], BF16) for i in range(2)]
        pgp = [st.ps(f"pgp{i}", [128, 512]) for i in range(2)]
        ppp = [st.ps(f"ppp{i}", [128, 512]) for i in range(2)]
        pst = st.ps("pst", [128, 2048], BF16)
        xoT_d = io['xoT'].ap.rearrange("(k p) t -> p k t", p=128)
        pT_d = io['pT'].ap.rearrange("(k p) t -> p k t", p=128)
        GT = min(4, NT)
        for t in range(NT):
            g = t // GT; tt_ = t % GT
            rows = slice(t * 128, (t + 1) * 128)
            p.dma('sp', xr[:, :], io['x1'][rows, :], reads=[io['x1']], writes=[xr])
            p.dma('sp', mo[:, :], io['moe'][rows, :], reads=[io['moe']], writes=[mo])
            p.dma('sp', pTf[:, :, :], pT_d[:, :, rows], reads=[io['pT']], writes=[pTf])
            p.copy('pool', pTb, pTb[:, :, :], pTf, pTf[:, :, :])
            p.stt(u, u[:, :], xr, xr[:, :], ALPHA, mo, mo[:, :], ALU.mult, ALU.add)
            ln_tile(p, u, lnw2, lnb2, scr, x2f, x2b, "c")
            transpose_tile(p, x2b, ident, pst, x2T, 0)
            for c in range(4):
                cs = slice(c * 512, (c + 1) * 512)
                G = pgp[c % 2]; PP = ppp[c % 2]
                for k in range(16):
                    p.mm(G, G[:, :], x2T, x2T[:, k, :], pg, pg[:, k, cs], start=(k == 0), stop=(k == 15))
                for k in range(2):
                    p.mm(PP, PP[:, :], pTb, pTb[:, k, :], pp, pp[:, k, cs], start=(k == 0), stop=(k == 1))
                p.act(sg, sg[:, :], G, G[:, :], AF.Sigmoid)
                p.tt('dve', sg, sg[:, :], sg, sg[:, :], PP, PP[:, :], ALU.mult)
                p.stt(u, u[:, cs], x2f, x2f[:, cs], ALPHA, sg, sg[:, :], ALU.mult, ALU.add)
            ln_tile(p, u, lnw3, lnb3, scr, x3f, x3b if want_xoT else None, "c")
            p.dma('pool', io['xo'][rows, :], x3f[:, :], reads=[x3f], writes=[io['xo']])
            x3 = x3T[g % 2]
            if want_xoT:
                transpose_tile(p, x3b, ident, pst, x3, tt_ * 128)
            if want_xoT and tt_ == GT - 1:
                p.dma('pool', xoT_d[:, :, g * GT * 128:(g + 1) * GT * 128], x3[:, :, 0:GT * 128], reads=[x3], writes=[io['xoT']])


_S = 16384
_D = 2048
_T = 4096
_PROGS = {}
_GROUPS = [[0, 1, 2, 3], [4, 5, 6, 7]]
_P1_KEYS = [('w_in', [_D, NZ]), ('rwp', [128, 55]), ('rw_wup', [64, 2, 192]), ('rw_aup', [64, 2, 192]), ('rw_gup', [128, 192]),
            ('s5rows', [3, 1024]), ('s5cols', [128, 24]), ('s5bl', [2, 128, 1024]), ('s5cl', [2, 128, 1024]), ('s5d', [128, 1])]
_P2_KEYS = [('w_out', [_D, _D]), ('ln_w', [3, _D]), ('ln_b', [3, _D]), ('rg', [_D, 4]), ('rgb', [4]), ('re', [4, _D, 4]), ('reb', [4, 4]),
            ('w1', [16, _D, 512]), ('w3', [16, _D, 512]), ('w2', [16, 512, _D]), ('glu_w', [512, 512]), ('glu_bc', [128, 4]),
            ('ple_proj', [256, _D]), ('ple_gate', [_D, _D]), ('pT', [256, None])]

def stage_select(p, gath, rmask, yTr):
    with p.stage() as st:
        mk = st.sb("mk", [128, 4]); p.dma('sp', mk[:, :], rmask[:, :], reads=[rmask], writes=[mk])
        cand = [[st.sb(f"cand{i}_{r}", [128, _T], BF16) for r in range(4)] for i in range(2)]
        acc = [st.sb(f"acc{i}", [128, _T], BF16) for i in range(2)]
        n = 0
        for q in range(4):
            for i in range(6):
                r0 = i * 96; sz = 96
                cd = cand[n % 2]; a = acc[n % 2]
                e = 'dve'; n += 1
                for r in range(4):
                    p.dma('sp', cd[r][0:sz, :], gath[r * 6 + i, q * 96:(q + 1) * 96, :], reads=[gath], writes=[cd[r]])
                p.ts(e, a, a[0:sz, :], cd[0], cd[0][0:sz, :], mk[0:sz, 0:1], None, ALU.mult, extra_reads=[mk])
                for r in range(1, 4):
                    p.stt(a, a[0:sz, :], cd[r], cd[r][0:sz, :], mk[0:sz, r:r + 1], a, a[0:sz, :], ALU.mult, ALU.add, extra_reads=[mk], e=e)
                p.dma('pool', yTr[q, r0:r0 + sz, :], a[0:sz, :], reads=[a], writes=[yTr])

def _build_fused():
    S = _S; D = _D; T_ = _T
    nc = bass.Bass("TRN2", target_bir_lowering=False)
    p = Prog(nc)
    E = {}
    def ext(name, shape, dt=F32):
        E[name] = p.dram(name, shape, dt, kind="ExternalInput"); return E[name]
    ext('xT0', [4, D, T_]); ext('xres', [T_, D]); ext('pos', [S], I32); ext('rconst', [128, RC_N]); ext('ident', [128, 128])
    ext('rwconst', [128, RW_N]); ext('s5iota', [128, 512]); ext('rmask', [128, 4])
    for L in range(2):
        for k, sh in _P1_KEYS + _P2_KEYS:
            ext(f"{k}_{L}", [T_ if s is None else s for s in sh])
    xo_final = p.dram('xo_final', [T_, D], F32, kind="ExternalOutput")
    sc = {}
    sc['zT'] = p.dram('zT', [NZ, S], F32)
    for nm in ['qrT', 'krT']: sc[nm] = p.dram(nm, [2, 128, S], BF16)
    for nm in ['ktok', 'vtok', 'sbd']: sc[nm] = p.dram(nm, [2, S // 128, 128, 128], BF16)
    sc['yf'] = p.dram('yf', [128, S]); sc['y0'] = p.dram('y0', [192, S])
    yTloc = p.dram('yTloc', [4, 576, T_], BF16)
    gath = p.dram('gath', [24, 4 * 96, T_], BF16)
    yTr = p.dram('yTr', [4, 576, T_], BF16)
    sc2 = dict(x1=p.dram('x1', [T_, D]), x1T=p.dram('x1T', [D, T_], BF16), moe=p.dram('moe', [T_, D]),
               w1b=p.dram('w1b', [16, D, 512], BF16), w3b=p.dram('w3b', [16, D, 512], BF16), w2b=p.dram('w2b', [16, 512, D], BF16))
    xo0 = p.dram('xo0', [T_, D]); xoT = p.dram('xoT', [D, T_], BF16); xoT_dummy = p.dram('xoT_dummy', [D, T_], BF16)
    xTg = p.dram('xTg', [16, 4 * 128, T_], BF16)
    for L in range(2):
        io1 = dict(sc)
        for k, _ in _P1_KEYS: io1[k] = E[f"{k}_{L}"]
        io1.update(pos=E['pos'], rconst=E['rconst'], ident=E['ident'], rwconst=E['rwconst'], s5iota=E['s5iota'], yT=yTloc)
        io1['xT'] = E['xT0'] if L == 0 else xTg
        xsrc = None
        if L == 1:
            xsrc = lambda r: xTg.ap[:, r * 128:(r + 1) * 128, :].rearrange("k p t -> p k t")
        stage_inproj(p, S, io1, L == 0, xsrc=xsrc)
        stage_retention(p, S, io1)
        stage_s5(p, S, io1)
        io2 = dict(sc2)
        for k_, _ in _P2_KEYS: io2[k_] = E[f"{k_}_{L}"]
        precast_dma(p, io2)
        stage_rwkv(p, S, io1)
        for r in range(4):
            for i in range(6):
                p.collective("AllGather", yTloc, yTloc.ap[r, i * 96:(i + 1) * 96, :], gath, gath.ap[r * 6 + i], _GROUPS)
        stage_select(p, gath, E['rmask'], yTr)
        io2.update(yT=yTr, ident=E['ident'], xres=(E['xres'] if L == 0 else xo0), xo=(xo0 if L == 0 else xo_final),
                   xoT=(xoT if L == 0 else xoT_dummy))
        phase2(p, T_, io2, ST=512, precast=False, want_xoT=(L == 0))
        if L == 0:
            for k in range(16):
                p.collective("AllGather", xoT, xoT.ap[k * 128:(k + 1) * 128, :], xTg, xTg.ap[k], _GROUPS)
    p.finish([xo_final])
    return nc

def kernel(**inp):
    x = np.ascontiguousarray(np.asarray(inp['x'], dtype=np.float32))
    S = _S; D = _D
    if 'f' not in _PROGS:
        _PROGS['f'] = _build_fused()
    ident = np.eye(128, dtype=np.float32)
    rwc = rwkv_consts()
    positions = np.asarray(inp['positions']).astype(np.int32)
    g = lambda k: np.asarray(inp[k])
    maps = []
    for c in range(8):
        b, q = c // 4, c % 4
        r = q
        rmask = np.zeros((128, 4), np.float32); rmask[:, r] = 1.0
        m = dict(xT0=np.ascontiguousarray(x[b].T.reshape(D, 4, S // 4).transpose(1, 0, 2)),
                 xres=np.ascontiguousarray(x[b, r * _T:(r + 1) * _T]), pos=np.ascontiguousarray(positions[b]),
                 rconst=ret_consts(q), ident=ident, rwconst=rwc, rmask=rmask)
        for L in range(2):
            d1 = dict(w_in=np.ascontiguousarray(g('w_in')[L][:, core_cols(q)]))
            d1.update(rwkv_host_layout(q, g('rwkv_mu_prev')[L], g('rwkv_mu_next')[L], g('rwkv_w0')[L], g('rwkv_w_up')[L], g('rwkv_a0')[L],
                                       g('rwkv_a_up')[L], g('rwkv_g_up')[L], g('rwkv_k_k')[L], g('rwkv_k_a')[L], g('rwkv_r_k')[L],
                                       g('rwkv_lnx_w')[L], g('rwkv_lnx_b')[L]))
            s5 = s5_host_layout(q, g('s5_lam_re')[L], g('s5_lam_im')[L], g('s5_log_dt')[L], g('s5_b_re')[L], g('s5_b_im')[L],
                                g('s5_c_re')[L], g('s5_c_im')[L], g('s5_d')[L])
            m['s5iota'] = s5.pop('s5iota')
            d1.update(s5)
            d2 = dict(w_out=g('w_out')[L], ln_w=g('ln_w')[L], ln_b=g('ln_b')[L],
                      rg=g('moe_router_g')[L], rgb=g('moe_router_g_b')[L], re=g('moe_router_e')[L], reb=g('moe_router_e_b')[L],
                      w1=g('moe_w1')[L], w3=g('moe_w3')[L], w2=g('moe_w2')[L],
                      glu_w=g('s5_glu_w')[L], glu_bc=np.ascontiguousarray(g('s5_glu_b')[L].reshape(4, 128).T),
                      ple_proj=g('ple_proj')[L], ple_gate=g('ple_gate')[L],
                      pT=np.ascontiguousarray(g('p')[L, b, r * _T:(r + 1) * _T].T))
            for k, v in list(d1.items()) + list(d2.items()):
                m[f"{k}_{L}"] = np.ascontiguousarray(v)
        maps.append(m)
    res = run_bass_kernel_spmd(_PROGS['f'], maps, core_ids=list(range(8)))
    out = np.empty_like(x)
    for c in range(8):
        b, r = c // 4, c % 4
        out[b, r * _T:(r + 1) * _T] = np.asarray(res.results[c]['xo_final'])
    return out
```

```python
import math
import numpy as np
import concourse.bass as bass
import concourse.mybir as mybir
from concourse.bass_utils import run_bass_kernel_spmd
from contextlib import ExitStack
F32 = mybir.dt.float32; BF16 = mybir.dt.bfloat16; I32 = mybir.dt.int32
AF = mybir.ActivationFunctionType; ALU = mybir.AluOpType; AX = mybir.AxisListType


class T:
    def __init__(self, ap, name):
        self.ap = ap; self.name = name
        self.w = None
        self.r = []
    def __getitem__(self, k):
        return self.ap[k]


class Prog:
    NDMA = 8
    SEM_MAX = 30000
    def __init__(self, nc):
        self.nc = nc
        self.eng = {'pe': nc.tensor, 'dve': nc.vector, 'act': nc.scalar, 'pool': nc.gpsimd, 'sp': nc.sync}
        self.gen = {e: 0 for e in ['pe', 'dve', 'act', 'pool']}
        self.sem = {e: nc.alloc_semaphore("sem_" + e) for e in ['pe', 'dve', 'act', 'pool']}
        self.cnt = {e: 0 for e in self.sem}
        self.seen = {e: {} for e in self.eng}
        self.dsem = {q: [nc.alloc_semaphore(f"dsem_{q}{i}") for i in range(self.NDMA)] for q in ['sp', 'pool']}
        self.dcnt = {q: [0] * self.NDMA for q in self.dsem}
        self.dgen = {q: [0] * self.NDMA for q in self.dsem}
        self.dnext = {q: 0 for q in self.dsem}
        self.semobj = {}
        for e, s in self.sem.items(): self.semobj[('c', e, 0)] = s
        for q in self.dsem:
            for i, s in enumerate(self.dsem[q]): self.semobj[('d', q, i, 0)] = s
        self.ninst = 0
        self.nsb = 0

    def sb(self, name, shape, dt=F32):
        return T(self.nc.alloc_sbuf_tensor(name, list(shape), dt).ap(), name)
    def ps(self, name, shape, dt=F32):
        return T(self.nc.alloc_psum_tensor(name, list(shape), dt).ap(), name)
    def dram(self, name, shape, dt=F32, kind="Internal"):
        return T(self.nc.dram_tensor(name, list(shape), dt, kind=kind).ap(), name)

    def _wait(self, e, tok):
        if tok is None: return
        key, val = tok
        if self.seen[e].get(key, 0) >= val: return
        self.eng[e].wait_ge(self.semobj[key], val)
        self.seen[e][key] = val

    def _deps(self, e, reads, writes):
        for b in reads:
            if b.w is not None and not (e == 'pe' and b.w[0][0:2] == ('c', 'pe')):
                self._wait(e, b.w)
        for b in writes:
            if b.w is not None and not (e == 'pe' and b.w[0][0:2] == ('c', 'pe')):
                self._wait(e, b.w)
            for tok in b.r:
                if e == 'pe' and tok[0][0:2] == ('c', 'pe'): continue
                self._wait(e, tok)

    def _mark(self, tok, reads, writes):
        for b in reads:
            b.r = [t for t in b.r if t[0] != tok[0]] + [tok]
        for b in writes:
            b.w = tok; b.r = []

    def op(self, e, fn, reads=(), writes=()):
        self._deps(e, reads, writes)
        if self.cnt[e] >= self.SEM_MAX:
            self.gen[e] += 1; self.cnt[e] = 0
            self.sem[e] = self.nc.alloc_semaphore(f"sem_{e}_{self.gen[e]}")
            self.semobj[('c', e, self.gen[e])] = self.sem[e]
        inst = fn()
        self.cnt[e] += 1
        inst.then_inc(self.sem[e], 1)
        tok = (('c', e, self.gen[e]), self.cnt[e])
        self._mark(tok, reads, writes)
        self.ninst += 1
        return inst

    def dma(self, q, out, in_, reads=(), writes=(), **kw):
        i = self.dnext[q]; self.dnext[q] = (i + 1) % self.NDMA
        key = ('d', q, i, self.dgen[q][i])
        if self.dcnt[q][i] > 0:
            self._wait(q, (key, self.dcnt[q][i]))
        if self.dcnt[q][i] >= self.SEM_MAX:
            self.dgen[q][i] += 1; self.dcnt[q][i] = 0
            self.dsem[q][i] = self.nc.alloc_semaphore(f"dsem_{q}{i}_{self.dgen[q][i]}")
            key = ('d', q, i, self.dgen[q][i])
            self.semobj[key] = self.dsem[q][i]
        self._deps(q, reads, writes)
        inst = self.eng[q].dma_start(out=out, in_=in_, **kw)
        self.dcnt[q][i] += 16
        inst.then_inc(self.dsem[q][i], 16)
        tok = (key, self.dcnt[q][i])
        self._mark(tok, reads, writes)
        self.ninst += 1
        return inst

    def finish(self, outs):
        for b in outs:
            self._wait('sp', b.w)
        for e in ['pe', 'dve', 'act', 'pool']:
            if self.cnt[e] > 0:
                self._wait('sp', (('c', e, self.gen[e]), self.cnt[e]))
        for q in self.dsem:
            for i in range(self.NDMA):
                if self.dcnt[q][i] > 0:
                    self._wait('sp', (('d', q, i, self.dgen[q][i]), self.dcnt[q][i]))

    def mm(self, out, o_ap, lhsT, l_ap, rhs, r_ap, start=True, stop=True):
        return self.op('pe', lambda: self.nc.tensor.matmul(o_ap, l_ap, r_ap, start=start, stop=stop),
                       reads=[lhsT, rhs], writes=[out])
    def tr(self, out, o_ap, in_, i_ap, ident, id_ap):
        return self.op('pe', lambda: self.nc.tensor.transpose(o_ap, i_ap, id_ap), reads=[in_, ident], writes=[out])
    def act(self, out, o_ap, in_, i_ap, func, bias=None, scale=1.0, extra_reads=(), e='act', accum=None):
        kw = {}
        if bias is not None: kw['bias'] = bias
        if accum is not None: kw['accum_out'] = accum[1]
        wr = [out] + ([accum[0]] if accum is not None else [])
        return self.op('act', lambda: self.nc.scalar.activation(out=o_ap, in_=i_ap, func=func, scale=scale, **kw),
                       reads=[in_] + list(extra_reads), writes=wr)
    def tt(self, e, out, o_ap, a, a_ap, b, b_ap, op):
        en = self.eng[e]
        return self.op(e, lambda: en.tensor_tensor(out=o_ap, in0=a_ap, in1=b_ap, op=op), reads=[a, b], writes=[out])
    def ts(self, e, out, o_ap, a, a_ap, s1, s2, op0, op1=None, extra_reads=(), accum=None):
        en = self.eng[e]
        kw = {}
        if op1 is not None: kw['op1'] = op1
        wr = [out]
        if accum is not None:
            kw['accum_out'] = accum[1]; wr.append(accum[0])
        return self.op(e, lambda: en.tensor_scalar(out=o_ap, in0=a_ap, scalar1=s1, scalar2=s2, op0=op0, **kw),
                       reads=[a] + list(extra_reads), writes=wr)
    def stt(self, out, o_ap, a, a_ap, s, b, b_ap, op0, op1, extra_reads=(), e='dve'):
        en = self.eng[e]
        return self.op(e, lambda: en.scalar_tensor_tensor(out=o_ap, in0=a_ap, scalar=s, in1=b_ap, op0=op0, op1=op1),
                       reads=[a, b] + list(extra_reads), writes=[out])
    def copy(self, e, out, o_ap, in_, i_ap):
        if e == 'act':
            return self.act(out, o_ap, in_, i_ap, AF.Copy)
        en = self.eng[e]
        return self.op(e, lambda: en.tensor_copy(out=o_ap, in_=i_ap), reads=[in_], writes=[out])
    def memset(self, e, out, o_ap, val):
        en = self.eng[e]
        return self.op(e, lambda: en.memset(o_ap, val), writes=[out])

class Stage:
    def __init__(self, p):
        self.p = p; self.es = ExitStack()
    def __enter__(self):
        self.es.__enter__(); return self
    def __exit__(self, *a):
        self.p.barrier()
        return self.es.__exit__(*a)
    def sb(self, name, shape, dt=F32):
        self.p.nsb += 1; name = f"s{self.p.nsb}_{name}"
        h = self.es.enter_context(self.p.nc.sbuf_tensor(name, list(shape), dt))
        return T(h.ap(), name)
    def ps(self, name, shape, dt=F32):
        self.p.nsb += 1; name = f"s{self.p.nsb}_{name}"
        h = self.es.enter_context(self.p.nc.psum_tensor(name, list(shape), dt))
        return T(h.ap(), name)

def _barrier(self):
    toks = []
    for e in ['pe', 'dve', 'act', 'pool']:
        if self.cnt[e] > 0: toks.append((('c', e, self.gen[e]), self.cnt[e]))
    for q in self.dsem:
        for i in range(self.NDMA):
            if self.dcnt[q][i] > 0: toks.append((('d', q, i, self.dgen[q][i]), self.dcnt[q][i]))
    for e in self.eng:
        for tok in toks:
            if tok[0][0:2] == ('c', e) and e == 'pe': continue
            self._wait(e, tok)
Prog.barrier = _barrier
Prog.stage = lambda self: Stage(self)

def _collective(self, kind, in_T, in_ap, out_T, out_ap, groups):
    if not hasattr(self, 'ccsem'):
        self.ccsem = self.nc.alloc_semaphore("ccsem"); self.ccnt = 0
        self.semobj[('cc',)] = self.ccsem
    self._deps('pool', [in_T], [out_T])
    inst = self.nc.gpsimd.collective_compute(kind, ALU.bypass, replica_groups=groups, ins=[in_ap], outs=[out_ap])
    self.ccnt += 1
    inst.then_inc(self.ccsem, 1)
    tok = (('cc',), self.ccnt)
    self._mark(tok, [in_T], [out_T])
    self.ninst += 1
Prog.collective = _collective

PI = math.pi
C1 = 6.28125
C2 = 2 * math.pi - C1
INV2PI = 1.0 / (2 * math.pi)

RET_W = 768; RWKV_W = 768; RET_COLS = 3072; RWKV_COLS = 2688
NZ = 2112
COLTILES = [(i * 128, 128) for i in range(8)] + [(1024, 128), (1152, 64), (1216, 128), (1344, 64), (1408, 128), (1536, 64),
                                                  (1600, 128), (1728, 128), (1856, 128), (1984, 128)]

def ret_heads(q):
    return [2 * q, 2 * q + 1] if q < 2 else [q + 2, q + 2]

def core_cols(q):
    cols = []
    for h in ret_heads(q):
        for part in range(4):
            cols += list(range(part * RET_W + h * 128, part * RET_W + (h + 1) * 128))
    base = RET_COLS
    for part in range(3):
        cols += list(range(base + part * RWKV_W + q * 192, base + part * RWKV_W + (q + 1) * 192))
    cols += list(range(base + 3 * RWKV_W, base + 3 * RWKV_W + 384))
    cols += list(range(RET_COLS + RWKV_COLS + q * 128, RET_COLS + RWKV_COLS + (q + 1) * 128))
    assert len(cols) == NZ
    return cols

def ret_consts(q):
    import numpy as np
    C = 128
    cols = []
    inv = (10000.0 ** (-(np.arange(128) % 64).astype(np.float32) / np.float32(64))).astype(np.float32)
    cols.append(inv[:, None]); cols.append(np.where(np.arange(128) < 64, -1.0, 1.0).astype(np.float32)[:, None])
    pos = np.arange(C, dtype=np.float64)
    for h in ret_heads(q):
        lg = np.log(1.0 - 2.0 ** (-5.0 - h))
        cols.append(np.exp(lg * (C - 1 - pos))[:, None])
        cols.append(np.exp(lg * pos)[:, None])
        cols.append(np.full((128, 1), np.exp(lg * C)))
        cols.append(np.exp(lg * np.abs(pos[:, None] - pos[None, :])))
        cols.append(np.tile(np.exp(lg * (pos + 1.0))[None, :], (128, 4)))
        cols.append(np.tile(np.exp(lg * (C - pos))[None, :], (128, 4)))
    return np.concatenate(cols, axis=1).astype(np.float32)
RC_SLOT = 3 + 128 + 512 + 512
RC_N = 2 + 2 * RC_SLOT

def stage_inproj(p, S, io, x_f32, xsrc=None):
    nc = p.nc
    Q4 = S // 4
    with p.stage() as st:
        wb = st.sb("winb", [128, 16, NZ], BF16)
        wst = [st.sb(f"wst{i}", [128, NZ]) for i in range(2)]
        for k in range(16):
            s = wst[k % 2]
            p.dma('sp', s[:, :], io['w_in'][k * 128:(k + 1) * 128, :], reads=[io['w_in']], writes=[s])
            p.copy(['dve', 'pool'][k % 2], wb, wb[:, k, :], s, s[:, :])
        xb = [st.sb(f"xb{i}", [128, 16, 512], BF16) for i in range(2)]
        if x_f32:
            xf = [st.sb(f"xf{i}", [128, 8, 512]) for i in range(2)]
        zo = [st.sb(f"zo{i}", [128, 512]) for i in range(4)]
        ps = [st.ps(f"ps{i}", [128, 512]) for i in range(4)]
        n = 0
        for tt in range(S // 512):
            r = (tt * 512) // Q4; off = tt * 512 - r * Q4
            src = xsrc(r) if xsrc is not None else io['xT'][r].rearrange("(k p) t -> p k t", p=128)
            x = xb[tt % 2]
            if x_f32:
                for hf in range(2):
                    p.dma('sp', xf[hf][:, :, :], src[:, hf * 8:(hf + 1) * 8, off:off + 512], reads=[io['xT']], writes=[xf[hf]])
                    p.copy(['dve', 'act'][hf], x, x[:, hf * 8:(hf + 1) * 8, :], xf[hf], xf[hf][:, :, :])
            else:
                p.dma('sp', x[:, :, :], src[:, :, off:off + 512], reads=[io['xT']], writes=[x])
            for ci, (c0, w) in enumerate(COLTILES):
                P_ = ps[n % 4]; z = zo[n % 4]
                for k in range(16):
                    p.mm(P_, P_[0:w, :], wb, wb[:, k, c0:c0 + w], x, x[:, k, :], start=(k == 0), stop=(k == 15))
                if n % 2 == 0:
                    p.act(z, z[0:w, :], P_, P_[0:w, :], AF.Copy)
                else:
                    p.copy('dve', z, z[0:w, :], P_, P_[0:w, :])
                p.dma('pool', io['zT'][c0:c0 + w, tt * 512:(tt + 1) * 512], z[0:w, :], reads=[z], writes=[io['zT']])
                n += 1

def stage_retention(p, S, io):
    nc = p.nc
    NCK = S // 128; NTT = S // 512; Q4 = S // 4
    zT = io['zT']
    with p.stage() as st:
        rc = st.sb("rc", [128, RC_N])
        p.dma('sp', rc[:, :], io['rconst'][:, :], reads=[io['rconst']], writes=[rc])
        ident_f = st.sb("ident_f", [128, 128]); ident = st.sb("ident", [128, 128], BF16)
        p.dma('sp', ident_f[:, :], io['ident'][:, :], reads=[io['ident']], writes=[ident_f])
        p.copy('dve', ident, ident[:, :], ident_f, ident_f[:, :])
        posi = st.sb("posi", [128, 512], I32); ang = st.sb("ang", [128, 512]); a2 = st.sb("a2", [128, 512])
        ki = st.sb("ki", [128, 512], I32); kf = st.sb("kf", [128, 512])
        cos = st.sb("cos", [128, 512]); sin = st.sb("sin", [128, 512])
        zq = [st.sb(f"zq{i}", [128, 512]) for i in range(2)]; zs = [st.sb(f"zs{i}", [128, 512]) for i in range(2)]
        t1 = st.sb("t1", [128, 512]); t2 = st.sb("t2", [128, 512])
        rot = [st.sb(f"rot{i}", [128, 512], BF16) for i in range(2)]
        vf = st.sb("vf", [128, 512]); vb = st.sb("vb", [128, 512], BF16)
        tok = [st.sb(f"tok{i}", [128, 4, 128], BF16) for i in range(2)]
        ptr = [st.ps(f"ptr{i}", [128, 512], BF16) for i in range(2)]
        n = 0
        for tt in range(NTT):
            ts_ = slice(tt * 512, (tt + 1) * 512)
            p.dma('sp', posi[:, :], io['pos'].ap[ts_].rearrange("(o t) -> o t", o=1).partition_broadcast(128), reads=[io['pos']], writes=[posi])
            p.ts('dve', ang, ang[:, :], posi, posi[:, :], rc[:, 0:1], None, ALU.mult, extra_reads=[rc])
            for (shift, dst, scale) in [(0.0, sin, rc[:, 1:2]), (PI / 2, cos, 1.0)]:
                if shift != 0.0:
                    p.ts('dve', a2, a2[:, :], ang, ang[:, :], shift, None, ALU.add)
                    src = a2
                else:
                    src = ang
                p.ts('dve', ki, ki[:, :], src, src[:, :], INV2PI, None, ALU.mult)
                p.copy('act', kf, kf[:, :], ki, ki[:, :])
                p.stt(a2, a2[:, :], kf, kf[:, :], -C1, src, src[:, :], ALU.mult, ALU.add)
                p.stt(a2, a2[:, :], kf, kf[:, :], -C2, a2, a2[:, :], ALU.mult, ALU.add)
                p.ts('pool', a2, a2[:, :], a2, a2[:, :], -PI, PI, ALU.max, ALU.min)
                p.act(dst, dst[:, :], a2, a2[:, :], AF.Sin, scale=scale, extra_reads=[rc])
            for s in range(2):
                base = s * 512
                for which, (r0, scl, dstd) in enumerate([(base, 128.0 ** -0.5, io['qrT']), (base + 128, 1.0, io['krT'])]):
                    z = zq[which]; zw = zs[which]
                    p.dma('sp', z[:, :], zT[r0:r0 + 128, ts_], reads=[zT], writes=[z])
                    p.dma('sp', zw[0:64, :], zT[r0 + 64:r0 + 128, ts_], reads=[zT], writes=[zw])
                    p.dma('sp', zw[64:128, :], zT[r0:r0 + 64, ts_], reads=[zT], writes=[zw])
                    p.stt(t1, t1[:, :], z, z[:, :], scl, cos, cos[:, :], ALU.mult, ALU.mult)
                    p.stt(t2, t2[:, :], zw, zw[:, :], scl, sin, sin[:, :], ALU.mult, ALU.mult, e='dve')
                    ro = rot[which]
                    p.tt(['pool', 'dve'][which], ro, ro[:, :], t1, t1[:, :], t2, t2[:, :], ALU.add)
                    p.dma('pool', dstd[s, :, ts_], ro[:, :], reads=[ro], writes=[dstd])
                p.dma('sp', vf[:, :], zT[base + 256:base + 384, ts_], reads=[zT], writes=[vf])
                p.copy('act', vb, vb[:, :], vf, vf[:, :])
                for (srcb, dstd) in [(rot[1], io['ktok']), (vb, io['vtok'])]:
                    P_ = ptr[n % 2]; tk = tok[n % 2]; n += 1
                    for c4 in range(4):
                        p.tr(P_, P_[:, c4 * 128:(c4 + 1) * 128], srcb, srcb[:, c4 * 128:(c4 + 1) * 128], ident, ident[:, :])
                    p.act(tk, tk[:, :, :], P_, P_[:, :].rearrange("p (c d) -> p c d", c=4), AF.Copy)
                    p.dma('pool', dstd[s, tt * 4:(tt + 1) * 4].rearrange("c j d -> j c d"), tk[:, :, :], reads=[tk], writes=[dstd])
    with p.stage() as st:
        rc = st.sb("rc", [128, RC_N])
        p.dma('sp', rc[:, :], io['rconst'][:, :], reads=[io['rconst']], writes=[rc])
        Sf = [st.sb(f"Sb{s}", [128, 128]) for s in range(2)]
        Sb = [[st.sb(f"Sbb{s}_{i}", [128, 128], BF16) for i in range(2)] for s in range(2)]
        kt = [[st.sb(f"kt{s}_{i}", [128, 4, 128], BF16) for i in range(2)] for s in range(2)]
        vt = [[st.sb(f"vt{s}_{i}", [128, 4, 128], BF16) for i in range(2)] for s in range(2)]
        kd = [[st.sb(f"kd{s}_{i}", [128, 4, 128], BF16) for i in range(2)] for s in range(2)]
        pkv = [st.ps(f"pkv{i}", [128, 128]) for i in range(2)]
        for s in range(2):
            p.memset('dve', Sf[s], Sf[s][:, :], 0.0)
            p.memset('pool', Sb[s][0], Sb[s][0][:, :], 0.0)
            p.memset('pool', Sb[s][1], Sb[s][1][:, :], 0.0)
        for tt in range(NTT - 1, -1, -1):
            for s in range(2):
                o = 2 + s * RC_SLOT
                k_ = kt[s][tt % 2]; v_ = vt[s][tt % 2]; d_ = kd[s][tt % 2]
                p.dma('sp', k_[:, :, :], io['ktok'][s, tt * 4:(tt + 1) * 4].rearrange("c j d -> j c d"), reads=[io['ktok']], writes=[k_])
                p.dma('sp', v_[:, :, :], io['vtok'][s, tt * 4:(tt + 1) * 4].rearrange("c j d -> j c d"), reads=[io['vtok']], writes=[v_])
                p.act(d_, d_[:, :, :], k_, k_[:, :, :], AF.Identity, scale=rc[:, o + 1:o + 2], extra_reads=[rc])
                for c4 in range(3, -1, -1):
                    c = tt * 4 + c4
                    cur = Sb[s][c % 2]; nxt = Sb[s][(c + 1) % 2]
                    p.dma('pool', io['sbd'][s, c], cur[:, :], reads=[cur], writes=[io['sbd']])
                    if c == 0: continue
                    P_ = pkv[s]
                    p.mm(P_, P_[:, :], d_, d_[:, c4, :], v_, v_[:, c4, :])
                    p.stt(Sf[s], Sf[s][:, :], Sf[s], Sf[s][:, :], rc[:, o + 2:o + 3], P_, P_[:, :], ALU.mult, ALU.add, extra_reads=[rc])
                    p.act(nxt, nxt[:, :], Sf[s], Sf[s][:, :], AF.Copy)
    with p.stage() as st:
        rc = st.sb("rc", [128, RC_N])
        p.dma('sp', rc[:, :], io['rconst'][:, :], reads=[io['rconst']], writes=[rc])
        ones = st.sb("ones", [128, 128]); p.memset('dve', ones, ones[:, :], 1.0)
        Sf = [st.sb(f"Sf{s}", [128, 128]) for s in range(2)]
        Sfb = [[st.sb(f"Sfb{s}_{i}", [128, 128], BF16) for i in range(2)] for s in range(2)]
        bufs = {}
        for s in range(2):
            for nm, shp, dt in [('q', [128, 512], BF16), ('k', [128, 512], BF16), ('kt', [128, 4, 128], BF16), ('vt', [128, 4, 128], BF16),
                                ('sb', [128, 4, 128], BF16), ('g', [128, 512], F32), ('qf', [128, 512], BF16), ('qb', [128, 512], BF16),
                                ('kd', [128, 4, 128], BF16), ('scm', [128, 512], BF16), ('sq', [128, 512], F32), ('rs', [128, 512], F32),
                                ('sg', [128, 512], F32), ('t', [128, 512], F32), ('o', [128, 512], BF16)]:
                bufs[(s, nm)] = st.sb(f"r3{nm}{s}", shp, dt)
        psc = [st.ps(f"psc{i}", [128, 128]) for i in range(2)]
        pyT = [st.ps(f"pyT{i}", [128, 512]) for i in range(2)]
        pkv = [st.ps(f"pkv3{i}", [128, 128]) for i in range(2)]
        pss = st.ps("pss", [128, 512])
        for s in range(2):
            p.memset('dve', Sf[s], Sf[s][:, :], 0.0)
            p.memset('pool', Sfb[s][0], Sfb[s][0][:, :], 0.0)
        for tt in range(NTT):
            ts_ = slice(tt * 512, (tt + 1) * 512)
            r = (tt * 512) // Q4; off = tt * 512 - r * Q4
            for s in range(2):
                o = 2 + s * RC_SLOT
                B = lambda nm: bufs[(s, nm)]
                p.dma('sp', B('q')[:, :], io['qrT'][s, :, ts_], reads=[io['qrT']], writes=[B('q')])
                p.dma('sp', B('k')[:, :], io['krT'][s, :, ts_], reads=[io['krT']], writes=[B('k')])
                p.dma('sp', B('kt')[:, :, :], io['ktok'][s, tt * 4:(tt + 1) * 4].rearrange("c j d -> j c d"), reads=[io['ktok']], writes=[B('kt')])
                p.dma('sp', B('vt')[:, :, :], io['vtok'][s, tt * 4:(tt + 1) * 4].rearrange("c j d -> j c d"), reads=[io['vtok']], writes=[B('vt')])
                p.dma('sp', B('sb')[:, :, :], io['sbd'][s, tt * 4:(tt + 1) * 4].rearrange("c d e -> d c e"), reads=[io['sbd']], writes=[B('sb')])
                p.dma('sp', B('g')[:, :], zT[s * 512 + 384:s * 512 + 512, ts_], reads=[zT], writes=[B('g')])
                p.tt('pool', B('qf'), B('qf')[:, :], B('q'), B('q')[:, :], rc, rc[:, o + 3 + 128:o + 3 + 128 + 512], ALU.mult)
                p.tt('dve', B('qb'), B('qb')[:, :], B('q'), B('q')[:, :], rc, rc[:, o + 3 + 640:o + 3 + 640 + 512], ALU.mult)
                p.act(B('kd'), B('kd')[:, :, :], B('kt'), B('kt')[:, :, :], AF.Identity, scale=rc[:, o:o + 1], extra_reads=[rc])
                p.act(B('sg'), B('sg')[:, :], B('g'), B('g')[:, :], AF.Silu)
                Y = pyT[s]
                for c4 in range(4):
                    c = tt * 4 + c4
                    cs = slice(c4 * 128, (c4 + 1) * 128)
                    cur = Sfb[s][c % 2]; nxt = Sfb[s][(c + 1) % 2]
                    SC = psc[c % 2]
                    p.mm(SC, SC[:, :], B('k'), B('k')[:, cs], B('q'), B('q')[:, cs])
                    p.tt('dve', B('scm'), B('scm')[:, cs], SC, SC[:, :], rc, rc[:, o + 3:o + 3 + 128], ALU.mult)
                    p.mm(Y, Y[:, cs], B('vt'), B('vt')[:, c4, :], B('scm'), B('scm')[:, cs], start=True, stop=False)
                    p.mm(Y, Y[:, cs], cur, cur[:, :], B('qf'), B('qf')[:, cs], start=False, stop=False)
                    p.mm(Y, Y[:, cs], B('sb'), B('sb')[:, c4, :], B('qb'), B('qb')[:, cs], start=False, stop=True)
                    if c < NCK - 1:
                        P_ = pkv[s]
                        p.mm(P_, P_[:, :], B('kd'), B('kd')[:, c4, :], B('vt'), B('vt')[:, c4, :])
                        p.stt(Sf[s], Sf[s][:, :], Sf[s], Sf[s][:, :], rc[:, o + 2:o + 3], P_, P_[:, :], ALU.mult, ALU.add, extra_reads=[rc])
                        p.act(nxt, nxt[:, :], Sf[s], Sf[s][:, :], AF.Copy)
                p.act(B('sq'), B('sq')[:, :], Y, Y[:, :], AF.Square)
                p.mm(pss, pss[:, :], ones, ones[:, :], B('sq'), B('sq')[:, :])
                p.act(B('rs'), B('rs')[:, :], pss, pss[:, :], AF.Sqrt, bias=1e-6, scale=1.0 / 128)
                p.op('dve', lambda: nc.vector.reciprocal(out=B('rs')[:, :], in_=B('rs')[:, :]), reads=[B('rs')], writes=[B('rs')])
                p.tt('dve', B('t'), B('t')[:, :], Y, Y[:, :], B('rs'), B('rs')[:, :], ALU.mult)
                p.tt('pool', B('o'), B('o')[:, :], B('t'), B('t')[:, :], B('sg'), B('sg')[:, :], ALU.mult)
                p.dma('pool', io['yT'][r, s * 128:(s + 1) * 128, off:off + 512], B('o')[:, :], reads=[B('o')], writes=[io['yT']])

ZS5 = 1984
def sin_reduce(p, out, src, shift, ki, kf, tmp, scale=1.0, extra_reads=()):
    sl = tuple(slice(None) for _ in src.ap.shape)
    if shift != 0.0:
        p.ts('pool', tmp, tmp[sl], src, src[sl], shift, None, ALU.add)
        s = tmp
    else:
        s = src
    p.ts('dve', ki, ki[sl], s, s[sl], INV2PI, None, ALU.mult)
    p.copy('pool', kf, kf[sl], ki, ki[sl])
    p.stt(tmp, tmp[sl], kf, kf[sl], -C1, s, s[sl], ALU.mult, ALU.add)
    p.stt(tmp, tmp[sl], kf, kf[sl], -C2, tmp, tmp[sl], ALU.mult, ALU.add)
    p.ts('pool', tmp, tmp[sl], tmp, tmp[sl], -PI, PI, ALU.max, ALU.min)
    p.act(out, out[sl], tmp, tmp[sl], AF.Sin, scale=scale, extra_reads=extra_reads)

def s5_host_layout(q, lam_re, lam_im, log_dt, b_re, b_im, c_re, c_im, d_skip):
    import numpy as np
    gs = slice(8 * q, 8 * q + 8)
    lr = lam_re[:, gs, :]; li = lam_im[:, gs, :]; ld = log_dt[:, gs]
    rows = np.stack([lr.reshape(-1), li.reshape(-1), np.repeat(ld.reshape(-1), 64)], 0).astype(np.float32)
    def col(a):
        return np.ascontiguousarray(a.reshape(2, 4, 2, 64).transpose(2, 3, 0, 1).reshape(128, 8))
    cols = np.concatenate([col(lr), col(li), col(np.repeat(ld[:, :, None], 64, axis=2))], 1).astype(np.float32)
    bl = np.zeros((2, 128, 2, 4, 128), np.float32)
    cl = np.zeros((2, 128, 2, 4, 128), np.float32)
    for d in range(2):
        for g in range(8):
            j, gp = g // 2, g % 2
            for ri, (bsrc, csrc) in enumerate([(b_re, c_re), (b_im, c_im)]):
                bl[ri, g * 16:(g + 1) * 16, d, j, gp * 64:(gp + 1) * 64] = bsrc[d, 8 * q + g].T
                cl[ri, gp * 64:(gp + 1) * 64, d, j, g * 16:(g + 1) * 16] = csrc[d, 8 * q + g].T
    dcol = d_skip[128 * q:128 * (q + 1)].reshape(128, 1).astype(np.float32)
    return dict(s5rows=rows, s5cols=cols, s5bl=bl.reshape(2, 128, 1024), s5cl=cl.reshape(2, 128, 1024), s5d=dcol,
                s5iota=np.tile(np.arange(512, dtype=np.float32)[None, :], (128, 1)))

def stage_s5(p, S, io):
    nc = p.nc
    NTT = S // 512; Q4 = S // 4; TC = 512
    zT = io['zT']
    with p.stage() as st:
        bb = [st.sb(f"bb{i}", [128, 1024], BF16) for i in range(2)]
        cc = [st.sb(f"cc{i}", [128, 1024], BF16) for i in range(2)]
        rho = st.sb("rho", [128, 8]); cT = st.sb("cT", [128, 8]); sT = st.sb("sT", [128, 8]); thc = st.sb("thc", [128, 8])
        dcol = st.sb("dcol", [128, 1])
        p.dma('sp', dcol[:, :], io['s5d'][:, :], reads=[io['s5d']], writes=[dcol])
        with p.stage() as s2:
            f = lambda nm: s2.sb(nm, [128, 1024])
            lr, li, ld, dt, mag, th, sn, cs, t1, t2, t3, kf, tmp, cr, ci = [f(n) for n in
                ['lr', 'li', 'ld', 'dt', 'mag', 'th', 'sn', 'cs', 't1', 't2', 't3', 'kf', 'tmp', 'cr', 'ci']]
            ki = s2.sb("ki", [128, 1024], I32)
            for k, t in enumerate([lr, li, ld]):
                p.dma('sp', t[:, :], io['s5rows'][k:k + 1, :].partition_broadcast(128), reads=[io['s5rows']], writes=[t])
            p.act(dt, dt[:, :], ld, ld[:, :], AF.Exp)
            p.tt('dve', t1, t1[:, :], lr, lr[:, :], dt, dt[:, :], ALU.mult)
            p.act(mag, mag[:, :], t1, t1[:, :], AF.Exp)
            p.tt('dve', th, th[:, :], li, li[:, :], dt, dt[:, :], ALU.mult)
            sin_reduce(p, sn, th, 0.0, ki, kf, tmp)
            sin_reduce(p, cs, th, PI / 2, ki, kf, tmp)
            p.tt('dve', cs, cs[:, :], cs, cs[:, :], mag, mag[:, :], ALU.mult)
            p.tt('dve', sn, sn[:, :], sn, sn[:, :], mag, mag[:, :], ALU.mult)
            p.ts('dve', cs, cs[:, :], cs, cs[:, :], -1.0, None, ALU.add)
            p.tt('dve', t1, t1[:, :], lr, lr[:, :], lr, lr[:, :], ALU.mult)
            p.tt('dve', t2, t2[:, :], li, li[:, :], li, li[:, :], ALU.mult)
            p.tt('dve', t1, t1[:, :], t1, t1[:, :], t2, t2[:, :], ALU.add)
            p.op('dve', lambda: nc.vector.reciprocal(out=t1[:, :], in_=t1[:, :]), reads=[t1], writes=[t1])
            p.tt('dve', t2, t2[:, :], cs, cs[:, :], lr, lr[:, :], ALU.mult)
            p.tt('dve', t3, t3[:, :], sn, sn[:, :], li, li[:, :], ALU.mult)
            p.tt('dve', t2, t2[:, :], t2, t2[:, :], t3, t3[:, :], ALU.add)
            p.tt('dve', cr, cr[:, :], t2, t2[:, :], t1, t1[:, :], ALU.mult)
            p.tt('dve', t2, t2[:, :], sn, sn[:, :], lr, lr[:, :], ALU.mult)
            p.tt('dve', t3, t3[:, :], cs, cs[:, :], li, li[:, :], ALU.mult)
            p.tt('dve', t2, t2[:, :], t2, t2[:, :], t3, t3[:, :], ALU.subtract)
            p.tt('dve', ci, ci[:, :], t2, t2[:, :], t1, t1[:, :], ALU.mult)
            br = lr; bi = li
            p.dma('sp', br[:, :], io['s5bl'][0], reads=[io['s5bl']], writes=[br])
            p.dma('sp', bi[:, :], io['s5bl'][1], reads=[io['s5bl']], writes=[bi])
            p.tt('dve', t1, t1[:, :], cr, cr[:, :], br, br[:, :], ALU.mult)
            p.tt('dve', t2, t2[:, :], ci, ci[:, :], bi, bi[:, :], ALU.mult)
            p.tt('dve', bb[0], bb[0][:, :], t1, t1[:, :], t2, t2[:, :], ALU.subtract)
            p.tt('dve', t1, t1[:, :], cr, cr[:, :], bi, bi[:, :], ALU.mult)
            p.tt('dve', t2, t2[:, :], ci, ci[:, :], br, br[:, :], ALU.mult)
            p.tt('dve', bb[1], bb[1][:, :], t1, t1[:, :], t2, t2[:, :], ALU.add)
            p.dma('sp', t1[:, :], io['s5cl'][0], reads=[io['s5cl']], writes=[t1])
            p.dma('sp', t2[:, :], io['s5cl'][1], reads=[io['s5cl']], writes=[t2])
            p.copy('dve', cc[0], cc[0][:, :], t1, t1[:, :])
            p.ts('dve', cc[1], cc[1][:, :], t2, t2[:, :], -1.0, None, ALU.mult)
            c24 = s2.sb("c24", [128, 24]); dtc = s2.sb("dtc", [128, 8]); tq = s2.sb("tq", [128, 8]); tq2 = s2.sb("tq2", [128, 8])
            ki8 = s2.sb("ki8", [128, 8], I32); kf8 = s2.sb("kf8", [128, 8]); tmp8 = s2.sb("tmp8", [128, 8])
            p.dma('sp', c24[:, :], io['s5cols'][:, :], reads=[io['s5cols']], writes=[c24])
            p.act(dtc, dtc[:, :], c24, c24[:, 16:24], AF.Exp)
            p.tt('dve', tq, tq[:, :], c24, c24[:, 0:8], dtc, dtc[:, :], ALU.mult)
            p.act(rho, rho[:, :], tq, tq[:, :], AF.Exp)
            p.tt('dve', thc, thc[:, :], c24, c24[:, 8:16], dtc, dtc[:, :], ALU.mult)
            p.ts('dve', tq2, tq2[:, :], thc, thc[:, :], float(TC), None, ALU.mult)
            sin_reduce(p, sT, tq2, 0.0, ki8, kf8, tmp8)
            sin_reduce(p, cT, tq2, PI / 2, ki8, kf8, tmp8)
        iota = st.sb("iota", [128, TC]); p.dma('sp', iota[:, :], io['s5iota'][:, :], reads=[io['s5iota']], writes=[iota])
        cosT = [st.sb(f"cost{k}", [128, TC]) for k in range(8)]; sinT = [st.sb(f"sint{k}", [128, TC]) for k in range(8)]
        rhoT = [st.sb(f"rhot{k}", [128, TC]) for k in range(8)]
        ang = st.sb("ang", [128, TC]); kiT = st.sb("kiT", [128, TC], I32); kfT = st.sb("kfT", [128, TC]); tmpT = st.sb("tmpT", [128, TC])
        tb = st.sb("tb", [128, TC])
        for k in range(8):
            p.ts('dve', ang, ang[:, :], iota, iota[:, :], thc[:, k:k + 1], None, ALU.mult, extra_reads=[thc])
            for (shift, dst) in [(0.0, sinT[k]), (PI / 2, cosT[k])]:
                if k < 4:
                    sin_reduce(p, dst, ang, shift, kiT, kfT, tmpT)
                else:
                    sin_reduce(p, tb, ang, shift, kiT, kfT, tmpT)
                    p.copy('dve', dst, dst[:, :], tb, tb[:, ::-1])
            p.ts('dve', rhoT[k], rhoT[k][:, :], iota, iota[:, :], 0.0, rho[:, k:k + 1], ALU.mult, ALU.add, extra_reads=[rho])
        uf = [st.sb(f"uf{i}", [128, TC]) for i in range(2)]; ub = [st.sb(f"ub{i}", [128, TC], BF16) for i in range(2)]
        brs = [st.sb(f"brs{i}", [128, TC]) for i in range(2)]; bis = [st.sb(f"bis{i}", [128, TC]) for i in range(2)]
        T1 = [st.sb(f"t1_{i}", [128, TC]) for i in range(2)]; T2 = [st.sb(f"t2_{i}", [128, TC]) for i in range(2)]
        T3 = [st.sb(f"t3_{i}", [128, TC]) for i in range(2)]; T4 = [st.sb(f"t4_{i}", [128, TC]) for i in range(2)]
        MRE = [st.sb(f"mre{i}", [128, TC]) for i in range(2)]; MIM = [st.sb(f"mim{i}", [128, TC]) for i in range(2)]
        TN = [st.sb(f"tn{i}", [128, 4]) for i in range(2)]
        xr = [st.sb(f"xr{i}", [128, TC]) for i in range(2)]; xi = [st.sb(f"xi{i}", [128, TC]) for i in range(2)]
        xre = [st.sb(f"xre{i}", [128, TC], BF16) for i in range(2)]; xim = [st.sb(f"xim{i}", [128, TC], BF16) for i in range(2)]
        init = st.sb("init", [128, 16]); p.memset('dve', init, init[:, :], 0.0)
        tn = st.sb("tn", [128, 4])
        yo = [st.sb(f"yo{i}", [128, TC]) for i in range(2)]
        yfl = st.sb("yfl", [128, TC]); yb16 = [st.sb(f"yb16{i}", [128, TC], BF16) for i in range(2)]
        pb_re = [st.ps(f"pbre{i}", [128, TC]) for i in range(2)]; pb_im = [st.ps(f"pbim{i}", [128, TC]) for i in range(2)]
        py = [st.ps(f"py{i}", [128, TC]) for i in range(2)]
        n = 0
        for d in range(2):
            order = range(NTT) if d == 0 else range(NTT - 1, -1, -1)
            for it, tt in enumerate(order):
                ts_ = slice(tt * TC, (tt + 1) * TC)
                r = (tt * TC) // Q4; off = tt * TC - r * Q4
                u_f = uf[it % 2]; u_b = ub[it % 2]
                p.dma('sp', u_f[:, :], zT[ZS5:ZS5 + 128, ts_], reads=[zT], writes=[u_f])
                p.copy('pool', u_b, u_b[:, :], u_f, u_f[:, :])
                Y = py[it % 2]
                if d == 0:
                    vw = lambda a: a[:, :]
                    last = slice(TC - 1, TC)
                else:
                    vw = lambda a: a[:, ::-1]
                    last = slice(0, 1)
                for jp in ((0, 1), (2, 3)):
                    ks_ = [d * 4 + j for j in jp]
                    for u2, k in enumerate(ks_):
                        blk = slice(k * 128, (k + 1) * 128)
                        p.mm(pb_re[u2], pb_re[u2][:, :], bb[0], bb[0][:, blk], u_b, u_b[:, :])
                        p.mm(pb_im[u2], pb_im[u2][:, :], bb[1], bb[1][:, blk], u_b, u_b[:, :])
                        p.act(brs[u2], brs[u2][:, :], pb_re[u2], pb_re[u2][:, :], AF.Copy)
                        p.act(bis[u2], bis[u2][:, :], pb_im[u2], pb_im[u2][:, :], AF.Copy)
                    for u2, k in enumerate(ks_):
                        p.tt('dve', T1[u2], T1[u2][:, :], brs[u2], brs[u2][:, :], cosT[k], cosT[k][:, :], ALU.mult)
                        p.tt('pool', T2[u2], T2[u2][:, :], bis[u2], bis[u2][:, :], sinT[k], sinT[k][:, :], ALU.mult)
                        p.tt('dve', T3[u2], T3[u2][:, :], bis[u2], bis[u2][:, :], cosT[k], cosT[k][:, :], ALU.mult)
                        p.tt('pool', T4[u2], T4[u2][:, :], brs[u2], brs[u2][:, :], sinT[k], sinT[k][:, :], ALU.mult)
                    for u2, k in enumerate(ks_):
                        p.tt('dve', MRE[u2], MRE[u2][:, :], T1[u2], T1[u2][:, :], T2[u2], T2[u2][:, :], ALU.add)
                        p.tt('pool', MIM[u2], MIM[u2][:, :], T3[u2], T3[u2][:, :], T4[u2], T4[u2][:, :], ALU.subtract)
                    for u2, k in enumerate(ks_):
                        x_r = xr[u2]; x_i = xi[u2]; mre_ = MRE[u2]; mim_ = MIM[u2]
                        p.op('dve', lambda: nc.vector.tensor_tensor_scan(out=vw(x_r), data0=rhoT[k][:, :], data1=vw(mre_), initial=init[:, k:k + 1],
                                                                          op0=ALU.mult, op1=ALU.add), reads=[rhoT[k], mre_, init], writes=[x_r])
                        p.op('dve', lambda: nc.vector.tensor_tensor_scan(out=vw(x_i), data0=rhoT[k][:, :], data1=vw(mim_), initial=init[:, 8 + k:9 + k],
                                                                          op0=ALU.mult, op1=ALU.add), reads=[rhoT[k], mim_, init], writes=[x_i])
                    for u2, k in enumerate(ks_):
                        x_r = xr[u2]; x_i = xi[u2]; tn_ = TN[u2]
                        p.ts('pool', tn_, tn_[:, 0:1], x_r, x_r[:, last], cT[:, k:k + 1], None, ALU.mult, extra_reads=[cT])
                        p.ts('pool', tn_, tn_[:, 1:2], x_r, x_r[:, last], sT[:, k:k + 1], None, ALU.mult, extra_reads=[sT])
                        p.stt(tn_, tn_[:, 2:3], x_i, x_i[:, last], sT[:, k:k + 1], tn_, tn_[:, 0:1], ALU.mult, ALU.subtract, extra_reads=[sT])
                        p.ts('pool', init, init[:, k:k + 1], tn_, tn_[:, 2:3], -1.0, None, ALU.mult)
                        p.stt(init, init[:, 8 + k:9 + k], x_i, x_i[:, last], cT[:, k:k + 1], tn_, tn_[:, 1:2], ALU.mult, ALU.add, extra_reads=[cT])
                    for u2, k in enumerate(ks_):
                        x_r = xr[u2]; x_i = xi[u2]
                        p.tt('dve', T1[u2], T1[u2][:, :], x_r, x_r[:, :], cosT[k], cosT[k][:, :], ALU.mult)
                        p.tt('pool', T2[u2], T2[u2][:, :], x_i, x_i[:, :], sinT[k], sinT[k][:, :], ALU.mult)
                        p.tt('dve', T3[u2], T3[u2][:, :], x_r, x_r[:, :], sinT[k], sinT[k][:, :], ALU.mult)
                        p.tt('dve', T4[u2], T4[u2][:, :], x_i, x_i[:, :], cosT[k], cosT[k][:, :], ALU.mult)
                    for u2, k in enumerate(ks_):
                        p.tt('dve', xre[u2], xre[u2][:, :], T1[u2], T1[u2][:, :], T2[u2], T2[u2][:, :], ALU.subtract)
                        p.tt('pool', xim[u2], xim[u2][:, :], T3[u2], T3[u2][:, :], T4[u2], T4[u2][:, :], ALU.add)
                    for u2, k in enumerate(ks_):
                        blk = slice(k * 128, (k + 1) * 128)
                        j = jp[u2]
                        p.mm(Y, Y[:, :], cc[0], cc[0][:, blk], xre[u2], xre[u2][:, :], start=(j == 0), stop=False)
                        p.mm(Y, Y[:, :], cc[1], cc[1][:, blk], xim[u2], xim[u2][:, :], start=False, stop=(j == 3))
                if d == 0:
                    y_o = yo[it % 2]
                    p.act(y_o, y_o[:, :], Y, Y[:, :], AF.Copy)
                    p.dma('pool', io['yf'][:, ts_], y_o[:, :], reads=[y_o], writes=[io['yf']])
                else:
                    y_o = yo[it % 2]; yb = yb16[it % 2]
                    p.dma('sp', yfl[:, :], io['yf'][:, ts_], reads=[io['yf']], writes=[yfl])
                    p.tt('dve', y_o, y_o[:, :], Y, Y[:, :], yfl, yfl[:, :], ALU.add)
                    p.stt(y_o, y_o[:, :], u_f, u_f[:, :], dcol[:, 0:1], y_o, y_o[:, :], ALU.mult, ALU.add, extra_reads=[dcol])
                    p.act(yb, yb[:, :], y_o, y_o[:, :], AF.Gelu_apprx_tanh)
                    p.dma('pool', io['yT'][r, 448:576, off:off + TC], yb[:, :], reads=[yb], writes=[io['yT']])

ZR, ZK, ZV, ZWD, ZAD, ZGD = 1024, 1216, 1408, 1600, 1728, 1856
RW_SU, RW_SL, RW_U, RW_L, RW_I, RW_BLK, RW_MF, RW_MB, RW_N = 0, 768, 1536, 2304, 3072, 3840, 3968, 4480, 4992
NEG_EXP_HALF = -math.exp(-0.5)

def rwkv_consts():
    import numpy as np
    i = np.arange(128)
    su = (i[:, None] < i[None, :]).astype(np.float32); sl = su.T.copy()
    u = (i[:, None] <= i[None, :]).astype(np.float32); l = u.T.copy()
    I = np.eye(128, dtype=np.float32)
    blk = np.zeros((128, 128), np.float32); blk[:64, :64] = 1; blk[64:, 64:] = 1
    t = np.arange(512)
    mf = (t % 128 != 0).astype(np.float32); mb = (t % 128 != 127).astype(np.float32)
    return np.concatenate([np.tile(su, (1, 6)), np.tile(sl, (1, 6)), np.tile(u, (1, 6)), np.tile(l, (1, 6)), np.tile(I, (1, 6)), blk,
                           np.tile(mf[None], (128, 1)), np.tile(mb[None], (128, 1))], axis=1).astype(np.float32)

def rwkv_host_layout(q, mu_prev, mu_next, w0, w_up, a0, a_up, g_up, k_k, k_a, r_k, lnx_w, lnx_b):
    import numpy as np
    rwp = np.zeros((128, 55), np.float32)
    rkf = r_k.reshape(-1)
    for hh in range(3):
        ch = q * 192 + hh * 64 + np.arange(64)
        b = hh * 15
        for j, part in enumerate([0, 768, 1536]):
            rwp[:64, b + 2 * j] = mu_prev[part + ch]; rwp[:64, b + 2 * j + 1] = mu_next[part + ch]
        rwp[:64, b + 6] = k_k[ch]; rwp[:64, b + 7] = k_a[ch]; rwp[:64, b + 8] = rkf[ch]; rwp[:64, b + 9] = lnx_w[ch]; rwp[:64, b + 10] = lnx_b[ch]
        rwp[:64, b + 11] = w0[0, ch]; rwp[:64, b + 12] = w0[1, ch]; rwp[:64, b + 13] = a0[0, ch]; rwp[:64, b + 14] = a0[1, ch]
    for j, part in enumerate([2304, 2432]):
        for d in range(2):
            rwp[:64, 45 + 4 * j + 2 * d] = mu_prev[part + d * 64:part + (d + 1) * 64]
            rwp[:64, 46 + 4 * j + 2 * d] = mu_next[part + d * 64:part + (d + 1) * 64]
    rwp[:, 53] = mu_prev[2560:2688]; rwp[:, 54] = mu_next[2560:2688]
    cs = slice(q * 192, (q + 1) * 192)
    return dict(rwp=rwp, rw_wup=np.ascontiguousarray(np.stack([w_up[0][:, cs], w_up[1][:, cs]], 1)),
                rw_aup=np.ascontiguousarray(np.stack([a_up[0][:, cs], a_up[1][:, cs]], 1)),
                rw_gup=np.ascontiguousarray(g_up[:, cs]))

RW_DEBUG = [None]
def stage_rwkv(p, S, io):
    nc = p.nc
    NTT = S // 512; Q4 = S // 4
    zT = io['zT']
    H3 = range(3)
    with p.stage() as st:
        rwp = st.sb("rwp", [128, 55]); p.dma('sp', rwp[:, :], io['rwp'][:, :], reads=[io['rwp']], writes=[rwp])
        cst = st.sb("rwc", [128, RW_N]); p.dma('sp', cst[:, :], io['rwconst'][:, :], reads=[io['rwconst']], writes=[cst])
        ident_f = st.sb("ident_f", [128, 128]); ident = st.sb("ident", [128, 128], BF16)
        p.dma('sp', ident_f[:, :], io['ident'][:, :], reads=[io['ident']], writes=[ident_f])
        p.copy('dve', ident, ident[:, :], ident_f, ident_f[:, :])
        wtmp = st.sb("wtmp", [128, 384])
        wup = st.sb("wupb", [64, 2, 192], BF16); aup = st.sb("aupb", [64, 2, 192], BF16); gup = st.sb("gupb", [128, 192], BF16)
        p.dma('sp', wtmp[0:64, :], io['rw_wup'].ap.rearrange("r d c -> r (d c)"), reads=[io['rw_wup']], writes=[wtmp])
        p.copy('dve', wup, wup[:, :, :], wtmp, wtmp[0:64, :].rearrange("r (d c) -> r d c", d=2))
        p.dma('sp', wtmp[0:64, :], io['rw_aup'].ap.rearrange("r d c -> r (d c)"), reads=[io['rw_aup']], writes=[wtmp])
        p.copy('dve', aup, aup[:, :, :], wtmp, wtmp[0:64, :].rearrange("r (d c) -> r d c", d=2))
        p.dma('sp', wtmp[:, 0:192], io['rw_gup'][:, :], reads=[io['rw_gup']], writes=[wtmp])
        p.copy('dve', gup, gup[:, :], wtmp, wtmp[:, 0:192])
        c0 = st.sb("c0", [128, 14])
        pairs = [0, 2, 4, 15, 17, 19, 30, 32, 34, 45, 47, 49, 51, 53]
        for i, col in enumerate(pairs):
            p.tt('dve', c0, c0[:, i:i + 1], rwp, rwp[:, col:col + 1], rwp, rwp[:, col + 1:col + 2], ALU.add)
        p.ts('dve', c0, c0[:, :], c0, c0[:, :], -1.0, 1.0, ALU.mult, ALU.add)
        def fb(nm, P_=64, n=512, dt=F32): return st.sb(nm, [P_, n], dt)
        zh = [fb(f"zh{i}", 128, 514) for i in range(3)]
        sh = {}
        for hh in H3:
            for nm in ['r', 'k', 'v']:
                sh[(hh, nm)] = fb(f"sh{nm}{hh}")
        shwd = fb("shwd"); shad = fb("shad"); shgd = fb("shgd", 128)
        twd = fb("twd", dt=BF16); adb = fb("adb", dt=BF16); sgd = fb("sgd", 128, dt=BF16)
        lw = [fb(f"lw{h}") for h in H3]; cin = [fb(f"cin{h}") for h in H3]; cex = [fb(f"cex{h}") for h in H3]
        Ein = [fb(f"Ein{h}") for h in H3]; Eex = [fb(f"Eex{h}") for h in H3]; Eni = [fb(f"Eni{h}") for h in H3]
        aT = [fb(f"aT{h}") for h in H3]; kk = [fb(f"kk{h}") for h in H3]
        tA = [fb(f"tA{h}") for h in H3]; tB = [fb(f"tB{h}") for h in H3]
        at_ = [fb(f"at{h}", dt=BF16) for h in H3]; bt_ = [fb(f"bt{h}", dt=BF16) for h in H3]
        kt_ = [fb(f"kt{h}", dt=BF16) for h in H3]; rt_ = [fb(f"rt{h}", dt=BF16) for h in H3]
        vb_ = [fb(f"vb{h}", dt=BF16) for h in H3]
        tok = [st.sb(f"tok{i}", [128, 4, 192], BF16) for i in range(4)]
        W3 = lambda nm, dt=BF16: st.sb(nm, [128, 768], dt)
        M = [W3(f"M{i}") for i in range(2)]; N = [W3(f"N{i}") for i in range(2)]
        Pm = [W3(f"P{i}") for i in range(2)]; Qm = [W3(f"Q{i}") for i in range(2)]
        AKT = W3("AKT"); RBT = W3("RBT"); RKT = W3("RKT")
        AKV = st.sb("AKV", [128, 384], BF16); UV = st.sb("UV", [128, 384]); U = st.sb("U", [128, 192], BF16)
        TAT = st.sb("TAT", [64, 768], BF16)
        KVW = st.sb("KVW", [64, 384])
        S0 = st.sb("S0", [64, 192])
        S0b = [st.sb(f"S0b{i}", [64, 192], BF16) for i in range(2)]
        tS = st.sb("tS", [64, 192])
        ydall = st.sb("ydall", [64, 3, 512]); y0l = [fb(f"y0l{h}") for h in H3]
        pp1 = [fb(f"pp1{h}") for h in H3]; pp2 = [fb(f"pp2{h}") for h in H3]; pp3 = [fb(f"pp3{h}") for h in H3]
        yob = [fb(f"yob{h}", dt=BF16) for h in H3]
        pg = [st.ps(f"pg{i}", [128, 1024]) for i in range(2)]
        pch = st.ps("pch", [128, 512])
        pY = [st.ps(f"pY{i}", [128, 512]) for i in range(2)]
        ptk = st.ps("ptk", [128, 1024], BF16)
        gcount = [0]
        def PG():
            gcount[0] += 1
            return pg[gcount[0] % 2]
        ones64 = cst[0:64, RW_BLK:RW_BLK + 64]
        def blocksum(src):
            G = PG()
            p.mm(G, G[0:64, 0:512], cst, ones64, src, src[0:64, :])
            return G

        for d in range(2):
            p.memset('dve', S0, S0[:, :], 0.0)
            p.memset('pool', S0b[0], S0b[0][:, :], 0.0)
            p.memset('pool', S0b[1], S0b[1][:, :], 0.0)
            order = list(range(NTT)) if d == 0 else list(range(NTT - 1, -1, -1))
            mSU = RW_SU if d == 0 else RW_SL; mSL = RW_SL if d == 0 else RW_SU; mU = RW_U if d == 0 else RW_L
            gchunk = 0
            for tt in order:
                t0 = tt * 512
                r_ = t0 // Q4; off = t0 - r_ * Q4
                hbc = [0]
                def shift(row0, P_, dst, c0col, mpcol):
                    z = zh[hbc[0] % 3]; hbc[0] += 1
                    lo = max(t0 - 1, 0); hi = min(t0 + 513, S)
                    if t0 == 0: p.memset('pool', z, z[0:P_, 0:1], 0.0)
                    if t0 + 513 > S: p.memset('pool', z, z[0:P_, 513:514], 0.0)
                    p.dma('sp', z[0:P_, lo - (t0 - 1):hi - (t0 - 1)], zT[row0:row0 + P_, lo:hi], reads=[zT], writes=[z])
                    p.act(dst, dst[0:P_, :], z, z[0:P_, 1:513], AF.Identity, scale=c0[0:P_, c0col:c0col + 1], extra_reads=[c0])
                    p.stt(dst, dst[0:P_, :], z, z[0:P_, 0:512], rwp[0:P_, mpcol:mpcol + 1], dst, dst[0:P_, :], ALU.mult, ALU.add, extra_reads=[rwp])
                    p.stt(dst, dst[0:P_, :], z, z[0:P_, 2:514], rwp[0:P_, mpcol + 1:mpcol + 2], dst, dst[0:P_, :], ALU.mult, ALU.add, extra_reads=[rwp])
                for hh in H3:
                    for j, (nm, zrow) in enumerate([('r', ZR), ('k', ZK), ('v', ZV)]):
                        shift(zrow + hh * 64, 64, sh[(hh, nm)], hh * 3 + j, hh * 15 + 2 * j)
                shift(ZWD + d * 64, 64, shwd, 9 + d, 45 + 2 * d)
                shift(ZAD + d * 64, 64, shad, 11 + d, 49 + 2 * d)
                p.act(twd, twd[:, :], shwd, shwd[:, :], AF.Tanh)
                p.copy('act', adb, adb[:, :], shad, shad[:, :])
                if d == 1:
                    shift(ZGD, 128, shgd, 13, 53)
                    p.act(sgd, sgd[:, :], shgd, shgd[:, :], AF.Sigmoid)
                for hh in H3:
                    b = hh * 15
                    cs_ = slice(hh * 64, (hh + 1) * 64)
                    G = PG()
                    p.mm(G, G[0:64, 0:512], wup, wup[:, d, cs_], twd, twd[:, :])
                    p.act(lw[hh], lw[hh][:, :], G, G[0:64, 0:512], AF.Sigmoid, bias=rwp[0:64, b + 11 + d:b + 12 + d], extra_reads=[rwp])
                    if d == 0:
                        p.op('dve', lambda: nc.vector.tensor_tensor_scan(out=cin[hh][:, :], data0=cst[0:64, RW_MF:RW_MF + 512], data1=lw[hh][:, :],
                                                                          initial=0.0, op0=ALU.mult, op1=ALU.add), reads=[cst, lw[hh]], writes=[cin[hh]])
                    else:
                        mbv = cst[0:64, RW_MB:RW_MB + 512]
                        p.op('dve', lambda: nc.vector.tensor_tensor_scan(out=cin[hh][:, ::-1], data0=mbv[:, ::-1], data1=lw[hh][:, ::-1],
                                                                          initial=0.0, op0=ALU.mult, op1=ALU.add), reads=[cst, lw[hh]], writes=[cin[hh]])
                    p.tt('dve', cex[hh], cex[hh][:, :], cin[hh], cin[hh][:, :], lw[hh], lw[hh][:, :], ALU.subtract)
                    p.act(Ein[hh], Ein[hh][:, :], cin[hh], cin[hh][:, :], AF.Exp, scale=NEG_EXP_HALF)
                    p.act(Eex[hh], Eex[hh][:, :], cex[hh], cex[hh][:, :], AF.Exp, scale=NEG_EXP_HALF)
                    p.act(Eni[hh], Eni[hh][:, :], cin[hh], cin[hh][:, :], AF.Exp, scale=-NEG_EXP_HALF)
                    G = PG()
                    p.mm(G, G[0:64, 0:512], aup, aup[:, d, cs_], adb, adb[:, :])
                    p.act(aT[hh], aT[hh][:, :], G, G[0:64, 0:512], AF.Sigmoid, bias=rwp[0:64, b + 13 + d:b + 14 + d], extra_reads=[rwp])
                    ks = sh[(hh, 'k')]
                    p.act(kk[hh], kk[hh][:, :], ks, ks[:, :], AF.Identity, scale=rwp[0:64, b + 6:b + 7], extra_reads=[rwp])
                    p.act(tA[hh], tA[hh][:, :], ks, ks[:, :], AF.Square, scale=rwp[0:64, b + 6:b + 7], extra_reads=[rwp])
                    G = blocksum(tA[hh])
                    p.act(tB[hh], tB[hh][:, :], G, G[0:64, 0:512], AF.Sqrt)
                    p.ts('dve', tB[hh], tB[hh][:, :], tB[hh], tB[hh][:, :], 1e-12, None, ALU.max)
                    p.op('dve', lambda: nc.vector.reciprocal(out=tB[hh][:, :], in_=tB[hh][:, :]), reads=[tB[hh]], writes=[tB[hh]])
                    p.tt('dve', kk[hh], kk[hh][:, :], kk[hh], kk[hh][:, :], tB[hh], tB[hh][:, :], ALU.mult)
                    p.stt(at_[hh], at_[hh][:, :], kk[hh], kk[hh][:, :], -1.0, Eex[hh], Eex[hh][:, :], ALU.mult, ALU.mult)
                    p.tt('dve', tA[hh], tA[hh][:, :], kk[hh], kk[hh][:, :], aT[hh], aT[hh][:, :], ALU.mult)
                    p.tt('pool', bt_[hh], bt_[hh][:, :], tA[hh], tA[hh][:, :], Eni[hh], Eni[hh][:, :], ALU.mult)
                    p.ts('dve', tB[hh], tB[hh][:, :], aT[hh], aT[hh][:, :], -1.0, rwp[0:64, b + 7:b + 8], ALU.add, ALU.mult, extra_reads=[rwp])
                    p.stt(tB[hh], tB[hh][:, :], tB[hh], tB[hh][:, :], 1.0, ks, ks[:, :], ALU.add, ALU.mult)
                    p.tt('dve', kt_[hh], kt_[hh][:, :], tB[hh], tB[hh][:, :], Eni[hh], Eni[hh][:, :], ALU.mult)
                    rs_ = sh[(hh, 'r')]
                    p.tt('pool', rt_[hh], rt_[hh][:, :], rs_, rs_[:, :], Ein[hh], Ein[hh][:, :], ALU.mult)
                    p.copy('act', vb_[hh], vb_[hh][:, :], sh[(hh, 'v')], sh[(hh, 'v')][:, :])
                if RW_DEBUG[0] == 'prep': return
                pairs = [(0, 1), (2, 3)] if d == 0 else [(3, 2), (1, 0)]
                HS = [slice(i * 128, (i + 1) * 128) for i in range(6)]
                VS = [slice(i * 64, (i + 1) * 64) for i in range(6)]
                for pr in pairs:
                    CS = [slice(c4 * 128, (c4 + 1) * 128) for c4 in pr]
                    tks = []
                    for ci, c4 in enumerate(pr):
                        tk = tok[(gchunk + ci) % 4]; tks.append(tk)
                        for j, srcs in enumerate([at_, bt_, kt_, vb_]):
                            for hh in H3:
                                p.tr(ptk, ptk[:, j * 192 + hh * 64:j * 192 + (hh + 1) * 64], srcs[hh], srcs[hh][:, CS[ci]], ident, ident[0:64, 0:64])
                        p.act(tk, tk[:, :, :], ptk, ptk[:, 0:768].rearrange("p (j c) -> p j c", j=4), AF.Copy)
                    BL = [(ci, hh) for ci in range(2) for hh in H3]
                    def gram(dst, A_, B_, mask):
                        G = PG()
                        for bi, (ci, hh) in enumerate(BL):
                            p.mm(G, G[:, HS[bi]], A_[hh], A_[hh][:, CS[ci]], B_[hh], B_[hh][:, CS[ci]])
                        p.tt('dve', dst, dst[:, :], G, G[:, 0:768], cst, cst[:, mask:mask + 768], ALU.mult)
                    gram(M[0], bt_, at_, mSU)
                    gram(N[0], at_, bt_, mSL)
                    p.tt('dve', Pm[0], Pm[0][:, :], M[0], M[0][:, :], cst, cst[:, RW_I:RW_I + 768], ALU.add)
                    p.tt('pool', Qm[0], Qm[0][:, :], N[0], N[0][:, :], cst, cst[:, RW_I:RW_I + 768], ALU.add)
                    gram(AKT, kt_, at_, mSU)
                    gram(RBT, bt_, rt_, mU)
                    gram(RKT, kt_, rt_, mU)
                    for lev in range(1, 7):
                        Mo, No = M[(lev - 1) % 2], N[(lev - 1) % 2]; Mn, Nn = M[lev % 2], N[lev % 2]
                        Po, Qo = Pm[(lev - 1) % 2], Qm[(lev - 1) % 2]; Pn, Qn = Pm[lev % 2], Qm[lev % 2]
                        GM = PG()
                        for bi in range(6):
                            p.mm(GM, GM[:, HS[bi]], No, No[:, HS[bi]], Mo, Mo[:, HS[bi]])
                        p.act(Mn, Mn[:, :], GM, GM[:, 0:768], AF.Copy)
                        if lev < 6:
                            GN = PG()
                            for bi in range(6):
                                p.mm(GN, GN[:, HS[bi]], Mo, Mo[:, HS[bi]], No, No[:, HS[bi]])
                            p.act(Nn, Nn[:, :], GN, GN[:, 0:768], AF.Copy)
                        GP = PG()
                        for bi in range(6):
                            p.mm(GP, GP[:, HS[bi]], Qo, Qo[:, HS[bi]], Mn, Mn[:, HS[bi]])
                        p.tt('dve', Pn, Pn[:, :], GP, GP[:, 0:768], Po, Po[:, :], ALU.add)
                        if lev < 6:
                            GQ = PG()
                            for bi in range(6):
                                p.mm(GQ, GQ[:, HS[bi]], Po, Po[:, HS[bi]], Nn, Nn[:, HS[bi]])
                            p.tt('dve', Qn, Qn[:, :], GQ, GQ[:, 0:768], Qo, Qo[:, :], ALU.add)
                    PT = Pm[0]
                    G = PG()
                    for bi, (ci, hh) in enumerate(BL):
                        p.mm(G, G[:, VS[bi]], AKT, AKT[:, HS[bi]], tks[ci], tks[ci][:, 3, VS[hh]])
                    p.act(AKV, AKV[:, :], G, G[:, 0:384], AF.Copy)
                    G = PG()
                    for bi, (ci, hh) in enumerate(BL):
                        p.mm(G, G[:, VS[bi]], PT, PT[:, HS[bi]], AKV, AKV[:, VS[bi]])
                    p.act(UV, UV[:, :], G, G[:, 0:384], AF.Copy)
                    G = PG()
                    for bi, (ci, hh) in enumerate(BL):
                        p.mm(G, G[0:64, HS[bi]], tks[ci], tks[ci][:, 0, VS[hh]], PT, PT[:, HS[bi]])
                    p.act(TAT, TAT[:, :], G, G[0:64, 0:768], AF.Copy)
                    G = PG()
                    for bi, (ci, hh) in enumerate(BL):
                        p.mm(G, G[0:64, VS[bi]], tks[ci], tks[ci][:, 2, VS[hh]], tks[ci], tks[ci][:, 3, VS[hh]])
                    for bi, (ci, hh) in enumerate(BL):
                        c4 = pr[ci]
                        widx = c4 * 128 + 127 if d == 0 else c4 * 128
                        p.ts('dve', KVW, KVW[:, VS[bi]], G, G[0:64, VS[bi]], Ein[hh][:, widx:widx + 1], None, ALU.mult, extra_reads=[Ein[hh]])
                    for ci, c4 in enumerate(pr):
                        cs = CS[ci]; tk = tks[ci]
                        widx = c4 * 128 + 127 if d == 0 else c4 * 128
                        cur = S0b[gchunk % 2]; nxt = S0b[(gchunk + 1) % 2]
                        Yp = pY[gchunk % 2]
                        gchunk += 1
                        for hh in H3:
                            p.mm(pch, pch[:, VS[hh]], TAT, TAT[:, HS[ci * 3 + hh]], cur, cur[:, VS[hh]])
                        p.tt('dve', U, U[:, :], pch, pch[:, 0:192], UV, UV[:, ci * 192:(ci + 1) * 192], ALU.add)
                        for hh in H3:
                            bi = ci * 3 + hh
                            p.mm(Yp, Yp[0:64, HS[hh]], cur, cur[:, VS[hh]], rt_[hh], rt_[hh][:, cs], start=True, stop=False)
                            p.mm(Yp, Yp[0:64, HS[hh]], U, U[:, VS[hh]], RBT, RBT[:, HS[bi]], start=False, stop=False)
                            p.mm(Yp, Yp[0:64, HS[hh]], tk, tk[:, 3, VS[hh]], RKT, RKT[:, HS[bi]], start=False, stop=True)
                        p.act(ydall, ydall[:, :, cs], Yp, Yp[0:64, 0:384].rearrange("p (h t) -> p h t", h=3), AF.Copy)
                        for hh in H3:
                            p.mm(pch, pch[0:64, 256 + hh * 64:256 + (hh + 1) * 64], tk, tk[:, 1, VS[hh]], U, U[:, VS[hh]])
                        p.tt('dve', tS, tS[:, :], pch, pch[0:64, 256:448], S0, S0[:, :], ALU.add)
                        for hh in H3:
                            p.stt(S0, S0[:, VS[hh]], tS, tS[:, VS[hh]], Ein[hh][:, widx:widx + 1], KVW, KVW[:, VS[ci * 3 + hh]], ALU.mult, ALU.add,
                                  extra_reads=[Ein[hh]])
                        p.act(nxt, nxt[:, :], S0, S0[:, :], AF.Copy)
                for hh in H3:
                    b = hh * 15
                    rows = slice(hh * 64, (hh + 1) * 64)
                    if d == 0:
                        p.dma('pool', io['y0'][rows, t0:t0 + 512], ydall[:, hh, :], reads=[ydall], writes=[io['y0']])
                    else:
                        p.dma('sp', y0l[hh][:, :], io['y0'][rows, t0:t0 + 512], reads=[io['y0']], writes=[y0l[hh]])
                        y = pp3[hh]
                        p.tt('dve', y, y[:, :], ydall, ydall[:, hh, :], y0l[hh], y0l[hh][:, :], ALU.add)
                        G1 = blocksum(y)
                        p.tt('pool', pp1[hh], pp1[hh][:, :], y, y[:, :], y, y[:, :], ALU.mult)
                        G2 = blocksum(pp1[hh])
                        mean = pp2[hh]; var = tA[hh]
                        p.act(mean, mean[:, :], G1, G1[0:64, 0:512], AF.Copy, scale=1.0 / 64)
                        p.tt('pool', pp1[hh], pp1[hh][:, :], mean, mean[:, :], mean, mean[:, :], ALU.mult)
                        p.stt(var, var[:, :], G2, G2[0:64, 0:512], 1.0 / 64, pp1[hh], pp1[hh][:, :], ALU.mult, ALU.subtract)
                        p.act(var, var[:, :], var, var[:, :], AF.Sqrt, bias=64e-5)
                        p.op('dve', lambda: nc.vector.reciprocal(out=var[:, :], in_=var[:, :]), reads=[var], writes=[var])
                        p.tt('dve', y, y[:, :], y, y[:, :], mean, mean[:, :], ALU.subtract)
                        p.tt('dve', y, y[:, :], y, y[:, :], var, var[:, :], ALU.mult)
                        p.ts('dve', y, y[:, :], y, y[:, :], rwp[0:64, b + 9:b + 10], rwp[0:64, b + 10:b + 11], ALU.mult, ALU.add, extra_reads=[rwp])
                        rs_ = sh[(hh, 'r')]; ks = sh[(hh, 'k')]; vs_ = sh[(hh, 'v')]
                        p.stt(pp1[hh], pp1[hh][:, :], rs_, rs_[:, :], rwp[0:64, b + 8:b + 9], ks, ks[:, :], ALU.mult, ALU.mult, extra_reads=[rwp])
                        G3 = blocksum(pp1[hh])
                        p.tt('dve', pp2[hh], pp2[hh][:, :], G3, G3[0:64, 0:512], vs_, vs_[:, :], ALU.mult)
                        p.tt('pool', y, y[:, :], y, y[:, :], pp2[hh], pp2[hh][:, :], ALU.add)
                        G4 = PG()
                        p.mm(G4, G4[0:64, 0:512], gup, gup[:, rows], sgd, sgd[:, :])
                        p.tt('dve', yob[hh], yob[hh][:, :], y, y[:, :], G4, G4[0:64, 0:512], ALU.mult)
                        p.dma('pool', io['yT'][r_, 256 + hh * 64:256 + (hh + 1) * 64, off:off + 512], yob[hh][:, :], reads=[yob[hh]], writes=[io['yT']])

ALPHA = float(2.0 ** 0.5)
LN_EPS = 1e-5

def out_chunks():
    ch = []
    for q in range(4):
        heads = [2 * q, 2 * q + 1] if q < 2 else [q + 2]
        for s, h in enumerate(heads):
            ch.append((q, s * 128, 128, h * 128))
        ch.append((q, 256, 128, 768 + q * 192))
        ch.append((q, 384, 64, 768 + q * 192 + 128))
        ch.append((q, 448, 128, 1536 + q * 128))
    return ch

def ln_tile(p, u, lnw, lnb, scr, outf, outb, pfx, eps=LN_EPS):
    nc = p.nc
    st6 = scr['st6']; mv = scr['mv']; rs = scr['rs']; nm = scr['nm']; xn = u
    for c in range(4):
        p.op('dve', lambda c=c: nc.vector.bn_stats(out=st6[:, c * 6:(c + 1) * 6], in_=u[:, c * 512:(c + 1) * 512]), reads=[u], writes=[st6])
    p.op('dve', lambda: nc.vector.bn_aggr(out=mv[:, 0:2], in_=st6[:, 0:24]), reads=[st6], writes=[mv])
    p.ts('dve', rs, rs[:, 0:1], mv, mv[:, 1:2], eps, None, ALU.add)
    p.act(rs, rs[:, 0:1], rs, rs[:, 0:1], AF.Sqrt)
    p.op('dve', lambda: nc.vector.reciprocal(out=rs[:, 0:1], in_=rs[:, 0:1]), reads=[rs], writes=[rs])
    p.ts('dve', nm, nm[:, 0:1], mv, mv[:, 0:1], rs[:, 0:1], -1.0, ALU.mult, ALU.mult, extra_reads=[rs])
    p.act(xn, xn[:, :], u, u[:, :], AF.Identity, bias=nm[:, 0:1], scale=rs[:, 0:1], extra_reads=[nm, rs])
    p.tt('dve', xn, xn[:, :], xn, xn[:, :], lnw, lnw[:, :], ALU.mult)
    p.tt('dve', outf, outf[:, :], xn, xn[:, :], lnb, lnb[:, :], ALU.add)
    if outb is not None:
        p.act(outb, outb[:, :], outf, outf[:, :], AF.Copy)

def ln_scratch(st, pfx):
    return dict(st6=st.sb(pfx + "st6", [128, 24]), mv=st.sb(pfx + "mv", [128, 2]), rs=st.sb(pfx + "rs", [128, 1]),
                nm=st.sb(pfx + "nm", [128, 1]))

def load_bc(p, st, name, src_ap, n, q='sp'):
    t = st.sb(name, [128, n])
    p.dma(q, t[:, :], src_ap.partition_broadcast(128), writes=[t])
    return t

def transpose_tile(p, xb, ident, pst, xT, tcol, evac_e='act'):
    for k in range(16):
        p.tr(pst, pst[:, k * 128:(k + 1) * 128], xb, xb[:, k * 128:(k + 1) * 128], ident, ident[:, :])
    src = pst[:, :].rearrange("p (k t) -> p k t", k=16)
    dst = xT[:, :, tcol:tcol + 128]
    if evac_e == 'act':
        p.act(xT, dst, pst, src, AF.Copy)
    else:
        p.copy(evac_e, xT, dst, pst, src)

def cast_weights(p, srcs, dsts, n_per):
    with p.stage() as st:
        CH = 4096
        stg = [st.sb(f"cw_s{i}", [128, CH], F32) for i in range(3)]
        ob = [st.sb(f"cw_o{i}", [128, CH], BF16) for i in range(3)]
        i = 0
        engs = ['dve', 'pool', 'act']
        for (sT, sap), (dT, dap) in zip(srcs, dsts):
            n = sap.shape[1]
            for c0 in range(0, n, CH):
                w = min(CH, n - c0)
                s = stg[i % 3]; o = ob[i % 3]
                p.dma('sp', s[:, 0:w], sap[:, c0:c0 + w], reads=[sT], writes=[s])
                p.copy(engs[i % 3], o, o[:, 0:w], s, s[:, 0:w])
                p.dma('pool', dap[:, c0:c0 + w], o[:, 0:w], reads=[o], writes=[dT])
                i += 1

def precast_dma(p, io):
    for nm in ['w1', 'w3', 'w2']:
        sT = io[nm]; dT = io[nm + 'b']
        sv = sT.ap.rearrange("e a b -> (e a b)").rearrange("(r c) -> r c", c=2048)
        dv = dT.ap.rearrange("e a b -> (e a b)").rearrange("(r c) -> r c", c=2048)
        for r0 in range(0, 8192, 4096):
            p.dma('pool', dv[r0:r0 + 4096, :], sv[r0:r0 + 4096, :], reads=[sT], writes=[dT])

def phase2(p, T_, io, layer_last=False, ST=512, precast=True, want_xoT=True):
    nc = p.nc
    NT = T_ // 128
    chunks = out_chunks()
    NCH = len(chunks)
    srcs = []; dsts = []
    for nm in ['w1', 'w3', 'w2']:
        s = io[nm]; d = io[nm + 'b']
        srcs.append((s, s.ap.rearrange("e a b -> (e a b)").rearrange("(p n) -> p n", p=128)))
        dsts.append((d, d.ap.rearrange("e a b -> (e a b)").rearrange("(p n) -> p n", p=128)))
    if precast:
        cast_weights(p, srcs, dsts, None)

    with p.stage() as st:
        ident_f = st.sb("ident_f", [128, 128]); ident = st.sb("ident", [128, 128], BF16)
        p.dma('sp', ident_f[:, :], io['ident'][:, :], reads=[io['ident']], writes=[ident_f])
        p.copy('dve', ident, ident[:, :], ident_f, ident_f[:, :])
        wout = st.sb("wout", [128, NCH, 2048], BF16)
        wst = [st.sb(f"wst{i}", [128, 2048]) for i in range(2)]
        for j, (q, off, sz, r0) in enumerate(chunks):
            s = wst[j % 2]
            p.dma('sp', s[0:sz, :], io['w_out'][r0:r0 + sz, :], reads=[io['w_out']], writes=[s])
            p.copy(['dve', 'pool'][j % 2], wout, wout[0:sz, j, :], s, s[0:sz, :])
        lnw = load_bc(p, st, "lnw", io['ln_w'][0:1, :], 2048); lnb = load_bc(p, st, "lnb", io['ln_b'][0:1, :], 2048)
        scr = ln_scratch(st, "a")
        gluw_f = st.sb("gluw_f", [128, 4, 512]); gluw = st.sb("gluw", [128, 4, 512], BF16); glub = st.sb("glub", [128, 4])
        p.dma('sp', gluw_f[:, :, :], io['glu_w'].ap.rearrange("(k p) c -> p k c", p=128), reads=[io['glu_w']], writes=[gluw_f])
        p.copy('dve', gluw, gluw[:, :, :], gluw_f, gluw_f[:, :, :])
        p.dma('sp', glub[:, :], io['glu_bc'][:, :], reads=[io['glu_bc']], writes=[glub])
        ys5 = [st.sb(f"ys5{i}", [128, 4, 512], BF16) for i in range(2)]
        sgl = st.sb("sgl", [128, 512])
        s5j = [j for j, (q, off, sz, r0) in enumerate(chunks) if off == 448]
        ybuf = [st.sb(f"ybuf{i}", [128, NCH, 512], BF16) for i in range(2)]
        xr = wst
        u = st.sb("u", [128, 2048])
        x1f = [st.sb(f"x1f{i}", [128, 2048]) for i in range(1)]
        x1b = st.sb("x1b", [128, 2048], BF16)
        x1T = [st.sb(f"x1T{i}", [128, 16, 512], BF16) for i in range(1)]
        pm = [st.ps(f"pm{i}", [128, 512]) for i in range(4)]
        pst = st.ps("pst", [128, 2048], BF16)
        x1T_d = io['x1T'].ap.rearrange("(k p) t -> p k t", p=128)
        GT = min(4, NT)
        for t in range(NT):
            g = t // GT; tt_ = t % GT
            yb = ybuf[g % 2]
            if tt_ == 0:
                for j, (q, off, sz, r0) in enumerate(chunks):
                    p.dma('sp', yb[0:sz, j, 0:GT * 128], io['yT'][q, off:off + sz, g * GT * 128:(g + 1) * GT * 128], reads=[io['yT']], writes=[yb])
                W_ = GT * 128
                y5 = ys5[g % 2]
                for qo in range(4):
                    G = pm[qo]
                    for qi in range(4):
                        p.mm(G, G[:, 0:W_], gluw, gluw[:, qi, qo * 128:(qo + 1) * 128], yb, yb[:, s5j[qi], 0:W_], start=(qi == 0), stop=(qi == 3))
                    p.act(sgl, sgl[:, 0:W_], G, G[:, 0:W_], AF.Sigmoid, bias=glub[:, qo:qo + 1], extra_reads=[glub])
                    p.tt('dve', y5, y5[:, qo, 0:W_], sgl, sgl[:, 0:W_], yb, yb[:, s5j[qo], 0:W_], ALU.mult)
            xrt = xr[t % 2]
            p.dma('sp', xrt[:, :], io['xres'][t * 128:(t + 1) * 128, :], reads=[io['xres']], writes=[xrt])
            for c in range(4):
                for j, (q, off, sz, r0) in enumerate(chunks):
                    if j in s5j:
                        y5 = ys5[g % 2]
                        p.mm(pm[c], pm[c][:, :], y5, y5[:, s5j.index(j), tt_ * 128:(tt_ + 1) * 128], wout, wout[0:sz, j, c * 512:(c + 1) * 512],
                             start=(j == 0), stop=(j == NCH - 1))
                    else:
                        p.mm(pm[c], pm[c][:, :], yb, yb[0:sz, j, tt_ * 128:(tt_ + 1) * 128], wout, wout[0:sz, j, c * 512:(c + 1) * 512],
                             start=(j == 0), stop=(j == NCH - 1))
                p.stt(u, u[:, c * 512:(c + 1) * 512], xrt, xrt[:, c * 512:(c + 1) * 512], ALPHA, pm[c], pm[c][:, :], ALU.mult, ALU.add)
            xf = x1f[0]
            ln_tile(p, u, lnw, lnb, scr, xf, x1b, "a")
            p.dma('pool', io['x1'][t * 128:(t + 1) * 128, :], xf[:, :], reads=[xf], writes=[io['x1']])
            xT = x1T[0]
            transpose_tile(p, x1b, ident, pst, xT, tt_ * 128)
            if tt_ == GT - 1:
                p.dma('pool', x1T_d[:, :, g * GT * 128:(g + 1) * GT * 128], xT[:, :, 0:GT * 128], reads=[xT], writes=[io['x1T']])

    with p.stage() as st:
        ident_f = st.sb("ident_f", [128, 128]); ident = st.sb("ident", [128, 128], BF16)
        p.dma('sp', ident_f[:, :], io['ident'][:, :], reads=[io['ident']], writes=[ident_f])
        p.copy('dve', ident, ident[:, :], ident_f, ident_f[:, :])
        wr_f = st.sb("wr_f", [128, 16, 20]); wr = st.sb("wr", [128, 16, 20], BF16)
        p.dma('sp', wr_f[:, :, 0:4], io['rg'].ap.rearrange("(k p) g -> p k g", p=128), reads=[io['rg']], writes=[wr_f])
        for g in range(4):
            p.dma('sp', wr_f[:, :, 4 + 4 * g:8 + 4 * g], io['re'][g].rearrange("(k p) e -> p k e", p=128), reads=[io['re']], writes=[wr_f])
        p.copy('dve', wr, wr[:, :, :], wr_f, wr_f[:, :, :])
        rb = st.sb("rb", [128, 20])
        p.dma('sp', rb[:, 0:4], io['rgb'].ap.rearrange("(o g) -> o g", o=1).partition_broadcast(128), reads=[io['rgb']], writes=[rb])
        p.dma('sp', rb[:, 4:20], io['reb'].ap.rearrange("(o g) e -> o (g e)", o=1).partition_broadcast(128), reads=[io['reb']], writes=[rb])
        NS = ST // 128
        x1T = [st.sb(f"mx1T{i}", [128, 16, ST], BF16) for i in range(2)]
        yacc = st.sb("yacc", [128, NS, 2048])
        gate = st.sb("gate", [128, NS, 16])
        w1b = [st.sb(f"w1b{i}", [128, 16, 512], BF16) for i in range(1)]
        w3b = [st.sb(f"w3b{i}", [128, 16, 512], BF16) for i in range(1)]
        w2b = [st.sb(f"w2b{i}", [128, 4, 2048], BF16) for i in range(1)]
        hT = [st.sb(f"hT{i}", [128, 4, ST], BF16) for i in range(2)]
        sl = [st.sb(f"sl{i}", [128, ST]) for i in range(2)]
        lg = st.sb("lg", [128, 20]); r1 = st.sb("r1", [128, 8]); mg = st.sb("mg", [128, 4]); es = st.sb("es", [128, 4])
        tmp16 = st.sb("tmp16", [128, 16]); m1 = st.sb("m1", [128, 4]); m2 = st.sb("m2", [128, 4]); e2 = st.sb("e2", [128, 4])
        gi = st.sb("gi", [128, 4])
        pa = [st.ps(f"pa{i}", [128, 512]) for i in range(2)]
        pb = [st.ps(f"pb{i}", [128, 512]) for i in range(2)]
        py = [st.ps(f"py{i}", [128, 512]) for i in range(2)]
        x1T_d = io['x1T'].ap.rearrange("(k p) t -> p k t", p=128)
        NSUP = T_ // ST
        wcount = 0
        for s in range(NSUP):
            xT = x1T[s % 2]
            p.dma('sp', xT[:, :, :], x1T_d[:, :, s * ST:(s + 1) * ST], reads=[io['x1T']], writes=[xT])
            for m in range(NS):
                pl = pa[m % 2]
                for k in range(16):
                    p.mm(pl, pl[:, 0:20], xT, xT[:, k, m * 128:(m + 1) * 128], wr, wr[:, k, :], start=(k == 0), stop=(k == 15))
                p.tt('dve', lg, lg[:, :], pl, pl[:, 0:20], rb, rb[:, :], ALU.add)
                p.op('dve', lambda: nc.vector.tensor_reduce(out=r1[:, 0:1], in_=lg[:, 0:4], axis=AX.X, op=ALU.max), reads=[lg], writes=[r1])
                p.ts('dve', mg, mg[:, :], lg, lg[:, 0:4], r1[:, 0:1], None, ALU.is_equal, extra_reads=[r1])
                p.ts('dve', r1, r1[:, 1:2], r1, r1[:, 0:1], -1.0, None, ALU.mult)
                p.act(tmp16, tmp16[:, 0:4], lg, lg[:, 0:4], AF.Exp, bias=r1[:, 1:2], extra_reads=[r1], accum=(r1, r1[:, 2:3]))
                p.op('dve', lambda: nc.vector.reciprocal(out=r1[:, 3:4], in_=r1[:, 2:3]), reads=[r1], writes=[r1])
                p.ts('dve', es, es[:, :], lg, lg[:, 4:8], mg[:, 0:1], None, ALU.mult, extra_reads=[mg])
                for g in range(1, 4):
                    p.stt(es, es[:, :], lg, lg[:, 4 + 4 * g:8 + 4 * g], mg[:, g:g + 1], es, es[:, :], ALU.mult, ALU.add, extra_reads=[mg])
                p.op('dve', lambda: nc.vector.tensor_reduce(out=r1[:, 4:5], in_=es[:, :], axis=AX.X, op=ALU.max), reads=[es], writes=[r1])
                p.ts('dve', m1, m1[:, :], es, es[:, :], r1[:, 4:5], None, ALU.is_equal, extra_reads=[r1])
                p.stt(e2, e2[:, :], m1, m1[:, :], -1e30, es, es[:, :], ALU.mult, ALU.add)
                p.op('dve', lambda: nc.vector.tensor_reduce(out=r1[:, 5:6], in_=e2[:, :], axis=AX.X, op=ALU.max), reads=[e2], writes=[r1])
                p.ts('dve', m2, m2[:, :], e2, e2[:, :], r1[:, 5:6], None, ALU.is_equal, extra_reads=[r1])
                p.tt('dve', r1, r1[:, 6:7], r1, r1[:, 4:5], r1, r1[:, 5:6], ALU.subtract)
                p.act(r1, r1[:, 6:7], r1, r1[:, 6:7], AF.Sigmoid)
                p.ts('dve', r1, r1[:, 7:8], r1, r1[:, 6:7], -1.0, 1.0, ALU.mult, ALU.add)
                p.ts('dve', gi, gi[:, :], m1, m1[:, :], r1[:, 6:7], None, ALU.mult, extra_reads=[r1])
                p.stt(gi, gi[:, :], m2, m2[:, :], r1[:, 7:8], gi, gi[:, :], ALU.mult, ALU.add, extra_reads=[r1])
                p.ts('dve', gi, gi[:, :], gi, gi[:, :], r1[:, 3:4], None, ALU.mult, extra_reads=[r1])
                for g in range(4):
                    p.ts('dve', gate, gate[:, m, 4 * g:4 * g + 4], gi, gi[:, :], mg[:, g:g + 1], None, ALU.mult, extra_reads=[mg])
            for e in range(16):
                wb = 0
                p.dma('sp', w1b[wb][:, :, :], io['w1b'][e].rearrange("(k p) f -> p k f", p=128), reads=[io['w1b']], writes=[w1b[wb]])
                p.dma('sp', w3b[wb][:, :, :], io['w3b'][e].rearrange("(k p) f -> p k f", p=128), reads=[io['w3b']], writes=[w3b[wb]])
                p.dma('sp', w2b[wb][:, :, :], io['w2b'][e].rearrange("(k p) f -> p k f", p=128), reads=[io['w2b']], writes=[w2b[wb]])
                h = hT[e % 2]
                for f in range(4):
                    a = pa[f % 2]; b = pb[f % 2]
                    for k in range(16):
                        p.mm(a, a[:, 0:ST], w1b[wb], w1b[wb][:, k, f * 128:(f + 1) * 128], xT, xT[:, k, :], start=(k == 0), stop=(k == 15))
                    for k in range(16):
                        p.mm(b, b[:, 0:ST], w3b[wb], w3b[wb][:, k, f * 128:(f + 1) * 128], xT, xT[:, k, :], start=(k == 0), stop=(k == 15))
                    s_ = sl[f % 2]
                    p.act(s_, s_[:, :], a, a[:, 0:ST], AF.Silu)
                    p.tt('dve', h, h[:, f, :], s_, s_[:, :], b, b[:, 0:ST], ALU.mult)
                i = 0
                for m in range(NS):
                    for c in range(4):
                        y = py[i % 2]; i += 1
                        for f in range(4):
                            p.mm(y, y[:, :], h, h[:, f, m * 128:(m + 1) * 128], w2b[wb], w2b[wb][:, f, c * 512:(c + 1) * 512], start=(f == 0), stop=(f == 3))
                        if e == 0:
                            p.ts('dve', yacc, yacc[:, m, c * 512:(c + 1) * 512], y, y[:, :], gate[:, m, e:e + 1], None, ALU.mult, extra_reads=[gate])
                        else:
                            p.stt(yacc, yacc[:, m, c * 512:(c + 1) * 512], y, y[:, :], gate[:, m, e:e + 1], yacc, yacc[:, m, c * 512:(c + 1) * 512],
                                  ALU.mult, ALU.add, extra_reads=[gate])
            for m in range(NS):
                t = s * NS + m
                p.dma('pool', io['moe'][t * 128:(t + 1) * 128, :], yacc[:, m, :], reads=[yacc], writes=[io['moe']])

    with p.stage() as st:
        ident_f = st.sb("ident_f", [128, 128]); ident = st.sb("ident", [128, 128], BF16)
        p.dma('sp', ident_f[:, :], io['ident'][:, :], reads=[io['ident']], writes=[ident_f])
        p.copy('dve', ident, ident[:, :], ident_f, ident_f[:, :])
        lnw2 = load_bc(p, st, "lnw2", io['ln_w'][1:2, :], 2048); lnb2 = load_bc(p, st, "lnb2", io['ln_b'][1:2, :], 2048)
        lnw3 = load_bc(p, st, "lnw3", io['ln_w'][2:3, :], 2048); lnb3 = load_bc(p, st, "lnb3", io['ln_b'][2:3, :], 2048)
        scr = ln_scratch(st, "c")
        pg = st.sb("pg", [128, 16, 2048], BF16); pp = st.sb("pp", [128, 2, 2048], BF16)
        wst = [st.sb(f"wst{i}", [128, 2048]) for i in range(2)]
        for k in range(16):
            s = wst[k % 2]
            p.dma('sp', s[:, :], io['ple_gate'][k * 128:(k + 1) * 128, :], reads=[io['ple_gate']], writes=[s])
            p.copy(['dve', 'pool'][k % 2], pg, pg[:, k, :], s, s[:, :])
        for k in range(2):
            s = wst[k % 2]
            p.dma('sp', s[:, :], io['ple_proj'][k * 128:(k + 1) * 128, :], reads=[io['ple_proj']], writes=[s])
            p.copy(['dve', 'pool'][k % 2], pp, pp[:, k, :], s, s[:, :])
        xr = wst[0]; mo = wst[1]
        x2T = st.sb("cx2T", [128, 16, 128], BF16)
        pTf = st.sb("pTf", [128, 2, 128]); pTb = st.sb("pTb", [128, 2, 128], BF16)
        sg = st.sb("sg", [128, 512])
        u = st.sb("u", [128, 2048])
        x2f = st.sb("x2f", [128, 2048]); x2b = st.sb("x2b", [128, 2048], BF16)
        x3f = st.sb("x3f", [128, 2048]); x3b = st.sb("x3b", [128, 2048], BF16)
        x3T = [st.sb(f"x3T{i}", [128, 16, 512], BF16) for i in range(2)]
        pgp = [st.ps(f"pgp{i}", [128, 512]) for i in range(2)]
        ppp = [st.ps(f"ppp{i}", [128, 512]) for i in range(2)]
        pst = st.ps("pst", [128, 2048], BF16)
        xoT_d = io['xoT'].ap.rearrange("(k p) t -> p k t", p=128)
        pT_d = io['pT'].ap.rearrange("(k p) t -> p k t", p=128)
        GT = min(4, NT)
        for t in range(NT):
            g = t // GT; tt_ = t % GT
            rows = slice(t * 128, (t + 1) * 128)
            p.dma('sp', xr[:, :], io['x1'][rows, :], reads=[io['x1']], writes=[xr])
            p.dma('sp', mo[:, :], io['moe'][rows, :], reads=[io['moe']], writes=[mo])
            p.dma('sp', pTf[:, :, :], pT_d[:, :, rows], reads=[io['pT']], writes=[pTf])
            p.copy('pool', pTb, pTb[:, :, :], pTf, pTf[:, :, :])
            p.stt(u, u[:, :], xr, xr[:, :], ALPHA, mo, mo[:, :], ALU.mult, ALU.add)
            ln_tile(p, u, lnw2, lnb2, scr, x2f, x2b, "c")
            transpose_tile(p, x2b, ident, pst, x2T, 0)
            for c in range(4):
                cs = slice(c * 512, (c + 1) * 512)
                G = pgp[c % 2]; PP = ppp[c % 2]
                for k in range(16):
                    p.mm(G, G[:, :], x2T, x2T[:, k, :], pg, pg[:, k, cs], start=(k == 0), stop=(k == 15))
                for k in range(2):
                    p.mm(PP, PP[:, :], pTb, pTb[:, k, :], pp, pp[:, k, cs], start=(k == 0), stop=(k == 1))
                p.act(sg, sg[:, :], G, G[:, :], AF.Sigmoid)
                p.tt('dve', sg, sg[:, :], sg, sg[:, :], PP, PP[:, :], ALU.mult)
                p.stt(u, u[:, cs], x2f, x2f[:, cs], ALPHA, sg, sg[:, :], ALU.mult, ALU.add)
            ln_tile(p, u, lnw3, lnb3, scr, x3f, x3b if want_xoT else None, "c")
            p.dma('pool', io['xo'][rows, :], x3f[:, :], reads=[x3f], writes=[io['xo']])
            x3 = x3T[g % 2]
            if want_xoT:
                transpose_tile(p, x3b, ident, pst, x3, tt_ * 128)
            if want_xoT and tt_ == GT - 1:
                p.dma('pool', xoT_d[:, :, g * GT * 128:(g + 1) * GT * 128], x3[:, :, 0:GT * 128], reads=[x3], writes=[io['xoT']])


_S = 16384
_D = 2048
_T = 4096
_PROGS = {}
_GROUPS = [[0, 1, 2, 3], [4, 5, 6, 7]]
_P1_KEYS = [('w_in', [_D, NZ]), ('rwp', [128, 55]), ('rw_wup', [64, 2, 192]), ('rw_aup', [64, 2, 192]), ('rw_gup', [128, 192]),
            ('s5rows', [3, 1024]), ('s5cols', [128, 24]), ('s5bl', [2, 128, 1024]), ('s5cl', [2, 128, 1024]), ('s5d', [128, 1])]
_P2_KEYS = [('w_out', [_D, _D]), ('ln_w', [3, _D]), ('ln_b', [3, _D]), ('rg', [_D, 4]), ('rgb', [4]), ('re', [4, _D, 4]), ('reb', [4, 4]),
            ('w1', [16, _D, 512]), ('w3', [16, _D, 512]), ('w2', [16, 512, _D]), ('glu_w', [512, 512]), ('glu_bc', [128, 4]),
            ('ple_proj', [256, _D]), ('ple_gate', [_D, _D]), ('pT', [256, None])]

def stage_select(p, gath, rmask, yTr):
    with p.stage() as st:
        mk = st.sb("mk", [128, 4]); p.dma('sp', mk[:, :], rmask[:, :], reads=[rmask], writes=[mk])
        cand = [[st.sb(f"cand{i}_{r}", [128, _T], BF16) for r in range(4)] for i in range(2)]
        acc = [st.sb(f"acc{i}", [128, _T], BF16) for i in range(2)]
        n = 0
        for q in range(4):
            for i in range(6):
                r0 = i * 96; sz = 96
                cd = cand[n % 2]; a = acc[n % 2]
                e = 'dve'; n += 1
                for r in range(4):
                    p.dma('sp', cd[r][0:sz, :], gath[r * 6 + i, q * 96:(q + 1) * 96, :], reads=[gath], writes=[cd[r]])
                p.ts(e, a, a[0:sz, :], cd[0], cd[0][0:sz, :], mk[0:sz, 0:1], None, ALU.mult, extra_reads=[mk])
                for r in range(1, 4):
                    p.stt(a, a[0:sz, :], cd[r], cd[r][0:sz, :], mk[0:sz, r:r + 1], a, a[0:sz, :], ALU.mult, ALU.add, extra_reads=[mk], e=e)
                p.dma('pool', yTr[q, r0:r0 + sz, :], a[0:sz, :], reads=[a], writes=[yTr])

def _build_fused():
    S = _S; D = _D; T_ = _T
    nc = bass.Bass("TRN2", target_bir_lowering=False)
    p = Prog(nc)
    E = {}
    def ext(name, shape, dt=F32):
        E[name] = p.dram(name, shape, dt, kind="ExternalInput"); return E[name]
    ext('xT0', [4, D, T_]); ext('xres', [T_, D]); ext('pos', [S], I32); ext('rconst', [128, RC_N]); ext('ident', [128, 128])
    ext('rwconst', [128, RW_N]); ext('s5iota', [128, 512]); ext('rmask', [128, 4])
    for L in range(2):
        for k, sh in _P1_KEYS + _P2_KEYS:
            ext(f"{k}_{L}", [T_ if s is None else s for s in sh])
    xo_final = p.dram('xo_final', [T_, D], F32, kind="ExternalOutput")
    sc = {}
    sc['zT'] = p.dram('zT', [NZ, S], F32)
    for nm in ['qrT', 'krT']: sc[nm] = p.dram(nm, [2, 128, S], BF16)
    for nm in ['ktok', 'vtok', 'sbd']: sc[nm] = p.dram(nm, [2, S // 128, 128, 128], BF16)
    sc['yf'] = p.dram('yf', [128, S]); sc['y0'] = p.dram('y0', [192, S])
    yTloc = p.dram('yTloc', [4, 576, T_], BF16)
    gath = p.dram('gath', [24, 4 * 96, T_], BF16)
    yTr = p.dram('yTr', [4, 576, T_], BF16)
    sc2 = dict(x1=p.dram('x1', [T_, D]), x1T=p.dram('x1T', [D, T_], BF16), moe=p.dram('moe', [T_, D]),
               w1b=p.dram('w1b', [16, D, 512], BF16), w3b=p.dram('w3b', [16, D, 512], BF16), w2b=p.dram('w2b', [16, 512, D], BF16))
    xo0 = p.dram('xo0', [T_, D]); xoT = p.dram('xoT', [D, T_], BF16); xoT_dummy = p.dram('xoT_dummy', [D, T_], BF16)
    xTg = p.dram('xTg', [16, 4 * 128, T_], BF16)
    for L in range(2):
        io1 = dict(sc)
        for k, _ in _P1_KEYS: io1[k] = E[f"{k}_{L}"]
        io1.update(pos=E['pos'], rconst=E['rconst'], ident=E['ident'], rwconst=E['rwconst'], s5iota=E['s5iota'], yT=yTloc)
        io1['xT'] = E['xT0'] if L == 0 else xTg
        xsrc = None
        if L == 1:
            xsrc = lambda r: xTg.ap[:, r * 128:(r + 1) * 128, :].rearrange("k p t -> p k t")
        stage_inproj(p, S, io1, L == 0, xsrc=xsrc)
        stage_retention(p, S, io1)
        stage_s5(p, S, io1)
        io2 = dict(sc2)
        for k_, _ in _P2_KEYS: io2[k_] = E[f"{k_}_{L}"]
        precast_dma(p, io2)
        stage_rwkv(p, S, io1)
        for r in range(4):
            for i in range(6):
                p.collective("AllGather", yTloc, yTloc.ap[r, i * 96:(i + 1) * 96, :], gath, gath.ap[r * 6 + i], _GROUPS)
        stage_select(p, gath, E['rmask'], yTr)
        io2.update(yT=yTr, ident=E['ident'], xres=(E['xres'] if L == 0 else xo0), xo=(xo0 if L == 0 else xo_final),
                   xoT=(xoT if L == 0 else xoT_dummy))
        phase2(p, T_, io2, ST=512, precast=False, want_xoT=(L == 0))
        if L == 0:
            for k in range(16):
                p.collective("AllGather", xoT, xoT.ap[k * 128:(k + 1) * 128, :], xTg, xTg.ap[k], _GROUPS)
    p.finish([xo_final])
    return nc

def kernel(**inp):
    x = np.ascontiguousarray(np.asarray(inp['x'], dtype=np.float32))
    S = _S; D = _D
    if 'f' not in _PROGS:
        _PROGS['f'] = _build_fused()
    ident = np.eye(128, dtype=np.float32)
    rwc = rwkv_consts()
    positions = np.asarray(inp['positions']).astype(np.int32)
    g = lambda k: np.asarray(inp[k])
    maps = []
    for c in range(8):
        b, q = c // 4, c % 4
        r = q
        rmask = np.zeros((128, 4), np.float32); rmask[:, r] = 1.0
        m = dict(xT0=np.ascontiguousarray(x[b].T.reshape(D, 4, S // 4).transpose(1, 0, 2)),
                 xres=np.ascontiguousarray(x[b, r * _T:(r + 1) * _T]), pos=np.ascontiguousarray(positions[b]),
                 rconst=ret_consts(q), ident=ident, rwconst=rwc, rmask=rmask)
        for L in range(2):
            d1 = dict(w_in=np.ascontiguousarray(g('w_in')[L][:, core_cols(q)]))
            d1.update(rwkv_host_layout(q, g('rwkv_mu_prev')[L], g('rwkv_mu_next')[L], g('rwkv_w0')[L], g('rwkv_w_up')[L], g('rwkv_a0')[L],
                                       g('rwkv_a_up')[L], g('rwkv_g_up')[L], g('rwkv_k_k')[L], g('rwkv_k_a')[L], g('rwkv_r_k')[L],
                                       g('rwkv_lnx_w')[L], g('rwkv_lnx_b')[L]))
            s5 = s5_host_layout(q, g('s5_lam_re')[L], g('s5_lam_im')[L], g('s5_log_dt')[L], g('s5_b_re')[L], g('s5_b_im')[L],
                                g('s5_c_re')[L], g('s5_c_im')[L], g('s5_d')[L])
            m['s5iota'] = s5.pop('s5iota')
            d1.update(s5)
            d2 = dict(w_out=g('w_out')[L], ln_w=g('ln_w')[L], ln_b=g('ln_b')[L],
                      rg=g('moe_router_g')[L], rgb=g('moe_router_g_b')[L], re=g('moe_router_e')[L], reb=g('moe_router_e_b')[L],
                      w1=g('moe_w1')[L], w3=g('moe_w3')[L], w2=g('moe_w2')[L],
                      glu_w=g('s5_glu_w')[L], glu_bc=np.ascontiguousarray(g('s5_glu_b')[L].reshape(4, 128).T),
                      ple_proj=g('ple_proj')[L], ple_gate=g('ple_gate')[L],
                      pT=np.ascontiguousarray(g('p')[L, b, r * _T:(r + 1) * _T].T))
            for k, v in list(d1.items()) + list(d2.items()):
                m[f"{k}_{L}"] = np.ascontiguousarray(v)
        maps.append(m)
    res = run_bass_kernel_spmd(_PROGS['f'], maps, core_ids=list(range(8)))
    out = np.empty_like(x)
    for c in range(8):
        b, r = c // 4, c % 4
        out[b, r * _T:(r + 1) * _T] = np.asarray(res.results[c]['xo_final'])
    return out
```

```python
import math
import numpy as np
import concourse.bass as bass
import concourse.mybir as mybir
from concourse.bass_utils import run_bass_kernel_spmd
from contextlib import ExitStack
F32 = mybir.dt.float32; BF16 = mybir.dt.bfloat16; I32 = mybir.dt.int32
AF = mybir.ActivationFunctionType; ALU = mybir.AluOpType; AX = mybir.AxisListType


class T:
    def __init__(self, ap, name):
        self.ap = ap; self.name = name
        self.w = None
        self.r = []
    def __getitem__(self, k):
        return self.ap[k]


class Prog:
    NDMA = 16
    SEM_MAX = 30000
    def __init__(self, nc):
        self.nc = nc
        self.eng = {'pe': nc.tensor, 'dve': nc.vector, 'act': nc.scalar, 'pool': nc.gpsimd, 'sp': nc.sync}
        self.gen = {e: 0 for e in ['pe', 'dve', 'act', 'pool']}
        self.sem = {e: nc.alloc_semaphore("sem_" + e) for e in ['pe', 'dve', 'act', 'pool']}
        self.cnt = {e: 0 for e in self.sem}
        self.seen = {e: {} for e in self.eng}
        self.dsem = {q: [nc.alloc_semaphore(f"dsem_{q}{i}") for i in range(self.NDMA)] for q in ['sp', 'pool']}
        self.dcnt = {q: [0] * self.NDMA for q in self.dsem}
        self.dgen = {q: [0] * self.NDMA for q in self.dsem}
        self.dnext = {q: 0 for q in self.dsem}
        self.semobj = {}
        for e, s in self.sem.items(): self.semobj[('c', e, 0)] = s
        for q in self.dsem:
            for i, s in enumerate(self.dsem[q]): self.semobj[('d', q, i, 0)] = s
        self.ninst = 0
        self.nsb = 0

    def sb(self, name, shape, dt=F32):
        return T(self.nc.alloc_sbuf_tensor(name, list(shape), dt).ap(), name)
    def ps(self, name, shape, dt=F32):
        return T(self.nc.alloc_psum_tensor(name, list(shape), dt).ap(), name)
    def dram(self, name, shape, dt=F32, kind="Internal"):
        return T(self.nc.dram_tensor(name, list(shape), dt, kind=kind).ap(), name)

    def _wait(self, e, tok):
        if tok is None: return
        key, val = tok
        if self.seen[e].get(key, 0) >= val: return
        self.eng[e].wait_ge(self.semobj[key], val)
        self.seen[e][key] = val

    def _deps(self, e, reads, writes):
        for b in reads:
            if b.w is not None and not (e == 'pe' and b.w[0][0:2] == ('c', 'pe')):
                self._wait(e, b.w)
        for b in writes:
            if b.w is not None and not (e == 'pe' and b.w[0][0:2] == ('c', 'pe')):
                self._wait(e, b.w)
            for tok in b.r:
                if e == 'pe' and tok[0][0:2] == ('c', 'pe'): continue
                self._wait(e, tok)

    def _mark(self, tok, reads, writes):
        for b in reads:
            b.r = [t for t in b.r if t[0] != tok[0]] + [tok]
        for b in writes:
            b.w = tok; b.r = []

    def op(self, e, fn, reads=(), writes=()):
        self._deps(e, reads, writes)
        if self.cnt[e] >= self.SEM_MAX:
            self.gen[e] += 1; self.cnt[e] = 0
            self.sem[e] = self.nc.alloc_semaphore(f"sem_{e}_{self.gen[e]}")
            self.semobj[('c', e, self.gen[e])] = self.sem[e]
        inst = fn()
        self.cnt[e] += 1
        inst.then_inc(self.sem[e], 1)
        tok = (('c', e, self.gen[e]), self.cnt[e])
        self._mark(tok, reads, writes)
        self.ninst += 1
        return inst

    def dma(self, q, out, in_, reads=(), writes=(), **kw):
        i = self.dnext[q]; self.dnext[q] = (i + 1) % self.NDMA
        key = ('d', q, i, self.dgen[q][i])
        if self.dcnt[q][i] > 0:
            self._wait(q, (key, self.dcnt[q][i]))
        if self.dcnt[q][i] >= self.SEM_MAX:
            self.dgen[q][i] += 1; self.dcnt[q][i] = 0
            self.dsem[q][i] = self.nc.alloc_semaphore(f"dsem_{q}{i}_{self.dgen[q][i]}")
            key = ('d', q, i, self.dgen[q][i])
            self.semobj[key] = self.dsem[q][i]
        self._deps(q, reads, writes)
        inst = self.eng[q].dma_start(out=out, in_=in_, **kw)
        self.dcnt[q][i] += 16
        inst.then_inc(self.dsem[q][i], 16)
        tok = (key, self.dcnt[q][i])
        self._mark(tok, reads, writes)
        self.ninst += 1
        return inst

    def finish(self, outs):
        for b in outs:
            self._wait('sp', b.w)
        for e in ['pe', 'dve', 'act', 'pool']:
            if self.cnt[e] > 0:
                self._wait('sp', (('c', e, self.gen[e]), self.cnt[e]))
        for q in self.dsem:
            for i in range(self.NDMA):
                if self.dcnt[q][i] > 0:
                    self._wait('sp', (('d', q, i, self.dgen[q][i]), self.dcnt[q][i]))

    def mm(self, out, o_ap, lhsT, l_ap, rhs, r_ap, start=True, stop=True):
        return self.op('pe', lambda: self.nc.tensor.matmul(o_ap, l_ap, r_ap, start=start, stop=stop),
                       reads=[lhsT, rhs], writes=[out])
    def tr(self, out, o_ap, in_, i_ap, ident, id_ap):
        return self.op('pe', lambda: self.nc.tensor.transpose(o_ap, i_ap, id_ap), reads=[in_, ident], writes=[out])
    def act(self, out, o_ap, in_, i_ap, func, bias=None, scale=1.0, extra_reads=(), e='act', accum=None):
        kw = {}
        if bias is not None: kw['bias'] = bias
        if accum is not None: kw['accum_out'] = accum[1]
        wr = [out] + ([accum[0]] if accum is not None else [])
        return self.op('act', lambda: self.nc.scalar.activation(out=o_ap, in_=i_ap, func=func, scale=scale, **kw),
                       reads=[in_] + list(extra_reads), writes=wr)
    def tt(self, e, out, o_ap, a, a_ap, b, b_ap, op):
        en = self.eng[e]
        return self.op(e, lambda: en.tensor_tensor(out=o_ap, in0=a_ap, in1=b_ap, op=op), reads=[a, b], writes=[out])
    def ts(self, e, out, o_ap, a, a_ap, s1, s2, op0, op1=None, extra_reads=(), accum=None):
        en = self.eng[e]
        kw = {}
        if op1 is not None: kw['op1'] = op1
        wr = [out]
        if accum is not None:
            kw['accum_out'] = accum[1]; wr.append(accum[0])
        return self.op(e, lambda: en.tensor_scalar(out=o_ap, in0=a_ap, scalar1=s1, scalar2=s2, op0=op0, **kw),
                       reads=[a] + list(extra_reads), writes=wr)
    def stt(self, out, o_ap, a, a_ap, s, b, b_ap, op0, op1, extra_reads=(), e='dve'):
        en = self.eng[e]
        return self.op(e, lambda: en.scalar_tensor_tensor(out=o_ap, in0=a_ap, scalar=s, in1=b_ap, op0=op0, op1=op1),
                       reads=[a, b] + list(extra_reads), writes=[out])
    def copy(self, e, out, o_ap, in_, i_ap):
        if e == 'act':
            return self.act(out, o_ap, in_, i_ap, AF.Copy)
        en = self.eng[e]
        return self.op(e, lambda: en.tensor_copy(out=o_ap, in_=i_ap), reads=[in_], writes=[out])
    def memset(self, e, out, o_ap, val):
        en = self.eng[e]
        return self.op(e, lambda: en.memset(o_ap, val), writes=[out])

class Stage:
    def __init__(self, p):
        self.p = p; self.es = ExitStack()
    def __enter__(self):
        self.es.__enter__(); return self
    def __exit__(self, *a):
        self.p.barrier()
        return self.es.__exit__(*a)
    def sb(self, name, shape, dt=F32):
        self.p.nsb += 1; name = f"s{self.p.nsb}_{name}"
        h = self.es.enter_context(self.p.nc.sbuf_tensor(name, list(shape), dt))
        return T(h.ap(), name)
    def ps(self, name, shape, dt=F32):
        self.p.nsb += 1; name = f"s{self.p.nsb}_{name}"
        h = self.es.enter_context(self.p.nc.psum_tensor(name, list(shape), dt))
        return T(h.ap(), name)

def _barrier(self):
    toks = []
    for e in ['pe', 'dve', 'act', 'pool']:
        if self.cnt[e] > 0: toks.append((('c', e, self.gen[e]), self.cnt[e]))
    for q in self.dsem:
        for i in range(self.NDMA):
            if self.dcnt[q][i] > 0: toks.append((('d', q, i, self.dgen[q][i]), self.dcnt[q][i]))
    for e in self.eng:
        for tok in toks:
            if tok[0][0:2] == ('c', e) and e == 'pe': continue
            self._wait(e, tok)
Prog.barrier = _barrier
Prog.stage = lambda self: Stage(self)

def _collective(self, kind, in_T, in_ap, out_T, out_ap, groups):
    if not hasattr(self, 'ccsem'):
        self.ccsem = self.nc.alloc_semaphore("ccsem"); self.ccnt = 0
        self.semobj[('cc',)] = self.ccsem
    self._deps('pool', [in_T], [out_T])
    inst = self.nc.gpsimd.collective_compute(kind, ALU.bypass, replica_groups=groups, ins=[in_ap], outs=[out_ap])
    self.ccnt += 1
    inst.then_inc(self.ccsem, 1)
    tok = (('cc',), self.ccnt)
    self._mark(tok, [in_T], [out_T])
    self.ninst += 1
Prog.collective = _collective

PI = math.pi
C1 = 6.28125
C2 = 2 * math.pi - C1
INV2PI = 1.0 / (2 * math.pi)

RET_W = 768; RWKV_W = 768; RET_COLS = 3072; RWKV_COLS = 2688
NZ = 2112
COLTILES = [(i * 128, 128) for i in range(8)] + [(1024, 128), (1152, 64), (1216, 128), (1344, 64), (1408, 128), (1536, 64),
                                                  (1600, 128), (1728, 128), (1856, 128), (1984, 128)]

def ret_heads(q):
    return [2 * q, 2 * q + 1] if q < 2 else [q + 2, q + 2]

def core_cols(q):
    cols = []
    for h in ret_heads(q):
        for part in range(4):
            cols += list(range(part * RET_W + h * 128, part * RET_W + (h + 1) * 128))
    base = RET_COLS
    for part in range(3):
        cols += list(range(base + part * RWKV_W + q * 192, base + part * RWKV_W + (q + 1) * 192))
    cols += list(range(base + 3 * RWKV_W, base + 3 * RWKV_W + 384))
    cols += list(range(RET_COLS + RWKV_COLS + q * 128, RET_COLS + RWKV_COLS + (q + 1) * 128))
    assert len(cols) == NZ
    return cols

def ret_consts(q):
    import numpy as np
    C = 128
    cols = []
    inv = (10000.0 ** (-(np.arange(128) % 64).astype(np.float32) / np.float32(64))).astype(np.float32)
    cols.append(inv[:, None]); cols.append(np.where(np.arange(128) < 64, -1.0, 1.0).astype(np.float32)[:, None])
    pos = np.arange(C, dtype=np.float64)
    for h in ret_heads(q):
        lg = np.log(1.0 - 2.0 ** (-5.0 - h))
        cols.append(np.exp(lg * (C - 1 - pos))[:, None])
        cols.append(np.exp(lg * pos)[:, None])
        cols.append(np.full((128, 1), np.exp(lg * C)))
        cols.append(np.exp(lg * np.abs(pos[:, None] - pos[None, :])))
        cols.append(np.tile(np.exp(lg * (pos + 1.0))[None, :], (128, 4)))
        cols.append(np.tile(np.exp(lg * (C - pos))[None, :], (128, 4)))
    return np.concatenate(cols, axis=1).astype(np.float32)
RC_SLOT = 3 + 128 + 512 + 512
RC_N = 2 + 2 * RC_SLOT

def stage_inproj(p, S, io, x_f32, xsrc=None):
    nc = p.nc
    Q4 = S // 4
    with p.stage() as st:
        wb = st.sb("winb", [128, 16, NZ], BF16)
        wst = [st.sb(f"wst{i}", [128, NZ]) for i in range(2)]
        for k in range(16):
            s = wst[k % 2]
            p.dma('sp', s[:, :], io['w_in'][k * 128:(k + 1) * 128, :], reads=[io['w_in']], writes=[s])
            p.copy(['dve', 'pool'][k % 2], wb, wb[:, k, :], s, s[:, :])
        xb = [st.sb(f"xb{i}", [128, 16, 512], BF16) for i in range(2)]
        if x_f32:
            xf = [st.sb(f"xf{i}", [128, 8, 512]) for i in range(2)]
        zo = [st.sb(f"zo{i}", [128, 512]) for i in range(4)]
        ps = [st.ps(f"ps{i}", [128, 512]) for i in range(4)]
        n = 0
        for tt in range(S // 512):
            r = (tt * 512) // Q4; off = tt * 512 - r * Q4
            src = xsrc(r) if xsrc is not None else io['xT'][r].rearrange("(k p) t -> p k t", p=128)
            x = xb[tt % 2]
            if x_f32:
                for hf in range(2):
                    p.dma('sp', xf[hf][:, :, :], src[:, hf * 8:(hf + 1) * 8, off:off + 512], reads=[io['xT']], writes=[xf[hf]])
                    p.copy(['dve', 'act'][hf], x, x[:, hf * 8:(hf + 1) * 8, :], xf[hf], xf[hf][:, :, :])
            else:
                p.dma('sp', x[:, :, :], src[:, :, off:off + 512], reads=[io['xT']], writes=[x])
            for ci, (c0, w) in enumerate(COLTILES):
                P_ = ps[n % 4]; z = zo[n % 4]
                for k in range(16):
                    p.mm(P_, P_[0:w, :], wb, wb[:, k, c0:c0 + w], x, x[:, k, :], start=(k == 0), stop=(k == 15))
                if n % 2 == 0:
                    p.act(z, z[0:w, :], P_, P_[0:w, :], AF.Copy)
                else:
                    p.copy('dve', z, z[0:w, :], P_, P_[0:w, :])
                p.dma('pool', io['zT'][c0:c0 + w, tt * 512:(tt + 1) * 512], z[0:w, :], reads=[z], writes=[io['zT']])
                n += 1

def stage_retention(p, S, io):
    nc = p.nc
    NCK = S // 128; NTT = S // 512; Q4 = S // 4
    zT = io['zT']
    with p.stage() as st:
        rc = st.sb("rc", [128, RC_N])
        p.dma('sp', rc[:, :], io['rconst'][:, :], reads=[io['rconst']], writes=[rc])
        ident_f = st.sb("ident_f", [128, 128]); ident = st.sb("ident", [128, 128], BF16)
        p.dma('sp', ident_f[:, :], io['ident'][:, :], reads=[io['ident']], writes=[ident_f])
        p.copy('dve', ident, ident[:, :], ident_f, ident_f[:, :])
        posi = st.sb("posi", [128, 512], I32); ang = st.sb("ang", [128, 512]); a2 = st.sb("a2", [128, 512])
        ki = st.sb("ki", [128, 512], I32); kf = st.sb("kf", [128, 512])
        cos = st.sb("cos", [128, 512]); sin = st.sb("sin", [128, 512])
        zq = [st.sb(f"zq{i}", [128, 512]) for i in range(2)]; zs = [st.sb(f"zs{i}", [128, 512]) for i in range(2)]
        t1 = st.sb("t1", [128, 512]); t2 = st.sb("t2", [128, 512])
        rot = [st.sb(f"rot{i}", [128, 512], BF16) for i in range(2)]
        vf = st.sb("vf", [128, 512]); vb = st.sb("vb", [128, 512], BF16)
        tok = [st.sb(f"tok{i}", [128, 4, 128], BF16) for i in range(2)]
        ptr = [st.ps(f"ptr{i}", [128, 512], BF16) for i in range(2)]
        n = 0
        for tt in range(NTT):
            ts_ = slice(tt * 512, (tt + 1) * 512)
            p.dma('sp', posi[:, :], io['pos'].ap[ts_].rearrange("(o t) -> o t", o=1).partition_broadcast(128), reads=[io['pos']], writes=[posi])
            p.ts('dve', ang, ang[:, :], posi, posi[:, :], rc[:, 0:1], None, ALU.mult, extra_reads=[rc])
            for (shift, dst, scale) in [(0.0, sin, rc[:, 1:2]), (PI / 2, cos, 1.0)]:
                if shift != 0.0:
                    p.ts('dve', a2, a2[:, :], ang, ang[:, :], shift, None, ALU.add)
                    src = a2
                else:
                    src = ang
                p.ts('dve', ki, ki[:, :], src, src[:, :], INV2PI, None, ALU.mult)
                p.copy('act', kf, kf[:, :], ki, ki[:, :])
                p.stt(a2, a2[:, :], kf, kf[:, :], -C1, src, src[:, :], ALU.mult, ALU.add)
                p.stt(a2, a2[:, :], kf, kf[:, :], -C2, a2, a2[:, :], ALU.mult, ALU.add)
                p.ts('pool', a2, a2[:, :], a2, a2[:, :], -PI, PI, ALU.max, ALU.min)
                p.act(dst, dst[:, :], a2, a2[:, :], AF.Sin, scale=scale, extra_reads=[rc])
            for s in range(2):
                base = s * 512
                for which, (r0, scl, dstd) in enumerate([(base, 128.0 ** -0.5, io['qrT']), (base + 128, 1.0, io['krT'])]):
                    z = zq[which]; zw = zs[which]
                    p.dma('sp', z[:, :], zT[r0:r0 + 128, ts_], reads=[zT], writes=[z])
                    p.dma('sp', zw[0:64, :], zT[r0 + 64:r0 + 128, ts_], reads=[zT], writes=[zw])
                    p.dma('sp', zw[64:128, :], zT[r0:r0 + 64, ts_], reads=[zT], writes=[zw])
                    p.stt(t1, t1[:, :], z, z[:, :], scl, cos, cos[:, :], ALU.mult, ALU.mult)
                    p.stt(t2, t2[:, :], zw, zw[:, :], scl, sin, sin[:, :], ALU.mult, ALU.mult, e='dve')
                    ro = rot[which]
                    p.tt(['pool', 'dve'][which], ro, ro[:, :], t1, t1[:, :], t2, t2[:, :], ALU.add)
                    p.dma('pool', dstd[s, :, ts_], ro[:, :], reads=[ro], writes=[dstd])
                p.dma('sp', vf[:, :], zT[base + 256:base + 384, ts_], reads=[zT], writes=[vf])
                p.copy('act', vb, vb[:, :], vf, vf[:, :])
                for (srcb, dstd) in [(rot[1], io['ktok']), (vb, io['vtok'])]:
                    P_ = ptr[n % 2]; tk = tok[n % 2]; n += 1
                    for c4 in range(4):
                        p.tr(P_, P_[:, c4 * 128:(c4 + 1) * 128], srcb, srcb[:, c4 * 128:(c4 + 1) * 128], ident, ident[:, :])
                    p.act(tk, tk[:, :, :], P_, P_[:, :].rearrange("p (c d) -> p c d", c=4), AF.Copy)
                    p.dma('pool', dstd[s, tt * 4:(tt + 1) * 4].rearrange("c j d -> j c d"), tk[:, :, :], reads=[tk], writes=[dstd])
    with p.stage() as st:
        rc = st.sb("rc", [128, RC_N])
        p.dma('sp', rc[:, :], io['rconst'][:, :], reads=[io['rconst']], writes=[rc])
        Sf = [st.sb(f"Sb{s}", [128, 128]) for s in range(2)]
        Sb = [[st.sb(f"Sbb{s}_{i}", [128, 128], BF16) for i in range(2)] for s in range(2)]
        kt = [[st.sb(f"kt{s}_{i}", [128, 4, 128], BF16) for i in range(2)] for s in range(2)]
        vt = [[st.sb(f"vt{s}_{i}", [128, 4, 128], BF16) for i in range(2)] for s in range(2)]
        kd = [[st.sb(f"kd{s}_{i}", [128, 4, 128], BF16) for i in range(2)] for s in range(2)]
        pkv = [st.ps(f"pkv{i}", [128, 128]) for i in range(2)]
        for s in range(2):
            p.memset('dve', Sf[s], Sf[s][:, :], 0.0)
            p.memset('pool', Sb[s][0], Sb[s][0][:, :], 0.0)
            p.memset('pool', Sb[s][1], Sb[s][1][:, :], 0.0)
        for tt in range(NTT - 1, -1, -1):
            for s in range(2):
                o = 2 + s * RC_SLOT
                k_ = kt[s][tt % 2]; v_ = vt[s][tt % 2]; d_ = kd[s][tt % 2]
                p.dma('sp', k_[:, :, :], io['ktok'][s, tt * 4:(tt + 1) * 4].rearrange("c j d -> j c d"), reads=[io['ktok']], writes=[k_])
                p.dma('sp', v_[:, :, :], io['vtok'][s, tt * 4:(tt + 1) * 4].rearrange("c j d -> j c d"), reads=[io['vtok']], writes=[v_])
                p.act(d_, d_[:, :, :], k_, k_[:, :, :], AF.Identity, scale=rc[:, o + 1:o + 2], extra_reads=[rc])
                for c4 in range(3, -1, -1):
                    c = tt * 4 + c4
                    cur = Sb[s][c % 2]; nxt = Sb[s][(c + 1) % 2]
                    p.dma('pool', io['sbd'][s, c], cur[:, :], reads=[cur], writes=[io['sbd']])
                    if c == 0: continue
                    P_ = pkv[s]
                    p.mm(P_, P_[:, :], d_, d_[:, c4, :], v_, v_[:, c4, :])
                    p.stt(Sf[s], Sf[s][:, :], Sf[s], Sf[s][:, :], rc[:, o + 2:o + 3], P_, P_[:, :], ALU.mult, ALU.add, extra_reads=[rc])
                    p.act(nxt, nxt[:, :], Sf[s], Sf[s][:, :], AF.Copy)
    with p.stage() as st:
        rc = st.sb("rc", [128, RC_N])
        p.dma('sp', rc[:, :], io['rconst'][:, :], reads=[io['rconst']], writes=[rc])
        ones = st.sb("ones", [128, 128]); p.memset('dve', ones, ones[:, :], 1.0)
        Sf = [st.sb(f"Sf{s}", [128, 128]) for s in range(2)]
        Sfb = [[st.sb(f"Sfb{s}_{i}", [128, 128], BF16) for i in range(2)] for s in range(2)]
        bufs = {}
        for s in range(2):
            for nm, shp, dt in [('q', [128, 512], BF16), ('k', [128, 512], BF16), ('kt', [128, 4, 128], BF16), ('vt', [128, 4, 128], BF16),
                                ('sb', [128, 4, 128], BF16), ('g', [128, 512], F32), ('qf', [128, 512], BF16), ('qb', [128, 512], BF16),
                                ('kd', [128, 4, 128], BF16), ('scm', [128, 512], BF16), ('sq', [128, 512], F32), ('rs', [128, 512], F32),
                                ('sg', [128, 512], F32), ('t', [128, 512], F32), ('o', [128, 512], BF16)]:
                bufs[(s, nm)] = st.sb(f"r3{nm}{s}", shp, dt)
        psc = [st.ps(f"psc{i}", [128, 128]) for i in range(2)]
        pyT = [st.ps(f"pyT{i}", [128, 512]) for i in range(2)]
        pkv = [st.ps(f"pkv3{i}", [128, 128]) for i in range(2)]
        pss = st.ps("pss", [128, 512])
        for s in range(2):
            p.memset('dve', Sf[s], Sf[s][:, :], 0.0)
            p.memset('pool', Sfb[s][0], Sfb[s][0][:, :], 0.0)
        for tt in range(NTT):
            ts_ = slice(tt * 512, (tt + 1) * 512)
            r = (tt * 512) // Q4; off = tt * 512 - r * Q4
            for s in range(2):
                o = 2 + s * RC_SLOT
                B = lambda nm: bufs[(s, nm)]
                p.dma('sp', B('q')[:, :], io['qrT'][s, :, ts_], reads=[io['qrT']], writes=[B('q')])
                p.dma('sp', B('k')[:, :], io['krT'][s, :, ts_], reads=[io['krT']], writes=[B('k')])
                p.dma('sp', B('kt')[:, :, :], io['ktok'][s, tt * 4:(tt + 1) * 4].rearrange("c j d -> j c d"), reads=[io['ktok']], writes=[B('kt')])
                p.dma('sp', B('vt')[:, :, :], io['vtok'][s, tt * 4:(tt + 1) * 4].rearrange("c j d -> j c d"), reads=[io['vtok']], writes=[B('vt')])
                p.dma('sp', B('sb')[:, :, :], io['sbd'][s, tt * 4:(tt + 1) * 4].rearrange("c d e -> d c e"), reads=[io['sbd']], writes=[B('sb')])
                p.dma('sp', B('g')[:, :], zT[s * 512 + 384:s * 512 + 512, ts_], reads=[zT], writes=[B('g')])
                p.tt('pool', B('qf'), B('qf')[:, :], B('q'), B('q')[:, :], rc, rc[:, o + 3 + 128:o + 3 + 128 + 512], ALU.mult)
                p.tt('dve', B('qb'), B('qb')[:, :], B('q'), B('q')[:, :], rc, rc[:, o + 3 + 640:o + 3 + 640 + 512], ALU.mult)
                p.act(B('kd'), B('kd')[:, :, :], B('kt'), B('kt')[:, :, :], AF.Identity, scale=rc[:, o:o + 1], extra_reads=[rc])
                p.act(B('sg'), B('sg')[:, :], B('g'), B('g')[:, :], AF.Silu)
                Y = pyT[s]
                for c4 in range(4):
                    c = tt * 4 + c4
                    cs = slice(c4 * 128, (c4 + 1) * 128)
                    cur = Sfb[s][c % 2]; nxt = Sfb[s][(c + 1) % 2]
                    SC = psc[c % 2]
                    p.mm(SC, SC[:, :], B('k'), B('k')[:, cs], B('q'), B('q')[:, cs])
                    p.tt('dve', B('scm'), B('scm')[:, cs], SC, SC[:, :], rc, rc[:, o + 3:o + 3 + 128], ALU.mult)
                    p.mm(Y, Y[:, cs], B('vt'), B('vt')[:, c4, :], B('scm'), B('scm')[:, cs], start=True, stop=False)
                    p.mm(Y, Y[:, cs], cur, cur[:, :], B('qf'), B('qf')[:, cs], start=False, stop=False)
                    p.mm(Y, Y[:, cs], B('sb'), B('sb')[:, c4, :], B('qb'), B('qb')[:, cs], start=False, stop=True)
                    if c < NCK - 1:
                        P_ = pkv[s]
                        p.mm(P_, P_[:, :], B('kd'), B('kd')[:, c4, :], B('vt'), B('vt')[:, c4, :])
                        p.stt(Sf[s], Sf[s][:, :], Sf[s], Sf[s][:, :], rc[:, o + 2:o + 3], P_, P_[:, :], ALU.mult, ALU.add, extra_reads=[rc])
                        p.act(nxt, nxt[:, :], Sf[s], Sf[s][:, :], AF.Copy)
                p.act(B('sq'), B('sq')[:, :], Y, Y[:, :], AF.Square)
                p.mm(pss, pss[:, :], ones, ones[:, :], B('sq'), B('sq')[:, :])
                p.act(B('rs'), B('rs')[:, :], pss, pss[:, :], AF.Sqrt, bias=1e-6, scale=1.0 / 128)
                p.op('dve', lambda: nc.vector.reciprocal(out=B('rs')[:, :], in_=B('rs')[:, :]), reads=[B('rs')], writes=[B('rs')])
                p.tt('dve', B('t'), B('t')[:, :], Y, Y[:, :], B('rs'), B('rs')[:, :], ALU.mult)
                p.tt('pool', B('o'), B('o')[:, :], B('t'), B('t')[:, :], B('sg'), B('sg')[:, :], ALU.mult)
                p.dma('pool', io['yT'][r, s * 128:(s + 1) * 128, off:off + 512], B('o')[:, :], reads=[B('o')], writes=[io['yT']])

ZS5 = 1984
def sin_reduce(p, out, src, shift, ki, kf, tmp, scale=1.0, extra_reads=()):
    sl = tuple(slice(None) for _ in src.ap.shape)
    if shift != 0.0:
        p.ts('pool', tmp, tmp[sl], src, src[sl], shift, None, ALU.add)
        s = tmp
    else:
        s = src
    p.ts('dve', ki, ki[sl], s, s[sl], INV2PI, None, ALU.mult)
    p.copy('pool', kf, kf[sl], ki, ki[sl])
    p.stt(tmp, tmp[sl], kf, kf[sl], -C1, s, s[sl], ALU.mult, ALU.add)
    p.stt(tmp, tmp[sl], kf, kf[sl], -C2, tmp, tmp[sl], ALU.mult, ALU.add)
    p.ts('pool', tmp, tmp[sl], tmp, tmp[sl], -PI, PI, ALU.max, ALU.min)
    p.act(out, out[sl], tmp, tmp[sl], AF.Sin, scale=scale, extra_reads=extra_reads)

def s5_host_layout(q, lam_re, lam_im, log_dt, b_re, b_im, c_re, c_im, d_skip):
    import numpy as np
    gs = slice(8 * q, 8 * q + 8)
    lr = lam_re[:, gs, :]; li = lam_im[:, gs, :]; ld = log_dt[:, gs]
    rows = np.stack([lr.reshape(-1), li.reshape(-1), np.repeat(ld.reshape(-1), 64)], 0).astype(np.float32)
    def col(a):
        return np.ascontiguousarray(a.reshape(2, 4, 2, 64).transpose(2, 3, 0, 1).reshape(128, 8))
    cols = np.concatenate([col(lr), col(li), col(np.repeat(ld[:, :, None], 64, axis=2))], 1).astype(np.float32)
    bl = np.zeros((2, 128, 2, 4, 128), np.float32)
    cl = np.zeros((2, 128, 2, 4, 128), np.float32)
    for d in range(2):
        for g in range(8):
            j, gp = g // 2, g % 2
            for ri, (bsrc, csrc) in enumerate([(b_re, c_re), (b_im, c_im)]):
                bl[ri, g * 16:(g + 1) * 16, d, j, gp * 64:(gp + 1) * 64] = bsrc[d, 8 * q + g].T
                cl[ri, gp * 64:(gp + 1) * 64, d, j, g * 16:(g + 1) * 16] = csrc[d, 8 * q + g].T
    dcol = d_skip[128 * q:128 * (q + 1)].reshape(128, 1).astype(np.float32)
    return dict(s5rows=rows, s5cols=cols, s5bl=bl.reshape(2, 128, 1024), s5cl=cl.reshape(2, 128, 1024), s5d=dcol,
                s5iota=np.tile(np.arange(512, dtype=np.float32)[None, :], (128, 1)))

def stage_s5(p, S, io):
    nc = p.nc
    NTT = S // 512; Q4 = S // 4; TC = 512
    zT = io['zT']
    with p.stage() as st:
        bb = [st.sb(f"bb{i}", [128, 1024], BF16) for i in range(2)]
        cc = [st.sb(f"cc{i}", [128, 1024], BF16) for i in range(2)]
        rho = st.sb("rho", [128, 8]); cT = st.sb("cT", [128, 8]); sT = st.sb("sT", [128, 8]); thc = st.sb("thc", [128, 8])
        dcol = st.sb("dcol", [128, 1])
        p.dma('sp', dcol[:, :], io['s5d'][:, :], reads=[io['s5d']], writes=[dcol])
        with p.stage() as s2:
            f = lambda nm: s2.sb(nm, [128, 1024])
            lr, li, ld, dt, mag, th, sn, cs, t1, t2, t3, kf, tmp, cr, ci = [f(n) for n in
                ['lr', 'li', 'ld', 'dt', 'mag', 'th', 'sn', 'cs', 't1', 't2', 't3', 'kf', 'tmp', 'cr', 'ci']]
            ki = s2.sb("ki", [128, 1024], I32)
            for k, t in enumerate([lr, li, ld]):
                p.dma('sp', t[:, :], io['s5rows'][k:k + 1, :].partition_broadcast(128), reads=[io['s5rows']], writes=[t])
            p.act(dt, dt[:, :], ld, ld[:, :], AF.Exp)
            p.tt('dve', t1, t1[:, :], lr, lr[:, :], dt, dt[:, :], ALU.mult)
            p.act(mag, mag[:, :], t1, t1[:, :], AF.Exp)
            p.tt('dve', th, th[:, :], li, li[:, :], dt, dt[:, :], ALU.mult)
            sin_reduce(p, sn, th, 0.0, ki, kf, tmp)
            sin_reduce(p, cs, th, PI / 2, ki, kf, tmp)
            p.tt('dve', cs, cs[:, :], cs, cs[:, :], mag, mag[:, :], ALU.mult)
            p.tt('dve', sn, sn[:, :], sn, sn[:, :], mag, mag[:, :], ALU.mult)
            p.ts('dve', cs, cs[:, :], cs, cs[:, :], -1.0, None, ALU.add)
            p.tt('dve', t1, t1[:, :], lr, lr[:, :], lr, lr[:, :], ALU.mult)
            p.tt('dve', t2, t2[:, :], li, li[:, :], li, li[:, :], ALU.mult)
            p.tt('dve', t1, t1[:, :], t1, t1[:, :], t2, t2[:, :], ALU.add)
            p.op('dve', lambda: nc.vector.reciprocal(out=t1[:, :], in_=t1[:, :]), reads=[t1], writes=[t1])
            p.tt('dve', t2, t2[:, :], cs, cs[:, :], lr, lr[:, :], ALU.mult)
            p.tt('dve', t3, t3[:, :], sn, sn[:, :], li, li[:, :], ALU.mult)
            p.tt('dve', t2, t2[:, :], t2, t2[:, :], t3, t3[:, :], ALU.add)
            p.tt('dve', cr, cr[:, :], t2, t2[:, :], t1, t1[:, :], ALU.mult)
            p.tt('dve', t2, t2[:, :], sn, sn[:, :], lr, lr[:, :], ALU.mult)
            p.tt('dve', t3, t3[:, :], cs, cs[:, :], li, li[:, :], ALU.mult)
            p.tt('dve', t2, t2[:, :], t2, t2[:, :], t3, t3[:, :], ALU.subtract)
            p.tt('dve', ci, ci[:, :], t2, t2[:, :], t1, t1[:, :], ALU.mult)
            br = lr; bi = li
            p.dma('sp', br[:, :], io['s5bl'][0], reads=[io['s5bl']], writes=[br])
            p.dma('sp', bi[:, :], io['s5bl'][1], reads=[io['s5bl']], writes=[bi])
            p.tt('dve', t1, t1[:, :], cr, cr[:, :], br, br[:, :], ALU.mult)
            p.tt('dve', t2, t2[:, :], ci, ci[:, :], bi, bi[:, :], ALU.mult)
            p.tt('dve', bb[0], bb[0][:, :], t1, t1[:, :], t2, t2[:, :], ALU.subtract)
            p.tt('dve', t1, t1[:, :], cr, cr[:, :], bi, bi[:, :], ALU.mult)
            p.tt('dve', t2, t2[:, :], ci, ci[:, :], br, br[:, :], ALU.mult)
            p.tt('dve', bb[1], bb[1][:, :], t1, t1[:, :], t2, t2[:, :], ALU.add)
            p.dma('sp', t1[:, :], io['s5cl'][0], reads=[io['s5cl']], writes=[t1])
            p.dma('sp', t2[:, :], io['s5cl'][1], reads=[io['s5cl']], writes=[t2])
            p.copy('dve', cc[0], cc[0][:, :], t1, t1[:, :])
            p.ts('dve', cc[1], cc[1][:, :], t2, t2[:, :], -1.0, None, ALU.mult)
            c24 = s2.sb("c24", [128, 24]); dtc = s2.sb("dtc", [128, 8]); tq = s2.sb("tq", [128, 8]); tq2 = s2.sb("tq2", [128, 8])
            ki8 = s2.sb("ki8", [128, 8], I32); kf8 = s2.sb("kf8", [128, 8]); tmp8 = s2.sb("tmp8", [128, 8])
            p.dma('sp', c24[:, :], io['s5cols'][:, :], reads=[io['s5cols']], writes=[c24])
            p.act(dtc, dtc[:, :], c24, c24[:, 16:24], AF.Exp)
            p.tt('dve', tq, tq[:, :], c24, c24[:, 0:8], dtc, dtc[:, :], ALU.mult)
            p.act(rho, rho[:, :], tq, tq[:, :], AF.Exp)
            p.tt('dve', thc, thc[:, :], c24, c24[:, 8:16], dtc, dtc[:, :], ALU.mult)
            p.ts('dve', tq2, tq2[:, :], thc, thc[:, :], float(TC), None, ALU.mult)
            sin_reduce(p, sT, tq2, 0.0, ki8, kf8, tmp8)
            sin_reduce(p, cT, tq2, PI / 2, ki8, kf8, tmp8)
        iota = st.sb("iota", [128, TC]); p.dma('sp', iota[:, :], io['s5iota'][:, :], reads=[io['s5iota']], writes=[iota])
        cosT = [st.sb(f"cost{k}", [128, TC]) for k in range(8)]; sinT = [st.sb(f"sint{k}", [128, TC]) for k in range(8)]
        rhoT = [st.sb(f"rhot{k}", [128, TC]) for k in range(8)]
        ang = st.sb("ang", [128, TC]); kiT = st.sb("kiT", [128, TC], I32); kfT = st.sb("kfT", [128, TC]); tmpT = st.sb("tmpT", [128, TC])
        tb = st.sb("tb", [128, TC])
        for k in range(8):
            p.ts('dve', ang, ang[:, :], iota, iota[:, :], thc[:, k:k + 1], None, ALU.mult, extra_reads=[thc])
            for (shift, dst) in [(0.0, sinT[k]), (PI / 2, cosT[k])]:
                if k < 4:
                    sin_reduce(p, dst, ang, shift, kiT, kfT, tmpT)
                else:
                    sin_reduce(p, tb, ang, shift, kiT, kfT, tmpT)
                    p.copy('dve', dst, dst[:, :], tb, tb[:, ::-1])
            p.ts('dve', rhoT[k], rhoT[k][:, :], iota, iota[:, :], 0.0, rho[:, k:k + 1], ALU.mult, ALU.add, extra_reads=[rho])
        uf = [st.sb(f"uf{i}", [128, TC]) for i in range(2)]; ub = [st.sb(f"ub{i}", [128, TC], BF16) for i in range(2)]
        brs = [st.sb(f"brs{i}", [128, TC]) for i in range(2)]; bis = [st.sb(f"bis{i}", [128, TC]) for i in range(2)]
        T1 = [st.sb(f"t1_{i}", [128, TC]) for i in range(2)]; T2 = [st.sb(f"t2_{i}", [128, TC]) for i in range(2)]
        T3 = [st.sb(f"t3_{i}", [128, TC]) for i in range(2)]; T4 = [st.sb(f"t4_{i}", [128, TC]) for i in range(2)]
        MRE = [st.sb(f"mre{i}", [128, TC]) for i in range(2)]; MIM = [st.sb(f"mim{i}", [128, TC]) for i in range(2)]
        TN = [st.sb(f"tn{i}", [128, 4]) for i in range(2)]
        xr = [st.sb(f"xr{i}", [128, TC]) for i in range(2)]; xi = [st.sb(f"xi{i}", [128, TC]) for i in range(2)]
        xre = [st.sb(f"xre{i}", [128, TC], BF16) for i in range(2)]; xim = [st.sb(f"xim{i}", [128, TC], BF16) for i in range(2)]
        init = st.sb("init", [128, 16]); p.memset('dve', init, init[:, :], 0.0)
        tn = st.sb("tn", [128, 4])
        yo = [st.sb(f"yo{i}", [128, TC]) for i in range(2)]
        yfl = st.sb("yfl", [128, TC]); yb16 = [st.sb(f"yb16{i}", [128, TC], BF16) for i in range(2)]
        pb_re = [st.ps(f"pbre{i}", [128, TC]) for i in range(2)]; pb_im = [st.ps(f"pbim{i}", [128, TC]) for i in range(2)]
        py = [st.ps(f"py{i}", [128, TC]) for i in range(2)]
        n = 0
        for d in range(2):
            order = range(NTT) if d == 0 else range(NTT - 1, -1, -1)
            for it, tt in enumerate(order):
                ts_ = slice(tt * TC, (tt + 1) * TC)
                r = (tt * TC) // Q4; off = tt * TC - r * Q4
                u_f = uf[it % 2]; u_b = ub[it % 2]
                p.dma('sp', u_f[:, :], zT[ZS5:ZS5 + 128, ts_], reads=[zT], writes=[u_f])
                p.copy('pool', u_b, u_b[:, :], u_f, u_f[:, :])
                Y = py[it % 2]
                if d == 0:
                    vw = lambda a: a[:, :]
                    last = slice(TC - 1, TC)
                else:
                    vw = lambda a: a[:, ::-1]
                    last = slice(0, 1)
                for jp in ((0, 1), (2, 3)):
                    ks_ = [d * 4 + j for j in jp]
                    for u2, k in enumerate(ks_):
                        blk = slice(k * 128, (k + 1) * 128)
                        p.mm(pb_re[u2], pb_re[u2][:, :], bb[0], bb[0][:, blk], u_b, u_b[:, :])
                        p.mm(pb_im[u2], pb_im[u2][:, :], bb[1], bb[1][:, blk], u_b, u_b[:, :])
                        p.act(brs[u2], brs[u2][:, :], pb_re[u2], pb_re[u2][:, :], AF.Copy)
                        p.act(bis[u2], bis[u2][:, :], pb_im[u2], pb_im[u2][:, :], AF.Copy)
                    for u2, k in enumerate(ks_):
                        p.tt('dve', T1[u2], T1[u2][:, :], brs[u2], brs[u2][:, :], cosT[k], cosT[k][:, :], ALU.mult)
                        p.tt('pool', T2[u2], T2[u2][:, :], bis[u2], bis[u2][:, :], sinT[k], sinT[k][:, :], ALU.mult)
                        p.tt('dve', T3[u2], T3[u2][:, :], bis[u2], bis[u2][:, :], cosT[k], cosT[k][:, :], ALU.mult)
                        p.tt('dve', T4[u2], T4[u2][:, :], brs[u2], brs[u2][:, :], sinT[k], sinT[k][:, :], ALU.mult)
                    for u2, k in enumerate(ks_):
                        p.tt('dve', MRE[u2], MRE[u2][:, :], T1[u2], T1[u2][:, :], T2[u2], T2[u2][:, :], ALU.add)
                        p.tt('pool', MIM[u2], MIM[u2][:, :], T3[u2], T3[u2][:, :], T4[u2], T4[u2][:, :], ALU.subtract)
                    for u2, k in enumerate(ks_):
                        x_r = xr[u2]; x_i = xi[u2]; mre_ = MRE[u2]; mim_ = MIM[u2]
                        p.op('dve', lambda: nc.vector.tensor_tensor_scan(out=vw(x_r), data0=rhoT[k][:, :], data1=vw(mre_), initial=init[:, k:k + 1],
                                                                          op0=ALU.mult, op1=ALU.add), reads=[rhoT[k], mre_, init], writes=[x_r])
                        p.op('dve', lambda: nc.vector.tensor_tensor_scan(out=vw(x_i), data0=rhoT[k][:, :], data1=vw(mim_), initial=init[:, 8 + k:9 + k],
                                                                          op0=ALU.mult, op1=ALU.add), reads=[rhoT[k], mim_, init], writes=[x_i])
                    for u2, k in enumerate(ks_):
                        x_r = xr[u2]; x_i = xi[u2]; tn_ = TN[u2]
                        p.ts('pool', tn_, tn_[:, 0:1], x_r, x_r[:, last], cT[:, k:k + 1], None, ALU.mult, extra_reads=[cT])
                        p.ts('pool', tn_, tn_[:, 1:2], x_r, x_r[:, last], sT[:, k:k + 1], None, ALU.mult, extra_reads=[sT])
                        p.stt(tn_, tn_[:, 2:3], x_i, x_i[:, last], sT[:, k:k + 1], tn_, tn_[:, 0:1], ALU.mult, ALU.subtract, extra_reads=[sT])
                        p.ts('pool', init, init[:, k:k + 1], tn_, tn_[:, 2:3], -1.0, None, ALU.mult)
                        p.stt(init, init[:, 8 + k:9 + k], x_i, x_i[:, last], cT[:, k:k + 1], tn_, tn_[:, 1:2], ALU.mult, ALU.add, extra_reads=[cT])
                    for u2, k in enumerate(ks_):
                        x_r = xr[u2]; x_i = xi[u2]
                        p.tt('dve', T1[u2], T1[u2][:, :], x_r, x_r[:, :], cosT[k], cosT[k][:, :], ALU.mult)
                        p.tt('pool', T2[u2], T2[u2][:, :], x_i, x_i[:, :], sinT[k], sinT[k][:, :], ALU.mult)
                        p.tt('dve', T3[u2], T3[u2][:, :], x_r, x_r[:, :], sinT[k], sinT[k][:, :], ALU.mult)
                        p.tt('dve', T4[u2], T4[u2][:, :], x_i, x_i[:, :], cosT[k], cosT[k][:, :], ALU.mult)
                    for u2, k in enumerate(ks_):
                        p.tt('dve', xre[u2], xre[u2][:, :], T1[u2], T1[u2][:, :], T2[u2], T2[u2][:, :], ALU.subtract)
                        p.tt('pool', xim[u2], xim[u2][:, :], T3[u2], T3[u2][:, :], T4[u2], T4[u2][:, :], ALU.add)
                    for u2, k in enumerate(ks_):
                        blk = slice(k * 128, (k + 1) * 128)
                        j = jp[u2]
                        p.mm(Y, Y[:, :], cc[0], cc[0][:, blk], xre[u2], xre[u2][:, :], start=(j == 0), stop=False)
                        p.mm(Y, Y[:, :], cc[1], cc[1][:, blk], xim[u2], xim[u2][:, :], start=False, stop=(j == 3))
                if d == 0:
                    y_o = yo[it % 2]
                    p.act(y_o, y_o[:, :], Y, Y[:, :], AF.Copy)
                    p.dma('pool', io['yf'][:, ts_], y_o[:, :], reads=[y_o], writes=[io['yf']])
                else:
                    y_o = yo[it % 2]; yb = yb16[it % 2]
                    p.dma('sp', yfl[:, :], io['yf'][:, ts_], reads=[io['yf']], writes=[yfl])
                    p.tt('dve', y_o, y_o[:, :], Y, Y[:, :], yfl, yfl[:, :], ALU.add)
                    p.stt(y_o, y_o[:, :], u_f, u_f[:, :], dcol[:, 0:1], y_o, y_o[:, :], ALU.mult, ALU.add, extra_reads=[dcol])
                    p.act(yb, yb[:, :], y_o, y_o[:, :], AF.Gelu_apprx_tanh)
                    p.dma('pool', io['yT'][r, 448:576, off:off + TC], yb[:, :], reads=[yb], writes=[io['yT']])

ZR, ZK, ZV, ZWD, ZAD, ZGD = 1024, 1216, 1408, 1600, 1728, 1856
RW_SU, RW_SL, RW_U, RW_L, RW_I, RW_BLK, RW_MF, RW_MB, RW_N = 0, 768, 1536, 2304, 3072, 3840, 3968, 4480, 4992
NEG_EXP_HALF = -math.exp(-0.5)

def rwkv_consts():
    import numpy as np
    i = np.arange(128)
    su = (i[:, None] < i[None, :]).astype(np.float32); sl = su.T.copy()
    u = (i[:, None] <= i[None, :]).astype(np.float32); l = u.T.copy()
    I = np.eye(128, dtype=np.float32)
    blk = np.zeros((128, 128), np.float32); blk[:64, :64] = 1; blk[64:, 64:] = 1
    t = np.arange(512)
    mf = (t % 128 != 0).astype(np.float32); mb = (t % 128 != 127).astype(np.float32)
    return np.concatenate([np.tile(su, (1, 6)), np.tile(sl, (1, 6)), np.tile(u, (1, 6)), np.tile(l, (1, 6)), np.tile(I, (1, 6)), blk,
                           np.tile(mf[None], (128, 1)), np.tile(mb[None], (128, 1))], axis=1).astype(np.float32)

def rwkv_host_layout(q, mu_prev, mu_next, w0, w_up, a0, a_up, g_up, k_k, k_a, r_k, lnx_w, lnx_b):
    import numpy as np
    rwp = np.zeros((128, 55), np.float32)
    rkf = r_k.reshape(-1)
    for hh in range(3):
        ch = q * 192 + hh * 64 + np.arange(64)
        b = hh * 15
        for j, part in enumerate([0, 768, 1536]):
            rwp[:64, b + 2 * j] = mu_prev[part + ch]; rwp[:64, b + 2 * j + 1] = mu_next[part + ch]
        rwp[:64, b + 6] = k_k[ch]; rwp[:64, b + 7] = k_a[ch]; rwp[:64, b + 8] = rkf[ch]; rwp[:64, b + 9] = lnx_w[ch]; rwp[:64, b + 10] = lnx_b[ch]
        rwp[:64, b + 11] = w0[0, ch]; rwp[:64, b + 12] = w0[1, ch]; rwp[:64, b + 13] = a0[0, ch]; rwp[:64, b + 14] = a0[1, ch]
    for j, part in enumerate([2304, 2432]):
        for d in range(2):
            rwp[:64, 45 + 4 * j + 2 * d] = mu_prev[part + d * 64:part + (d + 1) * 64]
            rwp[:64, 46 + 4 * j + 2 * d] = mu_next[part + d * 64:part + (d + 1) * 64]
    rwp[:, 53] = mu_prev[2560:2688]; rwp[:, 54] = mu_next[2560:2688]
    cs = slice(q * 192, (q + 1) * 192)
    return dict(rwp=rwp, rw_wup=np.ascontiguousarray(np.stack([w_up[0][:, cs], w_up[1][:, cs]], 1)),
                rw_aup=np.ascontiguousarray(np.stack([a_up[0][:, cs], a_up[1][:, cs]], 1)),
                rw_gup=np.ascontiguousarray(g_up[:, cs]))

RW_DEBUG = [None]
def stage_rwkv(p, S, io):
    nc = p.nc
    NTT = S // 512; Q4 = S // 4
    zT = io['zT']
    H3 = range(3)
    with p.stage() as st:
        rwp = st.sb("rwp", [128, 55]); p.dma('sp', rwp[:, :], io['rwp'][:, :], reads=[io['rwp']], writes=[rwp])
        cst = st.sb("rwc", [128, RW_N]); p.dma('sp', cst[:, :], io['rwconst'][:, :], reads=[io['rwconst']], writes=[cst])
        ident_f = st.sb("ident_f", [128, 128]); ident = st.sb("ident", [128, 128], BF16)
        p.dma('sp', ident_f[:, :], io['ident'][:, :], reads=[io['ident']], writes=[ident_f])
        p.copy('dve', ident, ident[:, :], ident_f, ident_f[:, :])
        wtmp = st.sb("wtmp", [128, 384])
        wup = st.sb("wupb", [64, 2, 192], BF16); aup = st.sb("aupb", [64, 2, 192], BF16); gup = st.sb("gupb", [128, 192], BF16)
        p.dma('sp', wtmp[0:64, :], io['rw_wup'].ap.rearrange("r d c -> r (d c)"), reads=[io['rw_wup']], writes=[wtmp])
        p.copy('dve', wup, wup[:, :, :], wtmp, wtmp[0:64, :].rearrange("r (d c) -> r d c", d=2))
        p.dma('sp', wtmp[0:64, :], io['rw_aup'].ap.rearrange("r d c -> r (d c)"), reads=[io['rw_aup']], writes=[wtmp])
        p.copy('dve', aup, aup[:, :, :], wtmp, wtmp[0:64, :].rearrange("r (d c) -> r d c", d=2))
        p.dma('sp', wtmp[:, 0:192], io['rw_gup'][:, :], reads=[io['rw_gup']], writes=[wtmp])
        p.copy('dve', gup, gup[:, :], wtmp, wtmp[:, 0:192])
        c0 = st.sb("c0", [128, 14])
        pairs = [0, 2, 4, 15, 17, 19, 30, 32, 34, 45, 47, 49, 51, 53]
        for i, col in enumerate(pairs):
            p.tt('dve', c0, c0[:, i:i + 1], rwp, rwp[:, col:col + 1], rwp, rwp[:, col + 1:col + 2], ALU.add)
        p.ts('dve', c0, c0[:, :], c0, c0[:, :], -1.0, 1.0, ALU.mult, ALU.add)
        def fb(nm, P_=64, n=512, dt=F32): return st.sb(nm, [P_, n], dt)
        zh = [fb(f"zh{i}", 128, 514) for i in range(3)]
        sh = {}
        for hh in H3:
            for nm in ['r', 'k', 'v']:
                sh[(hh, nm)] = fb(f"sh{nm}{hh}")
        shwd = fb("shwd"); shad = fb("shad"); shgd = fb("shgd", 128)
        twd = fb("twd", dt=BF16); adb = fb("adb", dt=BF16); sgd = fb("sgd", 128, dt=BF16)
        lw = [fb(f"lw{h}") for h in H3]; cin = [fb(f"cin{h}") for h in H3]; cex = [fb(f"cex{h}") for h in H3]
        Ein = [fb(f"Ein{h}") for h in H3]; Eex = [fb(f"Eex{h}") for h in H3]; Eni = [fb(f"Eni{h}") for h in H3]
        aT = [fb(f"aT{h}") for h in H3]; kk = [fb(f"kk{h}") for h in H3]
        tA = [fb(f"tA{h}") for h in H3]; tB = [fb(f"tB{h}") for h in H3]
        at_ = [fb(f"at{h}", dt=BF16) for h in H3]; bt_ = [fb(f"bt{h}", dt=BF16) for h in H3]
        kt_ = [fb(f"kt{h}", dt=BF16) for h in H3]; rt_ = [fb(f"rt{h}", dt=BF16) for h in H3]
        vb_ = [fb(f"vb{h}", dt=BF16) for h in H3]
        tok = [st.sb(f"tok{i}", [128, 4, 192], BF16) for i in range(4)]
        W3 = lambda nm, dt=BF16: st.sb(nm, [128, 768], dt)
        M = [W3(f"M{i}") for i in range(2)]; N = [W3(f"N{i}") for i in range(2)]
        Pm = [W3(f"P{i}") for i in range(2)]; Qm = [W3(f"Q{i}") for i in range(2)]
        AKT = W3("AKT"); RBT = W3("RBT"); RKT = W3("RKT")
        AKV = st.sb("AKV", [128, 384], BF16); UV = st.sb("UV", [128, 384]); U = st.sb("U", [128, 192], BF16)
        TAT = st.sb("TAT", [64, 768], BF16)
        KVW = st.sb("KVW", [64, 384])
        S0 = st.sb("S0", [64, 192])
        S0b = [st.sb(f"S0b{i}", [64, 192], BF16) for i in range(2)]
        tS = st.sb("tS", [64, 192])
        ydall = st.sb("ydall", [64, 3, 512]); y0l = [fb(f"y0l{h}") for h in H3]
        pp1 = [fb(f"pp1{h}") for h in H3]; pp2 = [fb(f"pp2{h}") for h in H3]; pp3 = [fb(f"pp3{h}") for h in H3]
        yob = [fb(f"yob{h}", dt=BF16) for h in H3]
        pg = [st.ps(f"pg{i}", [128, 1024]) for i in range(2)]
        pch = st.ps("pch", [128, 512])
        pY = [st.ps(f"pY{i}", [128, 512]) for i in range(2)]
        ptk = st.ps("ptk", [128, 1024], BF16)
        gcount = [0]
        def PG():
            gcount[0] += 1
            return pg[gcount[0] % 2]
        ones64 = cst[0:64, RW_BLK:RW_BLK + 64]
        def blocksum(src):
            G = PG()
            p.mm(G, G[0:64, 0:512], cst, ones64, src, src[0:64, :])
            return G

        for d in range(2):
            p.memset('dve', S0, S0[:, :], 0.0)
            p.memset('pool', S0b[0], S0b[0][:, :], 0.0)
            p.memset('pool', S0b[1], S0b[1][:, :], 0.0)
            order = list(range(NTT)) if d == 0 else list(range(NTT - 1, -1, -1))
            mSU = RW_SU if d == 0 else RW_SL; mSL = RW_SL if d == 0 else RW_SU; mU = RW_U if d == 0 else RW_L
            gchunk = 0
            for tt in order:
                t0 = tt * 512
                r_ = t0 // Q4; off = t0 - r_ * Q4
                hbc = [0]
                def shift(row0, P_, dst, c0col, mpcol):
                    z = zh[hbc[0] % 3]; hbc[0] += 1
                    lo = max(t0 - 1, 0); hi = min(t0 + 513, S)
                    if t0 == 0: p.memset('pool', z, z[0:P_, 0:1], 0.0)
                    if t0 + 513 > S: p.memset('pool', z, z[0:P_, 513:514], 0.0)
                    p.dma('sp', z[0:P_, lo - (t0 - 1):hi - (t0 - 1)], zT[row0:row0 + P_, lo:hi], reads=[zT], writes=[z])
                    p.act(dst, dst[0:P_, :], z, z[0:P_, 1:513], AF.Identity, scale=c0[0:P_, c0col:c0col + 1], extra_reads=[c0])
                    p.stt(dst, dst[0:P_, :], z, z[0:P_, 0:512], rwp[0:P_, mpcol:mpcol + 1], dst, dst[0:P_, :], ALU.mult, ALU.add, extra_reads=[rwp])
                    p.stt(dst, dst[0:P_, :], z, z[0:P_, 2:514], rwp[0:P_, mpcol + 1:mpcol + 2], dst, dst[0:P_, :], ALU.mult, ALU.add, extra_reads=[rwp])
                for hh in H3:
                    for j, (nm, zrow) in enumerate([('r', ZR), ('k', ZK), ('v', ZV)]):
                        shift(zrow + hh * 64, 64, sh[(hh, nm)], hh * 3 + j, hh * 15 + 2 * j)
                shift(ZWD + d * 64, 64, shwd, 9 + d, 45 + 2 * d)
                shift(ZAD + d * 64, 64, shad, 11 + d, 49 + 2 * d)
                p.act(twd, twd[:, :], shwd, shwd[:, :], AF.Tanh)
                p.copy('act', adb, adb[:, :], shad, shad[:, :])
                if d == 1:
                    shift(ZGD, 128, shgd, 13, 53)
                    p.act(sgd, sgd[:, :], shgd, shgd[:, :], AF.Sigmoid)
                for hh in H3:
                    b = hh * 15
                    cs_ = slice(hh * 64, (hh + 1) * 64)
                    G = PG()
                    p.mm(G, G[0:64, 0:512], wup, wup[:, d, cs_], twd, twd[:, :])
                    p.act(lw[hh], lw[hh][:, :], G, G[0:64, 0:512], AF.Sigmoid, bias=rwp[0:64, b + 11 + d:b + 12 + d], extra_reads=[rwp])
                    if d == 0:
                        p.op('dve', lambda: nc.vector.tensor_tensor_scan(out=cin[hh][:, :], data0=cst[0:64, RW_MF:RW_MF + 512], data1=lw[hh][:, :],
                                                                          initial=0.0, op0=ALU.mult, op1=ALU.add), reads=[cst, lw[hh]], writes=[cin[hh]])
                    else:
                        mbv = cst[0:64, RW_MB:RW_MB + 512]
                        p.op('dve', lambda: nc.vector.tensor_tensor_scan(out=cin[hh][:, ::-1], data0=mbv[:, ::-1], data1=lw[hh][:, ::-1],
                                                                          initial=0.0, op0=ALU.mult, op1=ALU.add), reads=[cst, lw[hh]], writes=[cin[hh]])
                    p.tt('dve', cex[hh], cex[hh][:, :], cin[hh], cin[hh][:, :], lw[hh], lw[hh][:, :], ALU.subtract)
                    p.act(Ein[hh], Ein[hh][:, :], cin[hh], cin[hh][:, :], AF.Exp, scale=NEG_EXP_HALF)
                    p.act(Eex[hh], Eex[hh][:, :], cex[hh], cex[hh][:, :], AF.Exp, scale=NEG_EXP_HALF)
                    p.act(Eni[hh], Eni[hh][:, :], cin[hh], cin[hh][:, :], AF.Exp, scale=-NEG_EXP_HALF)
                    G = PG()
                    p.mm(G, G[0:64, 0:512], aup, aup[:, d, cs_], adb, adb[:, :])
                    p.act(aT[hh], aT[hh][:, :], G, G[0:64, 0:512], AF.Sigmoid, bias=rwp[0:64, b + 13 + d:b + 14 + d], extra_reads=[rwp])
                    ks = sh[(hh, 'k')]
                    p.act(kk[hh], kk[hh][:, :], ks, ks[:, :], AF.Identity, scale=rwp[0:64, b + 6:b + 7], extra_reads=[rwp])
                    p.act(tA[hh], tA[hh][:, :], ks, ks[:, :], AF.Square, scale=rwp[0:64, b + 6:b + 7], extra_reads=[rwp])
                    G = blocksum(tA[hh])
                    p.act(tB[hh], tB[hh][:, :], G, G[0:64, 0:512], AF.Sqrt)
                    p.ts('dve', tB[hh], tB[hh][:, :], tB[hh], tB[hh][:, :], 1e-12, None, ALU.max)
                    p.op('dve', lambda: nc.vector.reciprocal(out=tB[hh][:, :], in_=tB[hh][:, :]), reads=[tB[hh]], writes=[tB[hh]])
                    p.tt('dve', kk[hh], kk[hh][:, :], kk[hh], kk[hh][:, :], tB[hh], tB[hh][:, :], ALU.mult)
                    p.stt(at_[hh], at_[hh][:, :], kk[hh], kk[hh][:, :], -1.0, Eex[hh], Eex[hh][:, :], ALU.mult, ALU.mult)
                    p.tt('dve', tA[hh], tA[hh][:, :], kk[hh], kk[hh][:, :], aT[hh], aT[hh][:, :], ALU.mult)
                    p.tt('pool', bt_[hh], bt_[hh][:, :], tA[hh], tA[hh][:, :], Eni[hh], Eni[hh][:, :], ALU.mult)
                    p.ts('dve', tB[hh], tB[hh][:, :], aT[hh], aT[hh][:, :], -1.0, rwp[0:64, b + 7:b + 8], ALU.add, ALU.mult, extra_reads=[rwp])
                    p.stt(tB[hh], tB[hh][:, :], tB[hh], tB[hh][:, :], 1.0, ks, ks[:, :], ALU.add, ALU.mult)
                    p.tt('dve', kt_[hh], kt_[hh][:, :], tB[hh], tB[hh][:, :], Eni[hh], Eni[hh][:, :], ALU.mult)
                    rs_ = sh[(hh, 'r')]
                    p.tt('pool', rt_[hh], rt_[hh][:, :], rs_, rs_[:, :], Ein[hh], Ein[hh][:, :], ALU.mult)
                    p.copy('act', vb_[hh], vb_[hh][:, :], sh[(hh, 'v')], sh[(hh, 'v')][:, :])
                if RW_DEBUG[0] == 'prep': return
                pairs = [(0, 1), (2, 3)] if d == 0 else [(3, 2), (1, 0)]
                HS = [slice(i * 128, (i + 1) * 128) for i in range(6)]
                VS = [slice(i * 64, (i + 1) * 64) for i in range(6)]
                for pr in pairs:
                    CS = [slice(c4 * 128, (c4 + 1) * 128) for c4 in pr]
                    tks = []
                    for ci, c4 in enumerate(pr):
                        tk = tok[(gchunk + ci) % 4]; tks.append(tk)
                        for j, srcs in enumerate([at_, bt_, kt_, vb_]):
                            for hh in H3:
                                p.tr(ptk, ptk[:, j * 192 + hh * 64:j * 192 + (hh + 1) * 64], srcs[hh], srcs[hh][:, CS[ci]], ident, ident[0:64, 0:64])
                        p.act(tk, tk[:, :, :], ptk, ptk[:, 0:768].rearrange("p (j c) -> p j c", j=4), AF.Copy)
                    BL = [(ci, hh) for ci in range(2) for hh in H3]
                    def gram(dst, A_, B_, mask):
                        G = PG()
                        for bi, (ci, hh) in enumerate(BL):
                            p.mm(G, G[:, HS[bi]], A_[hh], A_[hh][:, CS[ci]], B_[hh], B_[hh][:, CS[ci]])
                        p.tt('dve', dst, dst[:, :], G, G[:, 0:768], cst, cst[:, mask:mask + 768], ALU.mult)
                    gram(M[0], bt_, at_, mSU)
                    gram(N[0], at_, bt_, mSL)
                    p.tt('dve', Pm[0], Pm[0][:, :], M[0], M[0][:, :], cst, cst[:, RW_I:RW_I + 768], ALU.add)
                    p.tt('pool', Qm[0], Qm[0][:, :], N[0], N[0][:, :], cst, cst[:, RW_I:RW_I + 768], ALU.add)
                    gram(AKT, kt_, at_, mSU)
                    gram(RBT, bt_, rt_, mU)
                    gram(RKT, kt_, rt_, mU)
                    for lev in range(1, 7):
                        Mo, No = M[(lev - 1) % 2], N[(lev - 1) % 2]; Mn, Nn = M[lev % 2], N[lev % 2]
                        Po, Qo = Pm[(lev - 1) % 2], Qm[(lev - 1) % 2]; Pn, Qn = Pm[lev % 2], Qm[lev % 2]
                        GM = PG()
                        for bi in range(6):
                            p.mm(GM, GM[:, HS[bi]], No, No[:, HS[bi]], Mo, Mo[:, HS[bi]])
                        p.act(Mn, Mn[:, :], GM, GM[:, 0:768], AF.Copy)
                        if lev < 6:
                            GN = PG()
                            for bi in range(6):
                                p.mm(GN, GN[:, HS[bi]], Mo, Mo[:, HS[bi]], No, No[:, HS[bi]])
                            p.copy('dve', Nn, Nn[:, :], GN, GN[:, 0:768])
                        GP = PG()
                        for bi in range(6):
                            p.mm(GP, GP[:, HS[bi]], Qo, Qo[:, HS[bi]], Mn, Mn[:, HS[bi]])
                        p.tt('dve', Pn, Pn[:, :], GP, GP[:, 0:768], Po, Po[:, :], ALU.add)
                        if lev < 6:
                            GQ = PG()
                            for bi in range(6):
                                p.mm(GQ, GQ[:, HS[bi]], Po, Po[:, HS[bi]], Nn, Nn[:, HS[bi]])
                            p.tt('dve', Qn, Qn[:, :], GQ, GQ[:, 0:768], Qo, Qo[:, :], ALU.add)
                    PT = Pm[0]
                    G = PG()
                    for bi, (ci, hh) in enumerate(BL):
                        p.mm(G, G[:, VS[bi]], AKT, AKT[:, HS[bi]], tks[ci], tks[ci][:, 3, VS[hh]])
                    p.act(AKV, AKV[:, :], G, G[:, 0:384], AF.Copy)
                    G = PG()
                    for bi, (ci, hh) in enumerate(BL):
                        p.mm(G, G[:, VS[bi]], PT, PT[:, HS[bi]], AKV, AKV[:, VS[bi]])
                    p.act(UV, UV[:, :], G, G[:, 0:384], AF.Copy)
                    G = PG()
                    for bi, (ci, hh) in enumerate(BL):
                        p.mm(G, G[0:64, HS[bi]], tks[ci], tks[ci][:, 0, VS[hh]], PT, PT[:, HS[bi]])
                    p.act(TAT, TAT[:, :], G, G[0:64, 0:768], AF.Copy)
                    G = PG()
                    for bi, (ci, hh) in enumerate(BL):
                        p.mm(G, G[0:64, VS[bi]], tks[ci], tks[ci][:, 2, VS[hh]], tks[ci], tks[ci][:, 3, VS[hh]])
                    for bi, (ci, hh) in enumerate(BL):
                        c4 = pr[ci]
                        widx = c4 * 128 + 127 if d == 0 else c4 * 128
                        p.ts('dve', KVW, KVW[:, VS[bi]], G, G[0:64, VS[bi]], Ein[hh][:, widx:widx + 1], None, ALU.mult, extra_reads=[Ein[hh]])
                    for ci, c4 in enumerate(pr):
                        cs = CS[ci]; tk = tks[ci]
                        widx = c4 * 128 + 127 if d == 0 else c4 * 128
                        cur = S0b[gchunk % 2]; nxt = S0b[(gchunk + 1) % 2]
                        Yp = pY[gchunk % 2]
                        gchunk += 1
                        for hh in H3:
                            p.mm(pch, pch[:, VS[hh]], TAT, TAT[:, HS[ci * 3 + hh]], cur, cur[:, VS[hh]])
                        p.tt('dve', U, U[:, :], pch, pch[:, 0:192], UV, UV[:, ci * 192:(ci + 1) * 192], ALU.add)
                        for hh in H3:
                            bi = ci * 3 + hh
                            p.mm(Yp, Yp[0:64, HS[hh]], cur, cur[:, VS[hh]], rt_[hh], rt_[hh][:, cs], start=True, stop=False)
                            p.mm(Yp, Yp[0:64, HS[hh]], U, U[:, VS[hh]], RBT, RBT[:, HS[bi]], start=False, stop=False)
                            p.mm(Yp, Yp[0:64, HS[hh]], tk, tk[:, 3, VS[hh]], RKT, RKT[:, HS[bi]], start=False, stop=True)
                        p.act(ydall, ydall[:, :, cs], Yp, Yp[0:64, 0:384].rearrange("p (h t) -> p h t", h=3), AF.Copy)
                        for hh in H3:
                            p.mm(pch, pch[0:64, 256 + hh * 64:256 + (hh + 1) * 64], tk, tk[:, 1, VS[hh]], U, U[:, VS[hh]])
                        p.tt('dve', tS, tS[:, :], pch, pch[0:64, 256:448], S0, S0[:, :], ALU.add)
                        for hh in H3:
                            p.stt(S0, S0[:, VS[hh]], tS, tS[:, VS[hh]], Ein[hh][:, widx:widx + 1], KVW, KVW[:, VS[ci * 3 + hh]], ALU.mult, ALU.add,
                                  extra_reads=[Ein[hh]])
                        p.act(nxt, nxt[:, :], S0, S0[:, :], AF.Copy)
                for hh in H3:
                    b = hh * 15
                    rows = slice(hh * 64, (hh + 1) * 64)
                    if d == 0:
                        p.dma('pool', io['y0'][rows, t0:t0 + 512], ydall[:, hh, :], reads=[ydall], writes=[io['y0']])
                    else:
                        p.dma('sp', y0l[hh][:, :], io['y0'][rows, t0:t0 + 512], reads=[io['y0']], writes=[y0l[hh]])
                        y = pp3[hh]
                        p.tt('dve', y, y[:, :], ydall, ydall[:, hh, :], y0l[hh], y0l[hh][:, :], ALU.add)
                        G1 = blocksum(y)
                        p.tt('pool', pp1[hh], pp1[hh][:, :], y, y[:, :], y, y[:, :], ALU.mult)
                        G2 = blocksum(pp1[hh])
                        mean = pp2[hh]; var = tA[hh]
                        p.act(mean, mean[:, :], G1, G1[0:64, 0:512], AF.Copy, scale=1.0 / 64)
                        p.tt('pool', pp1[hh], pp1[hh][:, :], mean, mean[:, :], mean, mean[:, :], ALU.mult)
                        p.stt(var, var[:, :], G2, G2[0:64, 0:512], 1.0 / 64, pp1[hh], pp1[hh][:, :], ALU.mult, ALU.subtract)
                        p.act(var, var[:, :], var, var[:, :], AF.Sqrt, bias=64e-5)
                        p.op('dve', lambda: nc.vector.reciprocal(out=var[:, :], in_=var[:, :]), reads=[var], writes=[var])
                        p.tt('pool', y, y[:, :], y, y[:, :], mean, mean[:, :], ALU.subtract)
                        p.tt('pool', y, y[:, :], y, y[:, :], var, var[:, :], ALU.mult)
                        p.ts('dve', y, y[:, :], y, y[:, :], rwp[0:64, b + 9:b + 10], rwp[0:64, b + 10:b + 11], ALU.mult, ALU.add, extra_reads=[rwp])
                        rs_ = sh[(hh, 'r')]; ks = sh[(hh, 'k')]; vs_ = sh[(hh, 'v')]
                        p.stt(pp1[hh], pp1[hh][:, :], rs_, rs_[:, :], rwp[0:64, b + 8:b + 9], ks, ks[:, :], ALU.mult, ALU.mult, extra_reads=[rwp])
                        G3 = blocksum(pp1[hh])
                        p.tt('dve', pp2[hh], pp2[hh][:, :], G3, G3[0:64, 0:512], vs_, vs_[:, :], ALU.mult)
                        p.tt('pool', y, y[:, :], y, y[:, :], pp2[hh], pp2[hh][:, :], ALU.add)
                        G4 = PG()
                        p.mm(G4, G4[0:64, 0:512], gup, gup[:, rows], sgd, sgd[:, :])
                        p.tt('dve', yob[hh], yob[hh][:, :], y, y[:, :], G4, G4[0:64, 0:512], ALU.mult)
                        p.dma('pool', io['yT'][r_, 256 + hh * 64:256 + (hh + 1) * 64, off:off + 512], yob[hh][:, :], reads=[yob[hh]], writes=[io['yT']])

ALPHA = float(2.0 ** 0.5)
LN_EPS = 1e-5

def out_chunks():
    ch = []
    for q in range(4):
        heads = [2 * q, 2 * q + 1] if q < 2 else [q + 2]
        for s, h in enumerate(heads):
            ch.append((q, s * 128, 128, h * 128))
        ch.append((q, 256, 128, 768 + q * 192))
        ch.append((q, 384, 64, 768 + q * 192 + 128))
        ch.append((q, 448, 128, 1536 + q * 128))
    return ch

def ln_tile(p, u, lnw, lnb, scr, outf, outb, pfx, eps=LN_EPS):
    nc = p.nc
    st6 = scr['st6']; mv = scr['mv']; rs = scr['rs']; nm = scr['nm']; xn = u
    for c in range(4):
        p.op('dve', lambda c=c: nc.vector.bn_stats(out=st6[:, c * 6:(c + 1) * 6], in_=u[:, c * 512:(c + 1) * 512]), reads=[u], writes=[st6])
    p.op('dve', lambda: nc.vector.bn_aggr(out=mv[:, 0:2], in_=st6[:, 0:24]), reads=[st6], writes=[mv])
    p.ts('dve', rs, rs[:, 0:1], mv, mv[:, 1:2], eps, None, ALU.add)
    p.act(rs, rs[:, 0:1], rs, rs[:, 0:1], AF.Sqrt)
    p.op('dve', lambda: nc.vector.reciprocal(out=rs[:, 0:1], in_=rs[:, 0:1]), reads=[rs], writes=[rs])
    p.ts('dve', nm, nm[:, 0:1], mv, mv[:, 0:1], rs[:, 0:1], -1.0, ALU.mult, ALU.mult, extra_reads=[rs])
    p.act(xn, xn[:, :], u, u[:, :], AF.Identity, bias=nm[:, 0:1], scale=rs[:, 0:1], extra_reads=[nm, rs])
    p.tt('dve', xn, xn[:, :], xn, xn[:, :], lnw, lnw[:, :], ALU.mult)
    p.tt('dve', outf, outf[:, :], xn, xn[:, :], lnb, lnb[:, :], ALU.add)
    if outb is not None:
        p.act(outb, outb[:, :], outf, outf[:, :], AF.Copy)

def ln_scratch(st, pfx):
    return dict(st6=st.sb(pfx + "st6", [128, 24]), mv=st.sb(pfx + "mv", [128, 2]), rs=st.sb(pfx + "rs", [128, 1]),
                nm=st.sb(pfx + "nm", [128, 1]))

def load_bc(p, st, name, src_ap, n, q='sp'):
    t = st.sb(name, [128, n])
    p.dma(q, t[:, :], src_ap.partition_broadcast(128), writes=[t])
    return t

def transpose_tile(p, xb, ident, pst, xT, tcol, evac_e='act'):
    for k in range(16):
        p.tr(pst, pst[:, k * 128:(k + 1) * 128], xb, xb[:, k * 128:(k + 1) * 128], ident, ident[:, :])
    src = pst[:, :].rearrange("p (k t) -> p k t", k=16)
    dst = xT[:, :, tcol:tcol + 128]
    if evac_e == 'act':
        p.act(xT, dst, pst, src, AF.Copy)
    else:
        p.copy(evac_e, xT, dst, pst, src)

def cast_weights(p, srcs, dsts, n_per):
    with p.stage() as st:
        CH = 4096
        stg = [st.sb(f"cw_s{i}", [128, CH], F32) for i in range(3)]
        ob = [st.sb(f"cw_o{i}", [128, CH], BF16) for i in range(3)]
        i = 0
        engs = ['dve', 'pool', 'act']
        for (sT, sap), (dT, dap) in zip(srcs, dsts):
            n = sap.shape[1]
            for c0 in range(0, n, CH):
                w = min(CH, n - c0)
                s = stg[i % 3]; o = ob[i % 3]
                p.dma('sp', s[:, 0:w], sap[:, c0:c0 + w], reads=[sT], writes=[s])
                p.copy(engs[i % 3], o, o[:, 0:w], s, s[:, 0:w])
                p.dma('pool', dap[:, c0:c0 + w], o[:, 0:w], reads=[o], writes=[dT])
                i += 1

def precast_dma(p, io):
    for nm in ['w1', 'w3', 'w2']:
        sT = io[nm]; dT = io[nm + 'b']
        sv = sT.ap.rearrange("e a b -> (e a b)").rearrange("(r c) -> r c", c=2048)
        dv = dT.ap.rearrange("e a b -> (e a b)").rearrange("(r c) -> r c", c=2048)
        for r0 in range(0, 8192, 4096):
            p.dma('pool', dv[r0:r0 + 4096, :], sv[r0:r0 + 4096, :], reads=[sT], writes=[dT])

def phase2(p, T_, io, layer_last=False, ST=512, precast=True, want_xoT=True):
    nc = p.nc
    NT = T_ // 128
    chunks = out_chunks()
    NCH = len(chunks)
    srcs = []; dsts = []
    for nm in ['w1', 'w3', 'w2']:
        s = io[nm]; d = io[nm + 'b']
        srcs.append((s, s.ap.rearrange("e a b -> (e a b)").rearrange("(p n) -> p n", p=128)))
        dsts.append((d, d.ap.rearrange("e a b -> (e a b)").rearrange("(p n) -> p n", p=128)))
    if precast:
        cast_weights(p, srcs, dsts, None)

    with p.stage() as st:
        ident_f = st.sb("ident_f", [128, 128]); ident = st.sb("ident", [128, 128], BF16)
        p.dma('sp', ident_f[:, :], io['ident'][:, :], reads=[io['ident']], writes=[ident_f])
        p.copy('dve', ident, ident[:, :], ident_f, ident_f[:, :])
        wout = st.sb("wout", [128, NCH, 2048], BF16)
        wst = [st.sb(f"wst{i}", [128, 2048]) for i in range(2)]
        for j, (q, off, sz, r0) in enumerate(chunks):
            s = wst[j % 2]
            p.dma('sp', s[0:sz, :], io['w_out'][r0:r0 + sz, :], reads=[io['w_out']], writes=[s])
            p.copy(['dve', 'pool'][j % 2], wout, wout[0:sz, j, :], s, s[0:sz, :])
        lnw = load_bc(p, st, "lnw", io['ln_w'][0:1, :], 2048); lnb = load_bc(p, st, "lnb", io['ln_b'][0:1, :], 2048)
        scr = ln_scratch(st, "a")
        gluw_f = st.sb("gluw_f", [128, 4, 512]); gluw = st.sb("gluw", [128, 4, 512], BF16); glub = st.sb("glub", [128, 4])
        p.dma('sp', gluw_f[:, :, :], io['glu_w'].ap.rearrange("(k p) c -> p k c", p=128), reads=[io['glu_w']], writes=[gluw_f])
        p.copy('dve', gluw, gluw[:, :, :], gluw_f, gluw_f[:, :, :])
        p.dma('sp', glub[:, :], io['glu_bc'][:, :], reads=[io['glu_bc']], writes=[glub])
        ys5 = [st.sb(f"ys5{i}", [128, 4, 512], BF16) for i in range(2)]
        sgl = st.sb("sgl", [128, 512])
        s5j = [j for j, (q, off, sz, r0) in enumerate(chunks) if off == 448]
        ybuf = [st.sb(f"ybuf{i}", [128, NCH, 512], BF16) for i in range(2)]
        xr = wst
        u = st.sb("u", [128, 2048])
        x1f = [st.sb(f"x1f{i}", [128, 2048]) for i in range(1)]
        x1b = st.sb("x1b", [128, 2048], BF16)
        x1T = [st.sb(f"x1T{i}", [128, 16, 512], BF16) for i in range(1)]
        pm = [st.ps(f"pm{i}", [128, 512]) for i in range(4)]
        pst = st.ps("pst", [128, 2048], BF16)
        x1T_d = io['x1T'].ap.rearrange("(k p) t -> p k t", p=128)
        GT = min(4, NT)
        for t in range(NT):
            g = t // GT; tt_ = t % GT
            yb = ybuf[g % 2]
            if tt_ == 0:
                for j, (q, off, sz, r0) in enumerate(chunks):
                    p.dma('sp', yb[0:sz, j, 0:GT * 128], io['yT'][q, off:off + sz, g * GT * 128:(g + 1) * GT * 128], reads=[io['yT']], writes=[yb])
                W_ = GT * 128
                y5 = ys5[g % 2]
                for qo in range(4):
                    G = pm[qo]
                    for qi in range(4):
                        p.mm(G, G[:, 0:W_], gluw, gluw[:, qi, qo * 128:(qo + 1) * 128], yb, yb[:, s5j[qi], 0:W_], start=(qi == 0), stop=(qi == 3))
                    p.act(sgl, sgl[:, 0:W_], G, G[:, 0:W_], AF.Sigmoid, bias=glub[:, qo:qo + 1], extra_reads=[glub])
                    p.tt('dve', y5, y5[:, qo, 0:W_], sgl, sgl[:, 0:W_], yb, yb[:, s5j[qo], 0:W_], ALU.mult)
            xrt = xr[t % 2]
            p.dma('sp', xrt[:, :], io['xres'][t * 128:(t + 1) * 128, :], reads=[io['xres']], writes=[xrt])
            for c in range(4):
                for j, (q, off, sz, r0) in enumerate(chunks):
                    if j in s5j:
                        y5 = ys5[g % 2]
                        p.mm(pm[c], pm[c][:, :], y5, y5[:, s5j.index(j), tt_ * 128:(tt_ + 1) * 128], wout, wout[0:sz, j, c * 512:(c + 1) * 512],
                             start=(j == 0), stop=(j == NCH - 1))
                    else:
                        p.mm(pm[c], pm[c][:, :], yb, yb[0:sz, j, tt_ * 128:(tt_ + 1) * 128], wout, wout[0:sz, j, c * 512:(c + 1) * 512],
                             start=(j == 0), stop=(j == NCH - 1))
                p.stt(u, u[:, c * 512:(c + 1) * 512], xrt, xrt[:, c * 512:(c + 1) * 512], ALPHA, pm[c], pm[c][:, :], ALU.mult, ALU.add)
            xf = x1f[0]
            ln_tile(p, u, lnw, lnb, scr, xf, x1b, "a")
            p.dma('pool', io['x1'][t * 128:(t + 1) * 128, :], xf[:, :], reads=[xf], writes=[io['x1']])
            xT = x1T[0]
            transpose_tile(p, x1b, ident, pst, xT, tt_ * 128)
            if tt_ == GT - 1:
                p.dma('pool', x1T_d[:, :, g * GT * 128:(g + 1) * GT * 128], xT[:, :, 0:GT * 128], reads=[xT], writes=[io['x1T']])

    with p.stage() as st:
        ident_f = st.sb("ident_f", [128, 128]); ident = st.sb("ident", [128, 128], BF16)
        p.dma('sp', ident_f[:, :], io['ident'][:, :], reads=[io['ident']], writes=[ident_f])
        p.copy('dve', ident, ident[:, :], ident_f, ident_f[:, :])
        wr_f = st.sb("wr_f", [128, 16, 20]); wr = st.sb("wr", [128, 16, 20], BF16)
        p.dma('sp', wr_f[:, :, 0:4], io['rg'].ap.rearrange("(k p) g -> p k g", p=128), reads=[io['rg']], writes=[wr_f])
        for g in range(4):
            p.dma('sp', wr_f[:, :, 4 + 4 * g:8 + 4 * g], io['re'][g].rearrange("(k p) e -> p k e", p=128), reads=[io['re']], writes=[wr_f])
        p.copy('dve', wr, wr[:, :, :], wr_f, wr_f[:, :, :])
        rb = st.sb("rb", [128, 20])
        p.dma('sp', rb[:, 0:4], io['rgb'].ap.rearrange("(o g) -> o g", o=1).partition_broadcast(128), reads=[io['rgb']], writes=[rb])
        p.dma('sp', rb[:, 4:20], io['reb'].ap.rearrange("(o g) e -> o (g e)", o=1).partition_broadcast(128), reads=[io['reb']], writes=[rb])
        NS = ST // 128
        x1T = [st.sb(f"mx1T{i}", [128, 16, ST], BF16) for i in range(2)]
        yacc = st.sb("yacc", [128, NS, 2048])
        gate = st.sb("gate", [128, NS, 16])
        w1b = [st.sb(f"w1b{i}", [128, 16, 512], BF16) for i in range(1)]
        w3b = [st.sb(f"w3b{i}", [128, 16, 512], BF16) for i in range(1)]
        w2b = [st.sb(f"w2b{i}", [128, 4, 2048], BF16) for i in range(1)]
        hT = [st.sb(f"hT{i}", [128, 4, ST], BF16) for i in range(2)]
        sl = [st.sb(f"sl{i}", [128, ST]) for i in range(2)]
        lg = st.sb("lg", [128, 20]); r1 = st.sb("r1", [128, 8]); mg = st.sb("mg", [128, 4]); es = st.sb("es", [128, 4])
        tmp16 = st.sb("tmp16", [128, 16]); m1 = st.sb("m1", [128, 4]); m2 = st.sb("m2", [128, 4]); e2 = st.sb("e2", [128, 4])
        gi = st.sb("gi", [128, 4])
        pa = [st.ps(f"pa{i}", [128, 512]) for i in range(2)]
        pb = [st.ps(f"pb{i}", [128, 512]) for i in range(2)]
        py = [st.ps(f"py{i}", [128, 512]) for i in range(2)]
        x1T_d = io['x1T'].ap.rearrange("(k p) t -> p k t", p=128)
        NSUP = T_ // ST
        wcount = 0
        for s in range(NSUP):
            xT = x1T[s % 2]
            p.dma('sp', xT[:, :, :], x1T_d[:, :, s * ST:(s + 1) * ST], reads=[io['x1T']], writes=[xT])
            for m in range(NS):
                pl = pa[m % 2]
                for k in range(16):
                    p.mm(pl, pl[:, 0:20], xT, xT[:, k, m * 128:(m + 1) * 128], wr, wr[:, k, :], start=(k == 0), stop=(k == 15))
                p.tt('dve', lg, lg[:, :], pl, pl[:, 0:20], rb, rb[:, :], ALU.add)
                p.op('dve', lambda: nc.vector.tensor_reduce(out=r1[:, 0:1], in_=lg[:, 0:4], axis=AX.X, op=ALU.max), reads=[lg], writes=[r1])
                p.ts('dve', mg, mg[:, :], lg, lg[:, 0:4], r1[:, 0:1], None, ALU.is_equal, extra_reads=[r1])
                p.ts('dve', r1, r1[:, 1:2], r1, r1[:, 0:1], -1.0, None, ALU.mult)
                p.act(tmp16, tmp16[:, 0:4], lg, lg[:, 0:4], AF.Exp, bias=r1[:, 1:2], extra_reads=[r1], accum=(r1, r1[:, 2:3]))
                p.op('dve', lambda: nc.vector.reciprocal(out=r1[:, 3:4], in_=r1[:, 2:3]), reads=[r1], writes=[r1])
                p.ts('dve', es, es[:, :], lg, lg[:, 4:8], mg[:, 0:1], None, ALU.mult, extra_reads=[mg])
                for g in range(1, 4):
                    p.stt(es, es[:, :], lg, lg[:, 4 + 4 * g:8 + 4 * g], mg[:, g:g + 1], es, es[:, :], ALU.mult, ALU.add, extra_reads=[mg])
                p.op('dve', lambda: nc.vector.tensor_reduce(out=r1[:, 4:5], in_=es[:, :], axis=AX.X, op=ALU.max), reads=[es], writes=[r1])
                p.ts('dve', m1, m1[:, :], es, es[:, :], r1[:, 4:5], None, ALU.is_equal, extra_reads=[r1])
                p.stt(e2, e2[:, :], m1, m1[:, :], -1e30, es, es[:, :], ALU.mult, ALU.add)
                p.op('dve', lambda: nc.vector.tensor_reduce(out=r1[:, 5:6], in_=e2[:, :], axis=AX.X, op=ALU.max), reads=[e2], writes=[r1])
                p.ts('dve', m2, m2[:, :], e2, e2[:, :], r1[:, 5:6], None, ALU.is_equal, extra_reads=[r1])
                p.tt('dve', r1, r1[:, 6:7], r1, r1[:, 4:5], r1, r1[:, 5:6], ALU.subtract)
                p.act(r1, r1[:, 6:7], r1, r1[:, 6:7], AF.Sigmoid)
                p.ts('dve', r1, r1[:, 7:8], r1, r1[:, 6:7], -1.0, 1.0, ALU.mult, ALU.add)
                p.ts('dve', gi, gi[:, :], m1, m1[:, :], r1[:, 6:7], None, ALU.mult, extra_reads=[r1])
                p.stt(gi, gi[:, :], m2, m2[:, :], r1[:, 7:8], gi, gi[:, :], ALU.mult, ALU.add, extra_reads=[r1])
                p.ts('dve', gi, gi[:, :], gi, gi[:, :], r1[:, 3:4], None, ALU.mult, extra_reads=[r1])
                for g in range(4):
                    p.ts('dve', gate, gate[:, m, 4 * g:4 * g + 4], gi, gi[:, :], mg[:, g:g + 1], None, ALU.mult, extra_reads=[mg])
            for e in range(16):
                wb = 0
                p.dma('sp', w1b[wb][:, :, :], io['w1b'][e].rearrange("(k p) f -> p k f", p=128), reads=[io['w1b']], writes=[w1b[wb]])
                p.dma('sp', w3b[wb][:, :, :], io['w3b'][e].rearrange("(k p) f -> p k f", p=128), reads=[io['w3b']], writes=[w3b[wb]])
                p.dma('sp', w2b[wb][:, :, :], io['w2b'][e].rearrange("(k p) f -> p k f", p=128), reads=[io['w2b']], writes=[w2b[wb]])
                h = hT[e % 2]
                for f in range(4):
                    a = pa[f % 2]; b = pb[f % 2]
                    for k in range(16):
                        p.mm(a, a[:, 0:ST], w1b[wb], w1b[wb][:, k, f * 128:(f + 1) * 128], xT, xT[:, k, :], start=(k == 0), stop=(k == 15))
                    for k in range(16):
                        p.mm(b, b[:, 0:ST], w3b[wb], w3b[wb][:, k, f * 128:(f + 1) * 128], xT, xT[:, k, :], start=(k == 0), stop=(k == 15))
                    s_ = sl[f % 2]
                    p.act(s_, s_[:, :], a, a[:, 0:ST], AF.Silu)
                    p.tt('dve', h, h[:, f, :], s_, s_[:, :], b, b[:, 0:ST], ALU.mult)
                i = 0
                for m in range(NS):
                    for c in range(4):
                        y = py[i % 2]; i += 1
                        for f in range(4):
                            p.mm(y, y[:, :], h, h[:, f, m * 128:(m + 1) * 128], w2b[wb], w2b[wb][:, f, c * 512:(c + 1) * 512], start=(f == 0), stop=(f == 3))
                        if e == 0:
                            p.ts('dve', yacc, yacc[:, m, c * 512:(c + 1) * 512], y, y[:, :], gate[:, m, e:e + 1], None, ALU.mult, extra_reads=[gate])
                        else:
                            p.stt(yacc, yacc[:, m, c * 512:(c + 1) * 512], y, y[:, :], gate[:, m, e:e + 1], yacc, yacc[:, m, c * 512:(c + 1) * 512],
                                  ALU.mult, ALU.add, extra_reads=[gate])
            for m in range(NS):
                t = s * NS + m
                p.dma('pool', io['moe'][t * 128:(t + 1) * 128, :], yacc[:, m, :], reads=[yacc], writes=[io['moe']])

    with p.stage() as st:
        ident_f = st.sb("ident_f", [128, 128]); ident = st.sb("ident", [128, 128], BF16)
        p.dma('sp', ident_f[:, :], io['ident'][:, :], reads=[io['ident']], writes=[ident_f])
        p.copy('dve', ident, ident[:, :], ident_f, ident_f[:, :])
        lnw2 = load_bc(p, st, "lnw2", io['ln_w'][1:2, :], 2048); lnb2 = load_bc(p, st, "lnb2", io['ln_b'][1:2, :], 2048)
        lnw3 = load_bc(p, st, "lnw3", io['ln_w'][2:3, :], 2048); lnb3 = load_bc(p, st, "lnb3", io['ln_b'][2:3, :], 2048)
        scr = ln_scratch(st, "c")
        pg = st.sb("pg", [128, 16, 2048], BF16); pp = st.sb("pp", [128, 2, 2048], BF16)
        wst = [st.sb(f"wst{i}", [128, 2048]) for i in range(2)]
        for k in range(16):
            s = wst[k % 2]
            p.dma('sp', s[:, :], io['ple_gate'][k * 128:(k + 1) * 128, :], reads=[io['ple_gate']], writes=[s])
            p.copy(['dve', 'pool'][k % 2], pg, pg[:, k, :], s, s[:, :])
        for k in range(2):
            s = wst[k % 2]
            p.dma('sp', s[:, :], io['ple_proj'][k * 128:(k + 1) * 128, :], reads=[io['ple_proj']], writes=[s])
            p.copy(['dve', 'pool'][k % 2], pp, pp[:, k, :], s, s[:, :])
        xr = wst[0]; mo = wst[1]
        x2T = st.sb("cx2T", [128, 16, 128], BF16)
        pTf = st.sb("pTf", [128, 2, 128]); pTb = st.sb("pTb", [128, 2, 128], BF16)
        sg = st.sb("sg", [128, 512])
        u = st.sb("u", [128, 2048])
        x2f = st.sb("x2f", [128, 2048]); x2b = st.sb("x2b", [128, 2048], BF16)
        x3f = st.sb("x3f", [128, 2048]); x3b = st.sb("x3b", [128, 2048], BF16)
        x3T = [st.sb(f"x3T{i}", [128, 16, 512], BF16) for i in range(2)]
        pgp = [st.ps(f"pgp{i}", [128, 512]) for i in range(2)]
        ppp = [st.ps(f"ppp{i}", [128, 512]) for i in range(2)]
        pst = st.ps("pst", [128, 2048], BF16)
        xoT_d = io['xoT'].ap.rearrange("(k p) t -> p k t", p=128)
        pT_d = io['pT'].ap.rearrange("(k p) t -> p k t", p=128)
        GT = min(4, NT)
        for t in range(NT):
            g = t // GT; tt_ = t % GT
            rows = slice(t * 128, (t + 1) * 128)
            p.dma('sp', xr[:, :], io['x1'][rows, :], reads=[io['x1']], writes=[xr])
            p.dma('sp', mo[:, :], io['moe'][rows, :], reads=[io['moe']], writes=[mo])
            p.dma('sp', pTf[:, :, :], pT_d[:, :, rows], reads=[io['pT']], writes=[pTf])
            p.copy('pool', pTb, pTb[:, :, :], pTf, pTf[:, :, :])
            p.stt(u, u[:, :], xr, xr[:, :], ALPHA, mo, mo[:, :], ALU.mult, ALU.add)
            ln_tile(p, u, lnw2, lnb2, scr, x2f, x2b, "c")
            transpose_tile(p, x2b, ident, pst, x2T, 0)
            for c in range(4):
                cs = slice(c * 512, (c + 1) * 512)
                G = pgp[c % 2]; PP = ppp[c % 2]
                for k in range(16):
                    p.mm(G, G[:, :], x2T, x2T[:, k, :], pg, pg[:, k, cs], start=(k == 0), stop=(k == 15))
                for k in range(2):
                    p.mm(PP, PP[:, :], pTb, pTb[:, k, :], pp, pp[:, k, cs], start=(k == 0), stop=(k == 1))
                p.act(sg, sg[:, :], G, G[:, :], AF.Sigmoid)
                p.tt('dve', sg, sg[:, :], sg, sg[:, :], PP, PP[:, :], ALU.mult)
                p.stt(u, u[:, cs], x2f, x2f[:, cs], ALPHA, sg, sg[:, :], ALU.mult, ALU.add)
            ln_tile(p, u, lnw3, lnb3, scr, x3f, x3b if want_xoT else None, "c")
            p.dma('pool', io['xo'][rows, :], x3f[:, :], reads=[x3f], writes=[io['xo']])
            x3 = x3T[g % 2]
            if want_xoT:
                transpose_tile(p, x3b, ident, pst, x3, tt_ * 128)
            if want_xoT and tt_ == GT - 1:
                p.dma('pool', xoT_d[:, :, g * GT * 128:(g + 1) * GT * 128], x3[:, :, 0:GT * 128], reads=[x3], writes=[io['xoT']])


_S = 16384
_D = 2048
_T = 4096
_PROGS = {}
_GROUPS = [[0, 1, 2, 3], [4, 5, 6, 7]]
_P1_KEYS = [('w_in', [_D, NZ]), ('rwp', [128, 55]), ('rw_wup', [64, 2, 192]), ('rw_aup', [64, 2, 192]), ('rw_gup', [128, 192]),
            ('s5rows', [3, 1024]), ('s5cols', [128, 24]), ('s5bl', [2, 128, 1024]), ('s5cl', [2, 128, 1024]), ('s5d', [128, 1])]
_P2_KEYS = [('w_out', [_D, _D]), ('ln_w', [3, _D]), ('ln_b', [3, _D]), ('rg', [_D, 4]), ('rgb', [4]), ('re', [4, _D, 4]), ('reb', [4, 4]),
            ('w1', [16, _D, 512]), ('w3', [16, _D, 512]), ('w2', [16, 512, _D]), ('glu_w', [512, 512]), ('glu_bc', [128, 4]),
            ('ple_proj', [256, _D]), ('ple_gate', [_D, _D]), ('pT', [256, None])]

def stage_select(p, gath, rmask, yTr):
    with p.stage() as st:
        mk = st.sb("mk", [128, 4]); p.dma('sp', mk[:, :], rmask[:, :], reads=[rmask], writes=[mk])
        cand = [[st.sb(f"cand{i}_{r}", [128, _T], BF16) for r in range(4)] for i in range(2)]
        acc = [st.sb(f"acc{i}", [128, _T], BF16) for i in range(2)]
        n = 0
        for q in range(4):
            for i in range(6):
                r0 = i * 96; sz = 96
                cd = cand[n % 2]; a = acc[n % 2]
                e = 'dve'; n += 1
                for r in range(4):
                    p.dma('sp', cd[r][0:sz, :], gath[r * 6 + i, q * 96:(q + 1) * 96, :], reads=[gath], writes=[cd[r]])
                p.ts(e, a, a[0:sz, :], cd[0], cd[0][0:sz, :], mk[0:sz, 0:1], None, ALU.mult, extra_reads=[mk])
                for r in range(1, 4):
                    p.stt(a, a[0:sz, :], cd[r], cd[r][0:sz, :], mk[0:sz, r:r + 1], a, a[0:sz, :], ALU.mult, ALU.add, extra_reads=[mk], e=e)
                p.dma('pool', yTr[q, r0:r0 + sz, :], a[0:sz, :], reads=[a], writes=[yTr])

def _build_fused():
    S = _S; D = _D; T_ = _T
    nc = bass.Bass("TRN2", target_bir_lowering=False)
    p = Prog(nc)
    E = {}
    def ext(name, shape, dt=F32):
        E[name] = p.dram(name, shape, dt, kind="ExternalInput"); return E[name]
    ext('xT0', [4, D, T_]); ext('xres', [T_, D]); ext('pos', [S], I32); ext('rconst', [128, RC_N]); ext('ident', [128, 128])
    ext('rwconst', [128, RW_N]); ext('s5iota', [128, 512]); ext('rmask', [128, 4])
    for L in range(2):
        for k, sh in _P1_KEYS + _P2_KEYS:
            ext(f"{k}_{L}", [T_ if s is None else s for s in sh])
    xo_final = p.dram('xo_final', [T_, D], F32, kind="ExternalOutput")
    sc = {}
    sc['zT'] = p.dram('zT', [NZ, S], F32)
    for nm in ['qrT', 'krT']: sc[nm] = p.dram(nm, [2, 128, S], BF16)
    for nm in ['ktok', 'vtok', 'sbd']: sc[nm] = p.dram(nm, [2, S // 128, 128, 128], BF16)
    sc['yf'] = p.dram('yf', [128, S]); sc['y0'] = p.dram('y0', [192, S])
    yTloc = p.dram('yTloc', [4, 576, T_], BF16)
    gath = p.dram('gath', [24, 4 * 96, T_], BF16)
    yTr = p.dram('yTr', [4, 576, T_], BF16)
    sc2 = dict(x1=p.dram('x1', [T_, D]), x1T=p.dram('x1T', [D, T_], BF16), moe=p.dram('moe', [T_, D]),
               w1b=p.dram('w1b', [16, D, 512], BF16), w3b=p.dram('w3b', [16, D, 512], BF16), w2b=p.dram('w2b', [16, 512, D], BF16))
    xo0 = p.dram('xo0', [T_, D]); xoT = p.dram('xoT', [D, T_], BF16); xoT_dummy = p.dram('xoT_dummy', [D, T_], BF16)
    xTg = p.dram('xTg', [16, 4 * 128, T_], BF16)
    for L in range(2):
        io1 = dict(sc)
        for k, _ in _P1_KEYS: io1[k] = E[f"{k}_{L}"]
        io1.update(pos=E['pos'], rconst=E['rconst'], ident=E['ident'], rwconst=E['rwconst'], s5iota=E['s5iota'], yT=yTloc)
        io1['xT'] = E['xT0'] if L == 0 else xTg
        xsrc = None
        if L == 1:
            xsrc = lambda r: xTg.ap[:, r * 128:(r + 1) * 128, :].rearrange("k p t -> p k t")
        stage_inproj(p, S, io1, L == 0, xsrc=xsrc)
        stage_retention(p, S, io1)
        stage_s5(p, S, io1)
        io2 = dict(sc2)
        for k_, _ in _P2_KEYS: io2[k_] = E[f"{k_}_{L}"]
        precast_dma(p, io2)
        stage_rwkv(p, S, io1)
        for r in range(4):
            for i in range(6):
                p.collective("AllGather", yTloc, yTloc.ap[r, i * 96:(i + 1) * 96, :], gath, gath.ap[r * 6 + i], _GROUPS)
        stage_select(p, gath, E['rmask'], yTr)
        io2.update(yT=yTr, ident=E['ident'], xres=(E['xres'] if L == 0 else xo0), xo=(xo0 if L == 0 else xo_final),
                   xoT=(xoT if L == 0 else xoT_dummy))
        phase2(p, T_, io2, ST=512, precast=False, want_xoT=(L == 0))
        if L == 0:
            for k in range(16):
                p.collective("AllGather", xoT, xoT.ap[k * 128:(k + 1) * 128, :], xTg, xTg.ap[k], _GROUPS)
    p.finish([xo_final])
    return nc

def kernel(**inp):
    x = np.ascontiguousarray(np.asarray(inp['x'], dtype=np.float32))
    S = _S; D = _D
    if 'f' not in _PROGS:
        _PROGS['f'] = _build_fused()
    ident = np.eye(128, dtype=np.float32)
    rwc = rwkv_consts()
    positions = np.asarray(inp['positions']).astype(np.int32)
    g = lambda k: np.asarray(inp[k])
    maps = []
    for c in range(8):
        b, q = c // 4, c % 4
        r = q
        rmask = np.zeros((128, 4), np.float32); rmask[:, r] = 1.0
        m = dict(xT0=np.ascontiguousarray(x[b].T.reshape(D, 4, S // 4).transpose(1, 0, 2)),
                 xres=np.ascontiguousarray(x[b, r * _T:(r + 1) * _T]), pos=np.ascontiguousarray(positions[b]),
                 rconst=ret_consts(q), ident=ident, rwconst=rwc, rmask=rmask)
        for L in range(2):
            d1 = dict(w_in=np.ascontiguousarray(g('w_in')[L][:, core_cols(q)]))
            d1.update(rwkv_host_layout(q, g('rwkv_mu_prev')[L], g('rwkv_mu_next')[L], g('rwkv_w0')[L], g('rwkv_w_up')[L], g('rwkv_a0')[L],
                                       g('rwkv_a_up')[L], g('rwkv_g_up')[L], g('rwkv_k_k')[L], g('rwkv_k_a')[L], g('rwkv_r_k')[L],
                                       g('rwkv_lnx_w')[L], g('rwkv_lnx_b')[L]))
            s5 = s5_host_layout(q, g('s5_lam_re')[L], g('s5_lam_im')[L], g('s5_log_dt')[L], g('s5_b_re')[L], g('s5_b_im')[L],
                                g('s5_c_re')[L], g('s5_c_im')[L], g('s5_d')[L])
            m['s5iota'] = s5.pop('s5iota')
            d1.update(s5)
            d2 = dict(w_out=g('w_out')[L], ln_w=g('ln_w')[L], ln_b=g('ln_b')[L],
                      rg=g('moe_router_g')[L], rgb=g('moe_router_g_b')[L], re=g('moe_router_e')[L], reb=g('moe_router_e_b')[L],
                      w1=g('moe_w1')[L], w3=g('moe_w3')[L], w2=g('moe_w2')[L],
                      glu_w=g('s5_glu_w')[L], glu_bc=np.ascontiguousarray(g('s5_glu_b')[L].reshape(4, 128).T),
                      ple_proj=g('ple_proj')[L], ple_gate=g('ple_gate')[L],
                      pT=np.ascontiguousarray(g('p')[L, b, r * _T:(r + 1) * _T].T))
            for k, v in list(d1.items()) + list(d2.items()):
                m[f"{k}_{L}"] = np.ascontiguousarray(v)
        maps.append(m)
    res = run_bass_kernel_spmd(_PROGS['f'], maps, core_ids=list(range(8)))
    out = np.empty_like(x)
    for c in range(8):
        b, r = c // 4, c % 4
        out[b, r * _T:(r + 1) * _T] = np.asarray(res.results[c]['xo_final'])
    return out
```
